# Optimizing a Trainium2 kernel written in Bass

```python
import math
import jax, jax.numpy as jnp
from jax import lax
import numpy as np

D_MODEL = 1024
BATCH = 8
SEQ = 4096
DEPTH = 2

F32 = jnp.float32
HEAD_DIM = 64
HG_WIDTH = D_MODEL // 4
HG_HEADS = HG_WIDTH // HEAD_DIM
HG_CHUNK = 64
GM_WIDTH = D_MODEL // 4
GM_GROUPS = GM_WIDTH // HEAD_DIM
GM_CHUNK = 128
NSA_WIDTH = D_MODEL // 2
NSA_HEADS = NSA_WIDTH // HEAD_DIM
NSA_KV_GROUPS = 2
NSA_HPG = NSA_HEADS // NSA_KV_GROUPS
NSA_KV_WIDTH = NSA_KV_GROUPS * HEAD_DIM
CMP_BLOCK = 32
CMP_STRIDE = 16
CMP_HIDDEN = 128
SEL_BLOCK = 64
N_SEL = 16
WINDOW = 512
NSA_QBLOCK = 64
N_GATES = 3
IMP_FORCE = 1e9
IMP_FUTURE = -1e9
NEG_INF = -1e30
MIX_WIDTH = HG_WIDTH + GM_WIDTH + NSA_WIDTH
IN_SPLITS = (HG_WIDTH,) * 4 + (GM_WIDTH,) * 2 + (NSA_WIDTH,) + (NSA_KV_WIDTH,) * 6 + (NSA_HEADS * N_GATES,)
IN_WIDTH = sum(IN_SPLITS)
N_EXPERTS = 32
TOP_K = 4
D_EXPERT = D_MODEL
SWIGLU_ALPHA = 1.702
SWIGLU_LIMIT = 7.0
EXPERT_BLOCK = 512
ROPE_THETA = 10000.0
DEEPNORM_ALPHA = (2 * DEPTH) ** 0.25
DEEPNORM_BETA = (8 * DEPTH) ** -0.25
LN_EPS = 1e-5
RMS_EPS = 1e-6

kernel_name = "hybrid_hgrn2_gmlp_nsa_moe_deepnorm"


def layer_norm(x, w, b):
    xf = x.astype(F32)
    mu = jnp.mean(xf, axis=-1, keepdims=True)
    var = jnp.mean(jnp.square(xf - mu), axis=-1, keepdims=True)
    return ((xf - mu) * lax.rsqrt(var + LN_EPS) * w.astype(F32) + b.astype(F32)).astype(x.dtype)


def rms_norm_heads(x, w):
    shp = x.shape
    xf = x.astype(F32).reshape(shp[:-1] + (shp[-1] // HEAD_DIM, HEAD_DIM))
    xf = xf * lax.rsqrt(jnp.mean(jnp.square(xf), axis=-1, keepdims=True) + RMS_EPS)
    return (xf.reshape(shp) * w.astype(F32)).astype(x.dtype)


def masked_softmax(s, mask):
    s = jnp.where(mask, s.astype(F32), NEG_INF)
    return jax.nn.softmax(s, axis=-1) * mask


def rope_tables(positions):
    inv = ROPE_THETA ** (-jnp.arange(0, HEAD_DIM, 2, dtype=F32) / HEAD_DIM)
    ang = positions.astype(F32)[..., None] * inv
    return jnp.cos(ang)[:, :, None, :], jnp.sin(ang)[:, :, None, :]


def apply_rope(x, cos, sin):
    xf = x.astype(F32)
    x1, x2 = xf[..., :HEAD_DIM // 2], xf[..., HEAD_DIM // 2:]
    return jnp.concatenate([x1 * cos - x2 * sin, x2 * cos + x1 * sin], axis=-1).astype(x.dtype)


def hgrn2_mixer(q, f_raw, v, gate, lb, norm_w):
    B, S, _ = q.shape
    H, Dk, C = HG_HEADS, HEAD_DIM, HG_CHUNK
    n_ch = S // C
    dt = q.dtype
    lb = lb.astype(F32)
    f_raw = f_raw.astype(F32)
    qf = jax.nn.silu(q.astype(F32))
    log_f = jnp.logaddexp(jnp.log(lb), jnp.log1p(-lb) + jax.nn.log_sigmoid(f_raw))
    k = (1.0 - lb) * jax.nn.sigmoid(-f_raw)

    def chunks(a):
        return a.reshape(B, n_ch, C, H, Dk).transpose(1, 0, 3, 2, 4)

    causal = jnp.tril(jnp.ones((C, C), dtype=bool))

    def step(state, inp):
        qc, kc, vc, gc = inp
        b = jnp.cumsum(gc, axis=2)
        diff = b[:, :, :, None, :] - b[:, :, None, :, :]
        decay = jnp.exp(jnp.where(causal[:, :, None], diff, -jnp.inf))
        attn = jnp.einsum('bhtk,bhtsk,bhsk->bhts', qc, decay, kc)
        o = (jnp.einsum('bhts,bhsv->bhtv', attn, vc)
             + jnp.einsum('bhtk,bhkv->bhtv', qc * jnp.exp(b), state))
        b_last = b[:, :, -1]
        state = (jnp.exp(b_last)[..., None] * state
                 + jnp.einsum('bhsk,bhsv->bhkv', kc * jnp.exp(b_last[:, :, None] - b), vc))
        return state, o

    s0 = jnp.zeros((B, H, Dk, Dk), F32)
    _, o = lax.scan(step, s0, (chunks(qf), chunks(k), chunks(v.astype(F32)), chunks(log_f)))
    o = o.transpose(1, 0, 3, 2, 4).reshape(B, S, H * Dk)
    return (rms_norm_heads(o, norm_w) * jax.nn.sigmoid(gate.astype(F32))).astype(dt)


def gmlp_mixer(u, v, ln_w, ln_b, w_s, b_s, norm_w):
    B, S, _ = u.shape
    n_ch = S // GM_CHUNK
    u = jax.nn.gelu(u)
    v = layer_norm(jax.nn.gelu(v), ln_w, ln_b)
    v = v.reshape(B, n_ch, GM_CHUNK, GM_GROUPS, HEAD_DIM)
    w = w_s * jnp.tril(jnp.ones((GM_CHUNK, GM_CHUNK), w_s.dtype))
    sv = jnp.einsum('gts,bnsgc->bntgc', w, v) + b_s.T[:, :, None]
    return rms_norm_heads(u * sv.reshape(B, S, GM_WIDTH), norm_w)


def compress_blocks(k, pe, w1, w2):
    B, S, G, Dh = k.shape
    n_cmp = (S - CMP_BLOCK) // CMP_STRIDE + 1
    idx = np.arange(n_cmp)[:, None] * CMP_STRIDE + np.arange(CMP_BLOCK)[None, :]
    blk = k[:, idx] + pe[:, None, :]
    blk = blk.transpose(0, 3, 1, 2, 4).reshape(B, G, n_cmp, CMP_BLOCK * Dh)
    return jax.nn.gelu(blk @ w1) @ w2


def nsa_mixer(q, k_c, v_c, k_s, v_s, k_w, v_w, gates, pe, w1, w2, cos, sin):
    B, S, H, Dh = q.shape
    G, HPG, QB = NSA_KV_GROUPS, NSA_HPG, NSA_QBLOCK
    nqb = S // QB
    scale = 1.0 / math.sqrt(Dh)
    kc = compress_blocks(k_c, pe[0], w1[0], w2[0])
    vc = compress_blocks(v_c, pe[1], w1[1], w2[1])
    n_cmp = kc.shape[2]
    cmp_end = jnp.asarray(np.arange(n_cmp) * CMP_STRIDE + CMP_BLOCK - 1)
    n_sb = S // SEL_BLOCK
    n_sel = min(N_SEL, n_sb)
    units = np.arange(n_cmp)[:, None] + np.arange(CMP_BLOCK // CMP_STRIDE)[None, :]
    overlap = jax.nn.one_hot(units // (SEL_BLOCK // CMP_STRIDE), n_sb, dtype=F32).sum(axis=1)
    q_rot = apply_rope(q, cos, sin)
    ks_r = apply_rope(k_s, cos, sin)
    kw_r = apply_rope(k_w, cos, sin)
    ks_blk = ks_r.reshape(B, n_sb, SEL_BLOCK, G, Dh).transpose(0, 3, 1, 2, 4)
    vs_blk = v_s.reshape(B, n_sb, SEL_BLOCK, G, Dh).transpose(0, 3, 1, 2, 4)
    pad = ((0, 0), (WINDOW, 0), (0, 0), (0, 0))
    kw_pad = jnp.pad(kw_r, pad)
    vw_pad = jnp.pad(v_w, pad)
    b_idx = jnp.arange(B)[:, None, None, None]
    g_idx = jnp.arange(G)[None, :, None, None]

    def to_blocks(a):
        return a.reshape((B, nqb, QB) + a.shape[2:]).swapaxes(0, 1)

    def grouped(a):
        return a.reshape(B, QB, G, HPG, a.shape[-1]).transpose(0, 2, 3, 1, 4)

    def block_fn(args):
        qb_raw, qb_rot, gb, qi = args
        t = qi * QB + jnp.arange(QB)
        qc = grouped(qb_raw) * scale
        qr = grouped(qb_rot) * scale
        p_c = masked_softmax(jnp.einsum('bghqd,bgcd->bghqc', qc, kc), cmp_end[None, :] <= t[:, None])
        o_c = jnp.einsum('bghqc,bgcd->bghqd', p_c.astype(vc.dtype), vc)
        imp = jnp.einsum('bghqc,cj->bgqj', p_c, overlap)
        j = jnp.arange(n_sb)[None, :]
        cur = (t // SEL_BLOCK)[:, None]
        imp = jnp.where(j > cur, IMP_FUTURE, imp)
        imp = jnp.where((j == 0) | (j == cur) | (j == cur - 1), IMP_FORCE, imp)
        _, sel = lax.top_k(imp, n_sel)
        k_sel = ks_blk[b_idx, g_idx, sel].reshape(B, G, QB, n_sel * SEL_BLOCK, Dh)
        v_sel = vs_blk[b_idx, g_idx, sel].reshape(B, G, QB, n_sel * SEL_BLOCK, Dh)
        kpos = (sel[..., None] * SEL_BLOCK + jnp.arange(SEL_BLOCK)).reshape(B, G, QB, n_sel * SEL_BLOCK)
        m_s = kpos <= t[:, None]
        p_s = masked_softmax(jnp.einsum('bghqd,bgqkd->bghqk', qr, k_sel), m_s[:, :, None])
        o_s = jnp.einsum('bghqk,bgqkd->bghqd', p_s.astype(v_sel.dtype), v_sel)
        kw = lax.dynamic_slice_in_dim(kw_pad, qi * QB, QB + WINDOW, axis=1).transpose(0, 2, 1, 3)
        vw = lax.dynamic_slice_in_dim(vw_pad, qi * QB, QB + WINDOW, axis=1).transpose(0, 2, 1, 3)
        wpos = qi * QB - WINDOW + jnp.arange(QB + WINDOW)
        m_w = (wpos[None, :] <= t[:, None]) & (wpos[None, :] > t[:, None] - WINDOW)
        p_w = masked_softmax(jnp.einsum('bghqd,bgkd->bghqk', qr, kw), m_w)
        o_w = jnp.einsum('bghqk,bgkd->bghqd', p_w.astype(vw.dtype), vw)
        g = grouped(gb)
        o = g[..., 0:1] * o_c + g[..., 1:2] * o_s + g[..., 2:3] * o_w
        return o.transpose(0, 3, 1, 2, 4).reshape(B, QB, H * Dh)

    out = lax.map(block_fn, (to_blocks(q), to_blocks(q_rot), to_blocks(gates), jnp.arange(nqb)))
    return out.swapaxes(0, 1).reshape(B, S, H * Dh)


def moe_ffn(x2d, router_w, router_b, w_up, b_up, w_down, b_down):
    N, D = x2d.shape
    logits = x2d.astype(F32) @ router_w.astype(F32) + router_b.astype(F32)
    top_logit, top_e = lax.top_k(logits, TOP_K)
    top_w = jax.nn.softmax(top_logit, axis=-1)
    n_assign = N * TOP_K
    flat_e = top_e.reshape(-1)
    order = jnp.argsort(flat_e)
    sorted_e = flat_e[order]
    sorted_tok = (order // TOP_K).astype(jnp.int32)
    counts = jnp.bincount(flat_e, length=N_EXPERTS)
    padded = (counts + EXPERT_BLOCK - 1) // EXPERT_BLOCK * EXPERT_BLOCK
    start = jnp.cumsum(counts) - counts
    pad_end = jnp.cumsum(padded)
    pad_start = pad_end - padded
    dest = pad_start[sorted_e] + jnp.arange(n_assign) - start[sorted_e]
    n_blocks = -(-(n_assign + N_EXPERTS * (EXPERT_BLOCK - 1)) // EXPERT_BLOCK)
    n_rows = n_blocks * EXPERT_BLOCK
    tok_buf = jnp.full((n_rows,), N, jnp.int32).at[dest].set(sorted_tok)
    w_buf = jnp.zeros((n_rows,), F32).at[dest].set(top_w.reshape(-1)[order])
    block_e = jnp.clip(jnp.searchsorted(pad_end, jnp.arange(n_blocks) * EXPERT_BLOCK, side='right'),
                       0, N_EXPERTS - 1)
    x_pad = jnp.concatenate([x2d, jnp.zeros((1, D), x2d.dtype)], axis=0)
    xb = x_pad[tok_buf].reshape(n_blocks, EXPERT_BLOCK, D)

    def expert_block(args):
        xe, e = args
        h = xe @ w_up[e] + b_up[e]
        glu = jnp.minimum(h[:, :D_EXPERT], SWIGLU_LIMIT)
        lin = jnp.clip(h[:, D_EXPERT:], -SWIGLU_LIMIT, SWIGLU_LIMIT)
        act = glu * jax.nn.sigmoid(SWIGLU_ALPHA * glu) * (lin + 1.0)
        return act @ w_down[e] + b_down[e]

    yb = lax.map(expert_block, (xb, block_e)).reshape(n_rows, D)
    out = jax.ops.segment_sum(yb * w_buf[:, None].astype(yb.dtype), tok_buf, num_segments=N + 1)
    return out[:N]


def setup_inputs(seed: int = 0) -> dict:
    key = jax.random.key(seed)
    ks = jax.random.split(key, 32)
    L, D, E, Fd = DEPTH, D_MODEL, N_EXPERTS, D_EXPERT

    def nrm(k, shape, scale):
        return scale * jax.random.normal(k, shape, F32)

    def gain(k, shape):
        return 1.0 + 0.1 * jax.random.normal(k, shape, F32)

    positions = (jnp.arange(SEQ, dtype=jnp.int32)[None, :]
                 + jax.random.randint(ks[1], (BATCH, 1), 0, 1024, dtype=jnp.int32))
    return {
        "x": nrm(ks[0], (BATCH, SEQ, D), 1.0),
        "positions": positions,
        "w_in": nrm(ks[2], (L, D, IN_WIDTH), D ** -0.5),
        "hg_lower_bounds": nrm(ks[3], (L, HG_WIDTH), 1.0),
        "hg_norm_w": gain(ks[4], (L, HG_WIDTH)),
        "gm_ln_w": gain(ks[5], (L, GM_WIDTH)),
        "gm_ln_b": nrm(ks[6], (L, GM_WIDTH), 0.02),
        "gm_spatial_w": nrm(ks[7], (L, GM_GROUPS, GM_CHUNK, GM_CHUNK), GM_CHUNK ** -0.5),
        "gm_spatial_b": gain(ks[8], (L, GM_GROUPS, GM_CHUNK)),
        "gm_norm_w": gain(ks[9], (L, GM_WIDTH)),
        "nsa_cmp_pe": nrm(ks[10], (L, 2, CMP_BLOCK, HEAD_DIM), 0.1),
        "nsa_cmp_w1": nrm(ks[11], (L, 2, CMP_BLOCK * HEAD_DIM, CMP_HIDDEN), (CMP_BLOCK * HEAD_DIM) ** -0.5),
        "nsa_cmp_w2": nrm(ks[12], (L, 2, CMP_HIDDEN, HEAD_DIM), CMP_HIDDEN ** -0.5),
        "nsa_norm_w": gain(ks[13], (L, NSA_WIDTH)),
        "w_out": nrm(ks[14], (L, MIX_WIDTH, D), DEEPNORM_BETA * MIX_WIDTH ** -0.5),
        "ln1_w": gain(ks[15], (L, D)),
        "ln1_b": nrm(ks[16], (L, D), 0.02),
        "router_w": nrm(ks[17], (L, D, E), D ** -0.5),
        "router_b": nrm(ks[18], (L, E), 0.01),
        "exp_w_up": nrm(ks[19], (L, E, D, 2 * Fd), D ** -0.5),
        "exp_b_up": nrm(ks[20], (L, E, 2 * Fd), 0.01),
        "exp_w_down": nrm(ks[21], (L, E, Fd, D), DEEPNORM_BETA * Fd ** -0.5),
        "exp_b_down": nrm(ks[22], (L, E, D), 0.01),
        "ln2_w": gain(ks[23], (L, D)),
        "ln2_b": nrm(ks[24], (L, D), 0.02),
    }


def reference(x, positions, w_in, hg_lower_bounds, hg_norm_w, gm_ln_w, gm_ln_b, gm_spatial_w,
              gm_spatial_b, gm_norm_w, nsa_cmp_pe, nsa_cmp_w1, nsa_cmp_w2, nsa_norm_w, w_out,
              ln1_w, ln1_b, router_w, router_b, exp_w_up, exp_b_up, exp_w_down, exp_b_down,
              ln2_w, ln2_b):
    B, S, D = x.shape
    cos, sin = rope_tables(positions)
    lb_all = jnp.cumsum(jax.nn.softmax(hg_lower_bounds.astype(F32), axis=0), axis=0)
    lb_all = lb_all - lb_all[0:1]
    split_points = [int(p) for p in np.cumsum(IN_SPLITS)[:-1]]

    def heads(a, n):
        return a.reshape(B, S, n, HEAD_DIM)

    for l in range(DEPTH):
        h = x @ w_in[l]
        (hg_q, hg_f, hg_i, hg_g, gm_u, gm_v, nsa_q, k_c, v_c, k_s, v_s, k_w, v_w,
         nsa_g) = jnp.split(h, split_points, axis=-1)
        y_hg = hgrn2_mixer(hg_q, hg_f, hg_i, hg_g, lb_all[l], hg_norm_w[l])
        y_gm = gmlp_mixer(gm_u, gm_v, gm_ln_w[l], gm_ln_b[l], gm_spatial_w[l], gm_spatial_b[l], gm_norm_w[l])
        gates = jax.nn.sigmoid(nsa_g).reshape(B, S, NSA_HEADS, N_GATES)
        y_nsa = nsa_mixer(heads(nsa_q, NSA_HEADS), heads(k_c, NSA_KV_GROUPS), heads(v_c, NSA_KV_GROUPS),
                          heads(k_s, NSA_KV_GROUPS), heads(v_s, NSA_KV_GROUPS), heads(k_w, NSA_KV_GROUPS),
                          heads(v_w, NSA_KV_GROUPS), gates, nsa_cmp_pe[l], nsa_cmp_w1[l], nsa_cmp_w2[l],
                          cos, sin)
        y_nsa = rms_norm_heads(y_nsa, nsa_norm_w[l])
        mix = jnp.concatenate([y_hg, y_gm, y_nsa], axis=-1) @ w_out[l]
        x = layer_norm(DEEPNORM_ALPHA * x + mix, ln1_w[l], ln1_b[l])
        moe_out = moe_ffn(x.reshape(B * S, D), router_w[l], router_b[l], exp_w_up[l], exp_b_up[l],
                          exp_w_down[l], exp_b_down[l]).reshape(B, S, D)
        x = layer_norm(DEEPNORM_ALPHA * x + moe_out, ln2_w[l], ln2_b[l])
    return x
```

```python
import math
from contextlib import ExitStack

import numpy as np
import concourse.bass as bass
import concourse.mybir as mybir
from concourse.bass_utils import run_bass_kernel_spmd

F32 = mybir.dt.float32
BF16 = mybir.dt.bfloat16
I32 = mybir.dt.int32
U32 = mybir.dt.uint32
AF = mybir.ActivationFunctionType
ALU = mybir.AluOpType
AX = mybir.AxisListType

S = 4096
D = 1024
NT = S // 128
L = 2
NE = 32
CAP = 1024
NCOLA = 1816
NCH_B = 14
WCOLS = NCOLA + NCH_B * 128
ALPHA = (2 * L) ** 0.25
PI = math.pi
TRASH = NE * CAP

COMPUTE = ("pe", "dve", "act", "pool")
DMAQ = ("sp", "act", "pool")
NDMASEM = 6
NCSEM = 8
SEM_KEYS = [(e, i) for e in COMPUTE for i in range(NCSEM)] + [(q + "_q", i) for q in DMAQ for i in range(NDMASEM)]


class Op:
    __slots__ = ("eng", "fn", "reads", "writes", "dma", "deps", "raw", "need_inc", "sem", "val", "idx")


class Prog:
    def __init__(self, nc, sems):
        self.nc = nc
        self.sems = sems
        self.eng = {"pe": nc.tensor, "dve": nc.vector, "act": nc.scalar, "pool": nc.gpsimd, "sp": nc.sync}
        self.ops = []
        self.last_writer = {}
        self.readers = {}
        self.cnt = {e: 0 for e in COMPUTE}
        self.dcnt = {q: 0 for q in DMAQ}
        self.waited = {}
        self.dma_last = {}
        self.nwait = 0
        self.ntotal = 0
        import os
        self.verbose = bool(os.environ.get("VERB"))

    def op(self, eng, fn, reads=(), writes=(), dma=False):
        o = Op()
        o.eng, o.fn, o.dma = eng, fn, dma
        o.reads, o.writes = tuple(reads), tuple(writes)
        o.need_inc = False
        o.idx = len(self.ops)
        deps = set()
        raw = set()
        lw, rd = self.last_writer, self.readers
        for k in o.reads:
            w = lw.get(k)
            if w is not None:
                deps.add(w)
                raw.add(w)
        for k in o.writes:
            w = lw.get(k)
            if w is not None:
                deps.add(w)
            r = rd.get(k)
            if r:
                deps.update(r)
        for k in o.reads:
            rd.setdefault(k, []).append(o.idx)
        for k in o.writes:
            lw[k] = o.idx
            rd[k] = []
        deps.discard(o.idx)
        o.deps = deps
        o.raw = raw
        self.ops.append(o)
        return o

    def dma(self, q, out, in_, reads=(), writes=(), **kw):
        e = self.eng[q]
        return self.op(q, lambda: e.dma_start(out=out, in_=in_, **kw), reads, writes, dma=True)

    def barrier(self):
        ops = self.ops
        tails = set()
        last_by_eng = {}
        for o in ops:
            if o.dma:
                tails.add(o.idx)
            else:
                last_by_eng[o.eng] = o.idx
        tails.update(last_by_eng.values())
        for en in ("pe", "dve", "act", "pool", "sp"):
            o = Op()
            o.eng, o.dma, o.reads, o.writes, o.need_inc = en, False, (), (), False
            e = self.eng[en]
            o.fn = (lambda e=e: e.nop())
            o.idx = len(ops)
            o.deps = set(tails)
            o.raw = set()
            ops.append(o)
        self.emit()

    def emit(self):
        ops, sems = self.ops, self.sems
        def needs_sync(o, d):
            p = ops[d]
            return p.dma or p.eng != o.eng or (o.eng != "pe" and d in o.raw)
        for o in ops:
            for d in o.deps:
                if needs_sync(o, d):
                    ops[d].need_inc = True
        cnt, dcnt, waited = self.cnt, self.dcnt, self.waited
        for o in ops:
            e = self.eng[o.eng]
            pre = []
            if o.dma:
                i = dcnt[o.eng]
                dcnt[o.eng] += 1
                sk = (o.eng + "_q", i % NDMASEM)
                o.sem = sk
                o.val = 16 * (i // NDMASEM + 1)
                self.dma_last[sk] = o.val
                if o.val > 16:
                    pre.append((sk, o.val - 16))
            for d in o.deps:
                p = ops[d]
                if p.dma or needs_sync(o, d):
                    pre.append((p.sem, p.val))
            best = {}
            for sk, v in pre:
                if v > best.get(sk, 0):
                    best[sk] = v
            for sk, v in best.items():
                if waited.get((o.eng, sk), 0) >= v:
                    continue
                waited[(o.eng, sk)] = v
                e.wait_ge(sems[sk], v)
                self.nwait += 1
                if self.verbose:
                    print("   wait", o.eng, sk, v)
            ins = o.fn()
            if self.verbose:
                print("op", o.idx, o.eng, "dma" if o.dma else "", o.writes, "inc" if (o.need_inc or o.dma) else "", getattr(o, "sem", None) if o.dma else "", (o.val if o.dma else ""))
            if o.dma:
                ins.then_inc(sems[o.sem], 16)
            elif o.need_inc:
                i = cnt[o.eng]
                cnt[o.eng] += 1
                o.sem = (o.eng, i % NCSEM)
                o.val = i // NCSEM + 1
                ins.then_inc(sems[o.sem], 1)
        self.ntotal += len(ops)
        self.ops = []
        self.last_writer = {}
        self.readers = {}

    def finish(self):
        self.emit()
        e = self.eng["sp"]
        for sk, v in self.dma_last.items():
            e.wait_ge(self.sems[sk], v)


def host_consts():
    c = {}
    c["ident"] = np.eye(128, dtype=np.float32)
    s = np.arange(128)[:, None]
    t = np.arange(128)[None, :]
    same = (s // 64) == (t // 64)
    mid = (t // 64) * 64 + 31
    c["mcum"] = (same & (s <= t)).astype(np.float32)
    c["mmid"] = (c["mcum"] - (same & (s <= mid)).astype(np.float32))
    c["mrem"] = (same & (s > t)).astype(np.float32)
    c["tri"] = (s <= t).astype(np.float32)
    c["trit"] = (s >= t).astype(np.float32)
    inv = (10000.0 ** (-np.arange(0, 64, 2, dtype=np.float32) / 64)).astype(np.float32)
    p = np.arange(128)
    sgn = np.where((p % 64) < 32, -1.0, 1.0).astype(np.float32)
    col = np.zeros((128, 4), np.float32)
    col[:, 0] = inv[p % 32]
    col[:, 1] = sgn
    col[:, 2] = 2 * PI * sgn
    col[:, 3] = -PI
    c["ropecol"] = col
    jj = np.arange(64)[:, None, None]
    kt_ = np.arange(32)[None, :, None]
    kk_ = np.arange(128)[None, None, :]
    E = (jj == 2 * kt_ + kk_ // 64).astype(np.float32)
    c["edup"] = np.concatenate([E, E], 0)
    p_ = np.arange(128)[:, None]
    q_ = np.arange(512)[None, :]
    c["win01"] = np.stack([((128 * dl + p_ - q_ <= 0) & (128 * dl + p_ - q_ > -512)).astype(np.float32) for dl in range(-4, 4)], 1)
    c["cmp01"] = np.stack([(16 * p_ + 31 - q_ <= u).astype(np.float32) for u in (0, 512, 1024, 1536, 2048)], 1)
    tq = np.arange(S).reshape(NT, 128)
    cur = (tq // 64)[:, :, None]
    jb = np.arange(64)[None, None, :]
    fut = jb > cur
    frc = (jb == 0) | (jb == cur) | (jb == cur - 1)
    keep = (~fut & ~frc).astype(np.float32)
    add = np.where(frc, 1e9, np.where(fut, -1e9, 0.0)).astype(np.float32)
    c["wpad"] = np.ascontiguousarray(np.maximum(0, 511 - tq).T.astype(np.float32))
    c["impkeep"] = np.ascontiguousarray(keep.transpose(1, 0, 2))
    c["impadd"] = np.ascontiguousarray(add.transpose(1, 0, 2))
    cc = np.arange(256)
    ov = np.zeros((256, 65), np.float32)
    for c_ in range(255):
        for uu in (c_, c_ + 1):
            ov[c_, uu // 4] += 1.0
    ov[:, 64] = 1.0
    c["ovext"] = np.ascontiguousarray(ov.reshape(2, 128, 65).transpose(1, 0, 2))
    c["eoff"] = np.tile((np.arange(NE, dtype=np.float32) * CAP)[None, :], (128, 1)).astype(np.float32)
    c["trashp"] = (TRASH + np.arange(128, dtype=np.float32)).reshape(128, 1).astype(np.float32)
    return c


def w_in_perm():
    hg_q, hg_f, hg_i, hg_g, gm_u, gm_v, q, k_c, v_c, k_s, v_s, k_w, v_w, gts = (
        0, 256, 512, 768, 1024, 1280, 1536, 2048, 2176, 2304, 2432, 2560, 2688, 2816)
    A = list(range(0, 1536)) + list(range(v_s, v_s + 128)) + list(range(v_w, v_w + 128)) + list(range(gts, gts + 24))

    def sw(c0, n):
        out = []
        for h in range(n // 64):
            b = c0 + h * 64
            out += list(range(b + 32, b + 64)) + list(range(b, b + 32))
        return out
    B = (list(range(q, q + 512)) + sw(q, 512) + list(range(k_s, k_s + 128)) + sw(k_s, 128)
         + list(range(k_w, k_w + 128)) + sw(k_w, 128) + list(range(k_c, k_c + 128)) + list(range(v_c, v_c + 128)))
    perm = np.array(A + B, dtype=np.int64)
    assert perm.shape[0] == WCOLS
    return perm


class K:
    pass


def build(nlayers=L, dbg=False, stop_after=None, stage=None, inject_nsa=False, phases=(1, 2, 3, 4, 5)):
    nc = bass.Bass("TRN2", target_bir_lowering=False)
    k = K()
    global _LASTK
    _LASTK = k
    k.stage = stage
    import os
    k.dbgv = int(os.environ.get('DBGV', '0'))
    k.nc = nc
    k.dbg = dbg

    def din(name, shape, dt=F32):
        return nc.dram_tensor(name, list(shape), dt, kind="ExternalInput").ap()

    def dscr(name, shape, dt, out=False):
        return nc.dram_tensor(name, list(shape), dt, kind=("ExternalOutput" if (out and dbg) else "Internal")).ap()

    k.x = din("x", [S, D])
    k.pos = din("pos", [1, S], I32)
    k.w_in = din("w_in", [L, D, WCOLS])
    k.lbraw = din("hg_lb", [L, 256])
    k.hg_nw = din("hg_nw", [L, 256])
    k.gm_lnw = din("gm_lnw", [L, 256])
    k.gm_lnb = din("gm_lnb", [L, 256])
    k.gm_ws = din("gm_ws", [L, 4, 128, 128])
    k.gm_bs = din("gm_bs", [L, 4, 128])
    k.gm_nw = din("gm_nw", [L, 256])
    k.nsa_pe = din("nsa_pe", [L, 2, 32, 64]); k.nsa_w1 = din("nsa_w1", [L, 2, 2048, 128])
    k.nsa_w2 = din("nsa_w2", [L, 2, 128, 64]); k.nsa_nw = din("nsa_nw", [L, 512])
    k.w_out = din("w_out", [L, D, D])
    k.ln1_w = din("ln1_w", [L, D]); k.ln1_b = din("ln1_b", [L, D])
    k.ln2_w = din("ln2_w", [L, D]); k.ln2_b = din("ln2_b", [L, D])
    k.router_w = din("router_w", [L, D, NE]); k.router_b = din("router_b", [L, NE])
    k.exp_w_up = din("exp_w_up", [L, NE, D, 2 * D]); k.exp_b_up = din("exp_b_up", [L, NE, 2 * D])
    k.exp_w_down = din("exp_w_down", [L, NE, D, D]); k.exp_b_down = din("exp_b_down", [L, NE, D])
    k.inject_nsa = inject_nsa
    if dbg and inject_nsa:
        k.ynsa_in = din("ynsa_in", [L, S, 512], BF16)
    k.cst = {n: din("c_" + n, v.shape) for n, v in host_consts().items()}
    k.out = nc.dram_tensor("out", [S, D], F32, kind="ExternalOutput").ap()
    k.ft = dscr("ft", [12, 128, S], BF16, out=True)
    k.tmv = dscr("tmv", [S, 256], BF16, out=True)
    k.gts = dscr("gts", [S, 24], F32, out=True)
    k.y = dscr("y", [S, D], BF16, out=True)
    k.x1 = dscr("x1", [S, D], F32, out=True)
    k.x2 = dscr("x2", [S, D], F32, out=True)
    k.xbuf = dscr("xbuf", [TRASH + 128, D], BF16)
    k.ybuf = dscr("ybuf", [TRASH + 128, D], BF16)
    k.moe_dbg = dscr("moe_dbg", [S, D], F32, out=True) if dbg else None
    k.dbgout = dscr("dbgout", [24, 128, 512], F32, out=True)
    k.ndump = 0
    k.dumpnames = []

    with ExitStack() as es:
        sems = {}
        for sk in SEM_KEYS:
            nm = sk if isinstance(sk, str) else f"{sk[0]}{sk[1]}"
            sems[sk] = es.enter_context(nc.semaphore("s_" + nm))
        P = Prog(nc, sems)
        k.P = P
        k.es = es
        setup_consts(k)
        for l in range(nlayers if stop_after != "setup" else 0):
            if 1 in phases:
                phase1(k, l)
            if stop_after == (l, 1):
                break
            if 2 in phases:
                phase2(k, l)
            if stop_after == (l, 2):
                break
            if 3 in phases:
                phase3(k, l)
            if stop_after == (l, 3):
                break
            if 4 in phases:
                phase4(k, l)
            if 5 in phases:
                phase5(k, l, last=(l == nlayers - 1))
        P.finish()
        print("[build] ops", P.ntotal, "waits", P.nwait, "cnt", P.cnt, "dcnt", P.dcnt)
    return nc


def setup_consts(k):
    nc, P, es = k.nc, k.P, k.es
    sb = lambda name, shape, dt: es.enter_context(nc.sbuf_tensor(name, shape, dt))
    k.ident_f = sb("ident_f", [128, 128], F32)
    k.ident_b = sb("ident_b", [128, 128], BF16)
    k.mcum = sb("mcum", [128, 128], F32)
    k.mmid = sb("mmid", [128, 128], F32)
    k.mrem = sb("mrem", [128, 128], F32)
    k.tri = sb("tri", [128, 128], F32)
    k.trit = sb("trit", [128, 128], F32)
    k.mcum_b = sb("mcum_b", [128, 128], BF16)
    k.mmid_b = sb("mmid_b", [128, 128], BF16)
    k.mrem_b = sb("mrem_b", [128, 128], BF16)
    k.ones_b = sb("ones_b", [128, 128], BF16)
    k.ropecol = sb("ropecol", [128, 4], F32)
    k.ones_f = sb("ones_f", [128, 128], F32)
    k.idx4 = sb("idx4", [128, NT, 4], I32)
    k.w4 = sb("w4", [128, NT, 4], F32)
    k.eoff = sb("eoff", [128, NE], F32)
    k.trashp = sb("trashp", [128, 1], F32)
    k.ustrict_b = sb("ustrict_b", [128, 128], BF16)
    for n, t in (("ident", k.ident_f), ("mcum", k.mcum), ("mmid", k.mmid), ("mrem", k.mrem), ("tri", k.tri),
                 ("trit", k.trit), ("ropecol", k.ropecol), ("eoff", k.eoff), ("trashp", k.trashp)):
        P.dma("sp", t[:], k.cst[n], writes=[n])
    P.op("dve", lambda: nc.vector.tensor_copy(out=k.ident_b[:], in_=k.ident_f[:]), reads=["ident"], writes=["ident_b"])
    P.op("dve", lambda: nc.vector.memset(k.ones_f[:], 1.0), writes=["ones_f"])
    P.op("dve", lambda: nc.vector.memset(k.ones_b[:], 1.0), writes=["ones_b"])
    P.op("dve", lambda: nc.vector.tensor_sub(out=k.ones_f[:], in0=k.tri[:], in1=k.ident_f[:]), reads=["tri", "ident", "ones_f"], writes=["ustr_tmp"])
    P.op("dve", lambda: nc.vector.tensor_copy(out=k.ustrict_b[:], in_=k.ones_f[:]), reads=["ustr_tmp"], writes=["ustrict_b"])
    P.op("dve", lambda: nc.vector.memset(k.ones_f[:], 1.0), reads=["ustrict_b"], writes=["ones_f"])
    P.op("dve", lambda: nc.vector.tensor_copy(out=k.mcum_b[:], in_=k.mcum[:]), reads=["mcum"], writes=["mcum_b"])
    P.op("dve", lambda: nc.vector.tensor_copy(out=k.mmid_b[:], in_=k.mmid[:]), reads=["mmid"], writes=["mmid_b"])
    P.op("dve", lambda: nc.vector.tensor_copy(out=k.mrem_b[:], in_=k.mrem[:]), reads=["mrem"], writes=["mrem_b"])
    P.barrier()


def setup_rope(k, es, l):
    nc, P = k.nc, k.P
    sb = lambda name, shape, dt: es.enter_context(nc.sbuf_tensor(f"{name}l{l}", shape, dt))
    k.ropeC = sb("ropeC", [128, S], F32)
    k.ropeS = sb("ropeS", [128, S], F32)
    with nc.sbuf_tensor(f"posil{l}", [128, S], I32) as posi, nc.sbuf_tensor(f"ropeTl{l}", [128, S], F32) as ropeT, nc.sbuf_tensor(f"ropeT2l{l}", [128, S], F32) as ropeT2:
        k.ropeT, k.ropeT2 = ropeT, ropeT2
        P.dma("sp", posi[:], k.pos.partition_broadcast(128), writes=["posi"])
        C, Sg, col = k.ropeC, k.ropeS, k.ropecol
        P.op("dve", lambda: nc.vector.tensor_copy(out=C[:], in_=posi[:]), reads=["posi"], writes=["C"])
        P.op("dve", lambda: nc.vector.tensor_scalar(out=C[:], in0=C[:], scalar1=col[:, 0:1], scalar2=None, op0=ALU.mult),
             reads=["C", "ropecol"], writes=["C"])
        def red(T, add):
            P.op("dve", lambda: nc.vector.tensor_scalar(out=T[:], in0=C[:], scalar1=1.0 / (2 * PI), scalar2=add, op0=ALU.mult, op1=ALU.add),
                 reads=["C"], writes=["T" + str(add)])
            P.op("dve", lambda: nc.vector.tensor_copy(out=posi[:], in_=T[:]), reads=["T" + str(add)], writes=["posi"])
            P.op("dve", lambda: nc.vector.tensor_copy(out=k.ropeT[:], in_=posi[:]), reads=["posi"], writes=["ropeT"])
            P.op("dve", lambda: nc.vector.tensor_sub(out=T[:], in0=T[:], in1=k.ropeT[:]), reads=["ropeT", "T" + str(add)], writes=["T" + str(add)])
            P.op("dve", lambda: nc.vector.tensor_scalar(out=k.ropeT[:], in0=T[:], scalar1=0.5, scalar2=None, op0=ALU.is_gt), reads=["T" + str(add)], writes=["ropeT"])
            P.op("dve", lambda: nc.vector.tensor_sub(out=T[:], in0=T[:], in1=k.ropeT[:]), reads=["ropeT", "T" + str(add)], writes=["T" + str(add)])
        red(Sg, 0.0)
        P.op("act", lambda: nc.scalar.activation(out=Sg[:], in_=Sg[:], func=AF.Sin, scale=col[:, 2:3]), reads=["T0.0", "ropecol"], writes=["S"])
        red(k.ropeT2, 0.25)
        P.op("act", lambda: nc.scalar.activation(out=C[:], in_=k.ropeT2[:], func=AF.Sin, scale=2 * PI), reads=["T0.25", "C"], writes=["C"])
        P.barrier()


def dump(k, name, ap, key, width, parts=128):
    if not k.dbg:
        return
    i = k.ndump
    k.ndump += 1
    k.dumpnames.append(name)
    k.P.dma("pool", k.dbgout[i, 0:parts, 0:width], ap, reads=[key], writes=[("dbgout", i)])


def bcast_row(k, P, q, dst, src_row, key):
    P.dma(q, dst, src_row.partition_broadcast(128), writes=[key])


class _Stop(Exception):
    pass


def phase1(k, l):
    with ExitStack() as es:
        setup_rope(k, es, l)
        try:
            _phase1(k, l, es)
        except _Stop:
            pass
        k.P.barrier()


def ck(k, n):
    if k.stage == n:
        raise _Stop()


def _phase1(k, l, es):
    nc, P = k.nc, k.P
    if True:
        sb = lambda name, shape, dt: es.enter_context(nc.sbuf_tensor(f"p1l{l}_{name}", shape, dt))
        ps = lambda name, shape, dt: es.enter_context(nc.psum_tensor(f"p1l{l}_{name}", shape, dt))
        wb = sb("wb", [128, 8, WCOLS], BF16)
        xt = [sb(f"xt{i}", [128, D], F32) for i in range(2)]
        xb = [sb(f"xb{i}", [128, D], BF16) for i in range(2)]
        xTb = [sb(f"xTb{i}", [128, 8, 512], BF16) for i in range(2)]
        hg_nw = sb("hg_nw", [128, 256], F32)
        gm_lnw = sb("gm_lnw", [128, 256], F32)
        gm_lnb = sb("gm_lnb", [128, 256], F32)
        gm_nw = sb("gm_nw", [128, 256], F32)
        lbt = sb("lbt", [128, 2, 256], F32)
        lb = sb("lb", [128, 256], F32)
        oml = sb("oml", [128, 256], F32)
        wsn = sb("wsn", [128, 4, 128], F32)
        wsb = sb("wsb", [128, 4, 128], BF16)
        wsT = sb("wsT", [128, 4, 128], BF16)
        bsT = sb("bsT", [128, 4], F32)
        pT = ps("pT", [128, 8, 128], BF16)
        pm = [ps(f"pm{i}", [128, 512], F32) for i in range(4)]
        pa = ps("pa", [128, 512], F32)
        pb = ps("pb", [128, 512], F32)
        pc = ps("pc", [128, 512], F32)
        pcb = pc[:].bitcast(BF16)

        x_src = k.x if l == 0 else k.x2
        wv = k.w_in[l].rearrange("(c p) n -> p c n", p=128)
        half = WCOLS // 2
        for c in range(8):
            for hh in range(2):
                P.dma("pool", wb[:, c, hh * half:(hh + 1) * half], wv[:, c, hh * half:(hh + 1) * half], writes=[("wb", c), ("wbser", (2 * c + hh) % 2)])
        bcast_row(k, P, "sp", hg_nw[:], k.hg_nw[l:l + 1, :], "hg_nw")
        bcast_row(k, P, "sp", gm_lnw[:], k.gm_lnw[l:l + 1, :], "gm_lnw")
        bcast_row(k, P, "sp", gm_lnb[:], k.gm_lnb[l:l + 1, :], "gm_lnb")
        bcast_row(k, P, "sp", gm_nw[:], k.gm_nw[l:l + 1, :], "gm_nw")
        if l > 0:
            P.dma("sp", lbt[:].rearrange("p a n -> p (a n)"),
                  k.lbraw.rearrange("a n -> (a n)").unsqueeze(0).partition_broadcast(128), writes=["lbt"])
            P.op("dve", lambda: nc.vector.tensor_sub(out=lb[:], in0=lbt[:, 1, :], in1=lbt[:, 0, :]), reads=["lbt"], writes=["lb"])
            P.op("act", lambda: nc.scalar.activation(out=lb[:], in_=lb[:], func=AF.Sigmoid), reads=["lb"], writes=["lb"])
            P.op("dve", lambda: nc.vector.tensor_scalar(out=oml[:], in0=lb[:], scalar1=-1.0, scalar2=1.0, op0=ALU.mult, op1=ALU.add),
                 reads=["lb"], writes=["oml"])
        P.dma("sp", wsn[:], k.gm_ws[l].rearrange("g t s -> t g s"), writes=["wsn"])
        P.dma("sp", bsT[:], k.gm_bs[l].rearrange("g t -> t g"), writes=["bsT"], allow_slow_non_contiguous=True)
        trit = k.trit
        for g in range(4):
            P.op("dve", lambda g=g: nc.vector.tensor_tensor(out=wsb[:, g, :], in0=wsn[:, g, :], in1=trit[:], op=ALU.mult),
                 reads=["wsn"], writes=["wsb"])
        for g in range(4):
            P.op("pe", lambda g=g: nc.tensor.transpose(out=pT[:, g, :], in_=wsb[:, g, :], identity=k.ident_b[:]),
                 reads=["wsb", "ident_b"], writes=["pT"])
        P.op("dve", lambda: nc.vector.tensor_copy(out=wsT[:], in_=pT[:, 0:4, :]), reads=["pT"], writes=["wsT"])

        st_f = sb("st_f", [128, 2, 64], F32)
        st_b = sb("st_b", [128, 2, 64], BF16)
        P.op("dve", lambda: nc.vector.memset(st_f[:], 0.0), writes=["st_f"])
        P.op("dve", lambda: nc.vector.memset(st_b[:], 0.0), writes=["st_b"])

        W = {}
        for n in ("qf", "sg", "sgate", "logf", "kk", "ebm", "enbm", "eb", "erem", "t1", "t2", "osq", "nwg", "gsq", "ginn", "gsv"):
            W[n] = sb(n, [128, 256], F32)
        gx = sb("gx", [128, 512], F32)
        gg = sb("gg", [128, 512], F32)
        gt = sb("gt", [128, 512], F32)
        vb = sb("vb", [128, 256], BF16)
        qe = sb("qe", [128, 256], BF16)
        ke = sb("ke", [128, 256], BF16)
        qb = sb("qb", [128, 256], BF16)
        kd = sb("kd", [128, 256], BF16)
        vln = sb("vln", [128, 256], BF16)
        lfh = sb("lfh", [128, 256], BF16)
        lfl = sb("lfl", [128, 256], BF16)
        trT = sb("trT", [128, 6, 128], BF16)
        attb = sb("attb", [128, 4, 128], BF16)
        ebl = sb("ebl", [128, 2, 2], F32)
        ss4 = sb("ss4", [128, 8], F32)
        rs4 = sb("rs4", [128, 8], F32)
        bnst = sb("bnst", [128, 6], F32)
        bnag = sb("bnag", [128, 2], F32)
        ytile = [sb(f"ytile{i}", [128, 512], BF16) for i in range(2)]
        vstage = [sb(f"vstage{i}", [128, 256], BF16) for i in range(2)]
        gstage = [sb(f"gstage{i}", [128, 24], F32) for i in range(2)]
        fst_raw = [sb(f"fst_raw{i}", [128, 512], BF16) for i in range(2)]
        fst_rot = [sb(f"fst_rot{i}", [128, 512], BF16) for i in range(2)]
        r1 = [sb(f"r1_{i}", [128, 512], F32) for i in range(2)]
        r2 = [sb(f"r2_{i}", [128, 512], F32) for i in range(2)]

        V = nc.vector
        A = nc.scalar
        nfm = 0
        ck(k, 0)
        for tb in range(S // 512):
            xTc = xTb[tb % 2]
            kx = ("xTb", tb % 2)
            for tt in range(4):
                t = tb * 4 + tt
                i2 = t % 2
                P.dma("sp", xt[i2][:], x_src[t * 128:(t + 1) * 128, :], writes=[("xt", i2)])
                P.op("act", lambda i2=i2: A.copy(out=xb[i2][:], in_=xt[i2][:]), reads=[("xt", i2)], writes=[("xb", i2)])
                for c in range(8):
                    P.op("pe", lambda c=c, i2=i2: nc.tensor.transpose(out=pT[:, c, :], in_=xb[i2][:, c * 128:(c + 1) * 128], identity=k.ident_b[:]),
                         reads=[("xb", i2), "ident_b"], writes=["pT"])
                P.op("dve", lambda tt=tt, xTc=xTc: V.tensor_copy(out=xTc[:, :, tt * 128:(tt + 1) * 128], in_=pT[:]), reads=["pT"], writes=[kx])
            ck(k, 1)
            blk = slice(tb * 512, (tb + 1) * 512)

            def fm(ch, dst, xTc=xTc, kx=kx):
                for c in range(8):
                    P.op("pe", lambda c=c, ch=ch, dst=dst, xTc=xTc: nc.tensor.matmul(dst[:], lhsT=wb[:, c, NCOLA + ch * 128:NCOLA + (ch + 1) * 128], rhs=xTc[:, c, :],
                                                                                     start=(c == 0), stop=(c == 7)), reads=[("wb", c), kx], writes=[("pm", id(dst))])
            plan = [(0, 4, 0, 4), (1, 5, 1, 5), (2, 6, 2, 6), (3, 7, 3, 7), (8, 9, None, 8), (10, 11, None, 9)]
            for (cr, cs, fraw, frot) in plan:
                ck(k, 1.5 + 0.01 * nfm)
                j = nfm % 2
                nfm += 1
                pA, pB = pm[2 * j], pm[2 * j + 1]
                fm(cr, pA)
                ck(k, 1.21)
                fm(cs, pB)
                ck(k, 1.22)
                if k.dbgv == 13:
                    P.op("dve", lambda j=j, pA=pA: V.tensor_tensor(out=r1[j][:], in0=pA[:], in1=k.ropeC[:, blk], op=ALU.mult),
                         reads=[("pm", id(pA))], writes=[("r1", j)])
                if fraw is not None and k.dbgv == 14:
                    P.op("dve", lambda j=j, pA=pA: V.tensor_copy(out=fst_raw[j][:], in_=pA[:]), reads=[("pm", id(pA))], writes=[("fst_raw", j)])
                elif fraw is not None:
                    P.op("act", lambda j=j, pA=pA: A.copy(out=fst_raw[j][:], in_=pA[:]), reads=[("pm", id(pA))] + ([("r1", j)] if k.dbgv == 13 else []), writes=[("fst_raw", j)])
                    ck(k, 1.23)
                    P.dma("sp", k.ft[fraw, :, blk], fst_raw[j][:], reads=[("fst_raw", j)], writes=[("ft", fraw, tb)])
                ck(k, 1.24)
                P.op("act", lambda j=j, pA=pA: A.copy(out=r1[j][:], in_=pA[:]), reads=[("pm", id(pA))], writes=[("r1", j)])
                P.op("act", lambda j=j, pB=pB: A.copy(out=r2[j][:], in_=pB[:]), reads=[("pm", id(pB))], writes=[("r2", j)])
                P.op("dve", lambda j=j, blk=blk: V.tensor_tensor(out=r1[j][:], in0=r1[j][:], in1=k.ropeC[:, blk], op=ALU.mult), reads=[("r1", j)], writes=[("r1", j)])
                P.op("dve", lambda j=j, blk=blk: V.tensor_tensor(out=r2[j][:], in0=r2[j][:], in1=k.ropeS[:, blk], op=ALU.mult), reads=[("r2", j)], writes=[("r2", j)])
                P.op("dve", lambda j=j: V.tensor_tensor(out=fst_rot[j][:], in0=r1[j][:], in1=r2[j][:], op=ALU.add),
                     reads=[("r1", j), ("r2", j)], writes=[("fst_rot", j)])
                ck(k, 1.243)
                P.dma("sp", k.ft[frot, :, blk], fst_rot[j][:], reads=[("fst_rot", j)], writes=[("ft", frot, tb)])
            for (cr, fi) in ((12, 10), (13, 11)):
                j = nfm % 2
                nfm += 1
                pA = pm[2 * j]
                fm(cr, pA)
                P.op("act", lambda j=j, pA=pA: A.copy(out=fst_raw[j][:], in_=pA[:]), reads=[("pm", id(pA))], writes=[("fst_raw", j)])
                P.dma("sp", k.ft[fi, :, blk], fst_raw[j][:], reads=[("fst_raw", j)], writes=[("ft", fi, tb)])

            ck(k, 2)
            for tt in range(4):
                t = tb * 4 + tt
                i2 = t % 2
                rows = slice(t * 128, (t + 1) * 128)
                xs = lambda c, xTc=xTc, tt=tt: xTc[:, c, tt * 128:(tt + 1) * 128]
                colgrp = [(0, 512), (512, 512), (1024, 512), (1536, 280)]
                for gi, (c0, wdt) in enumerate(colgrp):
                    for c in range(8):
                        P.op("pe", lambda c=c, gi=gi, c0=c0, wdt=wdt, xs=xs: nc.tensor.matmul(pm[gi][:, 0:wdt], lhsT=xs(c), rhs=wb[:, c, c0:c0 + wdt],
                                                                                       start=(c == 0), stop=(c == 7)),
                             reads=[("wb", c), kx], writes=[("pm", id(pm[gi]))])
                kp = [("pm", id(pm[gi])) for gi in range(4)]
                P.op("act", lambda: A.activation(out=W["qf"][:], in_=pm[0][:, 0:256], func=AF.Silu), reads=[kp[0]], writes=["qf"])
                P.op("act", lambda: A.activation(out=W["sg"][:], in_=pm[0][:, 256:512], func=AF.Sigmoid), reads=[kp[0]], writes=["sg"])
                P.op("act", lambda: A.activation(out=W["sgate"][:], in_=pm[1][:, 256:512], func=AF.Sigmoid), reads=[kp[1]], writes=["sgate"])
                P.op("act", lambda i2=i2: A.activation(out=gstage[i2][:], in_=pm[3][:, 256:280], func=AF.Sigmoid), reads=[kp[3]], writes=[("gstage", i2)])
                P.op("act", lambda: A.copy(out=vb[:], in_=pm[1][:, 0:256]), reads=[kp[1]], writes=["vb"])
                P.op("act", lambda i2=i2: A.copy(out=vstage[i2][:], in_=pm[3][:, 0:256]), reads=[kp[3]], writes=[("vstage", i2)])
                P.op("act", lambda: A.copy(out=gx[:], in_=pm[2][:]), reads=[kp[2]], writes=["gx"])
                P.dma("sp", k.tmv[rows, :], vstage[i2][:], reads=[("vstage", i2)], writes=[("tmv", t)])
                P.dma("sp", k.gts[rows, :], gstage[i2][:], reads=[("gstage", i2)], writes=[("gts", t)])

                ck(k, 3)
                if l == 0:
                    fsrc = W["sg"]
                    kf = "sg"
                else:
                    P.op("dve", lambda: V.tensor_tensor(out=W["t1"][:], in0=W["sg"][:], in1=oml[:], op=ALU.mult), reads=["sg", "oml"], writes=["t1"])
                    P.op("dve", lambda: V.tensor_tensor(out=W["t1"][:], in0=W["t1"][:], in1=lb[:], op=ALU.add), reads=["t1", "lb"], writes=["t1"])
                    fsrc = W["t1"]
                    kf = "t1"
                P.op("act", lambda fsrc=fsrc: A.activation(out=W["logf"][:], in_=fsrc[:], func=AF.Ln), reads=[kf], writes=["logf"])
                P.op("dve", lambda fsrc=fsrc: V.tensor_scalar(out=W["kk"][:], in0=fsrc[:], scalar1=-1.0, scalar2=1.0, op0=ALU.mult, op1=ALU.add),
                     reads=[kf], writes=["kk"])
                ck(k, 3.5)
                lf = W["logf"]
                P.op("dve", lambda: V.tensor_copy(out=lfh[:], in_=lf[:]), reads=["logf"], writes=["lfh"])
                P.op("dve", lambda: V.tensor_tensor(out=W["t2"][:], in0=lf[:], in1=lfh[:], op=ALU.subtract), reads=["logf", "lfh"], writes=["t2"])
                P.op("dve", lambda: V.tensor_copy(out=lfl[:], in_=W["t2"][:]), reads=["t2"], writes=["lfl"])
                for (dst, mat, kn) in ((pa[:, 0:256], k.mmid_b, "pa"), (pa[:, 256:512], k.mcum_b, "pa"), (pb[:, 0:256], k.mrem_b, "pb")):
                    P.op("pe", lambda dst=dst, mat=mat: nc.tensor.matmul(dst, lhsT=mat[:], rhs=lfh[:], start=True, stop=False), reads=["lfh"], writes=[kn])
                    P.op("pe", lambda dst=dst, mat=mat: nc.tensor.matmul(dst, lhsT=mat[:], rhs=lfl[:], start=False, stop=True), reads=["lfl"], writes=[kn])
                for c2 in range(2):
                    for hh in range(2):
                        for pi, part in enumerate((lfh, lfl)):
                            P.op("pe", lambda c2=c2, hh=hh, part=part, pi=pi: nc.tensor.matmul((pb[:, 256 + hh:257 + hh] if c2 == 0 else pm[2][:, hh:hh + 1]),
                                                                                             lhsT=part[c2 * 64:(c2 + 1) * 64, hh * 128:(hh + 1) * 128],
                                                                                             rhs=k.ones_b[c2 * 64:(c2 + 1) * 64, 0:1], start=(pi == 0), stop=(pi == 1)),
                                 reads=["lfh", "lfl", "ones_b"], writes=(["pb"] if c2 == 0 else [kp[2]]))
                P.op("act", lambda: A.activation(out=W["ebm"][:], in_=pa[:, 0:256], func=AF.Exp), reads=["pa"], writes=["ebm"])
                P.op("act", lambda: A.activation(out=W["enbm"][:], in_=pa[:, 0:256], func=AF.Exp, scale=-1.0), reads=["pa"], writes=["enbm"])
                P.op("act", lambda: A.activation(out=W["eb"][:], in_=pa[:, 256:512], func=AF.Exp), reads=["pa"], writes=["eb"])
                P.op("act", lambda: A.activation(out=W["erem"][:], in_=pb[:, 0:256], func=AF.Exp), reads=["pb"], writes=["erem"])
                P.op("act", lambda: A.activation(out=ebl[:, 0, :], in_=pb[:, 256:258], func=AF.Exp), reads=["pb"], writes=["ebl"])
                P.op("act", lambda: A.activation(out=ebl[:, 1, :], in_=pm[2][:, 0:2], func=AF.Exp), reads=[kp[2]], writes=["ebl"])
                P.op("dve", lambda: V.tensor_tensor(out=qe[:], in0=W["qf"][:], in1=W["ebm"][:], op=ALU.mult), reads=["qf", "ebm"], writes=["qe"])
                P.op("dve", lambda: V.tensor_tensor(out=ke[:], in0=W["kk"][:], in1=W["enbm"][:], op=ALU.mult), reads=["kk", "enbm"], writes=["ke"])
                P.op("dve", lambda: V.tensor_tensor(out=qb[:], in0=W["qf"][:], in1=W["eb"][:], op=ALU.mult), reads=["qf", "eb"], writes=["qb"])
                P.op("dve", lambda: V.tensor_tensor(out=kd[:], in0=W["kk"][:], in1=W["erem"][:], op=ALU.mult), reads=["kk", "erem"], writes=["kd"])
                for i, (src, kn) in enumerate(((qe, "qe"), (ke, "ke"), (qb, "qb"))):
                    for hh in range(2):
                        P.op("pe", lambda i=i, hh=hh, src=src: nc.tensor.transpose(out=pcb[:, (i * 2 + hh) * 128:(i * 2 + hh + 1) * 128],
                                                                                   in_=src[:, hh * 128:(hh + 1) * 128], identity=k.ident_b[:]),
                             reads=[kn, "ident_b"], writes=["pc"])
                P.op("dve", lambda: V.tensor_copy(out=trT[:].rearrange("p a b -> p (a b)"), in_=pcb[:, 0:768]), reads=["pc"], writes=["trT"])

                ck(k, 4)

                def hT(i, h):
                    return trT[(h % 2) * 64:(h % 2) * 64 + 64, i * 2 + h // 2, :]
                for h in range(4):
                    ck(k, 4.01 + 0.01 * h)
                    dst = (pa if h % 2 == 0 else pm[0])[:, (h // 2) * 128:(h // 2 + 1) * 128]
                    P.op("pe", lambda h=h, dst=dst: nc.tensor.matmul(dst, lhsT=hT(1, h), rhs=hT(0, h), start=True, stop=True),
                         reads=["trT"], writes=["pa" if h % 2 == 0 else kp[0]])
                ck(k, 4.1)
                P.op("act", lambda: A.copy(out=gt[:, 0:256], in_=pa[:, 0:256]), reads=["pa"], writes=["gt"])
                P.op("act", lambda: A.copy(out=gt[:, 256:512], in_=pm[0][:, 0:256]), reads=[kp[0]], writes=["gt"])
                ck(k, 4.2)
                for h in range(4):
                    gb = (h % 2) * 2 + h // 2
                    P.op("dve", lambda h=h, gb=gb: V.tensor_tensor(out=attb[:, h, :], in0=gt[:, gb * 128:(gb + 1) * 128], in1=k.mcum[:], op=ALU.mult),
                         reads=["gt", "mcum"], writes=["attb"])
                ck(k, 4.5)
                def obank(h):
                    return ((pc, "pc"), (pm[1], kp[1]), (pm[3], kp[3]), (pm[0], kp[0]))[h]
                for h in range(4):
                    ob, okey = obank(h)
                    P.op("pe", lambda h=h, ob=ob: nc.tensor.matmul(ob[:, 0:64], lhsT=attb[:, h, :], rhs=vb[:, h * 64:(h + 1) * 64],
                                                                   start=True, stop=False), reads=["attb", "vb", "trT"], writes=[okey])
                for c2 in range(2):
                    rs = slice(c2 * 64, (c2 + 1) * 64)
                    for h in range(4):
                        hp = slice((h % 2) * 64, (h % 2) * 64 + 64)
                        ob, okey = obank(h)
                        P.op("pe", lambda h=h, rs=rs, hp=hp, ob=ob: nc.tensor.matmul(ob[rs, 0:64], lhsT=hT(2, h)[:, rs], rhs=st_b[hp, h // 2, :],
                                                                                     start=False, stop=True), reads=["trT", "st_b"], writes=[okey])
                    for hh in range(2):
                        P.op("pe", lambda hh=hh, rs=rs: nc.tensor.matmul(pa[:, hh * 128:(hh + 1) * 128], lhsT=kd[rs, hh * 128:(hh + 1) * 128],
                                                                         rhs=vb[rs, hh * 128:(hh + 1) * 128], start=True, stop=True),
                             reads=["kd", "vb", "attb"], writes=["pa"])
                    P.op("act", lambda: A.copy(out=W["t2"][:], in_=pa[:, 0:256]), reads=["pa"], writes=["t2"])
                    for h in range(4):
                        hp = slice((h % 2) * 64, (h % 2) * 64 + 64)
                        P.op("dve", lambda h=h, c2=c2, hp=hp: V.scalar_tensor_tensor(out=st_f[hp, h // 2, :], in0=st_f[hp, h // 2, :],
                                                                                    scalar=ebl[hp, c2, h // 2:h // 2 + 1],
                                                                                    in1=W["t2"][hp, h * 64:(h + 1) * 64], op0=ALU.mult, op1=ALU.add),
                             reads=["st_f", "ebl", "t2"], writes=["st_f"])
                    P.op("dve", lambda: V.tensor_copy(out=st_b[:], in_=st_f[:]), reads=["st_f"], writes=["st_b"])
                ck(k, 5)
                yt = ytile[i2]
                ky = ("ytile", i2)
                tk = (["t1"] if l > 0 else [])
                for h in range(4):
                    ob, okey = obank(h)
                    P.op("act", lambda h=h, ob=ob: A.copy(out=W["t1"][:, h * 64:(h + 1) * 64], in_=ob[:, 0:64]), reads=[okey] + tk, writes=["osb"] + tk)
                P.op("act", lambda: A.activation(out=W["osq"][:], in_=W["t1"][:], func=AF.Square), reads=["osb"], writes=["osq"])
                P.op("dve", lambda: V.tensor_reduce(out=ss4[:, 0:4], in_=W["osq"][:].rearrange("p (h d) -> p h d", h=4), axis=AX.X, op=ALU.add),
                     reads=["osq"], writes=["ss4"])
                P.op("dve", lambda: V.tensor_scalar(out=rs4[:, 0:4], in0=ss4[:, 0:4], scalar1=1.0 / 64, scalar2=1e-6, op0=ALU.mult, op1=ALU.add),
                     reads=["ss4"], writes=["rs4"])
                P.op("act", lambda: A.activation(out=rs4[:, 0:4], in_=rs4[:, 0:4], func=AF.Ln), reads=["rs4"], writes=["rs4"])
                P.op("act", lambda: A.activation(out=rs4[:, 0:4], in_=rs4[:, 0:4], func=AF.Exp, scale=-0.5), reads=["rs4"], writes=["rs4"])
                P.op("dve", lambda: V.tensor_tensor(out=W["nwg"][:], in0=W["sgate"][:], in1=hg_nw[:], op=ALU.mult), reads=["sgate", "hg_nw"], writes=["nwg"])
                for h in range(4):
                    P.op("dve", lambda h=h, yt=yt: V.scalar_tensor_tensor(out=yt[:, h * 64:(h + 1) * 64], in0=W["t1"][:, h * 64:(h + 1) * 64],
                                                                         scalar=rs4[:, h:h + 1], in1=W["nwg"][:, h * 64:(h + 1) * 64],
                                                                         op0=ALU.mult, op1=ALU.mult), reads=["osb", "rs4", "nwg"], writes=[ky])

                ck(k, 6)
                P.op("dve", lambda: V.tensor_tensor(out=gt[:], in0=gx[:], in1=gx[:], op=ALU.mult), reads=["gx"], writes=["gt"])
                P.op("dve", lambda: V.tensor_scalar(out=gt[:], in0=gt[:], scalar1=0.044715, scalar2=1.0, op0=ALU.mult, op1=ALU.add), reads=["gt"], writes=["gt"])
                P.op("dve", lambda: V.tensor_tensor(out=gt[:], in0=gt[:], in1=gx[:], op=ALU.mult), reads=["gt", "gx"], writes=["gt"])
                P.op("act", lambda: A.activation(out=gt[:], in_=gt[:], func=AF.Sigmoid, scale=1.5957691216057308), reads=["gt"], writes=["gt"])
                P.op("dve", lambda: V.tensor_tensor(out=gg[:], in0=gt[:], in1=gx[:], op=ALU.mult), reads=["gt", "gx"], writes=["gg"])
                P.op("dve", lambda: V.bn_stats(out=bnst[:], in_=gg[:, 256:512]), reads=["gg"], writes=["bnst"])
                P.op("dve", lambda: V.bn_aggr(out=bnag[:], in_=bnst[:]), reads=["bnst"], writes=["bnag"])
                P.op("dve", lambda: V.tensor_scalar(out=rs4[:, 4:5], in0=bnag[:, 1:2], scalar1=1e-5, scalar2=None, op0=ALU.add),
                     reads=["bnag"], writes=["rs4b"])
                P.op("act", lambda: A.activation(out=rs4[:, 4:5], in_=rs4[:, 4:5], func=AF.Ln), reads=["rs4b"], writes=["rs4b"])
                P.op("act", lambda: A.activation(out=rs4[:, 4:5], in_=rs4[:, 4:5], func=AF.Exp, scale=-0.5), reads=["rs4b"], writes=["rs4b"])
                P.op("dve", lambda: V.tensor_scalar(out=W["ginn"][:], in0=gg[:, 256:512], scalar1=bnag[:, 0:1], scalar2=rs4[:, 4:5],
                                                    op0=ALU.subtract, op1=ALU.mult), reads=["gg", "bnag", "rs4b"], writes=["ginn"])
                P.op("dve", lambda: V.tensor_tensor(out=W["ginn"][:], in0=W["ginn"][:], in1=gm_lnw[:], op=ALU.mult), reads=["ginn", "gm_lnw"], writes=["ginn"])
                P.op("dve", lambda: V.tensor_tensor(out=vln[:], in0=W["ginn"][:], in1=gm_lnb[:], op=ALU.add), reads=["ginn", "gm_lnb"], writes=["vln"])
                for g in range(4):
                    P.op("pe", lambda g=g: nc.tensor.matmul(pb[:, 256 + g * 64:256 + (g + 1) * 64], lhsT=wsT[:, g, :], rhs=vln[:, g * 64:(g + 1) * 64],
                                                            start=True, stop=True), reads=["wsT", "vln"], writes=["pb"])
                P.op("act", lambda: A.copy(out=W["ginn"][:], in_=pb[:, 256:512]), reads=["pb", "ginn"], writes=["ginn"])
                for g in range(4):
                    P.op("dve", lambda g=g: V.scalar_tensor_tensor(out=W["gsv"][:, g * 64:(g + 1) * 64], in0=W["ginn"][:, g * 64:(g + 1) * 64],
                                                                  scalar=bsT[:, g:g + 1], in1=gg[:, g * 64:(g + 1) * 64], op0=ALU.add, op1=ALU.mult),
                         reads=["ginn", "bsT", "gg"], writes=["gsv"])
                P.op("act", lambda: A.activation(out=W["gsq"][:], in_=W["gsv"][:], func=AF.Square), reads=["gsv"], writes=["gsq"])
                P.op("dve", lambda: V.tensor_reduce(out=ss4[:, 4:8], in_=W["gsq"][:].rearrange("p (h d) -> p h d", h=4), axis=AX.X, op=ALU.add),
                     reads=["gsq"], writes=["ss4b"])
                P.op("dve", lambda: V.tensor_scalar(out=ss4[:, 4:8], in0=ss4[:, 4:8], scalar1=1.0 / 64, scalar2=1e-6, op0=ALU.mult, op1=ALU.add),
                     reads=["ss4b"], writes=["ss4b"])
                P.op("act", lambda: A.activation(out=ss4[:, 4:8], in_=ss4[:, 4:8], func=AF.Ln), reads=["ss4b"], writes=["ss4b"])
                P.op("act", lambda: A.activation(out=ss4[:, 4:8], in_=ss4[:, 4:8], func=AF.Exp, scale=-0.5), reads=["ss4b"], writes=["ss4b"])
                for g in range(4):
                    P.op("dve", lambda g=g, yt=yt: V.scalar_tensor_tensor(out=yt[:, 256 + g * 64:256 + (g + 1) * 64], in0=W["gsv"][:, g * 64:(g + 1) * 64],
                                                                         scalar=ss4[:, 4 + g:5 + g], in1=gm_nw[:, g * 64:(g + 1) * 64],
                                                                         op0=ALU.mult, op1=ALU.mult), reads=["gsv", "ss4b", "gm_nw"], writes=[ky])
                P.dma("sp", k.y[rows, 0:512], yt[:], reads=[ky], writes=[("y", t)])
                if t == 0:
                    dump(k, "gg", gg[:], "gg", 512)
                    dump(k, "bnag", bnag[:], "bnag", 2)
                    dump(k, "rs4", rs4[:], "rs4b", 8)
                    dump(k, "vln", vln[:], "vln", 256)
                    dump(k, "gsv", W["gsv"][:], "gsv", 256)
                    dump(k, "ss4", ss4[:], "ss4b", 8)
                    dump(k, "wsT", wsT[:].rearrange("p a b -> p (a b)"), "wsT", 512)
                    dump(k, "logf", W["logf"][:], "logf", 256)
                    dump(k, "kk", W["kk"][:], "kk", 256)
                    dump(k, "qf", W["qf"][:], "qf", 256)
                    dump(k, "ebm", W["ebm"][:], "ebm", 256)
                    dump(k, "eb", W["eb"][:], "eb", 256)
                    dump(k, "erem", W["erem"][:], "erem", 256)
                    dump(k, "ebl", ebl[:].rearrange("p a b -> p (a b)"), "ebl", 4)
                    dump(k, "osb", W["t1"][:], "osb", 256)
                    dump(k, "attb", attb[:].rearrange("p a b -> p (a b)"), "attb", 512)
                    dump(k, "st_f", st_f[:].rearrange("p a b -> p (a b)"), "st_f", 128)
                    dump(k, "rs4h", rs4[:], "rs4", 8)
                    dump(k, "nwg", W["nwg"][:], "nwg", 256)
                ck(k, 7)


NSA_BIG = 30000.0


def phase2(k, l):
    nc, P = k.nc, k.P
    V, A, G = nc.vector, nc.scalar, nc.gpsimd
    with ExitStack() as es:
        sb = lambda name, shape, dt: es.enter_context(nc.sbuf_tensor(f"p2l{l}_{name}", shape, dt))
        ps = lambda name, shape, dt: es.enter_context(nc.psum_tensor(f"p2l{l}_{name}", shape, dt))
        edup = sb("edup", [128, 32, 128], BF16)
        win01 = sb("win01", [128, 8, 512], BF16)
        cmp01 = sb("cmp01", [128, 5, 512], BF16)
        impkeep = sb("impkeep", [128, NT, 64], F32)
        impadd = sb("impadd", [128, NT, 64], F32)
        ovext = sb("ovext", [128, 2, 65], BF16)
        wpad = sb("wpad", [128, NT], F32)
        gts = sb("gts", [128, NT, 24], F32)
        nw = sb("nw", [128, 512], F32)
        w1 = [sb(f"w1_{i}", [64, 32, 128], BF16) for i in range(2)]
        w2kd = sb("w2kd", [128, 128], BF16)
        w2v = sb("w2v", [128, 64], BF16)
        peT = sb("peT", [64, 2, 32], BF16)
        craw = sb("craw", [64, S], BF16)
        cb = sb("cb", [128, 1], F32)
        hx = sb("hx", [128, 256], F32)
        ht = sb("ht", [128, 256], F32)
        hb = sb("hb", [128, 256], BF16)
        kcd = sb("kcd", [128, 256], BF16)
        vc = sb("vc", [128, 2, 65], BF16)
        qr = [sb(f"qr{i}", [128, S], BF16) for i in range(2)]
        qo = [sb(f"qo{i}", [128, S], BF16) for i in range(2)]
        ksd = sb("ksd", [128, S], BF16)
        kwd = sb("kwd", [128, S], BF16)
        vsg = sb("vsg", [128, NT, 65], BF16)
        vwg = sb("vwg", [128, NT, 65], BF16)
        ec = [[[sb(f"ec{ci}{par}{ct}", [128, 512], BF16) for ct in range(2)] for par in range(2)] for ci in range(2)]
        pt = [[sb(f"pt{par}{i}", [128, 512], BF16) for i in range(2)] for par in range(2)]
        osb = [sb(f"osb{par}", [65, 512], F32) for par in range(2)]
        ob = sb("ob", [128, 3, 4, 4, 65], F32)
        impsb = sb("impsb", [128, 4, 4, 64], F32)
        impw = sb("impw", [128, 4, 64], F32)
        imp = sb("imp", [128, 64], F32)
        imp3 = sb("imp3", [128, 64], F32)
        m8a = sb("m8a", [128, 8], F32)
        m8b = sb("m8b", [128, 8], F32)
        rdn = sb("rdn", [128, 4], F32)
        mdup = sb("mdup", [128, 4, 128], BF16)
        MT = sb("MT", [128, 512], BF16)
        den3 = sb("den3", [128, 3, 4], F32)
        coef = sb("coef", [128, 3, 4], F32)
        acc = sb("acc", [128, 4, 64], F32)
        tm2 = sb("tm2", [128, 4, 64], F32)
        ss = sb("ss", [128, 4], F32)
        yst = [sb(f"yst{i}", [128, 256], BF16) for i in range(2)]
        Sb = [[ps(f"S{par}{i}", [128, 512], F32) for i in range(2)] for par in range(2)]
        Ob = [ps(f"O{par}", [128, 512], F32) for par in range(2)]
        X = ps("X", [128, 512], F32)
        Y = ps("Y", [128, 512], F32)
        Xb = X[:].bitcast(BF16)

        flat = lambda t: t[:].rearrange("p a b -> p (a b)")

        def cast_load(dst_flat, src_flat, n, key):
            for o in range(0, n, 2048):
                w = min(2048, n - o)
                P.dma("pool", dst_flat[:, o:o + w], src_flat[:, o:o + w], writes=[key])
        cast_load(flat(edup), k.cst["edup"].rearrange("p a b -> p (a b)"), 32 * 128, "edup")
        cast_load(flat(win01), k.cst["win01"].rearrange("p a b -> p (a b)"), 8 * 512, "win01")
        cast_load(flat(cmp01), k.cst["cmp01"].rearrange("p a b -> p (a b)"), 5 * 512, "cmp01")
        cast_load(flat(ovext), k.cst["ovext"].rearrange("p a b -> p (a b)"), 130, "ovext")
        P.dma("sp", impkeep[:], k.cst["impkeep"], writes=["impkeep"])
        P.dma("sp", impadd[:], k.cst["impadd"], writes=["impadd"])
        P.dma("sp", wpad[:], k.cst["wpad"], writes=["wpad"])
        P.dma("sp", gts[:], k.gts.rearrange("(t p) c -> p t c", p=128), writes=["gts"])
        bcast_row(k, P, "sp", nw[:], k.nsa_nw[l:l + 1, :], "nw")
        for kv in range(2):
            wv = k.nsa_w1[l, kv].rearrange("(j d) m -> d j m", d=64)
            for hh in range(2):
                P.dma("pool", w1[kv][:, hh * 16:(hh + 1) * 16, :], wv[:, hh * 16:(hh + 1) * 16, :], writes=[("w1", kv)])
        P.dma("pool", w2kd[:, 0:64], k.nsa_w2[l, 0], writes=["w2kd"])
        P.dma("pool", w2kd[:, 64:128], k.nsa_w2[l, 0], writes=["w2kd"])
        P.dma("pool", w2v[:], k.nsa_w2[l, 1], writes=["w2v"])
        P.dma("pool", peT[:], k.nsa_pe[l].rearrange("k j d -> d k j"), writes=["peT"], allow_slow_non_contiguous=True)

        def evac_O(par, br, hl):
            P.op("act", lambda par=par: A.copy(out=osb[par][:], in_=Ob[par][0:65, :]), reads=[("O", par)], writes=[("osb", par)])
            for qt in range(4):
                P.op("pe", lambda par=par, qt=qt: nc.tensor.transpose(out=X[:, qt * 65:(qt + 1) * 65], in_=osb[par][:, qt * 128:(qt + 1) * 128], identity=k.ident_f[0:65, 0:65]),
                     reads=[("osb", par), "ident"], writes=["X"])
            P.op("act", lambda br=br, hl=hl: A.copy(out=ob[:, br, :, hl, :], in_=X[:, 0:260].rearrange("p (a b) -> p a b", a=4)), reads=["X"], writes=[("ob", br)])

        for g in range(2):
            for kv in range(2):
                P.dma("sp", craw[:], k.ft[10 + kv, g * 64:(g + 1) * 64, :], writes=["craw"])
                for j in range(32):
                    P.op("pe", lambda j=j, kv=kv: nc.tensor.matmul(X[:, 300:301], lhsT=w1[kv][:, j, :], rhs=peT[:, kv, j:j + 1], start=(j == 0), stop=(j == 31)),
                         reads=[("w1", kv), "peT"], writes=["X"])
                for j in range(32):
                    P.op("pe", lambda j=j, kv=kv: nc.tensor.matmul(X[:, 0:255], lhsT=w1[kv][:, j, :], rhs=craw[:, j:j + 16 * 254 + 1:16], start=(j == 0), stop=(j == 31)),
                         reads=[("w1", kv), "craw"], writes=["X"])
                P.op("act", lambda: A.copy(out=cb[:], in_=X[:, 300:301]), reads=["X"], writes=["cb"])
                P.op("dve", lambda: V.memset(hx[:], 0.0), writes=["hx"])
                P.op("act", lambda: A.activation(out=hx[:, 0:255], in_=X[:, 0:255], func=AF.Identity, bias=cb[:, 0:1], scale=1.0), reads=["X", "cb", "hx"], writes=["hx"])
                P.op("dve", lambda: V.tensor_tensor(out=ht[:], in0=hx[:], in1=hx[:], op=ALU.mult), reads=["hx"], writes=["ht"])
                P.op("dve", lambda: V.tensor_scalar(out=ht[:], in0=ht[:], scalar1=0.044715, scalar2=1.0, op0=ALU.mult, op1=ALU.add), reads=["ht"], writes=["ht"])
                P.op("dve", lambda: V.tensor_tensor(out=ht[:], in0=ht[:], in1=hx[:], op=ALU.mult), reads=["ht", "hx"], writes=["ht"])
                P.op("act", lambda: A.activation(out=ht[:], in_=ht[:], func=AF.Sigmoid, scale=1.5957691216057308), reads=["ht"], writes=["ht"])
                P.op("dve", lambda: V.tensor_tensor(out=hb[:], in0=ht[:], in1=hx[:], op=ALU.mult), reads=["ht", "hx"], writes=["hb"])
                if kv == 0:
                    P.op("pe", lambda: nc.tensor.matmul(X[:, 0:256], lhsT=w2kd[:], rhs=hb[:], start=True, stop=True), reads=["w2kd", "hb"], writes=["X"])
                    P.op("act", lambda: A.copy(out=kcd[:], in_=X[:, 0:256]), reads=["X"], writes=["kcd"])
                else:
                    for ct in range(2):
                        P.op("pe", lambda ct=ct: nc.tensor.matmul(X[:, ct * 64:(ct + 1) * 64], lhsT=hb[:, ct * 128:(ct + 1) * 128], rhs=w2v[:], start=True, stop=True),
                             reads=["w2v", "hb"], writes=["X"])
                    P.op("dve", lambda: V.memset(vc[:], 1.0), writes=["vc"])
                    P.op("act", lambda: A.copy(out=vc[:, :, 0:64], in_=X[:, 0:128].rearrange("p (a b) -> p a b", a=2)), reads=["X", "vc"], writes=["vc"])
            for ci in range(2):
                P.dma("sp", qr[ci][:], k.ft[2 * g + ci], writes=[("qr", ci)])
                P.dma("act", qo[ci][:], k.ft[4 + 2 * g + ci], writes=[("qo", ci)])
            for hh in range(2):
                P.dma("sp", ksd[hh * 64:(hh + 1) * 64, :], k.ft[8, g * 64:(g + 1) * 64, :], writes=["ksd"])
                P.dma("act", kwd[hh * 64:(hh + 1) * 64, :], k.ft[9, g * 64:(g + 1) * 64, :], writes=["kwd"])
            P.op("dve", lambda: V.memset(vsg[:], 1.0), writes=["vsg"])
            P.op("dve", lambda: V.memset(vwg[:], 1.0), writes=["vwg"])
            P.dma("sp", vsg[:, :, 0:64], k.tmv[:, g * 64:(g + 1) * 64].rearrange("(t p) c -> p t c", p=128), reads=["vsg"], writes=["vsg"])
            P.dma("act", vwg[:, :, 0:64], k.tmv[:, 128 + g * 64:128 + (g + 1) * 64].rearrange("(t p) c -> p t c", p=128), reads=["vwg"], writes=["vwg"])

            for Q in range(S // 512):
                qs = slice(Q * 512, (Q + 1) * 512)
                cts = []
                for ct in range(2):
                    u = 512 * Q - 2048 * ct
                    if u < -480:
                        continue
                    cts.append((ct, (None if u > 2048 else (0, 512, 1024, 1536, 2048).index(u))))
                for ci in range(2):
                    for n_, (ct, mi) in enumerate(cts):
                        for par in range(2):
                            hp = slice(par * 64, (par + 1) * 64)
                            P.op("pe", lambda par=par, hp=hp, ct=ct, ci=ci, qs=qs: nc.tensor.matmul(Sb[par][0][:], lhsT=kcd[hp, ct * 128:(ct + 1) * 128], rhs=qr[ci][hp, qs], start=True, stop=True),
                                 reads=["kcd", ("qr", ci)], writes=[("S", par, 0)])
                        for par in range(2):
                            e_ = ec[ci][par][ct]
                            ke = ("ec", ci, par, ct)
                            P.op("act", lambda par=par, e_=e_: A.activation(out=e_[:], in_=Sb[par][0][:], func=AF.Exp, scale=0.125), reads=[("S", par, 0)], writes=[ke])
                            if mi is not None:
                                P.op("pool", lambda e_=e_, mi=mi: G.tensor_tensor(out=e_[:], in0=e_[:], in1=cmp01[:, mi, :], op=ALU.mult), reads=[ke, "cmp01"], writes=[ke])
                            P.op("pe", lambda par=par, ct=ct, e_=e_, n_=n_: nc.tensor.matmul(Ob[par][0:65, :], lhsT=vc[:, ct, :], rhs=e_[:], start=(n_ == 0), stop=(n_ == len(cts) - 1)),
                                 reads=["vc", ke], writes=[("O", par)])
                    for par in range(2):
                        evac_O(par, 0, 2 * ci + par)
                for half in range(2):
                    for q2 in range(2):
                        qt = half * 2 + q2
                        for hl in range(4):
                            ci, par = hl // 2, hl % 2
                            for n_, (ct, mi) in enumerate(cts):
                                e_ = ec[ci][par][ct]
                                P.op("pe", lambda e_=e_, ct=ct, qt=qt, q2=q2, hl=hl, n_=n_: nc.tensor.matmul(Y[:, q2 * 256 + hl * 64:q2 * 256 + (hl + 1) * 64],
                                                                                                           lhsT=e_[:, qt * 128:(qt + 1) * 128], rhs=ovext[:, ct, 0:64],
                                                                                                           start=(n_ == 0), stop=(n_ == len(cts) - 1)),
                                     reads=[("ec", ci, par, ct), "ovext"], writes=["Y"])
                    P.op("act", lambda half=half: A.copy(out=impsb[:, half * 2:(half + 1) * 2, :, :].rearrange("p a b c -> p (a b c)"), in_=Y[:]), reads=["Y"], writes=["impsb"])
                for qt in range(4):
                    t = Q * 4 + qt
                    P.op("dve", lambda qt=qt: V.tensor_scalar(out=rdn[:], in0=ob[:, 0, qt, :, 64], scalar1=1e-30, scalar2=None, op0=ALU.max), reads=[("ob", 0)], writes=["rdn"])
                    P.op("dve", lambda: V.reciprocal(out=rdn[:], in_=rdn[:]), reads=["rdn"], writes=["rdn"])
                    P.op("dve", lambda qt=qt: V.tensor_tensor(out=impw[:], in0=impsb[:, qt, :, :], in1=rdn[:].unsqueeze(2).to_broadcast([128, 4, 64]), op=ALU.mult),
                         reads=["impsb", "rdn"], writes=["impw"])
                    P.op("dve", lambda: V.tensor_reduce(out=imp[:], in_=impw[:].rearrange("p h j -> p j h"), axis=AX.X, op=ALU.add), reads=["impw"], writes=["imp"])
                    P.op("dve", lambda t=t: V.tensor_tensor(out=imp[:], in0=imp[:], in1=impkeep[:, t, :], op=ALU.mult), reads=["imp", "impkeep"], writes=["imp"])
                    P.op("dve", lambda t=t: V.tensor_tensor(out=imp[:], in0=imp[:], in1=impadd[:, t, :], op=ALU.add), reads=["imp", "impadd"], writes=["imp"])
                    P.op("dve", lambda: V.max(out=m8a[:], in_=imp[:]), reads=["imp"], writes=["m8a"])
                    P.op("dve", lambda: V.match_replace(out=imp3[:], in_to_replace=m8a[:], in_values=imp[:], imm_value=-3.0e9), reads=["imp", "m8a"], writes=["imp3"])
                    P.op("dve", lambda: V.max(out=m8b[:], in_=imp3[:]), reads=["imp3"], writes=["m8b"])
                    P.op("dve", lambda: V.tensor_scalar(out=imp3[:], in0=imp[:], scalar1=m8b[:, 7:8], scalar2=None, op0=ALU.is_ge), reads=["imp", "m8b", "imp3"], writes=["imp3"])
                    for dd in range(2):
                        P.op("dve", lambda qt=qt, dd=dd: V.tensor_scalar(out=mdup[:, qt, dd * 64:(dd + 1) * 64], in0=imp3[:], scalar1=-1.0, scalar2=NSA_BIG, op0=ALU.add, op1=ALU.mult),
                             reads=["imp3"], writes=["mdup"])
                    P.op("pe", lambda qt=qt: nc.tensor.transpose(out=Xb[:, qt * 128:(qt + 1) * 128], in_=mdup[:, qt, :], identity=k.ident_b[:]), reads=["mdup", "ident_b"], writes=["X"])
                P.op("dve", lambda: V.tensor_copy(out=MT[:], in_=Xb[:, 0:512]), reads=["X"], writes=["MT"])
                for br, (kd_, kkey, vt, vkey) in ((1, (ksd, "ksd", vsg, "vsg")), (2, (kwd, "kwd", vwg, "vwg"))):
                    kt0 = 0 if br == 1 else max(0, 4 * Q - 4)
                    kts = list(range(kt0, 4 * Q + 4))
                    for ci in range(2):
                        for n_, kt in enumerate(kts):
                            bi = n_ % 2
                            for par in range(2):
                                hp = slice(par * 64, (par + 1) * 64)
                                P.op("pe", lambda par=par, hp=hp, kt=kt, ci=ci, qs=qs, bi=bi, kd_=kd_, br=br: nc.tensor.matmul(Sb[par][bi][:], lhsT=kd_[hp, kt * 128:(kt + 1) * 128], rhs=qo[ci][hp, qs],
                                                                                                                          start=True, stop=(br == 2)),
                                     reads=[kkey, ("qo", ci)], writes=[("S", par, bi)])
                            if br == 1:
                                for par in range(2):
                                    hp = slice(par * 64, (par + 1) * 64)
                                    P.op("pe", lambda par=par, hp=hp, kt=kt, bi=bi: nc.tensor.matmul(Sb[par][bi][:], lhsT=edup[hp, kt, :], rhs=MT[hp, :], start=False, stop=True),
                                         reads=["edup", "MT"], writes=[("S", par, bi)])
                            for par in range(2):
                                p_ = pt[par][bi]
                                kp_ = ("pt", par, bi)
                                P.op("act", lambda par=par, bi=bi, p_=p_: A.activation(out=p_[:], in_=Sb[par][bi][:], func=AF.Exp, scale=0.125), reads=[("S", par, bi)], writes=[kp_])
                                dl = kt - 4 * Q
                                if br == 2 or dl >= 0:
                                    P.op("pool", lambda p_=p_, dl=dl: G.tensor_tensor(out=p_[:], in0=p_[:], in1=win01[:, dl + 4, :], op=ALU.mult), reads=[kp_, "win01"], writes=[kp_])
                                P.op("pe", lambda par=par, kt=kt, p_=p_, n_=n_, vt=vt, nk=len(kts): nc.tensor.matmul(Ob[par][0:65, :], lhsT=vt[:, kt, :], rhs=p_[:], start=(n_ == 0), stop=(n_ == nk - 1)),
                                     reads=[vkey, kp_], writes=[("O", par)])
                        for par in range(2):
                            evac_O(par, br, 2 * ci + par)
                for qt in range(4):
                    t = Q * 4 + qt
                    i2 = qt % 2
                    P.op("dve", lambda qt=qt: V.tensor_scalar(out=den3[:], in0=ob[:, :, qt, :, 64], scalar1=1e-30, scalar2=None, op0=ALU.max), reads=[("ob", 0), ("ob", 1), ("ob", 2)], writes=["den3"])
                    if Q == 0:
                        P.op("dve", lambda t=t: V.tensor_scalar(out=den3[:, 2, :], in0=den3[:, 2, :], scalar1=wpad[:, t:t + 1], scalar2=None, op0=ALU.add),
                             reads=["den3", "wpad"], writes=["den3"])
                    P.op("dve", lambda: V.reciprocal(out=den3[:], in_=den3[:]), reads=["den3"], writes=["den3"])
                    P.op("dve", lambda t=t, g=g: V.tensor_tensor(out=coef[:], in0=den3[:], in1=gts[:, t, g * 12:(g + 1) * 12].rearrange("p (h b) -> p b h", b=3), op=ALU.mult),
                         reads=["den3", "gts"], writes=["coef"])
                    for br in range(3):
                        dst = acc if br == 0 else tm2
                        P.op("dve", lambda br=br, qt=qt, dst=dst: V.tensor_tensor(out=dst[:], in0=ob[:, br, qt, :, 0:64], in1=coef[:, br, :].unsqueeze(2).to_broadcast([128, 4, 64]), op=ALU.mult),
                             reads=[("ob", br), "coef"], writes=["acc" if br == 0 else "tm2"])
                        if br > 0:
                            P.op("dve", lambda: V.tensor_tensor(out=acc[:], in0=acc[:], in1=tm2[:], op=ALU.add), reads=["acc", "tm2"], writes=["acc"])
                    P.op("act", lambda: A.activation(out=tm2[:], in_=acc[:], func=AF.Square), reads=["acc", "tm2"], writes=["tm2"])
                    P.op("dve", lambda: V.tensor_reduce(out=ss[:], in_=tm2[:], axis=AX.X, op=ALU.add), reads=["tm2"], writes=["ss"])
                    P.op("dve", lambda: V.tensor_scalar(out=ss[:], in0=ss[:], scalar1=1.0 / 64, scalar2=1e-6, op0=ALU.mult, op1=ALU.add), reads=["ss"], writes=["ss"])
                    P.op("act", lambda: A.activation(out=ss[:], in_=ss[:], func=AF.Sqrt), reads=["ss"], writes=["ss"])
                    P.op("dve", lambda: V.reciprocal(out=ss[:], in_=ss[:]), reads=["ss"], writes=["ss"])
                    P.op("dve", lambda: V.tensor_tensor(out=acc[:], in0=acc[:], in1=ss[:].unsqueeze(2).to_broadcast([128, 4, 64]), op=ALU.mult), reads=["acc", "ss"], writes=["acc"])
                    P.op("dve", lambda g=g, i2=i2: V.tensor_tensor(out=yst[i2][:], in0=acc[:].rearrange("p h d -> p (h d)"), in1=nw[:, g * 256:(g + 1) * 256], op=ALU.mult),
                         reads=["acc", "nw"], writes=[("yst", i2)])
                    P.dma("sp", k.y[t * 128:(t + 1) * 128, 512 + g * 256:512 + (g + 1) * 256], yst[i2][:], reads=[("yst", i2)], writes=[("ynsa", t, g)])
        P.barrier()


TRASH = NE * CAP


def layer_norm_tile(k, P, src, dst, lnw, lnb, tmp, key_src, key_dst, sfx):
    nc = k.nc
    V, A = nc.vector, nc.scalar
    st, ag, rs = k.ln_st, k.ln_ag, k.ln_rs
    for hlf in range(2):
        P.op("dve", lambda hlf=hlf: V.bn_stats(out=st[:, hlf * 6:(hlf + 1) * 6], in_=src[:, hlf * 512:(hlf + 1) * 512]), reads=[key_src], writes=["ln_st"])
    P.op("dve", lambda: V.bn_aggr(out=ag[:], in_=st[:]), reads=["ln_st"], writes=["ln_ag"])
    P.op("dve", lambda: V.tensor_scalar(out=rs[:], in0=ag[:, 1:2], scalar1=1e-5, scalar2=None, op0=ALU.add), reads=["ln_ag"], writes=["ln_rs"])
    P.op("act", lambda: A.activation(out=rs[:], in_=rs[:], func=AF.Sqrt), reads=["ln_rs"], writes=["ln_rs"])
    P.op("dve", lambda: V.reciprocal(out=rs[:], in_=rs[:]), reads=["ln_rs"], writes=["ln_rs"])
    P.op("dve", lambda: V.tensor_scalar(out=tmp[:], in0=src[:], scalar1=ag[:, 0:1], scalar2=rs[:, 0:1], op0=ALU.subtract, op1=ALU.mult),
         reads=[key_src, "ln_ag", "ln_rs"], writes=["ln_tmp" + sfx])
    P.op("pool", lambda: nc.gpsimd.tensor_tensor(out=tmp[:], in0=tmp[:], in1=lnw[:], op=ALU.mult), reads=["ln_tmp" + sfx, "lnw" + sfx], writes=["ln_tmp" + sfx])
    P.op("pool", lambda: nc.gpsimd.tensor_tensor(out=dst[:], in0=tmp[:], in1=lnb[:], op=ALU.add), reads=["ln_tmp" + sfx, "lnb" + sfx], writes=[key_dst])


def phase3(k, l):
    nc, P = k.nc, k.P
    V, A = nc.vector, nc.scalar
    with ExitStack() as es:
        sb = lambda name, shape, dt: es.enter_context(nc.sbuf_tensor(f"p3l{l}_{name}", shape, dt))
        ps = lambda name, shape, dt: es.enter_context(nc.psum_tensor(f"p3l{l}_{name}", shape, dt))
        wo = sb("wo", [128, 8, D], BF16)
        rw = sb("rw", [128, 8, NE], F32)
        rb = sb("rb", [128, NE], F32)
        lnw = sb("lnw", [128, D], F32)
        lnb = sb("lnb", [128, D], F32)
        k.ln_st = sb("ln_st", [128, 12], F32)
        k.ln_ag = sb("ln_ag", [128, 2], F32)
        k.ln_rs = sb("ln_rs", [128, 1], F32)
        ytl = [sb(f"ytl{i}", [128, D], BF16) for i in range(2)]
        yT = sb("yT", [128, 8, 128], BF16)
        xt = [sb(f"xt{i}", [128, D], F32) for i in range(2)]
        mix = sb("mix", [128, D], F32)
        rr = sb("rr", [128, D], F32)
        tmp = sb("tmp", [128, D], F32)
        x1 = [sb(f"x1_{i}", [128, D], F32) for i in range(2)]
        x1b = [sb(f"x1b{i}", [128, D], BF16) for i in range(2)]
        x1T = sb("x1T", [128, 8, 128], F32)
        lg = sb("lg", [128, NE], F32)
        m8 = sb("m8", [128, 8], F32)
        nm = sb("nm", [128, 1], F32)
        msk = sb("msk", [128, NE], F32)
        mskb = sb("mskb", [128, NE], BF16)
        ex = sb("ex", [128, NE], F32)
        ssum = sb("ssum", [128, 1], F32)
        wts = sb("wts", [128, NE], F32)
        posf = sb("posf", [128, NE], F32)
        carry = sb("carry", [128, NE], F32)
        val = sb("val", [128, NE], F32)
        nsel = sb("nsel", [128, NE], F32)
        t8 = sb("t8", [128, 8], F32)
        oh = sb("oh", [128, NE], F32)
        idxf = sb("idxf", [128, 4], F32)
        pT = ps("pT", [128, 8, 128], BF16)
        pm = [ps(f"pm{i}", [128, 512], F32) for i in range(2)]
        pX = [ps(f"pX{i}", [128, 512], F32) for i in range(2)]
        pr = ps("pr", [128, 512], F32)

        x_src = k.x if l == 0 else k.x2
        wov = k.w_out[l].rearrange("(c p) n -> p c n", p=128)
        for c in range(8):
            P.dma("pool", wo[:, c, :], wov[:, c, :], writes=[("wo", c)])
        P.dma("sp", rw[:], k.router_w[l].rearrange("(c p) n -> p c n", p=128), writes=["rw"])
        bcast_row(k, P, "sp", rb[:], k.router_b[l:l + 1, :], "rb")
        bcast_row(k, P, "sp", lnw[:], k.ln1_w[l:l + 1, :], "lnw1")
        bcast_row(k, P, "sp", lnb[:], k.ln1_b[l:l + 1, :], "lnb1")
        P.op("dve", lambda: V.memset(carry[:], 0.0), writes=["carry"])
        if k.dbg and k.inject_nsa:
            P.dma("sp", k.y[:, 512:1024], k.ynsa_in[l], writes=["yinj"])
            P.barrier()

        for t in range(NT):
            i2 = t % 2
            rows = slice(t * 128, (t + 1) * 128)
            P.dma("sp", ytl[i2][:], k.y[rows, :], writes=[("ytl", i2)])
            P.dma("act", xt[i2][:], x_src[rows, :], writes=[("xt", i2)])
            for c in range(8):
                P.op("pe", lambda c=c, i2=i2: nc.tensor.transpose(out=pT[:, c, :], in_=ytl[i2][:, c * 128:(c + 1) * 128], identity=k.ident_b[:]),
                     reads=[("ytl", i2), "ident_b"], writes=["pT"])
            P.op("dve", lambda: V.tensor_copy(out=yT[:], in_=pT[:]), reads=["pT"], writes=["yT"])
            for hf in range(2):
                for c in range(8):
                    P.op("pe", lambda c=c, hf=hf: nc.tensor.matmul(pm[hf][:], lhsT=yT[:, c, :], rhs=wo[:, c, hf * 512:(hf + 1) * 512], start=(c == 0), stop=(c == 7)),
                         reads=["yT", ("wo", c)], writes=[("pm", hf)])
                P.op("act", lambda hf=hf: A.copy(out=mix[:, hf * 512:(hf + 1) * 512], in_=pm[hf][:]), reads=[("pm", hf)], writes=["mix"])
            P.op("dve", lambda i2=i2: V.scalar_tensor_tensor(out=rr[:], in0=xt[i2][:], scalar=ALPHA, in1=mix[:], op0=ALU.mult, op1=ALU.add),
                 reads=[("xt", i2), "mix"], writes=["rr"])
            layer_norm_tile(k, P, rr, x1[i2], lnw, lnb, tmp, "rr", ("x1", i2), "1")
            P.dma("sp", k.x1[rows, :], x1[i2][:], reads=[("x1", i2)], writes=[("x1d", t)])
            P.op("act", lambda i2=i2: A.copy(out=x1b[i2][:], in_=x1[i2][:]), reads=[("x1", i2)], writes=[("x1b", i2)])
            for c in range(8):
                P.op("pe", lambda c=c, i2=i2: nc.tensor.transpose(out=pX[c // 4][:, (c % 4) * 128:(c % 4 + 1) * 128], in_=x1[i2][:, c * 128:(c + 1) * 128], identity=k.ident_f[:]),
                     reads=[("x1", i2), "ident"], writes=[("pX", c // 4)])
            for q in range(2):
                P.op("act", lambda q=q: A.copy(out=x1T[:, q * 4:(q + 1) * 4, :].rearrange("p a b -> p (a b)"), in_=pX[q][:]), reads=[("pX", q)], writes=["x1T"])
            for c in range(8):
                P.op("pe", lambda c=c: nc.tensor.matmul(pr[:, 0:NE], lhsT=x1T[:, c, :], rhs=rw[:, c, :], start=(c == 0), stop=(c == 7)),
                     reads=["x1T", "rw"], writes=["pr"])
            P.op("act", lambda: A.copy(out=lg[:], in_=pr[:, 0:NE]), reads=["pr"], writes=["lg"])
            P.op("dve", lambda: V.tensor_tensor(out=lg[:], in0=lg[:], in1=rb[:], op=ALU.add), reads=["lg", "rb"], writes=["lg"])
            P.op("dve", lambda: V.max(out=m8[:], in_=lg[:]), reads=["lg"], writes=["m8"])
            P.op("dve", lambda: V.tensor_scalar(out=msk[:], in0=lg[:], scalar1=m8[:, 3:4], scalar2=None, op0=ALU.is_ge), reads=["lg", "m8"], writes=["msk"])
            P.op("dve", lambda: V.tensor_copy(out=mskb[:], in_=msk[:]), reads=["msk"], writes=["mskb"])
            P.op("dve", lambda: V.tensor_scalar(out=nm[:], in0=m8[:, 0:1], scalar1=-1.0, scalar2=None, op0=ALU.mult), reads=["m8"], writes=["nm"])
            P.op("act", lambda: A.activation(out=ex[:], in_=lg[:], func=AF.Exp, bias=nm[:, 0:1], scale=1.0), reads=["lg", "nm"], writes=["ex"])
            P.op("dve", lambda: V.tensor_tensor(out=ex[:], in0=ex[:], in1=msk[:], op=ALU.mult), reads=["ex", "msk"], writes=["ex"])
            P.op("dve", lambda: V.tensor_reduce(out=ssum[:], in_=ex[:], axis=AX.X, op=ALU.add), reads=["ex"], writes=["ssum"])
            P.op("dve", lambda: V.reciprocal(out=ssum[:], in_=ssum[:]), reads=["ssum"], writes=["ssum"])
            P.op("dve", lambda: V.tensor_scalar(out=wts[:], in0=ex[:], scalar1=ssum[:, 0:1], scalar2=None, op0=ALU.mult), reads=["ex", "ssum"], writes=["wts"])
            P.op("pe", lambda: nc.tensor.matmul(pr[:, 64:64 + NE], lhsT=k.ustrict_b[:], rhs=mskb[:], start=True, stop=True), reads=["mskb", "ustrict_b"], writes=["pr"])
            P.op("pe", lambda: nc.tensor.matmul(pr[:, 128:128 + NE], lhsT=k.ones_b[:], rhs=mskb[:], start=True, stop=True), reads=["mskb", "ones_b"], writes=["pr"])
            P.op("act", lambda: A.copy(out=posf[:], in_=pr[:, 64:64 + NE]), reads=["pr"], writes=["posf"])
            P.op("act", lambda: A.copy(out=oh[:], in_=pr[:, 128:128 + NE]), reads=["pr"], writes=["oh"])
            P.op("dve", lambda: V.tensor_tensor(out=posf[:], in0=posf[:], in1=carry[:], op=ALU.add), reads=["posf", "carry"], writes=["posf"])
            P.op("dve", lambda: V.tensor_tensor(out=carry[:], in0=carry[:], in1=oh[:], op=ALU.add), reads=["carry", "oh", "posf"], writes=["carry"])
            P.op("dve", lambda: V.tensor_scalar(out=val[:], in0=posf[:], scalar1=float(CAP) - 0.5, scalar2=None, op0=ALU.is_lt), reads=["posf"], writes=["val"])
            P.op("dve", lambda: V.tensor_tensor(out=val[:], in0=val[:], in1=msk[:], op=ALU.mult), reads=["val", "msk"], writes=["val"])
            P.op("dve", lambda: V.tensor_tensor(out=nsel[:], in0=posf[:], in1=k.eoff[:], op=ALU.add), reads=["posf", "eoff"], writes=["nsel"])
            P.op("dve", lambda: V.tensor_scalar(out=nsel[:], in0=nsel[:], scalar1=k.trashp[:, 0:1], scalar2=None, op0=ALU.subtract), reads=["nsel", "trashp"], writes=["nsel"])
            P.op("dve", lambda: V.tensor_tensor(out=nsel[:], in0=nsel[:], in1=val[:], op=ALU.mult), reads=["nsel", "val"], writes=["nsel"])
            P.op("dve", lambda: V.tensor_scalar(out=nsel[:], in0=nsel[:], scalar1=k.trashp[:, 0:1], scalar2=-1.0, op0=ALU.add, op1=ALU.mult), reads=["nsel", "trashp"], writes=["nsel"])
            P.op("dve", lambda: V.max(out=t8[:], in_=nsel[:]), reads=["nsel"], writes=["t8"])
            P.op("dve", lambda: V.tensor_scalar(out=idxf[:], in0=t8[:, 0:4], scalar1=-1.0, scalar2=None, op0=ALU.mult), reads=["t8"], writes=["idxf"])
            P.op("dve", lambda t=t: V.tensor_copy(out=k.idx4[:, t, :], in_=idxf[:]), reads=["idxf"], writes=[("idx4", t)])
            for kk_ in range(4):
                P.op("dve", lambda kk_=kk_: V.tensor_scalar(out=oh[:], in0=nsel[:], scalar1=t8[:, kk_:kk_ + 1], scalar2=None, op0=ALU.is_equal), reads=["nsel", "t8", "carry"], writes=["oh"])
                P.op("dve", lambda: V.tensor_tensor(out=oh[:], in0=oh[:], in1=wts[:], op=ALU.mult), reads=["oh", "wts"], writes=["oh"])
                P.op("dve", lambda kk_=kk_, t=t: V.tensor_reduce(out=k.w4[:, t, kk_:kk_ + 1], in_=oh[:], axis=AX.X, op=ALU.add), reads=["oh"], writes=[("w4", t)])
            for kk_ in range(4):
                P.op("pool", lambda kk_=kk_, t=t, i2=i2: nc.gpsimd.indirect_dma_start(out=k.xbuf, out_offset=bass.IndirectOffsetOnAxis(ap=k.idx4[:, t, kk_:kk_ + 1], axis=0),
                                                                                       in_=x1b[i2][:], in_offset=None),
                     reads=[("x1b", i2), ("idx4", t)], writes=[("xbuf", t, kk_)], dma=True)
            if k.dbg and t == 0:
                dump(k, "lg", lg[:], "lg", NE)
                dump(k, "wts", wts[:], "wts", NE)
                dump(k, "nsel", nsel[:], "nsel", NE)
                dump(k, "w4", k.w4[:, 0, :], ("w4", 0), 4)
                dump(k, "idxf", idxf[:], "idxf", 4)
        P.barrier()


def phase4(k, l):
    nc, P = k.nc, k.P
    V, A = nc.vector, nc.scalar
    NSL = CAP // 128
    with ExitStack() as es:
        sb = lambda name, shape, dt: es.enter_context(nc.sbuf_tensor(f"p4l{l}_{name}", shape, dt))
        ps = lambda name, shape, dt: es.enter_context(nc.psum_tensor(f"p4l{l}_{name}", shape, dt))
        wu = [sb(f"wu{i}", [128, 8, 2 * D], BF16) for i in range(2)]
        wd = [sb(f"wd{i}", [128, 8, D], BF16) for i in range(2)]
        bun = sb("bun", [NE, 2 * D], F32)
        bupT = sb("bupT", [128, 16, NE], F32)
        bdn = [sb(f"bdn{i}", [128, D], F32) for i in range(2)]
        xe = [sb(f"xe{i}", [128, D], BF16) for i in range(2)]
        XeT = sb("XeT", [128, 8, CAP], BF16)
        actT = sb("actT", [128, 8, CAP], BF16)
        gsb = sb("gsb", [128, CAP], F32)
        lsb = sb("lsb", [128, CAP], F32)
        sig = sb("sig", [128, CAP], F32)
        ysb = [sb(f"ysb{i}", [128, D], F32) for i in range(2)]
        yst = [sb(f"yst{i}", [128, D], BF16) for i in range(2)]
        zt = sb("zt", [128, D], BF16)
        pT = ps("pT", [128, 8, 128], BF16)
        pA = ps("pA", [128, 512], F32)
        pB = ps("pB", [128, 512], F32)
        pC = ps("pC", [128, 512], F32)
        pD = ps("pD", [128, 512], F32)

        P.op("dve", lambda: V.memset(zt[:], 0.0), writes=["zt"])
        P.dma("sp", k.ybuf[TRASH:TRASH + 128, :], zt[:], reads=["zt"], writes=["ybuf_trash"])
        P.dma("sp", bun[:], k.exp_b_up[l], writes=["bun"])
        for j in range(16):
            P.op("pe", lambda j=j: nc.tensor.transpose(out=pA[:, j * NE:(j + 1) * NE], in_=bun[:, j * 128:(j + 1) * 128], identity=k.ident_f[0:NE, 0:NE]),
                 reads=["bun", "ident"], writes=["pA"])
        P.op("act", lambda: A.copy(out=bupT[:].rearrange("p a b -> p (a b)"), in_=pA[:, 0:16 * NE]), reads=["pA"], writes=["bupT"])

        def load_w(e):
            b = e % 2
            wuv = k.exp_w_up[l, e].rearrange("(c p) n -> p c n", p=128)
            wdv = k.exp_w_down[l, e].rearrange("(c p) n -> p c n", p=128)
            for c in range(8):
                P.dma("pool", wu[b][:, c, :], wuv[:, c, :], writes=[("wu", b, c)])
            for c in range(8):
                P.dma("pool", wd[b][:, c, :], wdv[:, c, :], writes=[("wd", b, c)])
            P.dma("act", bdn[b][:], k.exp_b_down[l, e:e + 1, :].partition_broadcast(128), writes=[("bdn", b)])

        load_w(0)
        for e in range(NE):
            b = e % 2
            if e + 1 < NE:
                load_w(e + 1)
            for i in range(NSL):
                i2 = i % 2
                P.dma("sp", xe[i2][:], k.xbuf[e * CAP + i * 128:e * CAP + (i + 1) * 128, :], writes=[("xe", i2)])
                for c in range(8):
                    P.op("pe", lambda c=c, i2=i2: nc.tensor.transpose(out=pT[:, c, :], in_=xe[i2][:, c * 128:(c + 1) * 128], identity=k.ident_b[:]),
                         reads=[("xe", i2), "ident_b"], writes=["pT"])
                P.op("dve", lambda i=i: V.tensor_copy(out=XeT[:, :, i * 128:(i + 1) * 128], in_=pT[:]), reads=["pT"], writes=["XeT"])
            for j in range(8):
                groups = ((pA, "pA", j, 0), (pB, "pB", j, 512), (pC, "pC", 8 + j, 0), (pD, "pD", 8 + j, 512))
                for (dst, key, fc, n0) in groups:
                    for c in range(8):
                        P.op("pe", lambda c=c, dst=dst, fc=fc, n0=n0, b=b: nc.tensor.matmul(dst[:], lhsT=wu[b][:, c, fc * 128:(fc + 1) * 128], rhs=XeT[:, c, n0:n0 + 512],
                                                                                           start=(c == 0), stop=(c == 7)),
                             reads=[("wu", b, c), "XeT"], writes=[key])
                G, Lq, Sg = gsb, lsb, sig
                kg, kl, ks = "gsb", "lsb", "sig"
                for (dst, key, fc, n0) in groups:
                    T_, kt_ = (G, kg) if fc < 8 else (Lq, kl)
                    P.op("act", lambda dst=dst, fc=fc, n0=n0, e=e, T_=T_: A.activation(out=T_[:, n0:n0 + 512], in_=dst[:], func=AF.Identity, bias=bupT[:, fc, e:e + 1], scale=1.0),
                         reads=[key, "bupT"], writes=[kt_])
                P.op("dve", lambda: V.tensor_scalar(out=G[:], in0=G[:], scalar1=7.0, scalar2=None, op0=ALU.min), reads=[kg], writes=[kg])
                P.op("act", lambda: A.activation(out=Sg[:], in_=G[:], func=AF.Sigmoid, scale=1.702), reads=[kg], writes=[ks])
                P.op("pool", lambda: nc.gpsimd.tensor_scalar(out=Lq[:], in0=Lq[:], scalar1=-7.0, scalar2=7.0, op0=ALU.max, op1=ALU.min), reads=[kl], writes=[kl])
                P.op("dve", lambda: V.tensor_tensor(out=Sg[:], in0=G[:], in1=Sg[:], op=ALU.mult), reads=[kg, ks], writes=[ks])
                P.op("dve", lambda j=j: V.scalar_tensor_tensor(out=actT[:, j, :], in0=Lq[:], scalar=1.0, in1=Sg[:], op0=ALU.add, op1=ALU.mult),
                     reads=[kl, ks], writes=[("actT", j)])
            for i in range(NSL):
                i2 = i % 2
                for hf in range(2):
                    dstb, key = ((pA, "pA"), (pC, "pC"), (pB, "pB"), (pD, "pD"))[2 * i2 + hf]
                    for c in range(8):
                        P.op("pe", lambda c=c, dstb=dstb, hf=hf, i=i, b=b: nc.tensor.matmul(dstb[:], lhsT=actT[:, c, i * 128:(i + 1) * 128], rhs=wd[b][:, c, hf * 512:(hf + 1) * 512],
                                                                                           start=(c == 0), stop=(c == 7)),
                             reads=[("actT", c), ("wd", b, c)], writes=[key])
                    P.op("act", lambda dstb=dstb, hf=hf, i2=i2: A.copy(out=ysb[i2][:, hf * 512:(hf + 1) * 512], in_=dstb[:]), reads=[key], writes=[("ysb", i2)])
                P.op("pool", lambda i2=i2, b=b: nc.gpsimd.tensor_tensor(out=yst[i2][:], in0=ysb[i2][:], in1=bdn[b][:], op=ALU.add),
                     reads=[("ysb", i2), ("bdn", b)], writes=[("yst", i2)])
                P.dma("sp", k.ybuf[e * CAP + i * 128:e * CAP + (i + 1) * 128, :], yst[i2][:], reads=[("yst", i2)], writes=[("ybuf", e, i)])
        P.barrier()


def phase5(k, l, last):
    nc, P = k.nc, k.P
    V, A = nc.vector, nc.scalar
    with ExitStack() as es:
        sb = lambda name, shape, dt: es.enter_context(nc.sbuf_tensor(f"p5l{l}_{name}", shape, dt))
        lnw = sb("lnw", [128, D], F32)
        lnb = sb("lnb", [128, D], F32)
        k.ln_st = sb("ln_st", [128, 12], F32)
        k.ln_ag = sb("ln_ag", [128, 2], F32)
        k.ln_rs = sb("ln_rs", [128, 1], F32)
        gk = [[sb(f"gk{i}_{q}", [128, D], BF16) for q in range(4)] for i in range(2)]
        x1t = [sb(f"x1t{i}", [128, D], F32) for i in range(2)]
        acc = sb("acc", [128, D], F32)
        tmp = sb("tmp", [128, D], F32)
        x2 = [sb(f"x2_{i}", [128, D], F32) for i in range(2)]
        bcast_row(k, P, "sp", lnw[:], k.ln2_w[l:l + 1, :], "lnw2")
        bcast_row(k, P, "sp", lnb[:], k.ln2_b[l:l + 1, :], "lnb2")
        dst_d = k.out if last else k.x2
        for t in range(NT):
            i2 = t % 2
            rows = slice(t * 128, (t + 1) * 128)
            P.dma("sp", x1t[i2][:], k.x1[rows, :], writes=[("x1t", i2)])
            for q in range(4):
                P.op("pool", lambda q=q, t=t, i2=i2: nc.gpsimd.indirect_dma_start(out=gk[i2][q][:], out_offset=None, in_=k.ybuf,
                                                                                 in_offset=bass.IndirectOffsetOnAxis(ap=k.idx4[:, t, q:q + 1], axis=0)),
                     writes=[("gk", i2, q)], dma=True)
            P.op("dve", lambda t=t, i2=i2: V.tensor_scalar(out=acc[:], in0=gk[i2][0][:], scalar1=k.w4[:, t, 0:1], scalar2=None, op0=ALU.mult),
                 reads=[("gk", i2, 0)], writes=["acc"])
            for q in range(1, 4):
                P.op("dve", lambda q=q, t=t, i2=i2: V.scalar_tensor_tensor(out=acc[:], in0=gk[i2][q][:], scalar=k.w4[:, t, q:q + 1], in1=acc[:], op0=ALU.mult, op1=ALU.add),
                     reads=[("gk", i2, q), "acc"], writes=["acc"])
            if k.dbg and k.moe_dbg is not None:
                P.dma("sp", k.moe_dbg[rows, :], acc[:], reads=["acc"], writes=[("moe_dbg", t)])
            P.op("dve", lambda i2=i2: V.scalar_tensor_tensor(out=acc[:], in0=x1t[i2][:], scalar=ALPHA, in1=acc[:], op0=ALU.mult, op1=ALU.add),
                 reads=[("x1t", i2), "acc"], writes=["acc"])
            layer_norm_tile(k, P, acc, x2[i2], lnw, lnb, tmp, "acc", ("x2", i2), "2")
            P.dma("sp", dst_d[rows, :], x2[i2][:], reads=[("x2", i2)], writes=[("x2d", t)])
        P.barrier()


def make_inputs(inputs, b, perm, w_in_r=None):
    f32 = lambda n: np.ascontiguousarray(np.asarray(inputs[n], dtype=np.float32))
    if w_in_r is None:
        w_in_r = np.ascontiguousarray(np.asarray(inputs["w_in"], dtype=np.float32)[:, :, perm])
    m = {"x": np.ascontiguousarray(np.asarray(inputs["x"], dtype=np.float32)[b]),
         "pos": np.ascontiguousarray(np.asarray(inputs["positions"], dtype=np.int32)[b].reshape(1, -1)), "w_in": w_in_r,
         "hg_lb": f32("hg_lower_bounds"), "hg_nw": f32("hg_norm_w"), "gm_lnw": f32("gm_ln_w"), "gm_lnb": f32("gm_ln_b"),
         "gm_ws": f32("gm_spatial_w"), "gm_bs": f32("gm_spatial_b"), "gm_nw": f32("gm_norm_w"),
         "nsa_pe": f32("nsa_cmp_pe"), "nsa_w1": f32("nsa_cmp_w1"), "nsa_w2": f32("nsa_cmp_w2"), "nsa_nw": f32("nsa_norm_w"),
         "w_out": f32("w_out"), "ln1_w": f32("ln1_w"), "ln1_b": f32("ln1_b"), "ln2_w": f32("ln2_w"), "ln2_b": f32("ln2_b"),
         "router_w": f32("router_w"), "router_b": f32("router_b"), "exp_w_up": f32("exp_w_up"), "exp_b_up": f32("exp_b_up"),
         "exp_w_down": f32("exp_w_down"), "exp_b_down": f32("exp_b_down")}
    for n, v in host_consts().items():
        m["c_" + n] = v
    return m


def kernel(**inputs):
    perm = w_in_perm()
    w_in_r = np.ascontiguousarray(np.asarray(inputs["w_in"], dtype=np.float32)[:, :, perm])
    nc = build(nlayers=L, dbg=False)
    in_maps = [make_inputs(inputs, b, perm, w_in_r) for b in range(8)]
    res = run_bass_kernel_spmd(nc, in_maps, core_ids=list(range(8)))
    return np.stack([np.asarray(r["out"], dtype=np.float32) for r in res.results], axis=0)
```

```python
import math
from contextlib import ExitStack

import numpy as np
import concourse.bass as bass
import concourse.mybir as mybir
from concourse.bass_utils import run_bass_kernel_spmd

F32 = mybir.dt.float32
BF16 = mybir.dt.bfloat16
I32 = mybir.dt.int32
U32 = mybir.dt.uint32
AF = mybir.ActivationFunctionType
ALU = mybir.AluOpType
AX = mybir.AxisListType

S = 4096
D = 1024
NT = S // 128
L = 2
NE = 32
CAP = 1024
NCOLA = 1816
NCH_B = 14
WCOLS = NCOLA + NCH_B * 128
ALPHA = (2 * L) ** 0.25
PI = math.pi
TRASH = NE * CAP

COMPUTE = ("pe", "dve", "act", "pool")
DMAQ = ("sp", "act", "pool")
NDMASEM = 6
NCSEM = 8
SEM_KEYS = [(e, i) for e in COMPUTE for i in range(NCSEM)] + [(q + "_q", i) for q in DMAQ for i in range(NDMASEM)]


class Op:
    __slots__ = ("eng", "fn", "reads", "writes", "dma", "deps", "raw", "need_inc", "sem", "val", "idx")


class Prog:
    def __init__(self, nc, sems):
        self.nc = nc
        self.sems = sems
        self.eng = {"pe": nc.tensor, "dve": nc.vector, "act": nc.scalar, "pool": nc.gpsimd, "sp": nc.sync}
        self.ops = []
        self.last_writer = {}
        self.readers = {}
        self.cnt = {e: 0 for e in COMPUTE}
        self.dcnt = {q: 0 for q in DMAQ}
        self.waited = {}
        self.dma_last = {}
        self.nwait = 0
        self.ntotal = 0
        import os
        self.verbose = bool(os.environ.get("VERB"))

    def op(self, eng, fn, reads=(), writes=(), dma=False):
        o = Op()
        o.eng, o.fn, o.dma = eng, fn, dma
        o.reads, o.writes = tuple(reads), tuple(writes)
        o.need_inc = False
        o.idx = len(self.ops)
        deps = set()
        raw = set()
        lw, rd = self.last_writer, self.readers
        for k in o.reads:
            w = lw.get(k)
            if w is not None:
                deps.add(w)
                raw.add(w)
        for k in o.writes:
            w = lw.get(k)
            if w is not None:
                deps.add(w)
            r = rd.get(k)
            if r:
                deps.update(r)
        for k in o.reads:
            rd.setdefault(k, []).append(o.idx)
        for k in o.writes:
            lw[k] = o.idx
            rd[k] = []
        deps.discard(o.idx)
        o.deps = deps
        o.raw = raw
        self.ops.append(o)
        return o

    def dma(self, q, out, in_, reads=(), writes=(), **kw):
        e = self.eng[q]
        return self.op(q, lambda: e.dma_start(out=out, in_=in_, **kw), reads, writes, dma=True)

    def barrier(self):
        ops = self.ops
        tails = set()
        last_by_eng = {}
        for o in ops:
            if o.dma:
                tails.add(o.idx)
            else:
                last_by_eng[o.eng] = o.idx
        tails.update(last_by_eng.values())
        for en in ("pe", "dve", "act", "pool", "sp"):
            o = Op()
            o.eng, o.dma, o.reads, o.writes, o.need_inc = en, False, (), (), False
            e = self.eng[en]
            o.fn = (lambda e=e: e.nop())
            o.idx = len(ops)
            o.deps = set(tails)
            o.raw = set()
            ops.append(o)
        self.emit()

    def emit(self):
        ops, sems = self.ops, self.sems
        def needs_sync(o, d):
            p = ops[d]
            return p.dma or p.eng != o.eng or (o.eng != "pe" and d in o.raw)
        for o in ops:
            for d in o.deps:
                if needs_sync(o, d):
                    ops[d].need_inc = True
        cnt, dcnt, waited = self.cnt, self.dcnt, self.waited
        for o in ops:
            e = self.eng[o.eng]
            pre = []
            if o.dma:
                i = dcnt[o.eng]
                dcnt[o.eng] += 1
                sk = (o.eng + "_q", i % NDMASEM)
                o.sem = sk
                o.val = 16 * (i // NDMASEM + 1)
                self.dma_last[sk] = o.val
                if o.val > 16:
                    pre.append((sk, o.val - 16))
            for d in o.deps:
                p = ops[d]
                if p.dma or needs_sync(o, d):
                    pre.append((p.sem, p.val))
            best = {}
            for sk, v in pre:
                if v > best.get(sk, 0):
                    best[sk] = v
            for sk, v in best.items():
                if waited.get((o.eng, sk), 0) >= v:
                    continue
                waited[(o.eng, sk)] = v
                e.wait_ge(sems[sk], v)
                self.nwait += 1
                if self.verbose:
                    print("   wait", o.eng, sk, v)
            ins = o.fn()
            if self.verbose:
                print("op", o.idx, o.eng, "dma" if o.dma else "", o.writes, "inc" if (o.need_inc or o.dma) else "", getattr(o, "sem", None) if o.dma else "", (o.val if o.dma else ""))
            if o.dma:
                ins.then_inc(sems[o.sem], 16)
            elif o.need_inc:
                i = cnt[o.eng]
                cnt[o.eng] += 1
                o.sem = (o.eng, i % NCSEM)
                o.val = i // NCSEM + 1
                ins.then_inc(sems[o.sem], 1)
        self.ntotal += len(ops)
        self.ops = []
        self.last_writer = {}
        self.readers = {}

    def finish(self):
        self.emit()
        e = self.eng["sp"]
        for sk, v in self.dma_last.items():
            e.wait_ge(self.sems[sk], v)


def host_consts():
    c = {}
    c["ident"] = np.eye(128, dtype=np.float32)
    s = np.arange(128)[:, None]
    t = np.arange(128)[None, :]
    same = (s // 64) == (t // 64)
    mid = (t // 64) * 64 + 31
    c["mcum"] = (same & (s <= t)).astype(np.float32)
    c["mmid"] = (c["mcum"] - (same & (s <= mid)).astype(np.float32))
    c["mrem"] = (same & (s > t)).astype(np.float32)
    c["tri"] = (s <= t).astype(np.float32)
    c["trit"] = (s >= t).astype(np.float32)
    inv = (10000.0 ** (-np.arange(0, 64, 2, dtype=np.float32) / 64)).astype(np.float32)
    p = np.arange(128)
    sgn = np.where((p % 64) < 32, -1.0, 1.0).astype(np.float32)
    col = np.zeros((128, 4), np.float32)
    col[:, 0] = inv[p % 32]
    col[:, 1] = sgn
    col[:, 2] = 2 * PI * sgn
    col[:, 3] = -PI
    c["ropecol"] = col
    jj = np.arange(64)[:, None, None]
    kt_ = np.arange(32)[None, :, None]
    kk_ = np.arange(128)[None, None, :]
    E = (jj == 2 * kt_ + kk_ // 64).astype(np.float32)
    c["edup"] = np.concatenate([E, E], 0)
    p_ = np.arange(128)[:, None]
    q_ = np.arange(512)[None, :]
    c["win01"] = np.stack([((128 * dl + p_ - q_ <= 0) & (128 * dl + p_ - q_ > -512)).astype(np.float32) for dl in range(-4, 4)], 1)
    c["cmp01"] = np.stack([(16 * p_ + 31 - q_ <= u).astype(np.float32) for u in (0, 512, 1024, 1536, 2048)], 1)
    tq = np.arange(S).reshape(NT, 128)
    cur = (tq // 64)[:, :, None]
    jb = np.arange(64)[None, None, :]
    fut = jb > cur
    frc = (jb == 0) | (jb == cur) | (jb == cur - 1)
    keep = (~fut & ~frc).astype(np.float32)
    add = np.where(frc, 1e9, np.where(fut, -1e9, 0.0)).astype(np.float32)
    c["wpad"] = np.ascontiguousarray(np.maximum(0, 511 - tq).T.astype(np.float32))
    c["impkeep"] = np.ascontiguousarray(keep.transpose(1, 0, 2))
    c["impadd"] = np.ascontiguousarray(add.transpose(1, 0, 2))
    cc = np.arange(256)
    ov = np.zeros((256, 65), np.float32)
    for c_ in range(255):
        for uu in (c_, c_ + 1):
            ov[c_, uu // 4] += 1.0
    ov[:, 64] = 1.0
    c["ovext"] = np.ascontiguousarray(ov.reshape(2, 128, 65).transpose(1, 0, 2))
    c["eoff"] = np.tile((np.arange(NE, dtype=np.float32) * CAP)[None, :], (128, 1)).astype(np.float32)
    c["trashp"] = (TRASH + np.arange(128, dtype=np.float32)).reshape(128, 1).astype(np.float32)
    return c


def w_in_perm():
    hg_q, hg_f, hg_i, hg_g, gm_u, gm_v, q, k_c, v_c, k_s, v_s, k_w, v_w, gts = (
        0, 256, 512, 768, 1024, 1280, 1536, 2048, 2176, 2304, 2432, 2560, 2688, 2816)
    A = list(range(0, 1536)) + list(range(v_s, v_s + 128)) + list(range(v_w, v_w + 128)) + list(range(gts, gts + 24))

    def sw(c0, n):
        out = []
        for h in range(n // 64):
            b = c0 + h * 64
            out += list(range(b + 32, b + 64)) + list(range(b, b + 32))
        return out
    B = (list(range(q, q + 512)) + sw(q, 512) + list(range(k_s, k_s + 128)) + sw(k_s, 128)
         + list(range(k_w, k_w + 128)) + sw(k_w, 128) + list(range(k_c, k_c + 128)) + list(range(v_c, v_c + 128)))
    perm = np.array(A + B, dtype=np.int64)
    assert perm.shape[0] == WCOLS
    return perm


class K:
    pass


def build(nlayers=L, dbg=False, stop_after=None, stage=None, inject_nsa=False, phases=(1, 2, 3, 4, 5)):
    nc = bass.Bass("TRN2", target_bir_lowering=False)
    k = K()
    global _LASTK
    _LASTK = k
    k.stage = stage
    import os
    k.dbgv = int(os.environ.get('DBGV', '0'))
    k.nc = nc
    k.dbg = dbg

    def din(name, shape, dt=F32):
        return nc.dram_tensor(name, list(shape), dt, kind="ExternalInput").ap()

    def dscr(name, shape, dt, out=False):
        return nc.dram_tensor(name, list(shape), dt, kind=("ExternalOutput" if (out and dbg) else "Internal")).ap()

    k.x = din("x", [S, D])
    k.pos = din("pos", [1, S], I32)
    k.w_in = din("w_in", [L, D, WCOLS])
    k.lbraw = din("hg_lb", [L, 256])
    k.hg_nw = din("hg_nw", [L, 256])
    k.gm_lnw = din("gm_lnw", [L, 256])
    k.gm_lnb = din("gm_lnb", [L, 256])
    k.gm_ws = din("gm_ws", [L, 4, 128, 128])
    k.gm_bs = din("gm_bs", [L, 4, 128])
    k.gm_nw = din("gm_nw", [L, 256])
    k.nsa_pe = din("nsa_pe", [L, 2, 32, 64]); k.nsa_w1 = din("nsa_w1", [L, 2, 2048, 128])
    k.nsa_w2 = din("nsa_w2", [L, 2, 128, 64]); k.nsa_nw = din("nsa_nw", [L, 512])
    k.w_out = din("w_out", [L, D, D])
    k.ln1_w = din("ln1_w", [L, D]); k.ln1_b = din("ln1_b", [L, D])
    k.ln2_w = din("ln2_w", [L, D]); k.ln2_b = din("ln2_b", [L, D])
    k.router_w = din("router_w", [L, D, NE]); k.router_b = din("router_b", [L, NE])
    k.exp_w_up = din("exp_w_up", [L, NE, D, 2 * D]); k.exp_b_up = din("exp_b_up", [L, NE, 2 * D])
    k.exp_w_down = din("exp_w_down", [L, NE, D, D]); k.exp_b_down = din("exp_b_down", [L, NE, D])
    k.inject_nsa = inject_nsa
    if dbg and inject_nsa:
        k.ynsa_in = din("ynsa_in", [L, S, 512], BF16)
    k.cst = {n: din("c_" + n, v.shape) for n, v in host_consts().items()}
    k.out = nc.dram_tensor("out", [S, D], F32, kind="ExternalOutput").ap()
    k.ft = dscr("ft", [12, 128, S], BF16, out=True)
    k.tmv = dscr("tmv", [S, 256], BF16, out=True)
    k.gts = dscr("gts", [S, 24], F32, out=True)
    k.y = dscr("y", [S, D], BF16, out=True)
    k.x1 = dscr("x1", [S, D], F32, out=True)
    k.x2 = dscr("x2", [S, D], F32, out=True)
    k.xbuf = dscr("xbuf", [TRASH + 128, D], BF16)
    k.ybuf = dscr("ybuf", [TRASH + 128, D], BF16)
    k.moe_dbg = dscr("moe_dbg", [S, D], F32, out=True) if dbg else None
    k.dbgout = dscr("dbgout", [24, 128, 512], F32, out=True)
    k.ndump = 0
    k.dumpnames = []

    with ExitStack() as es:
        sems = {}
        for sk in SEM_KEYS:
            nm = sk if isinstance(sk, str) else f"{sk[0]}{sk[1]}"
            sems[sk] = es.enter_context(nc.semaphore("s_" + nm))
        P = Prog(nc, sems)
        k.P = P
        k.es = es
        setup_consts(k)
        for l in range(nlayers if stop_after != "setup" else 0):
            if 1 in phases:
                phase1(k, l)
            if stop_after == (l, 1):
                break
            if 2 in phases:
                phase2(k, l)
            if stop_after == (l, 2):
                break
            if 3 in phases:
                phase3(k, l)
            if stop_after == (l, 3):
                break
            if 4 in phases:
                phase4(k, l)
            if 5 in phases:
                phase5(k, l, last=(l == nlayers - 1))
        P.finish()
        print("[build] ops", P.ntotal, "waits", P.nwait, "cnt", P.cnt, "dcnt", P.dcnt)
    return nc


def setup_consts(k):
    nc, P, es = k.nc, k.P, k.es
    sb = lambda name, shape, dt: es.enter_context(nc.sbuf_tensor(name, shape, dt))
    k.ident_f = sb("ident_f", [128, 128], F32)
    k.ident_b = sb("ident_b", [128, 128], BF16)
    k.mcum = sb("mcum", [128, 128], F32)
    k.mmid = sb("mmid", [128, 128], F32)
    k.mrem = sb("mrem", [128, 128], F32)
    k.tri = sb("tri", [128, 128], F32)
    k.trit = sb("trit", [128, 128], F32)
    k.mcum_b = sb("mcum_b", [128, 128], BF16)
    k.mmid_b = sb("mmid_b", [128, 128], BF16)
    k.mrem_b = sb("mrem_b", [128, 128], BF16)
    k.ones_b = sb("ones_b", [128, 128], BF16)
    k.ropecol = sb("ropecol", [128, 4], F32)
    k.ones_f = sb("ones_f", [128, 128], F32)
    k.idx4 = sb("idx4", [128, NT, 4], I32)
    k.w4 = sb("w4", [128, NT, 4], F32)
    k.eoff = sb("eoff", [128, NE], F32)
    k.trashp = sb("trashp", [128, 1], F32)
    k.ustrict_b = sb("ustrict_b", [128, 128], BF16)
    for n, t in (("ident", k.ident_f), ("mcum", k.mcum), ("mmid", k.mmid), ("mrem", k.mrem), ("tri", k.tri),
                 ("trit", k.trit), ("ropecol", k.ropecol), ("eoff", k.eoff), ("trashp", k.trashp)):
        P.dma("sp", t[:], k.cst[n], writes=[n])
    P.op("dve", lambda: nc.vector.tensor_copy(out=k.ident_b[:], in_=k.ident_f[:]), reads=["ident"], writes=["ident_b"])
    P.op("dve", lambda: nc.vector.memset(k.ones_f[:], 1.0), writes=["ones_f"])
    P.op("dve", lambda: nc.vector.memset(k.ones_b[:], 1.0), writes=["ones_b"])
    P.op("dve", lambda: nc.vector.tensor_sub(out=k.ones_f[:], in0=k.tri[:], in1=k.ident_f[:]), reads=["tri", "ident", "ones_f"], writes=["ustr_tmp"])
    P.op("dve", lambda: nc.vector.tensor_copy(out=k.ustrict_b[:], in_=k.ones_f[:]), reads=["ustr_tmp"], writes=["ustrict_b"])
    P.op("dve", lambda: nc.vector.memset(k.ones_f[:], 1.0), reads=["ustrict_b"], writes=["ones_f"])
    P.op("dve", lambda: nc.vector.tensor_copy(out=k.mcum_b[:], in_=k.mcum[:]), reads=["mcum"], writes=["mcum_b"])
    P.op("dve", lambda: nc.vector.tensor_copy(out=k.mmid_b[:], in_=k.mmid[:]), reads=["mmid"], writes=["mmid_b"])
    P.op("dve", lambda: nc.vector.tensor_copy(out=k.mrem_b[:], in_=k.mrem[:]), reads=["mrem"], writes=["mrem_b"])
    P.barrier()


def setup_rope(k, es, l):
    nc, P = k.nc, k.P
    sb = lambda name, shape, dt: es.enter_context(nc.sbuf_tensor(f"{name}l{l}", shape, dt))
    k.ropeC = sb("ropeC", [128, S], F32)
    k.ropeS = sb("ropeS", [128, S], F32)
    with nc.sbuf_tensor(f"posil{l}", [128, S], I32) as posi, nc.sbuf_tensor(f"ropeTl{l}", [128, S], F32) as ropeT, nc.sbuf_tensor(f"ropeT2l{l}", [128, S], F32) as ropeT2:
        k.ropeT, k.ropeT2 = ropeT, ropeT2
        P.dma("sp", posi[:], k.pos.partition_broadcast(128), writes=["posi"])
        C, Sg, col = k.ropeC, k.ropeS, k.ropecol
        P.op("dve", lambda: nc.vector.tensor_copy(out=C[:], in_=posi[:]), reads=["posi"], writes=["C"])
        P.op("dve", lambda: nc.vector.tensor_scalar(out=C[:], in0=C[:], scalar1=col[:, 0:1], scalar2=None, op0=ALU.mult),
             reads=["C", "ropecol"], writes=["C"])
        def red(T, add):
            P.op("dve", lambda: nc.vector.tensor_scalar(out=T[:], in0=C[:], scalar1=1.0 / (2 * PI), scalar2=add, op0=ALU.mult, op1=ALU.add),
                 reads=["C"], writes=["T" + str(add)])
            P.op("dve", lambda: nc.vector.tensor_copy(out=posi[:], in_=T[:]), reads=["T" + str(add)], writes=["posi"])
            P.op("dve", lambda: nc.vector.tensor_copy(out=k.ropeT[:], in_=posi[:]), reads=["posi"], writes=["ropeT"])
            P.op("dve", lambda: nc.vector.tensor_sub(out=T[:], in0=T[:], in1=k.ropeT[:]), reads=["ropeT", "T" + str(add)], writes=["T" + str(add)])
            P.op("dve", lambda: nc.vector.tensor_scalar(out=k.ropeT[:], in0=T[:], scalar1=0.5, scalar2=None, op0=ALU.is_gt), reads=["T" + str(add)], writes=["ropeT"])
            P.op("dve", lambda: nc.vector.tensor_sub(out=T[:], in0=T[:], in1=k.ropeT[:]), reads=["ropeT", "T" + str(add)], writes=["T" + str(add)])
        red(Sg, 0.0)
        P.op("act", lambda: nc.scalar.activation(out=Sg[:], in_=Sg[:], func=AF.Sin, scale=col[:, 2:3]), reads=["T0.0", "ropecol"], writes=["S"])
        red(k.ropeT2, 0.25)
        P.op("act", lambda: nc.scalar.activation(out=C[:], in_=k.ropeT2[:], func=AF.Sin, scale=2 * PI), reads=["T0.25", "C"], writes=["C"])
        P.barrier()


def dump(k, name, ap, key, width, parts=128):
    if not k.dbg:
        return
    i = k.ndump
    k.ndump += 1
    k.dumpnames.append(name)
    k.P.dma("pool", k.dbgout[i, 0:parts, 0:width], ap, reads=[key], writes=[("dbgout", i)])


def bcast_row(k, P, q, dst, src_row, key):
    P.dma(q, dst, src_row.partition_broadcast(128), writes=[key])


class _Stop(Exception):
    pass


def phase1(k, l):
    with ExitStack() as es:
        setup_rope(k, es, l)
        try:
            _phase1(k, l, es)
        except _Stop:
            pass
        k.P.barrier()


def ck(k, n):
    if k.stage == n:
        raise _Stop()


def _phase1(k, l, es):
    nc, P = k.nc, k.P
    if True:
        sb = lambda name, shape, dt: es.enter_context(nc.sbuf_tensor(f"p1l{l}_{name}", shape, dt))
        ps = lambda name, shape, dt: es.enter_context(nc.psum_tensor(f"p1l{l}_{name}", shape, dt))
        wb = sb("wb", [128, 8, WCOLS], BF16)
        xt = [sb(f"xt{i}", [128, D], F32) for i in range(2)]
        xb = [sb(f"xb{i}", [128, D], BF16) for i in range(2)]
        xTb = [sb(f"xTb{i}", [128, 8, 512], BF16) for i in range(2)]
        hg_nw = sb("hg_nw", [128, 256], F32)
        gm_lnw = sb("gm_lnw", [128, 256], F32)
        gm_lnb = sb("gm_lnb", [128, 256], F32)
        gm_nw = sb("gm_nw", [128, 256], F32)
        lbt = sb("lbt", [128, 2, 256], F32)
        lb = sb("lb", [128, 256], F32)
        oml = sb("oml", [128, 256], F32)
        wsn = sb("wsn", [128, 4, 128], F32)
        wsb = sb("wsb", [128, 4, 128], BF16)
        wsT = sb("wsT", [128, 4, 128], BF16)
        bsT = sb("bsT", [128, 4], F32)
        pT = ps("pT", [128, 8, 128], BF16)
        pm = [ps(f"pm{i}", [128, 512], F32) for i in range(4)]
        pa = ps("pa", [128, 512], F32)
        pb = ps("pb", [128, 512], F32)
        pc = ps("pc", [128, 512], F32)
        pcb = pc[:].bitcast(BF16)

        x_src = k.x if l == 0 else k.x2
        wv = k.w_in[l].rearrange("(c p) n -> p c n", p=128)
        half = WCOLS // 2
        for c in range(8):
            for hh in range(2):
                P.dma("pool", wb[:, c, hh * half:(hh + 1) * half], wv[:, c, hh * half:(hh + 1) * half], writes=[("wb", c), ("wbser", (2 * c + hh) % 2)])
        bcast_row(k, P, "sp", hg_nw[:], k.hg_nw[l:l + 1, :], "hg_nw")
        bcast_row(k, P, "sp", gm_lnw[:], k.gm_lnw[l:l + 1, :], "gm_lnw")
        bcast_row(k, P, "sp", gm_lnb[:], k.gm_lnb[l:l + 1, :], "gm_lnb")
        bcast_row(k, P, "sp", gm_nw[:], k.gm_nw[l:l + 1, :], "gm_nw")
        if l > 0:
            P.dma("sp", lbt[:].rearrange("p a n -> p (a n)"),
                  k.lbraw.rearrange("a n -> (a n)").unsqueeze(0).partition_broadcast(128), writes=["lbt"])
            P.op("dve", lambda: nc.vector.tensor_sub(out=lb[:], in0=lbt[:, 1, :], in1=lbt[:, 0, :]), reads=["lbt"], writes=["lb"])
            P.op("act", lambda: nc.scalar.activation(out=lb[:], in_=lb[:], func=AF.Sigmoid), reads=["lb"], writes=["lb"])
            P.op("dve", lambda: nc.vector.tensor_scalar(out=oml[:], in0=lb[:], scalar1=-1.0, scalar2=1.0, op0=ALU.mult, op1=ALU.add),
                 reads=["lb"], writes=["oml"])
        P.dma("sp", wsn[:], k.gm_ws[l].rearrange("g t s -> t g s"), writes=["wsn"])
        P.dma("sp", bsT[:], k.gm_bs[l].rearrange("g t -> t g"), writes=["bsT"], allow_slow_non_contiguous=True)
        trit = k.trit
        for g in range(4):
            P.op("dve", lambda g=g: nc.vector.tensor_tensor(out=wsb[:, g, :], in0=wsn[:, g, :], in1=trit[:], op=ALU.mult),
                 reads=["wsn"], writes=["wsb"])
        for g in range(4):
            P.op("pe", lambda g=g: nc.tensor.transpose(out=pT[:, g, :], in_=wsb[:, g, :], identity=k.ident_b[:]),
                 reads=["wsb", "ident_b"], writes=["pT"])
        P.op("dve", lambda: nc.vector.tensor_copy(out=wsT[:], in_=pT[:, 0:4, :]), reads=["pT"], writes=["wsT"])

        st_f = sb("st_f", [128, 2, 64], F32)
        st_b = sb("st_b", [128, 2, 64], BF16)
        P.op("dve", lambda: nc.vector.memset(st_f[:], 0.0), writes=["st_f"])
        P.op("dve", lambda: nc.vector.memset(st_b[:], 0.0), writes=["st_b"])

        W = {}
        for n in ("qf", "sg", "sgate", "logf", "kk", "ebm", "enbm", "eb", "erem", "t1", "t2", "osq", "nwg", "gsq", "ginn", "gsv"):
            W[n] = sb(n, [128, 256], F32)
        gx = sb("gx", [128, 512], F32)
        gg = sb("gg", [128, 512], F32)
        gt = sb("gt", [128, 512], F32)
        vb = sb("vb", [128, 256], BF16)
        qe = sb("qe", [128, 256], BF16)
        ke = sb("ke", [128, 256], BF16)
        qb = sb("qb", [128, 256], BF16)
        kd = sb("kd", [128, 256], BF16)
        vln = sb("vln", [128, 256], BF16)
        lfh = sb("lfh", [128, 256], BF16)
        lfl = sb("lfl", [128, 256], BF16)
        trT = sb("trT", [128, 6, 128], BF16)
        attb = sb("attb", [128, 4, 128], BF16)
        ebl = sb("ebl", [128, 2, 2], F32)
        ss4 = sb("ss4", [128, 8], F32)
        rs4 = sb("rs4", [128, 8], F32)
        bnst = sb("bnst", [128, 6], F32)
        bnag = sb("bnag", [128, 2], F32)
        ytile = [sb(f"ytile{i}", [128, 512], BF16) for i in range(2)]
        vstage = [sb(f"vstage{i}", [128, 256], BF16) for i in range(2)]
        gstage = [sb(f"gstage{i}", [128, 24], F32) for i in range(2)]
        fst_raw = [sb(f"fst_raw{i}", [128, 512], BF16) for i in range(2)]
        fst_rot = [sb(f"fst_rot{i}", [128, 512], BF16) for i in range(2)]
        r1 = [sb(f"r1_{i}", [128, 512], F32) for i in range(2)]
        r2 = [sb(f"r2_{i}", [128, 512], F32) for i in range(2)]

        V = nc.vector
        A = nc.scalar
        nfm = 0
        ck(k, 0)
        for tb in range(S // 512):
            xTc = xTb[tb % 2]
            kx = ("xTb", tb % 2)
            for tt in range(4):
                t = tb * 4 + tt
                i2 = t % 2
                P.dma("sp", xt[i2][:], x_src[t * 128:(t + 1) * 128, :], writes=[("xt", i2)])
                P.op("act", lambda i2=i2: A.copy(out=xb[i2][:], in_=xt[i2][:]), reads=[("xt", i2)], writes=[("xb", i2)])
                for c in range(8):
                    P.op("pe", lambda c=c, i2=i2: nc.tensor.transpose(out=pT[:, c, :], in_=xb[i2][:, c * 128:(c + 1) * 128], identity=k.ident_b[:]),
                         reads=[("xb", i2), "ident_b"], writes=["pT"])
                P.op("dve", lambda tt=tt, xTc=xTc: V.tensor_copy(out=xTc[:, :, tt * 128:(tt + 1) * 128], in_=pT[:]), reads=["pT"], writes=[kx])
            ck(k, 1)
            blk = slice(tb * 512, (tb + 1) * 512)

            def fm(ch, dst, xTc=xTc, kx=kx):
                for c in range(8):
                    P.op("pe", lambda c=c, ch=ch, dst=dst, xTc=xTc: nc.tensor.matmul(dst[:], lhsT=wb[:, c, NCOLA + ch * 128:NCOLA + (ch + 1) * 128], rhs=xTc[:, c, :],
                                                                                     start=(c == 0), stop=(c == 7)), reads=[("wb", c), kx], writes=[("pm", id(dst))])
            plan = [(0, 4, 0, 4), (1, 5, 1, 5), (2, 6, 2, 6), (3, 7, 3, 7), (8, 9, None, 8), (10, 11, None, 9)]
            for (cr, cs, fraw, frot) in plan:
                ck(k, 1.5 + 0.01 * nfm)
                j = nfm % 2
                nfm += 1
                pA, pB = pm[2 * j], pm[2 * j + 1]
                fm(cr, pA)
                ck(k, 1.21)
                fm(cs, pB)
                ck(k, 1.22)
                if k.dbgv == 13:
                    P.op("dve", lambda j=j, pA=pA: V.tensor_tensor(out=r1[j][:], in0=pA[:], in1=k.ropeC[:, blk], op=ALU.mult),
                         reads=[("pm", id(pA))], writes=[("r1", j)])
                if fraw is not None and k.dbgv == 14:
                    P.op("dve", lambda j=j, pA=pA: V.tensor_copy(out=fst_raw[j][:], in_=pA[:]), reads=[("pm", id(pA))], writes=[("fst_raw", j)])
                elif fraw is not None:
                    P.op("act", lambda j=j, pA=pA: A.copy(out=fst_raw[j][:], in_=pA[:]), reads=[("pm", id(pA))] + ([("r1", j)] if k.dbgv == 13 else []), writes=[("fst_raw", j)])
                    ck(k, 1.23)
                    P.dma("sp", k.ft[fraw, :, blk], fst_raw[j][:], reads=[("fst_raw", j)], writes=[("ft", fraw, tb)])
                ck(k, 1.24)
                P.op("act", lambda j=j, pA=pA: A.copy(out=r1[j][:], in_=pA[:]), reads=[("pm", id(pA))], writes=[("r1", j)])
                P.op("act", lambda j=j, pB=pB: A.copy(out=r2[j][:], in_=pB[:]), reads=[("pm", id(pB))], writes=[("r2", j)])
                P.op("dve", lambda j=j, blk=blk: V.tensor_tensor(out=r1[j][:], in0=r1[j][:], in1=k.ropeC[:, blk], op=ALU.mult), reads=[("r1", j)], writes=[("r1", j)])
                P.op("dve", lambda j=j, blk=blk: V.tensor_tensor(out=r2[j][:], in0=r2[j][:], in1=k.ropeS[:, blk], op=ALU.mult), reads=[("r2", j)], writes=[("r2", j)])
                P.op("dve", lambda j=j: V.tensor_tensor(out=fst_rot[j][:], in0=r1[j][:], in1=r2[j][:], op=ALU.add),
                     reads=[("r1", j), ("r2", j)], writes=[("fst_rot", j)])
                ck(k, 1.243)
                P.dma("sp", k.ft[frot, :, blk], fst_rot[j][:], reads=[("fst_rot", j)], writes=[("ft", frot, tb)])
            for (cr, fi) in ((12, 10), (13, 11)):
                j = nfm % 2
                nfm += 1
                pA = pm[2 * j]
                fm(cr, pA)
                P.op("act", lambda j=j, pA=pA: A.copy(out=fst_raw[j][:], in_=pA[:]), reads=[("pm", id(pA))], writes=[("fst_raw", j)])
                P.dma("sp", k.ft[fi, :, blk], fst_raw[j][:], reads=[("fst_raw", j)], writes=[("ft", fi, tb)])

            ck(k, 2)
            for tt in range(4):
                t = tb * 4 + tt
                i2 = t % 2
                rows = slice(t * 128, (t + 1) * 128)
                xs = lambda c, xTc=xTc, tt=tt: xTc[:, c, tt * 128:(tt + 1) * 128]
                colgrp = [(0, 512), (512, 512), (1024, 512), (1536, 280)]
                for gi, (c0, wdt) in enumerate(colgrp):
                    for c in range(8):
                        P.op("pe", lambda c=c, gi=gi, c0=c0, wdt=wdt, xs=xs: nc.tensor.matmul(pm[gi][:, 0:wdt], lhsT=xs(c), rhs=wb[:, c, c0:c0 + wdt],
                                                                                       start=(c == 0), stop=(c == 7)),
                             reads=[("wb", c), kx], writes=[("pm", id(pm[gi]))])
                kp = [("pm", id(pm[gi])) for gi in range(4)]
                P.op("act", lambda: A.activation(out=W["qf"][:], in_=pm[0][:, 0:256], func=AF.Silu), reads=[kp[0]], writes=["qf"])
                P.op("act", lambda: A.activation(out=W["sg"][:], in_=pm[0][:, 256:512], func=AF.Sigmoid), reads=[kp[0]], writes=["sg"])
                P.op("act", lambda: A.activation(out=W["sgate"][:], in_=pm[1][:, 256:512], func=AF.Sigmoid), reads=[kp[1]], writes=["sgate"])
                P.op("act", lambda i2=i2: A.activation(out=gstage[i2][:], in_=pm[3][:, 256:280], func=AF.Sigmoid), reads=[kp[3]], writes=[("gstage", i2)])
                P.op("act", lambda: A.copy(out=vb[:], in_=pm[1][:, 0:256]), reads=[kp[1]], writes=["vb"])
                P.op("act", lambda i2=i2: A.copy(out=vstage[i2][:], in_=pm[3][:, 0:256]), reads=[kp[3]], writes=[("vstage", i2)])
                P.op("act", lambda: A.copy(out=gx[:], in_=pm[2][:]), reads=[kp[2]], writes=["gx"])
                P.dma("sp", k.tmv[rows, :], vstage[i2][:], reads=[("vstage", i2)], writes=[("tmv", t)])
                P.dma("sp", k.gts[rows, :], gstage[i2][:], reads=[("gstage", i2)], writes=[("gts", t)])

                ck(k, 3)
                if l == 0:
                    fsrc = W["sg"]
                    kf = "sg"
                else:
                    P.op("dve", lambda: V.tensor_tensor(out=W["t1"][:], in0=W["sg"][:], in1=oml[:], op=ALU.mult), reads=["sg", "oml"], writes=["t1"])
                    P.op("dve", lambda: V.tensor_tensor(out=W["t1"][:], in0=W["t1"][:], in1=lb[:], op=ALU.add), reads=["t1", "lb"], writes=["t1"])
                    fsrc = W["t1"]
                    kf = "t1"
                P.op("act", lambda fsrc=fsrc: A.activation(out=W["logf"][:], in_=fsrc[:], func=AF.Ln), reads=[kf], writes=["logf"])
                P.op("dve", lambda fsrc=fsrc: V.tensor_scalar(out=W["kk"][:], in0=fsrc[:], scalar1=-1.0, scalar2=1.0, op0=ALU.mult, op1=ALU.add),
                     reads=[kf], writes=["kk"])
                ck(k, 3.5)
                lf = W["logf"]
                P.op("dve", lambda: V.tensor_copy(out=lfh[:], in_=lf[:]), reads=["logf"], writes=["lfh"])
                P.op("dve", lambda: V.tensor_tensor(out=W["t2"][:], in0=lf[:], in1=lfh[:], op=ALU.subtract), reads=["logf", "lfh"], writes=["t2"])
                P.op("dve", lambda: V.tensor_copy(out=lfl[:], in_=W["t2"][:]), reads=["t2"], writes=["lfl"])
                for (dst, mat, kn) in ((pa[:, 0:256], k.mmid_b, "pa"), (pa[:, 256:512], k.mcum_b, "pa"), (pb[:, 0:256], k.mrem_b, "pb")):
                    P.op("pe", lambda dst=dst, mat=mat: nc.tensor.matmul(dst, lhsT=mat[:], rhs=lfh[:], start=True, stop=False), reads=["lfh"], writes=[kn])
                    P.op("pe", lambda dst=dst, mat=mat: nc.tensor.matmul(dst, lhsT=mat[:], rhs=lfl[:], start=False, stop=True), reads=["lfl"], writes=[kn])
                for c2 in range(2):
                    for hh in range(2):
                        for pi, part in enumerate((lfh, lfl)):
                            P.op("pe", lambda c2=c2, hh=hh, part=part, pi=pi: nc.tensor.matmul((pb[:, 256 + hh:257 + hh] if c2 == 0 else pm[2][:, hh:hh + 1]),
                                                                                             lhsT=part[c2 * 64:(c2 + 1) * 64, hh * 128:(hh + 1) * 128],
                                                                                             rhs=k.ones_b[c2 * 64:(c2 + 1) * 64, 0:1], start=(pi == 0), stop=(pi == 1)),
                                 reads=["lfh", "lfl", "ones_b"], writes=(["pb"] if c2 == 0 else [kp[2]]))
                P.op("act", lambda: A.activation(out=W["ebm"][:], in_=pa[:, 0:256], func=AF.Exp), reads=["pa"], writes=["ebm"])
                P.op("act", lambda: A.activation(out=W["enbm"][:], in_=pa[:, 0:256], func=AF.Exp, scale=-1.0), reads=["pa"], writes=["enbm"])
                P.op("act", lambda: A.activation(out=W["eb"][:], in_=pa[:, 256:512], func=AF.Exp), reads=["pa"], writes=["eb"])
                P.op("act", lambda: A.activation(out=W["erem"][:], in_=pb[:, 0:256], func=AF.Exp), reads=["pb"], writes=["erem"])
                P.op("act", lambda: A.activation(out=ebl[:, 0, :], in_=pb[:, 256:258], func=AF.Exp), reads=["pb"], writes=["ebl"])
                P.op("act", lambda: A.activation(out=ebl[:, 1, :], in_=pm[2][:, 0:2], func=AF.Exp), reads=[kp[2]], writes=["ebl"])
                P.op("dve", lambda: V.tensor_tensor(out=qe[:], in0=W["qf"][:], in1=W["ebm"][:], op=ALU.mult), reads=["qf", "ebm"], writes=["qe"])
                P.op("dve", lambda: V.tensor_tensor(out=ke[:], in0=W["kk"][:], in1=W["enbm"][:], op=ALU.mult), reads=["kk", "enbm"], writes=["ke"])
                P.op("dve", lambda: V.tensor_tensor(out=qb[:], in0=W["qf"][:], in1=W["eb"][:], op=ALU.mult), reads=["qf", "eb"], writes=["qb"])
                P.op("dve", lambda: V.tensor_tensor(out=kd[:], in0=W["kk"][:], in1=W["erem"][:], op=ALU.mult), reads=["kk", "erem"], writes=["kd"])
                for i, (src, kn) in enumerate(((qe, "qe"), (ke, "ke"), (qb, "qb"))):
                    for hh in range(2):
                        P.op("pe", lambda i=i, hh=hh, src=src: nc.tensor.transpose(out=pcb[:, (i * 2 + hh) * 128:(i * 2 + hh + 1) * 128],
                                                                                   in_=src[:, hh * 128:(hh + 1) * 128], identity=k.ident_b[:]),
                             reads=[kn, "ident_b"], writes=["pc"])
                P.op("dve", lambda: V.tensor_copy(out=trT[:].rearrange("p a b -> p (a b)"), in_=pcb[:, 0:768]), reads=["pc"], writes=["trT"])

                ck(k, 4)

                def hT(i, h):
                    return trT[(h % 2) * 64:(h % 2) * 64 + 64, i * 2 + h // 2, :]
                for h in range(4):
                    ck(k, 4.01 + 0.01 * h)
                    dst = (pa if h % 2 == 0 else pm[0])[:, (h // 2) * 128:(h // 2 + 1) * 128]
                    P.op("pe", lambda h=h, dst=dst: nc.tensor.matmul(dst, lhsT=hT(1, h), rhs=hT(0, h), start=True, stop=True),
                         reads=["trT"], writes=["pa" if h % 2 == 0 else kp[0]])
                ck(k, 4.1)
                P.op("act", lambda: A.copy(out=gt[:, 0:256], in_=pa[:, 0:256]), reads=["pa"], writes=["gt"])
                P.op("act", lambda: A.copy(out=gt[:, 256:512], in_=pm[0][:, 0:256]), reads=[kp[0]], writes=["gt"])
                ck(k, 4.2)
                for h in range(4):
                    gb = (h % 2) * 2 + h // 2
                    P.op("dve", lambda h=h, gb=gb: V.tensor_tensor(out=attb[:, h, :], in0=gt[:, gb * 128:(gb + 1) * 128], in1=k.mcum[:], op=ALU.mult),
                         reads=["gt", "mcum"], writes=["attb"])
                ck(k, 4.5)
                def obank(h):
                    return ((pc, "pc"), (pm[1], kp[1]), (pm[3], kp[3]), (pm[0], kp[0]))[h]
                for h in range(4):
                    ob, okey = obank(h)
                    P.op("pe", lambda h=h, ob=ob: nc.tensor.matmul(ob[:, 0:64], lhsT=attb[:, h, :], rhs=vb[:, h * 64:(h + 1) * 64],
                                                                   start=True, stop=False), reads=["attb", "vb", "trT"], writes=[okey])
                for c2 in range(2):
                    rs = slice(c2 * 64, (c2 + 1) * 64)
                    for h in range(4):
                        hp = slice((h % 2) * 64, (h % 2) * 64 + 64)
                        ob, okey = obank(h)
                        P.op("pe", lambda h=h, rs=rs, hp=hp, ob=ob: nc.tensor.matmul(ob[rs, 0:64], lhsT=hT(2, h)[:, rs], rhs=st_b[hp, h // 2, :],
                                                                                     start=False, stop=True), reads=["trT", "st_b"], writes=[okey])
                    for hh in range(2):
                        P.op("pe", lambda hh=hh, rs=rs: nc.tensor.matmul(pa[:, hh * 128:(hh + 1) * 128], lhsT=kd[rs, hh * 128:(hh + 1) * 128],
                                                                         rhs=vb[rs, hh * 128:(hh + 1) * 128], start=True, stop=True),
                             reads=["kd", "vb", "attb"], writes=["pa"])
                    P.op("act", lambda: A.copy(out=W["t2"][:], in_=pa[:, 0:256]), reads=["pa"], writes=["t2"])
                    for h in range(4):
                        hp = slice((h % 2) * 64, (h % 2) * 64 + 64)
                        P.op("dve", lambda h=h, c2=c2, hp=hp: V.scalar_tensor_tensor(out=st_f[hp, h // 2, :], in0=st_f[hp, h // 2, :],
                                                                                    scalar=ebl[hp, c2, h // 2:h // 2 + 1],
                                                                                    in1=W["t2"][hp, h * 64:(h + 1) * 64], op0=ALU.mult, op1=ALU.add),
                             reads=["st_f", "ebl", "t2"], writes=["st_f"])
                    P.op("dve", lambda: V.tensor_copy(out=st_b[:], in_=st_f[:]), reads=["st_f"], writes=["st_b"])
                ck(k, 5)
                yt = ytile[i2]
                ky = ("ytile", i2)
                tk = (["t1"] if l > 0 else [])
                for h in range(4):
                    ob, okey = obank(h)
                    P.op("act", lambda h=h, ob=ob: A.copy(out=W["t1"][:, h * 64:(h + 1) * 64], in_=ob[:, 0:64]), reads=[okey] + tk, writes=["osb"] + tk)
                P.op("act", lambda: A.activation(out=W["osq"][:], in_=W["t1"][:], func=AF.Square), reads=["osb"], writes=["osq"])
                P.op("dve", lambda: V.tensor_reduce(out=ss4[:, 0:4], in_=W["osq"][:].rearrange("p (h d) -> p h d", h=4), axis=AX.X, op=ALU.add),
                     reads=["osq"], writes=["ss4"])
                P.op("dve", lambda: V.tensor_scalar(out=rs4[:, 0:4], in0=ss4[:, 0:4], scalar1=1.0 / 64, scalar2=1e-6, op0=ALU.mult, op1=ALU.add),
                     reads=["ss4"], writes=["rs4"])
                P.op("act", lambda: A.activation(out=rs4[:, 0:4], in_=rs4[:, 0:4], func=AF.Ln), reads=["rs4"], writes=["rs4"])
                P.op("act", lambda: A.activation(out=rs4[:, 0:4], in_=rs4[:, 0:4], func=AF.Exp, scale=-0.5), reads=["rs4"], writes=["rs4"])
                P.op("dve", lambda: V.tensor_tensor(out=W["nwg"][:], in0=W["sgate"][:], in1=hg_nw[:], op=ALU.mult), reads=["sgate", "hg_nw"], writes=["nwg"])
                for h in range(4):
                    P.op("dve", lambda h=h, yt=yt: V.scalar_tensor_tensor(out=yt[:, h * 64:(h + 1) * 64], in0=W["t1"][:, h * 64:(h + 1) * 64],
                                                                         scalar=rs4[:, h:h + 1], in1=W["nwg"][:, h * 64:(h + 1) * 64],
                                                                         op0=ALU.mult, op1=ALU.mult), reads=["osb", "rs4", "nwg"], writes=[ky])

                ck(k, 6)
                P.op("dve", lambda: V.tensor_tensor(out=gt[:], in0=gx[:], in1=gx[:], op=ALU.mult), reads=["gx"], writes=["gt"])
                P.op("dve", lambda: V.tensor_scalar(out=gt[:], in0=gt[:], scalar1=0.044715, scalar2=1.0, op0=ALU.mult, op1=ALU.add), reads=["gt"], writes=["gt"])
                P.op("dve", lambda: V.tensor_tensor(out=gt[:], in0=gt[:], in1=gx[:], op=ALU.mult), reads=["gt", "gx"], writes=["gt"])
                P.op("act", lambda: A.activation(out=gt[:], in_=gt[:], func=AF.Sigmoid, scale=1.5957691216057308), reads=["gt"], writes=["gt"])
                P.op("dve", lambda: V.tensor_tensor(out=gg[:], in0=gt[:], in1=gx[:], op=ALU.mult), reads=["gt", "gx"], writes=["gg"])
                P.op("dve", lambda: V.bn_stats(out=bnst[:], in_=gg[:, 256:512]), reads=["gg"], writes=["bnst"])
                P.op("dve", lambda: V.bn_aggr(out=bnag[:], in_=bnst[:]), reads=["bnst"], writes=["bnag"])
                P.op("dve", lambda: V.tensor_scalar(out=rs4[:, 4:5], in0=bnag[:, 1:2], scalar1=1e-5, scalar2=None, op0=ALU.add),
                     reads=["bnag"], writes=["rs4b"])
                P.op("act", lambda: A.activation(out=rs4[:, 4:5], in_=rs4[:, 4:5], func=AF.Ln), reads=["rs4b"], writes=["rs4b"])
                P.op("act", lambda: A.activation(out=rs4[:, 4:5], in_=rs4[:, 4:5], func=AF.Exp, scale=-0.5), reads=["rs4b"], writes=["rs4b"])
                P.op("dve", lambda: V.tensor_scalar(out=W["ginn"][:], in0=gg[:, 256:512], scalar1=bnag[:, 0:1], scalar2=rs4[:, 4:5],
                                                    op0=ALU.subtract, op1=ALU.mult), reads=["gg", "bnag", "rs4b"], writes=["ginn"])
                P.op("dve", lambda: V.tensor_tensor(out=W["ginn"][:], in0=W["ginn"][:], in1=gm_lnw[:], op=ALU.mult), reads=["ginn", "gm_lnw"], writes=["ginn"])
                P.op("dve", lambda: V.tensor_tensor(out=vln[:], in0=W["ginn"][:], in1=gm_lnb[:], op=ALU.add), reads=["ginn", "gm_lnb"], writes=["vln"])
                for g in range(4):
                    P.op("pe", lambda g=g: nc.tensor.matmul(pb[:, 256 + g * 64:256 + (g + 1) * 64], lhsT=wsT[:, g, :], rhs=vln[:, g * 64:(g + 1) * 64],
                                                            start=True, stop=True), reads=["wsT", "vln"], writes=["pb"])
                P.op("act", lambda: A.copy(out=W["ginn"][:], in_=pb[:, 256:512]), reads=["pb", "ginn"], writes=["ginn"])
                for g in range(4):
                    P.op("dve", lambda g=g: V.scalar_tensor_tensor(out=W["gsv"][:, g * 64:(g + 1) * 64], in0=W["ginn"][:, g * 64:(g + 1) * 64],
                                                                  scalar=bsT[:, g:g + 1], in1=gg[:, g * 64:(g + 1) * 64], op0=ALU.add, op1=ALU.mult),
                         reads=["ginn", "bsT", "gg"], writes=["gsv"])
                P.op("act", lambda: A.activation(out=W["gsq"][:], in_=W["gsv"][:], func=AF.Square), reads=["gsv"], writes=["gsq"])
                P.op("dve", lambda: V.tensor_reduce(out=ss4[:, 4:8], in_=W["gsq"][:].rearrange("p (h d) -> p h d", h=4), axis=AX.X, op=ALU.add),
                     reads=["gsq"], writes=["ss4b"])
                P.op("dve", lambda: V.tensor_scalar(out=ss4[:, 4:8], in0=ss4[:, 4:8], scalar1=1.0 / 64, scalar2=1e-6, op0=ALU.mult, op1=ALU.add),
                     reads=["ss4b"], writes=["ss4b"])
                P.op("act", lambda: A.activation(out=ss4[:, 4:8], in_=ss4[:, 4:8], func=AF.Ln), reads=["ss4b"], writes=["ss4b"])
                P.op("act", lambda: A.activation(out=ss4[:, 4:8], in_=ss4[:, 4:8], func=AF.Exp, scale=-0.5), reads=["ss4b"], writes=["ss4b"])
                for g in range(4):
                    P.op("dve", lambda g=g, yt=yt: V.scalar_tensor_tensor(out=yt[:, 256 + g * 64:256 + (g + 1) * 64], in0=W["gsv"][:, g * 64:(g + 1) * 64],
                                                                         scalar=ss4[:, 4 + g:5 + g], in1=gm_nw[:, g * 64:(g + 1) * 64],
                                                                         op0=ALU.mult, op1=ALU.mult), reads=["gsv", "ss4b", "gm_nw"], writes=[ky])
                P.dma("sp", k.y[rows, 0:512], yt[:], reads=[ky], writes=[("y", t)])
                if t == 0:
                    dump(k, "gg", gg[:], "gg", 512)
                    dump(k, "bnag", bnag[:], "bnag", 2)
                    dump(k, "rs4", rs4[:], "rs4b", 8)
                    dump(k, "vln", vln[:], "vln", 256)
                    dump(k, "gsv", W["gsv"][:], "gsv", 256)
                    dump(k, "ss4", ss4[:], "ss4b", 8)
                    dump(k, "wsT", wsT[:].rearrange("p a b -> p (a b)"), "wsT", 512)
                    dump(k, "logf", W["logf"][:], "logf", 256)
                    dump(k, "kk", W["kk"][:], "kk", 256)
                    dump(k, "qf", W["qf"][:], "qf", 256)
                    dump(k, "ebm", W["ebm"][:], "ebm", 256)
                    dump(k, "eb", W["eb"][:], "eb", 256)
                    dump(k, "erem", W["erem"][:], "erem", 256)
                    dump(k, "ebl", ebl[:].rearrange("p a b -> p (a b)"), "ebl", 4)
                    dump(k, "osb", W["t1"][:], "osb", 256)
                    dump(k, "attb", attb[:].rearrange("p a b -> p (a b)"), "attb", 512)
                    dump(k, "st_f", st_f[:].rearrange("p a b -> p (a b)"), "st_f", 128)
                    dump(k, "rs4h", rs4[:], "rs4", 8)
                    dump(k, "nwg", W["nwg"][:], "nwg", 256)
                ck(k, 7)


NSA_BIG = 30000.0


def phase2(k, l):
    nc, P = k.nc, k.P
    V, A, G = nc.vector, nc.scalar, nc.gpsimd
    with ExitStack() as es:
        sb = lambda name, shape, dt: es.enter_context(nc.sbuf_tensor(f"p2l{l}_{name}", shape, dt))
        ps = lambda name, shape, dt: es.enter_context(nc.psum_tensor(f"p2l{l}_{name}", shape, dt))
        edup = sb("edup", [128, 32, 128], BF16)
        win01 = sb("win01", [128, 8, 512], BF16)
        cmp01 = sb("cmp01", [128, 5, 512], BF16)
        impkeep = sb("impkeep", [128, NT, 64], F32)
        impadd = sb("impadd", [128, NT, 64], F32)
        ovext = sb("ovext", [128, 2, 65], BF16)
        wpad = sb("wpad", [128, NT], F32)
        gts = sb("gts", [128, NT, 24], F32)
        nw = sb("nw", [128, 512], F32)
        w1 = [sb(f"w1_{i}", [64, 32, 128], BF16) for i in range(2)]
        w2kd = sb("w2kd", [128, 128], BF16)
        w2v = sb("w2v", [128, 64], BF16)
        peT = sb("peT", [64, 2, 32], BF16)
        craw = sb("craw", [64, S], BF16)
        cb = sb("cb", [128, 1], F32)
        hx = sb("hx", [128, 256], F32)
        ht = sb("ht", [128, 256], F32)
        hb = sb("hb", [128, 256], BF16)
        kcd = sb("kcd", [128, 256], BF16)
        vc = sb("vc", [128, 2, 65], BF16)
        qr = [sb(f"qr{i}", [128, S], BF16) for i in range(2)]
        qo = [sb(f"qo{i}", [128, S], BF16) for i in range(2)]
        ksd = sb("ksd", [128, S], BF16)
        kwd = sb("kwd", [128, S], BF16)
        vsg = sb("vsg", [128, NT, 65], BF16)
        vwg = sb("vwg", [128, NT, 65], BF16)
        ec = [[[sb(f"ec{ci}{par}{ct}", [128, 512], BF16) for ct in range(2)] for par in range(2)] for ci in range(2)]
        pt = [[sb(f"pt{par}{i}", [128, 512], BF16) for i in range(2)] for par in range(2)]
        osb = [sb(f"osb{par}", [65, 512], F32) for par in range(2)]
        ob = sb("ob", [128, 3, 4, 4, 65], F32)
        impsb = sb("impsb", [128, 4, 4, 64], F32)
        impw = sb("impw", [128, 4, 64], F32)
        imp = sb("imp", [128, 64], F32)
        imp3 = sb("imp3", [128, 64], F32)
        m8a = sb("m8a", [128, 8], F32)
        m8b = sb("m8b", [128, 8], F32)
        rdn = sb("rdn", [128, 4], F32)
        mdup = sb("mdup", [128, 4, 128], BF16)
        MT = sb("MT", [128, 512], BF16)
        den3 = sb("den3", [128, 3, 4], F32)
        coef = sb("coef", [128, 3, 4], F32)
        acc = sb("acc", [128, 4, 64], F32)
        tm2 = sb("tm2", [128, 4, 64], F32)
        ss = sb("ss", [128, 4], F32)
        yst = [sb(f"yst{i}", [128, 256], BF16) for i in range(2)]
        Sb = [[ps(f"S{par}{i}", [128, 512], F32) for i in range(2)] for par in range(2)]
        Ob = [ps(f"O{par}", [128, 512], F32) for par in range(2)]
        X = ps("X", [128, 512], F32)
        Y = ps("Y", [128, 512], F32)
        Xb = X[:].bitcast(BF16)

        flat = lambda t: t[:].rearrange("p a b -> p (a b)")

        def cast_load(dst_flat, src_flat, n, key):
            for o in range(0, n, 2048):
                w = min(2048, n - o)
                P.dma("pool", dst_flat[:, o:o + w], src_flat[:, o:o + w], writes=[key])
        cast_load(flat(edup), k.cst["edup"].rearrange("p a b -> p (a b)"), 32 * 128, "edup")
        cast_load(flat(win01), k.cst["win01"].rearrange("p a b -> p (a b)"), 8 * 512, "win01")
        cast_load(flat(cmp01), k.cst["cmp01"].rearrange("p a b -> p (a b)"), 5 * 512, "cmp01")
        cast_load(flat(ovext), k.cst["ovext"].rearrange("p a b -> p (a b)"), 130, "ovext")
        P.dma("sp", impkeep[:], k.cst["impkeep"], writes=["impkeep"])
        P.dma("sp", impadd[:], k.cst["impadd"], writes=["impadd"])
        P.dma("sp", wpad[:], k.cst["wpad"], writes=["wpad"])
        P.dma("sp", gts[:], k.gts.rearrange("(t p) c -> p t c", p=128), writes=["gts"])
        bcast_row(k, P, "sp", nw[:], k.nsa_nw[l:l + 1, :], "nw")
        for kv in range(2):
            wv = k.nsa_w1[l, kv].rearrange("(j d) m -> d j m", d=64)
            for hh in range(2):
                P.dma("pool", w1[kv][:, hh * 16:(hh + 1) * 16, :], wv[:, hh * 16:(hh + 1) * 16, :], writes=[("w1", kv)])
        P.dma("pool", w2kd[:, 0:64], k.nsa_w2[l, 0], writes=["w2kd"])
        P.dma("pool", w2kd[:, 64:128], k.nsa_w2[l, 0], writes=["w2kd"])
        P.dma("pool", w2v[:], k.nsa_w2[l, 1], writes=["w2v"])
        P.dma("pool", peT[:], k.nsa_pe[l].rearrange("k j d -> d k j"), writes=["peT"], allow_slow_non_contiguous=True)

        def evac_O(par, br, hl):
            P.op("act", lambda par=par: A.copy(out=osb[par][:], in_=Ob[par][0:65, :]), reads=[("O", par)], writes=[("osb", par)])
            for qt in range(4):
                P.op("pe", lambda par=par, qt=qt: nc.tensor.transpose(out=X[:, qt * 65:(qt + 1) * 65], in_=osb[par][:, qt * 128:(qt + 1) * 128], identity=k.ident_f[0:65, 0:65]),
                     reads=[("osb", par), "ident"], writes=["X"])
            P.op("act", lambda br=br, hl=hl: A.copy(out=ob[:, br, :, hl, :], in_=X[:, 0:260].rearrange("p (a b) -> p a b", a=4)), reads=["X"], writes=[("ob", br)])

        for g in range(2):
            for kv in range(2):
                P.dma("sp", craw[:], k.ft[10 + kv, g * 64:(g + 1) * 64, :], writes=["craw"])
                for j in range(32):
                    P.op("pe", lambda j=j, kv=kv: nc.tensor.matmul(X[:, 300:301], lhsT=w1[kv][:, j, :], rhs=peT[:, kv, j:j + 1], start=(j == 0), stop=(j == 31)),
                         reads=[("w1", kv), "peT"], writes=["X"])
                for j in range(32):
                    P.op("pe", lambda j=j, kv=kv: nc.tensor.matmul(X[:, 0:255], lhsT=w1[kv][:, j, :], rhs=craw[:, j:j + 16 * 254 + 1:16], start=(j == 0), stop=(j == 31)),
                         reads=[("w1", kv), "craw"], writes=["X"])
                P.op("act", lambda: A.copy(out=cb[:], in_=X[:, 300:301]), reads=["X"], writes=["cb"])
                P.op("dve", lambda: V.memset(hx[:], 0.0), writes=["hx"])
                P.op("act", lambda: A.activation(out=hx[:, 0:255], in_=X[:, 0:255], func=AF.Identity, bias=cb[:, 0:1], scale=1.0), reads=["X", "cb", "hx"], writes=["hx"])
                P.op("dve", lambda: V.tensor_tensor(out=ht[:], in0=hx[:], in1=hx[:], op=ALU.mult), reads=["hx"], writes=["ht"])
                P.op("dve", lambda: V.tensor_scalar(out=ht[:], in0=ht[:], scalar1=0.044715, scalar2=1.0, op0=ALU.mult, op1=ALU.add), reads=["ht"], writes=["ht"])
                P.op("dve", lambda: V.tensor_tensor(out=ht[:], in0=ht[:], in1=hx[:], op=ALU.mult), reads=["ht", "hx"], writes=["ht"])
                P.op("act", lambda: A.activation(out=ht[:], in_=ht[:], func=AF.Sigmoid, scale=1.5957691216057308), reads=["ht"], writes=["ht"])
                P.op("dve", lambda: V.tensor_tensor(out=hb[:], in0=ht[:], in1=hx[:], op=ALU.mult), reads=["ht", "hx"], writes=["hb"])
                if kv == 0:
                    P.op("pe", lambda: nc.tensor.matmul(X[:, 0:256], lhsT=w2kd[:], rhs=hb[:], start=True, stop=True), reads=["w2kd", "hb"], writes=["X"])
                    P.op("act", lambda: A.copy(out=kcd[:], in_=X[:, 0:256]), reads=["X"], writes=["kcd"])
                else:
                    for ct in range(2):
                        P.op("pe", lambda ct=ct: nc.tensor.matmul(X[:, ct * 64:(ct + 1) * 64], lhsT=hb[:, ct * 128:(ct + 1) * 128], rhs=w2v[:], start=True, stop=True),
                             reads=["w2v", "hb"], writes=["X"])
                    P.op("dve", lambda: V.memset(vc[:], 1.0), writes=["vc"])
                    P.op("act", lambda: A.copy(out=vc[:, :, 0:64], in_=X[:, 0:128].rearrange("p (a b) -> p a b", a=2)), reads=["X", "vc"], writes=["vc"])
            for ci in range(2):
                P.dma("sp", qr[ci][:], k.ft[2 * g + ci], writes=[("qr", ci)])
                P.dma("act", qo[ci][:], k.ft[4 + 2 * g + ci], writes=[("qo", ci)])
            for hh in range(2):
                P.dma("sp", ksd[hh * 64:(hh + 1) * 64, :], k.ft[8, g * 64:(g + 1) * 64, :], writes=["ksd"])
                P.dma("act", kwd[hh * 64:(hh + 1) * 64, :], k.ft[9, g * 64:(g + 1) * 64, :], writes=["kwd"])
            P.op("dve", lambda: V.memset(vsg[:], 1.0), writes=["vsg"])
            P.op("dve", lambda: V.memset(vwg[:], 1.0), writes=["vwg"])
            P.dma("sp", vsg[:, :, 0:64], k.tmv[:, g * 64:(g + 1) * 64].rearrange("(t p) c -> p t c", p=128), reads=["vsg"], writes=["vsg"])
            P.dma("act", vwg[:, :, 0:64], k.tmv[:, 128 + g * 64:128 + (g + 1) * 64].rearrange("(t p) c -> p t c", p=128), reads=["vwg"], writes=["vwg"])

            for Q in range(S // 512):
                qs = slice(Q * 512, (Q + 1) * 512)
                cts = []
                for ct in range(2):
                    u = 512 * Q - 2048 * ct
                    if u < -480:
                        continue
                    cts.append((ct, (None if u > 2048 else (0, 512, 1024, 1536, 2048).index(u))))
                for ci in range(2):
                    for n_, (ct, mi) in enumerate(cts):
                        for par in range(2):
                            hp = slice(par * 64, (par + 1) * 64)
                            P.op("pe", lambda par=par, hp=hp, ct=ct, ci=ci, qs=qs: nc.tensor.matmul(Sb[par][0][:], lhsT=kcd[hp, ct * 128:(ct + 1) * 128], rhs=qr[ci][hp, qs], start=True, stop=True),
                                 reads=["kcd", ("qr", ci)], writes=[("S", par, 0)])
                        for par in range(2):
                            e_ = ec[ci][par][ct]
                            ke = ("ec", ci, par, ct)
                            P.op("act", lambda par=par, e_=e_: A.activation(out=e_[:], in_=Sb[par][0][:], func=AF.Exp, scale=0.125), reads=[("S", par, 0)], writes=[ke])
                            if mi is not None:
                                P.op("pool", lambda e_=e_, mi=mi: G.tensor_tensor(out=e_[:], in0=e_[:], in1=cmp01[:, mi, :], op=ALU.mult), reads=[ke, "cmp01"], writes=[ke])
                            P.op("pe", lambda par=par, ct=ct, e_=e_, n_=n_: nc.tensor.matmul(Ob[par][0:65, :], lhsT=vc[:, ct, :], rhs=e_[:], start=(n_ == 0), stop=(n_ == len(cts) - 1)),
                                 reads=["vc", ke], writes=[("O", par)])
                    for par in range(2):
                        evac_O(par, 0, 2 * ci + par)
                for half in range(2):
                    for q2 in range(2):
                        qt = half * 2 + q2
                        for hl in range(4):
                            ci, par = hl // 2, hl % 2
                            for n_, (ct, mi) in enumerate(cts):
                                e_ = ec[ci][par][ct]
                                P.op("pe", lambda e_=e_, ct=ct, qt=qt, q2=q2, hl=hl, n_=n_: nc.tensor.matmul(Y[:, q2 * 256 + hl * 64:q2 * 256 + (hl + 1) * 64],
                                                                                                           lhsT=e_[:, qt * 128:(qt + 1) * 128], rhs=ovext[:, ct, 0:64],
                                                                                                           start=(n_ == 0), stop=(n_ == len(cts) - 1)),
                                     reads=[("ec", ci, par, ct), "ovext"], writes=["Y"])
                    P.op("act", lambda half=half: A.copy(out=impsb[:, half * 2:(half + 1) * 2, :, :].rearrange("p a b c -> p (a b c)"), in_=Y[:]), reads=["Y"], writes=["impsb"])
                for qt in range(4):
                    t = Q * 4 + qt
                    P.op("dve", lambda qt=qt: V.tensor_scalar(out=rdn[:], in0=ob[:, 0, qt, :, 64], scalar1=1e-30, scalar2=None, op0=ALU.max), reads=[("ob", 0)], writes=["rdn"])
                    P.op("dve", lambda: V.reciprocal(out=rdn[:], in_=rdn[:]), reads=["rdn"], writes=["rdn"])
                    P.op("dve", lambda qt=qt: V.tensor_tensor(out=impw[:], in0=impsb[:, qt, :, :], in1=rdn[:].unsqueeze(2).to_broadcast([128, 4, 64]), op=ALU.mult),
                         reads=["impsb", "rdn"], writes=["impw"])
                    P.op("dve", lambda: V.tensor_reduce(out=imp[:], in_=impw[:].rearrange("p h j -> p j h"), axis=AX.X, op=ALU.add), reads=["impw"], writes=["imp"])
                    P.op("dve", lambda t=t: V.tensor_tensor(out=imp[:], in0=imp[:], in1=impkeep[:, t, :], op=ALU.mult), reads=["imp", "impkeep"], writes=["imp"])
                    P.op("dve", lambda t=t: V.tensor_tensor(out=imp[:], in0=imp[:], in1=impadd[:, t, :], op=ALU.add), reads=["imp", "impadd"], writes=["imp"])
                    P.op("dve", lambda: V.max(out=m8a[:], in_=imp[:]), reads=["imp"], writes=["m8a"])
                    P.op("dve", lambda: V.match_replace(out=imp3[:], in_to_replace=m8a[:], in_values=imp[:], imm_value=-3.0e9), reads=["imp", "m8a"], writes=["imp3"])
                    P.op("dve", lambda: V.max(out=m8b[:], in_=imp3[:]), reads=["imp3"], writes=["m8b"])
                    P.op("dve", lambda: V.tensor_scalar(out=imp3[:], in0=imp[:], scalar1=m8b[:, 7:8], scalar2=None, op0=ALU.is_ge), reads=["imp", "m8b", "imp3"], writes=["imp3"])
                    for dd in range(2):
                        P.op("dve", lambda qt=qt, dd=dd: V.tensor_scalar(out=mdup[:, qt, dd * 64:(dd + 1) * 64], in0=imp3[:], scalar1=-1.0, scalar2=NSA_BIG, op0=ALU.add, op1=ALU.mult),
                             reads=["imp3"], writes=["mdup"])
                    P.op("pe", lambda qt=qt: nc.tensor.transpose(out=Xb[:, qt * 128:(qt + 1) * 128], in_=mdup[:, qt, :], identity=k.ident_b[:]), reads=["mdup", "ident_b"], writes=["X"])
                P.op("dve", lambda: V.tensor_copy(out=MT[:], in_=Xb[:, 0:512]), reads=["X"], writes=["MT"])
                for br, (kd_, kkey, vt, vkey) in ((1, (ksd, "ksd", vsg, "vsg")), (2, (kwd, "kwd", vwg, "vwg"))):
                    kt0 = 0 if br == 1 else max(0, 4 * Q - 4)
                    kts = list(range(kt0, 4 * Q + 4))
                    for ci in range(2):
                        def scores(n_, ci=ci, br=br, kd_=kd_, kkey=kkey, kts=kts, qs=qs):
                            kt = kts[n_]
                            bi = n_ % 2
                            for par in range(2):
                                hp = slice(par * 64, (par + 1) * 64)
                                P.op("pe", lambda par=par, hp=hp, kt=kt, ci=ci, qs=qs, bi=bi, kd_=kd_, br=br: nc.tensor.matmul(Sb[par][bi][:], lhsT=kd_[hp, kt * 128:(kt + 1) * 128], rhs=qo[ci][hp, qs],
                                                                                                                          start=True, stop=(br == 2)),
                                     reads=[kkey, ("qo", ci)], writes=[("S", par, bi)])
                            if br == 1:
                                for par in range(2):
                                    hp = slice(par * 64, (par + 1) * 64)
                                    P.op("pe", lambda par=par, hp=hp, kt=kt, bi=bi: nc.tensor.matmul(Sb[par][bi][:], lhsT=edup[hp, kt, :], rhs=MT[hp, :], start=False, stop=True),
                                         reads=["edup", "MT"], writes=[("S", par, bi)])
                        scores(0)
                        for n_, kt in enumerate(kts):
                            bi = n_ % 2
                            if n_ + 1 < len(kts):
                                scores(n_ + 1)
                            for par in range(2):
                                p_ = pt[par][bi]
                                kp_ = ("pt", par, bi)
                                P.op("act", lambda par=par, bi=bi, p_=p_: A.activation(out=p_[:], in_=Sb[par][bi][:], func=AF.Exp, scale=0.125), reads=[("S", par, bi)], writes=[kp_])
                                dl = kt - 4 * Q
                                if br == 2 or dl >= 0:
                                    P.op("pool", lambda p_=p_, dl=dl: G.tensor_tensor(out=p_[:], in0=p_[:], in1=win01[:, dl + 4, :], op=ALU.mult), reads=[kp_, "win01"], writes=[kp_])
                                P.op("pe", lambda par=par, kt=kt, p_=p_, n_=n_, vt=vt, nk=len(kts): nc.tensor.matmul(Ob[par][0:65, :], lhsT=vt[:, kt, :], rhs=p_[:], start=(n_ == 0), stop=(n_ == nk - 1)),
                                     reads=[vkey, kp_], writes=[("O", par)])
                        for par in range(2):
                            evac_O(par, br, 2 * ci + par)
                for qt in range(4):
                    t = Q * 4 + qt
                    i2 = qt % 2
                    P.op("dve", lambda qt=qt: V.tensor_scalar(out=den3[:], in0=ob[:, :, qt, :, 64], scalar1=1e-30, scalar2=None, op0=ALU.max), reads=[("ob", 0), ("ob", 1), ("ob", 2)], writes=["den3"])
                    if Q == 0:
                        P.op("dve", lambda t=t: V.tensor_scalar(out=den3[:, 2, :], in0=den3[:, 2, :], scalar1=wpad[:, t:t + 1], scalar2=None, op0=ALU.add),
                             reads=["den3", "wpad"], writes=["den3"])
                    P.op("dve", lambda: V.reciprocal(out=den3[:], in_=den3[:]), reads=["den3"], writes=["den3"])
                    P.op("dve", lambda t=t, g=g: V.tensor_tensor(out=coef[:], in0=den3[:], in1=gts[:, t, g * 12:(g + 1) * 12].rearrange("p (h b) -> p b h", b=3), op=ALU.mult),
                         reads=["den3", "gts"], writes=["coef"])
                    for br in range(3):
                        dst = acc if br == 0 else tm2
                        P.op("dve", lambda br=br, qt=qt, dst=dst: V.tensor_tensor(out=dst[:], in0=ob[:, br, qt, :, 0:64], in1=coef[:, br, :].unsqueeze(2).to_broadcast([128, 4, 64]), op=ALU.mult),
                             reads=[("ob", br), "coef"], writes=["acc" if br == 0 else "tm2"])
                        if br > 0:
                            P.op("dve", lambda: V.tensor_tensor(out=acc[:], in0=acc[:], in1=tm2[:], op=ALU.add), reads=["acc", "tm2"], writes=["acc"])
                    P.op("act", lambda: A.activation(out=tm2[:], in_=acc[:], func=AF.Square), reads=["acc", "tm2"], writes=["tm2"])
                    P.op("dve", lambda: V.tensor_reduce(out=ss[:], in_=tm2[:], axis=AX.X, op=ALU.add), reads=["tm2"], writes=["ss"])
                    P.op("dve", lambda: V.tensor_scalar(out=ss[:], in0=ss[:], scalar1=1.0 / 64, scalar2=1e-6, op0=ALU.mult, op1=ALU.add), reads=["ss"], writes=["ss"])
                    P.op("act", lambda: A.activation(out=ss[:], in_=ss[:], func=AF.Sqrt), reads=["ss"], writes=["ss"])
                    P.op("dve", lambda: V.reciprocal(out=ss[:], in_=ss[:]), reads=["ss"], writes=["ss"])
                    P.op("dve", lambda: V.tensor_tensor(out=acc[:], in0=acc[:], in1=ss[:].unsqueeze(2).to_broadcast([128, 4, 64]), op=ALU.mult), reads=["acc", "ss"], writes=["acc"])
                    P.op("dve", lambda g=g, i2=i2: V.tensor_tensor(out=yst[i2][:], in0=acc[:].rearrange("p h d -> p (h d)"), in1=nw[:, g * 256:(g + 1) * 256], op=ALU.mult),
                         reads=["acc", "nw"], writes=[("yst", i2)])
                    P.dma("sp", k.y[t * 128:(t + 1) * 128, 512 + g * 256:512 + (g + 1) * 256], yst[i2][:], reads=[("yst", i2)], writes=[("ynsa", t, g)])
        P.barrier()


TRASH = NE * CAP


def layer_norm_tile(k, P, src, dst, lnw, lnb, tmp, key_src, key_dst, sfx):
    nc = k.nc
    V, A = nc.vector, nc.scalar
    st, ag, rs = k.ln_st, k.ln_ag, k.ln_rs
    for hlf in range(2):
        P.op("dve", lambda hlf=hlf: V.bn_stats(out=st[:, hlf * 6:(hlf + 1) * 6], in_=src[:, hlf * 512:(hlf + 1) * 512]), reads=[key_src], writes=["ln_st"])
    P.op("dve", lambda: V.bn_aggr(out=ag[:], in_=st[:]), reads=["ln_st"], writes=["ln_ag"])
    P.op("dve", lambda: V.tensor_scalar(out=rs[:], in0=ag[:, 1:2], scalar1=1e-5, scalar2=None, op0=ALU.add), reads=["ln_ag"], writes=["ln_rs"])
    P.op("act", lambda: A.activation(out=rs[:], in_=rs[:], func=AF.Sqrt), reads=["ln_rs"], writes=["ln_rs"])
    P.op("dve", lambda: V.reciprocal(out=rs[:], in_=rs[:]), reads=["ln_rs"], writes=["ln_rs"])
    P.op("dve", lambda: V.tensor_scalar(out=tmp[:], in0=src[:], scalar1=ag[:, 0:1], scalar2=rs[:, 0:1], op0=ALU.subtract, op1=ALU.mult),
         reads=[key_src, "ln_ag", "ln_rs"], writes=["ln_tmp" + sfx])
    P.op("dve", lambda: V.tensor_tensor(out=tmp[:], in0=tmp[:], in1=lnw[:], op=ALU.mult), reads=["ln_tmp" + sfx, "lnw" + sfx], writes=["ln_tmp" + sfx])
    P.op("dve", lambda: V.tensor_tensor(out=dst[:], in0=tmp[:], in1=lnb[:], op=ALU.add), reads=["ln_tmp" + sfx, "lnb" + sfx], writes=[key_dst])


def phase3(k, l):
    nc, P = k.nc, k.P
    V, A = nc.vector, nc.scalar
    with ExitStack() as es:
        sb = lambda name, shape, dt: es.enter_context(nc.sbuf_tensor(f"p3l{l}_{name}", shape, dt))
        ps = lambda name, shape, dt: es.enter_context(nc.psum_tensor(f"p3l{l}_{name}", shape, dt))
        wo = sb("wo", [128, 8, D], BF16)
        rw = sb("rw", [128, 8, NE], F32)
        rb = sb("rb", [128, NE], F32)
        lnw = sb("lnw", [128, D], F32)
        lnb = sb("lnb", [128, D], F32)
        k.ln_st = sb("ln_st", [128, 12], F32)
        k.ln_ag = sb("ln_ag", [128, 2], F32)
        k.ln_rs = sb("ln_rs", [128, 1], F32)
        ytl = [sb(f"ytl{i}", [128, D], BF16) for i in range(2)]
        yT = sb("yT", [128, 8, 128], BF16)
        xt = [sb(f"xt{i}", [128, D], F32) for i in range(2)]
        mix = sb("mix", [128, D], F32)
        rr = sb("rr", [128, D], F32)
        tmp = sb("tmp", [128, D], F32)
        x1 = [sb(f"x1_{i}", [128, D], F32) for i in range(2)]
        x1b = [sb(f"x1b{i}", [128, D], BF16) for i in range(2)]
        x1T = sb("x1T", [128, 8, 128], F32)
        lg = sb("lg", [128, NE], F32)
        m8 = sb("m8", [128, 8], F32)
        nm = sb("nm", [128, 1], F32)
        msk = sb("msk", [128, NE], F32)
        mskb = sb("mskb", [128, NE], BF16)
        ex = sb("ex", [128, NE], F32)
        ssum = sb("ssum", [128, 1], F32)
        wts = sb("wts", [128, NE], F32)
        posf = sb("posf", [128, NE], F32)
        carry = sb("carry", [128, NE], F32)
        val = sb("val", [128, NE], F32)
        nsel = sb("nsel", [128, NE], F32)
        t8 = sb("t8", [128, 8], F32)
        oh = sb("oh", [128, NE], F32)
        idxf = sb("idxf", [128, 4], F32)
        pT = ps("pT", [128, 8, 128], BF16)
        pm = [ps(f"pm{i}", [128, 512], F32) for i in range(2)]
        pX = [ps(f"pX{i}", [128, 512], F32) for i in range(2)]
        pr = ps("pr", [128, 512], F32)

        x_src = k.x if l == 0 else k.x2
        wov = k.w_out[l].rearrange("(c p) n -> p c n", p=128)
        for c in range(8):
            P.dma("pool", wo[:, c, :], wov[:, c, :], writes=[("wo", c)])
        P.dma("sp", rw[:], k.router_w[l].rearrange("(c p) n -> p c n", p=128), writes=["rw"])
        bcast_row(k, P, "sp", rb[:], k.router_b[l:l + 1, :], "rb")
        bcast_row(k, P, "sp", lnw[:], k.ln1_w[l:l + 1, :], "lnw1")
        bcast_row(k, P, "sp", lnb[:], k.ln1_b[l:l + 1, :], "lnb1")
        P.op("dve", lambda: V.memset(carry[:], 0.0), writes=["carry"])
        if k.dbg and k.inject_nsa:
            P.dma("sp", k.y[:, 512:1024], k.ynsa_in[l], writes=["yinj"])
            P.barrier()

        for t in range(NT):
            i2 = t % 2
            rows = slice(t * 128, (t + 1) * 128)
            P.dma("sp", ytl[i2][:], k.y[rows, :], writes=[("ytl", i2)])
            P.dma("act", xt[i2][:], x_src[rows, :], writes=[("xt", i2)])
            for c in range(8):
                P.op("pe", lambda c=c, i2=i2: nc.tensor.transpose(out=pT[:, c, :], in_=ytl[i2][:, c * 128:(c + 1) * 128], identity=k.ident_b[:]),
                     reads=[("ytl", i2), "ident_b"], writes=["pT"])
            P.op("dve", lambda: V.tensor_copy(out=yT[:], in_=pT[:]), reads=["pT"], writes=["yT"])
            for hf in range(2):
                for c in range(8):
                    P.op("pe", lambda c=c, hf=hf: nc.tensor.matmul(pm[hf][:], lhsT=yT[:, c, :], rhs=wo[:, c, hf * 512:(hf + 1) * 512], start=(c == 0), stop=(c == 7)),
                         reads=["yT", ("wo", c)], writes=[("pm", hf)])
                P.op("act", lambda hf=hf: A.copy(out=mix[:, hf * 512:(hf + 1) * 512], in_=pm[hf][:]), reads=[("pm", hf)], writes=["mix"])
            P.op("dve", lambda i2=i2: V.scalar_tensor_tensor(out=rr[:], in0=xt[i2][:], scalar=ALPHA, in1=mix[:], op0=ALU.mult, op1=ALU.add),
                 reads=[("xt", i2), "mix"], writes=["rr"])
            layer_norm_tile(k, P, rr, x1[i2], lnw, lnb, tmp, "rr", ("x1", i2), "1")
            P.dma("sp", k.x1[rows, :], x1[i2][:], reads=[("x1", i2)], writes=[("x1d", t)])
            P.op("act", lambda i2=i2: A.copy(out=x1b[i2][:], in_=x1[i2][:]), reads=[("x1", i2)], writes=[("x1b", i2)])
            for c in range(8):
                P.op("pe", lambda c=c, i2=i2: nc.tensor.transpose(out=pX[c // 4][:, (c % 4) * 128:(c % 4 + 1) * 128], in_=x1[i2][:, c * 128:(c + 1) * 128], identity=k.ident_f[:]),
                     reads=[("x1", i2), "ident"], writes=[("pX", c // 4)])
            for q in range(2):
                P.op("act", lambda q=q: A.copy(out=x1T[:, q * 4:(q + 1) * 4, :].rearrange("p a b -> p (a b)"), in_=pX[q][:]), reads=[("pX", q)], writes=["x1T"])
            for c in range(8):
                P.op("pe", lambda c=c: nc.tensor.matmul(pr[:, 0:NE], lhsT=x1T[:, c, :], rhs=rw[:, c, :], start=(c == 0), stop=(c == 7)),
                     reads=["x1T", "rw"], writes=["pr"])
            P.op("act", lambda: A.copy(out=lg[:], in_=pr[:, 0:NE]), reads=["pr"], writes=["lg"])
            P.op("dve", lambda: V.tensor_tensor(out=lg[:], in0=lg[:], in1=rb[:], op=ALU.add), reads=["lg", "rb"], writes=["lg"])
            P.op("dve", lambda: V.max(out=m8[:], in_=lg[:]), reads=["lg"], writes=["m8"])
            P.op("dve", lambda: V.tensor_scalar(out=msk[:], in0=lg[:], scalar1=m8[:, 3:4], scalar2=None, op0=ALU.is_ge), reads=["lg", "m8"], writes=["msk"])
            P.op("dve", lambda: V.tensor_copy(out=mskb[:], in_=msk[:]), reads=["msk"], writes=["mskb"])
            P.op("dve", lambda: V.tensor_scalar(out=nm[:], in0=m8[:, 0:1], scalar1=-1.0, scalar2=None, op0=ALU.mult), reads=["m8"], writes=["nm"])
            P.op("act", lambda: A.activation(out=ex[:], in_=lg[:], func=AF.Exp, bias=nm[:, 0:1], scale=1.0), reads=["lg", "nm"], writes=["ex"])
            P.op("dve", lambda: V.tensor_tensor(out=ex[:], in0=ex[:], in1=msk[:], op=ALU.mult), reads=["ex", "msk"], writes=["ex"])
            P.op("dve", lambda: V.tensor_reduce(out=ssum[:], in_=ex[:], axis=AX.X, op=ALU.add), reads=["ex"], writes=["ssum"])
            P.op("dve", lambda: V.reciprocal(out=ssum[:], in_=ssum[:]), reads=["ssum"], writes=["ssum"])
            P.op("dve", lambda: V.tensor_scalar(out=wts[:], in0=ex[:], scalar1=ssum[:, 0:1], scalar2=None, op0=ALU.mult), reads=["ex", "ssum"], writes=["wts"])
            P.op("pe", lambda: nc.tensor.matmul(pr[:, 64:64 + NE], lhsT=k.ustrict_b[:], rhs=mskb[:], start=True, stop=True), reads=["mskb", "ustrict_b"], writes=["pr"])
            P.op("pe", lambda: nc.tensor.matmul(pr[:, 128:128 + NE], lhsT=k.ones_b[:], rhs=mskb[:], start=True, stop=True), reads=["mskb", "ones_b"], writes=["pr"])
            P.op("act", lambda: A.copy(out=posf[:], in_=pr[:, 64:64 + NE]), reads=["pr"], writes=["posf"])
            P.op("act", lambda: A.copy(out=oh[:], in_=pr[:, 128:128 + NE]), reads=["pr"], writes=["oh"])
            P.op("dve", lambda: V.tensor_tensor(out=posf[:], in0=posf[:], in1=carry[:], op=ALU.add), reads=["posf", "carry"], writes=["posf"])
            P.op("dve", lambda: V.tensor_tensor(out=carry[:], in0=carry[:], in1=oh[:], op=ALU.add), reads=["carry", "oh", "posf"], writes=["carry"])
            P.op("dve", lambda: V.tensor_scalar(out=val[:], in0=posf[:], scalar1=float(CAP) - 0.5, scalar2=None, op0=ALU.is_lt), reads=["posf"], writes=["val"])
            P.op("dve", lambda: V.tensor_tensor(out=val[:], in0=val[:], in1=msk[:], op=ALU.mult), reads=["val", "msk"], writes=["val"])
            P.op("dve", lambda: V.tensor_tensor(out=nsel[:], in0=posf[:], in1=k.eoff[:], op=ALU.add), reads=["posf", "eoff"], writes=["nsel"])
            P.op("dve", lambda: V.tensor_scalar(out=nsel[:], in0=nsel[:], scalar1=k.trashp[:, 0:1], scalar2=None, op0=ALU.subtract), reads=["nsel", "trashp"], writes=["nsel"])
            P.op("dve", lambda: V.tensor_tensor(out=nsel[:], in0=nsel[:], in1=val[:], op=ALU.mult), reads=["nsel", "val"], writes=["nsel"])
            P.op("dve", lambda: V.tensor_scalar(out=nsel[:], in0=nsel[:], scalar1=k.trashp[:, 0:1], scalar2=-1.0, op0=ALU.add, op1=ALU.mult), reads=["nsel", "trashp"], writes=["nsel"])
            P.op("dve", lambda: V.max(out=t8[:], in_=nsel[:]), reads=["nsel"], writes=["t8"])
            P.op("dve", lambda: V.tensor_scalar(out=idxf[:], in0=t8[:, 0:4], scalar1=-1.0, scalar2=None, op0=ALU.mult), reads=["t8"], writes=["idxf"])
            P.op("dve", lambda t=t: V.tensor_copy(out=k.idx4[:, t, :], in_=idxf[:]), reads=["idxf"], writes=[("idx4", t)])
            for kk_ in range(4):
                P.op("dve", lambda kk_=kk_: V.tensor_scalar(out=oh[:], in0=nsel[:], scalar1=t8[:, kk_:kk_ + 1], scalar2=None, op0=ALU.is_equal), reads=["nsel", "t8", "carry"], writes=["oh"])
                P.op("dve", lambda: V.tensor_tensor(out=oh[:], in0=oh[:], in1=wts[:], op=ALU.mult), reads=["oh", "wts"], writes=["oh"])
                P.op("dve", lambda kk_=kk_, t=t: V.tensor_reduce(out=k.w4[:, t, kk_:kk_ + 1], in_=oh[:], axis=AX.X, op=ALU.add), reads=["oh"], writes=[("w4", t)])
            for kk_ in range(4):
                P.op("pool", lambda kk_=kk_, t=t, i2=i2: nc.gpsimd.indirect_dma_start(out=k.xbuf, out_offset=bass.IndirectOffsetOnAxis(ap=k.idx4[:, t, kk_:kk_ + 1], axis=0),
                                                                                       in_=x1b[i2][:], in_offset=None),
                     reads=[("x1b", i2), ("idx4", t)], writes=[("xbuf", t, kk_)], dma=True)
            if k.dbg and t == 0:
                dump(k, "lg", lg[:], "lg", NE)
                dump(k, "wts", wts[:], "wts", NE)
                dump(k, "nsel", nsel[:], "nsel", NE)
                dump(k, "w4", k.w4[:, 0, :], ("w4", 0), 4)
                dump(k, "idxf", idxf[:], "idxf", 4)
        P.barrier()


def phase4(k, l):
    nc, P = k.nc, k.P
    V, A = nc.vector, nc.scalar
    NSL = CAP // 128
    with ExitStack() as es:
        sb = lambda name, shape, dt: es.enter_context(nc.sbuf_tensor(f"p4l{l}_{name}", shape, dt))
        ps = lambda name, shape, dt: es.enter_context(nc.psum_tensor(f"p4l{l}_{name}", shape, dt))
        wu = [sb(f"wu{i}", [128, 8, 2 * D], BF16) for i in range(2)]
        wd = [sb(f"wd{i}", [128, 8, D], BF16) for i in range(2)]
        bupT = sb("bupT", [128, 16, NE], F32)
        bdn = [sb(f"bdn{i}", [128, D], F32) for i in range(2)]
        xe = [sb(f"xe{i}", [128, D], BF16) for i in range(4)]
        XeTs = [sb(f"XeT{i}", [128, 8, CAP], BF16) for i in range(2)]
        actT = sb("actT", [128, 8, CAP], BF16)
        gsb = [sb(f"gsb{i}", [128, CAP], F32) for i in range(2)]
        lsb = [sb(f"lsb{i}", [128, CAP], F32) for i in range(2)]
        sig = [sb(f"sig{i}", [128, CAP], F32) for i in range(2)]
        ysb = [sb(f"ysb{i}", [128, D], F32) for i in range(2)]
        yst = [sb(f"yst{i}", [128, D], BF16) for i in range(2)]
        pT = ps("pT", [128, 8, 128], BF16)
        pA = ps("pA", [128, 512], F32)
        pB = ps("pB", [128, 512], F32)
        pC = ps("pC", [128, 512], F32)
        pD = ps("pD", [128, 512], F32)

        P.op("dve", lambda: V.memset(yst[0][:], 0.0), writes=[("yst", 0)])
        P.dma("sp", k.ybuf[TRASH:TRASH + 128, :], yst[0][:], reads=[("yst", 0)], writes=["ybuf_trash"])
        P.dma("sp", gsb[0][0:NE, :], k.exp_b_up[l][:, 0:D], writes=[("gsb", 0)])
        P.dma("sp", lsb[0][0:NE, :], k.exp_b_up[l][:, D:2 * D], writes=[("lsb", 0)])
        for j in range(16):
            src = gsb[0] if j < 8 else lsb[0]
            P.op("pe", lambda j=j, src=src: nc.tensor.transpose(out=pA[:, j * NE:(j + 1) * NE], in_=src[0:NE, (j % 8) * 128:(j % 8 + 1) * 128], identity=k.ident_f[0:NE, 0:NE]),
                 reads=[("gsb", 0), ("lsb", 0), "ident"], writes=["pA"])
        P.op("act", lambda: A.copy(out=bupT[:].rearrange("p a b -> p (a b)"), in_=pA[:, 0:16 * NE]), reads=["pA"], writes=["bupT"])

        def load_w(e):
            import os
            if os.environ.get("NOLOADW") and e > 1:
                return
            b = e % 2
            wuv = k.exp_w_up[l, e].rearrange("(c p) n -> p c n", p=128)
            wdv = k.exp_w_down[l, e].rearrange("(c p) n -> p c n", p=128)
            for c in range(8):
                P.dma("pool", wu[b][:, c, :], wuv[:, c, :], writes=[("wu", b, c)])
            for c in range(8):
                P.dma("pool", wd[b][:, c, :], wdv[:, c, :], writes=[("wd", b, c)])
            P.dma("act", bdn[b][:], k.exp_b_down[l, e:e + 1, :].partition_broadcast(128), writes=[("bdn", b)])

        def gather(e):
            XeT = XeTs[e % 2]
            for i in range(NSL):
                i4 = i % 4
                P.dma("sp", xe[i4][:], k.xbuf[e * CAP + i * 128:e * CAP + (i + 1) * 128, :], writes=[("xe", i4)])
                for c in range(8):
                    P.op("pe", lambda c=c, i4=i4: nc.tensor.transpose(out=pT[:, c, :], in_=xe[i4][:, c * 128:(c + 1) * 128], identity=k.ident_b[:]),
                         reads=[("xe", i4), "ident_b"], writes=["pT"])
                P.op("dve", lambda i=i, XeT=XeT: V.tensor_copy(out=XeT[:, :, i * 128:(i + 1) * 128], in_=pT[:]), reads=["pT"], writes=[("XeT", e % 2)])

        load_w(0)
        for e in range(NE):
            b = e % 2
            if e + 1 < NE:
                load_w(e + 1)
            XeT = XeTs[b]
            kxe = ("XeT", b)
            if e == 0:
                gather(0)
            for j in range(8):
                groups = ((pA, "pA", j, 0), (pB, "pB", j, 512), (pC, "pC", 8 + j, 0), (pD, "pD", 8 + j, 512))
                for (dst, key, fc, n0) in groups:
                    for c in range(8):
                        P.op("pe", lambda c=c, dst=dst, fc=fc, n0=n0, b=b, XeT=XeT: nc.tensor.matmul(dst[:], lhsT=wu[b][:, c, fc * 128:(fc + 1) * 128], rhs=XeT[:, c, n0:n0 + 512],
                                                                                           start=(c == 0), stop=(c == 7)),
                             reads=[("wu", b, c), kxe], writes=[key])
                jb = j % 2
                G, Lq, Sg = gsb[jb], lsb[jb], sig[jb]
                kg, kl, ks = ("gsb", jb), ("lsb", jb), ("sig", jb)
                for (dst, key, fc, n0) in groups:
                    T_, kt_ = (G, kg) if fc < 8 else (Lq, kl)
                    P.op("act", lambda dst=dst, fc=fc, n0=n0, e=e, T_=T_: A.activation(out=T_[:, n0:n0 + 512], in_=dst[:], func=AF.Identity, bias=bupT[:, fc, e:e + 1], scale=1.0),
                         reads=[key, "bupT"], writes=[kt_])
                P.op("dve", lambda G=G: V.tensor_scalar(out=G[:], in0=G[:], scalar1=7.0, scalar2=None, op0=ALU.min), reads=[kg], writes=[kg])
                P.op("act", lambda G=G, Sg=Sg: A.activation(out=Sg[:], in_=G[:], func=AF.Sigmoid, scale=1.702), reads=[kg], writes=[ks])
                P.op("dve", lambda Lq=Lq: V.tensor_scalar(out=Lq[:], in0=Lq[:], scalar1=-7.0, scalar2=7.0, op0=ALU.max, op1=ALU.min), reads=[kl], writes=[kl])
                P.op("dve", lambda G=G, Sg=Sg: V.tensor_tensor(out=Sg[:], in0=G[:], in1=Sg[:], op=ALU.mult), reads=[kg, ks], writes=[ks])
                P.op("dve", lambda j=j, Lq=Lq, Sg=Sg: V.scalar_tensor_tensor(out=actT[:, j, :], in0=Lq[:], scalar=1.0, in1=Sg[:], op0=ALU.add, op1=ALU.mult),
                     reads=[kl, ks], writes=[("actT", j)])
            if e + 1 < NE:
                gather(e + 1)
            for i in range(NSL):
                i2 = i % 2
                for hf in range(2):
                    dstb, key = ((pA, "pA"), (pC, "pC"), (pB, "pB"), (pD, "pD"))[2 * i2 + hf]
                    for c in range(8):
                        P.op("pe", lambda c=c, dstb=dstb, hf=hf, i=i, b=b: nc.tensor.matmul(dstb[:], lhsT=actT[:, c, i * 128:(i + 1) * 128], rhs=wd[b][:, c, hf * 512:(hf + 1) * 512],
                                                                                           start=(c == 0), stop=(c == 7)),
                             reads=[("actT", c), ("wd", b, c)], writes=[key])
                    P.op("act", lambda dstb=dstb, hf=hf, i2=i2: A.copy(out=ysb[i2][:, hf * 512:(hf + 1) * 512], in_=dstb[:]), reads=[key], writes=[("ysb", i2)])
                P.op("dve", lambda i2=i2, b=b: V.tensor_tensor(out=yst[i2][:], in0=ysb[i2][:], in1=bdn[b][:], op=ALU.add),
                     reads=[("ysb", i2), ("bdn", b)], writes=[("yst", i2)])
                P.dma("sp", k.ybuf[e * CAP + i * 128:e * CAP + (i + 1) * 128, :], yst[i2][:], reads=[("yst", i2)], writes=[("ybuf", e, i)])
        P.barrier()


def phase5(k, l, last):
    nc, P = k.nc, k.P
    V, A = nc.vector, nc.scalar
    with ExitStack() as es:
        sb = lambda name, shape, dt: es.enter_context(nc.sbuf_tensor(f"p5l{l}_{name}", shape, dt))
        lnw = sb("lnw", [128, D], F32)
        lnb = sb("lnb", [128, D], F32)
        k.ln_st = sb("ln_st", [128, 12], F32)
        k.ln_ag = sb("ln_ag", [128, 2], F32)
        k.ln_rs = sb("ln_rs", [128, 1], F32)
        gk = [[sb(f"gk{i}_{q}", [128, D], BF16) for q in range(4)] for i in range(2)]
        x1t = [sb(f"x1t{i}", [128, D], F32) for i in range(2)]
        acc = sb("acc", [128, D], F32)
        tmp = sb("tmp", [128, D], F32)
        x2 = [sb(f"x2_{i}", [128, D], F32) for i in range(2)]
        bcast_row(k, P, "sp", lnw[:], k.ln2_w[l:l + 1, :], "lnw2")
        bcast_row(k, P, "sp", lnb[:], k.ln2_b[l:l + 1, :], "lnb2")
        dst_d = k.out if last else k.x2
        for t in range(NT):
            i2 = t % 2
            rows = slice(t * 128, (t + 1) * 128)
            P.dma("sp", x1t[i2][:], k.x1[rows, :], writes=[("x1t", i2)])
            for q in range(4):
                P.op("pool", lambda q=q, t=t, i2=i2: nc.gpsimd.indirect_dma_start(out=gk[i2][q][:], out_offset=None, in_=k.ybuf,
                                                                                 in_offset=bass.IndirectOffsetOnAxis(ap=k.idx4[:, t, q:q + 1], axis=0)),
                     writes=[("gk", i2, q)], dma=True)
            P.op("dve", lambda t=t, i2=i2: V.tensor_scalar(out=acc[:], in0=gk[i2][0][:], scalar1=k.w4[:, t, 0:1], scalar2=None, op0=ALU.mult),
                 reads=[("gk", i2, 0)], writes=["acc"])
            for q in range(1, 4):
                P.op("dve", lambda q=q, t=t, i2=i2: V.scalar_tensor_tensor(out=acc[:], in0=gk[i2][q][:], scalar=k.w4[:, t, q:q + 1], in1=acc[:], op0=ALU.mult, op1=ALU.add),
                     reads=[("gk", i2, q), "acc"], writes=["acc"])
            if k.dbg and k.moe_dbg is not None:
                P.dma("sp", k.moe_dbg[rows, :], acc[:], reads=["acc"], writes=[("moe_dbg", t)])
            P.op("dve", lambda i2=i2: V.scalar_tensor_tensor(out=acc[:], in0=x1t[i2][:], scalar=ALPHA, in1=acc[:], op0=ALU.mult, op1=ALU.add),
                 reads=[("x1t", i2), "acc"], writes=["acc"])
            layer_norm_tile(k, P, acc, x2[i2], lnw, lnb, tmp, "acc", ("x2", i2), "2")
            P.dma("sp", dst_d[rows, :], x2[i2][:], reads=[("x2", i2)], writes=[("x2d", t)])
        P.barrier()


def make_inputs(inputs, b, perm, w_in_r=None):
    f32 = lambda n: np.ascontiguousarray(np.asarray(inputs[n], dtype=np.float32))
    if w_in_r is None:
        w_in_r = np.ascontiguousarray(np.asarray(inputs["w_in"], dtype=np.float32)[:, :, perm])
    m = {"x": np.ascontiguousarray(np.asarray(inputs["x"], dtype=np.float32)[b]),
         "pos": np.ascontiguousarray(np.asarray(inputs["positions"], dtype=np.int32)[b].reshape(1, -1)), "w_in": w_in_r,
         "hg_lb": f32("hg_lower_bounds"), "hg_nw": f32("hg_norm_w"), "gm_lnw": f32("gm_ln_w"), "gm_lnb": f32("gm_ln_b"),
         "gm_ws": f32("gm_spatial_w"), "gm_bs": f32("gm_spatial_b"), "gm_nw": f32("gm_norm_w"),
         "nsa_pe": f32("nsa_cmp_pe"), "nsa_w1": f32("nsa_cmp_w1"), "nsa_w2": f32("nsa_cmp_w2"), "nsa_nw": f32("nsa_norm_w"),
         "w_out": f32("w_out"), "ln1_w": f32("ln1_w"), "ln1_b": f32("ln1_b"), "ln2_w": f32("ln2_w"), "ln2_b": f32("ln2_b"),
         "router_w": f32("router_w"), "router_b": f32("router_b"), "exp_w_up": f32("exp_w_up"), "exp_b_up": f32("exp_b_up"),
         "exp_w_down": f32("exp_w_down"), "exp_b_down": f32("exp_b_down")}
    for n, v in host_consts().items():
        m["c_" + n] = v
    return m


def kernel(**inputs):
    perm = w_in_perm()
    w_in_r = np.ascontiguousarray(np.asarray(inputs["w_in"], dtype=np.float32)[:, :, perm])
    nc = build(nlayers=L, dbg=False)
    in_maps = [make_inputs(inputs, b, perm, w_in_r) for b in range(8)]
    res = run_bass_kernel_spmd(nc, in_maps, core_ids=list(range(8)))
    return np.stack([np.asarray(r["out"], dtype=np.float32) for r in res.results], axis=0)
```

```python
import math
from contextlib import ExitStack

import numpy as np
import concourse.bass as bass
import concourse.mybir as mybir
from concourse.bass_utils import run_bass_kernel_spmd

F32 = mybir.dt.float32
BF16 = mybir.dt.bfloat16
I32 = mybir.dt.int32
U32 = mybir.dt.uint32
AF = mybir.ActivationFunctionType
ALU = mybir.AluOpType
AX = mybir.AxisListType

S = 4096
D = 1024
NT = S // 128
L = 2
NE = 32
CAP = 1024
NCOLA = 1816
NCH_B = 14
WCOLS = NCOLA + NCH_B * 128
ALPHA = (2 * L) ** 0.25
PI = math.pi
TRASH = NE * CAP

COMPUTE = ("pe", "dve", "act", "pool")
DMAQ = ("sp", "act", "pool")
NDMASEM = 6
NCSEM = 8
SEM_KEYS = [(e, i) for e in COMPUTE for i in range(NCSEM)] + [(q + "_q", i) for q in DMAQ for i in range(NDMASEM)]


class Op:
    __slots__ = ("eng", "fn", "reads", "writes", "dma", "deps", "raw", "need_inc", "sem", "val", "idx")


class Prog:
    def __init__(self, nc, sems):
        self.nc = nc
        self.sems = sems
        self.eng = {"pe": nc.tensor, "dve": nc.vector, "act": nc.scalar, "pool": nc.gpsimd, "sp": nc.sync}
        self.ops = []
        self.last_writer = {}
        self.readers = {}
        self.cnt = {e: 0 for e in COMPUTE}
        self.dcnt = {q: 0 for q in DMAQ}
        self.waited = {}
        self.dma_last = {}
        self.nwait = 0
        self.ntotal = 0
        import os
        self.verbose = bool(os.environ.get("VERB"))

    def op(self, eng, fn, reads=(), writes=(), dma=False):
        o = Op()
        o.eng, o.fn, o.dma = eng, fn, dma
        o.reads, o.writes = tuple(reads), tuple(writes)
        o.need_inc = False
        o.idx = len(self.ops)
        deps = set()
        raw = set()
        lw, rd = self.last_writer, self.readers
        for k in o.reads:
            w = lw.get(k)
            if w is not None:
                deps.add(w)
                raw.add(w)
        for k in o.writes:
            w = lw.get(k)
            if w is not None:
                deps.add(w)
            r = rd.get(k)
            if r:
                deps.update(r)
        for k in o.reads:
            rd.setdefault(k, []).append(o.idx)
        for k in o.writes:
            lw[k] = o.idx
            rd[k] = []
        deps.discard(o.idx)
        o.deps = deps
        o.raw = raw
        self.ops.append(o)
        return o

    def dma(self, q, out, in_, reads=(), writes=(), **kw):
        e = self.eng[q]
        return self.op(q, lambda: e.dma_start(out=out, in_=in_, **kw), reads, writes, dma=True)

    def barrier(self):
        ops = self.ops
        tails = set()
        last_by_eng = {}
        for o in ops:
            if o.dma:
                tails.add(o.idx)
            else:
                last_by_eng[o.eng] = o.idx
        tails.update(last_by_eng.values())
        for en in ("pe", "dve", "act", "pool", "sp"):
            o = Op()
            o.eng, o.dma, o.reads, o.writes, o.need_inc = en, False, (), (), False
            e = self.eng[en]
            o.fn = (lambda e=e: e.nop())
            o.idx = len(ops)
            o.deps = set(tails)
            o.raw = set()
            ops.append(o)
        self.emit()

    def emit(self):
        ops, sems = self.ops, self.sems
        def needs_sync(o, d):
            p = ops[d]
            return p.dma or p.eng != o.eng or (o.eng != "pe" and d in o.raw)
        for o in ops:
            for d in o.deps:
                if needs_sync(o, d):
                    ops[d].need_inc = True
        cnt, dcnt, waited = self.cnt, self.dcnt, self.waited
        for o in ops:
            e = self.eng[o.eng]
            pre = []
            if o.dma:
                i = dcnt[o.eng]
                dcnt[o.eng] += 1
                sk = (o.eng + "_q", i % NDMASEM)
                o.sem = sk
                o.val = 16 * (i // NDMASEM + 1)
                self.dma_last[sk] = o.val
                if o.val > 16:
                    pre.append((sk, o.val - 16))
            for d in o.deps:
                p = ops[d]
                if p.dma or needs_sync(o, d):
                    pre.append((p.sem, p.val))
            best = {}
            for sk, v in pre:
                if v > best.get(sk, 0):
                    best[sk] = v
            for sk, v in best.items():
                if waited.get((o.eng, sk), 0) >= v:
                    continue
                waited[(o.eng, sk)] = v
                e.wait_ge(sems[sk], v)
                self.nwait += 1
                if self.verbose:
                    print("   wait", o.eng, sk, v)
            ins = o.fn()
            if self.verbose:
                print("op", o.idx, o.eng, "dma" if o.dma else "", o.writes, "inc" if (o.need_inc or o.dma) else "", getattr(o, "sem", None) if o.dma else "", (o.val if o.dma else ""))
            if o.dma:
                ins.then_inc(sems[o.sem], 16)
            elif o.need_inc:
                i = cnt[o.eng]
                cnt[o.eng] += 1
                o.sem = (o.eng, i % NCSEM)
                o.val = i // NCSEM + 1
                ins.then_inc(sems[o.sem], 1)
        self.ntotal += len(ops)
        self.ops = []
        self.last_writer = {}
        self.readers = {}

    def finish(self):
        self.emit()
        e = self.eng["sp"]
        for sk, v in self.dma_last.items():
            e.wait_ge(self.sems[sk], v)


def host_consts():
    c = {}
    c["ident"] = np.eye(128, dtype=np.float32)
    s = np.arange(128)[:, None]
    t = np.arange(128)[None, :]
    same = (s // 64) == (t // 64)
    mid = (t // 64) * 64 + 31
    c["mcum"] = (same & (s <= t)).astype(np.float32)
    c["mmid"] = (c["mcum"] - (same & (s <= mid)).astype(np.float32))
    c["mrem"] = (same & (s > t)).astype(np.float32)
    c["tri"] = (s <= t).astype(np.float32)
    c["trit"] = (s >= t).astype(np.float32)
    inv = (10000.0 ** (-np.arange(0, 64, 2, dtype=np.float32) / 64)).astype(np.float32)
    p = np.arange(128)
    sgn = np.where((p % 64) < 32, -1.0, 1.0).astype(np.float32)
    col = np.zeros((128, 4), np.float32)
    col[:, 0] = inv[p % 32]
    col[:, 1] = sgn
    col[:, 2] = 2 * PI * sgn
    col[:, 3] = -PI
    c["ropecol"] = col
    jj = np.arange(64)[:, None, None]
    kt_ = np.arange(32)[None, :, None]
    kk_ = np.arange(128)[None, None, :]
    E = (jj == 2 * kt_ + kk_ // 64).astype(np.float32)
    c["edup"] = np.concatenate([E, E], 0)
    p_ = np.arange(128)[:, None]
    q_ = np.arange(512)[None, :]
    c["win01"] = np.stack([((128 * dl + p_ - q_ <= 0) & (128 * dl + p_ - q_ > -512)).astype(np.float32) for dl in range(-4, 4)], 1)
    c["cmp01"] = np.stack([(16 * p_ + 31 - q_ <= u).astype(np.float32) for u in (0, 512, 1024, 1536, 2048)], 1)
    tq = np.arange(S).reshape(NT, 128)
    cur = (tq // 64)[:, :, None]
    jb = np.arange(64)[None, None, :]
    fut = jb > cur
    frc = (jb == 0) | (jb == cur) | (jb == cur - 1)
    keep = (~fut & ~frc).astype(np.float32)
    add = np.where(frc, 1e9, np.where(fut, -1e9, 0.0)).astype(np.float32)
    c["wpad"] = np.ascontiguousarray(np.maximum(0, 511 - tq).T.astype(np.float32))
    c["impkeep"] = np.ascontiguousarray(keep.transpose(1, 0, 2))
    c["impadd"] = np.ascontiguousarray(add.transpose(1, 0, 2))
    cc = np.arange(256)
    ov = np.zeros((256, 65), np.float32)
    for c_ in range(255):
        for uu in (c_, c_ + 1):
            ov[c_, uu // 4] += 1.0
    ov[:, 64] = 1.0
    c["ovext"] = np.ascontiguousarray(ov.reshape(2, 128, 65).transpose(1, 0, 2))
    c["eoff"] = np.tile((np.arange(NE, dtype=np.float32) * CAP)[None, :], (128, 1)).astype(np.float32)
    c["trashp"] = (TRASH + np.arange(128, dtype=np.float32)).reshape(128, 1).astype(np.float32)
    return c


def w_in_perm():
    hg_q, hg_f, hg_i, hg_g, gm_u, gm_v, q, k_c, v_c, k_s, v_s, k_w, v_w, gts = (
        0, 256, 512, 768, 1024, 1280, 1536, 2048, 2176, 2304, 2432, 2560, 2688, 2816)
    A = list(range(0, 1536)) + list(range(v_s, v_s + 128)) + list(range(v_w, v_w + 128)) + list(range(gts, gts + 24))

    def sw(c0, n):
        out = []
        for h in range(n // 64):
            b = c0 + h * 64
            out += list(range(b + 32, b + 64)) + list(range(b, b + 32))
        return out
    B = (list(range(q, q + 512)) + sw(q, 512) + list(range(k_s, k_s + 128)) + sw(k_s, 128)
         + list(range(k_w, k_w + 128)) + sw(k_w, 128) + list(range(k_c, k_c + 128)) + list(range(v_c, v_c + 128)))
    perm = np.array(A + B, dtype=np.int64)
    assert perm.shape[0] == WCOLS
    return perm


class K:
    pass


def build(nlayers=L, dbg=False, stop_after=None, stage=None, inject_nsa=False, phases=(1, 2, 3, 4, 5)):
    nc = bass.Bass("TRN2", target_bir_lowering=False)
    k = K()
    global _LASTK
    _LASTK = k
    k.stage = stage
    import os
    k.dbgv = int(os.environ.get('DBGV', '0'))
    k.nc = nc
    k.dbg = dbg

    def din(name, shape, dt=F32):
        return nc.dram_tensor(name, list(shape), dt, kind="ExternalInput").ap()

    def dscr(name, shape, dt, out=False):
        return nc.dram_tensor(name, list(shape), dt, kind=("ExternalOutput" if (out and dbg) else "Internal")).ap()

    k.x = din("x", [S, D])
    k.pos = din("pos", [1, S], I32)
    k.w_in = din("w_in", [L, D, WCOLS])
    k.lbraw = din("hg_lb", [L, 256])
    k.hg_nw = din("hg_nw", [L, 256])
    k.gm_lnw = din("gm_lnw", [L, 256])
    k.gm_lnb = din("gm_lnb", [L, 256])
    k.gm_ws = din("gm_ws", [L, 4, 128, 128])
    k.gm_bs = din("gm_bs", [L, 4, 128])
    k.gm_nw = din("gm_nw", [L, 256])
    k.nsa_pe = din("nsa_pe", [L, 2, 32, 64]); k.nsa_w1 = din("nsa_w1", [L, 2, 2048, 128])
    k.nsa_w2 = din("nsa_w2", [L, 2, 128, 64]); k.nsa_nw = din("nsa_nw", [L, 512])
    k.w_out = din("w_out", [L, D, D])
    k.ln1_w = din("ln1_w", [L, D]); k.ln1_b = din("ln1_b", [L, D])
    k.ln2_w = din("ln2_w", [L, D]); k.ln2_b = din("ln2_b", [L, D])
    k.router_w = din("router_w", [L, D, NE]); k.router_b = din("router_b", [L, NE])
    k.exp_w_up = din("exp_w_up", [L, NE, D, 2 * D]); k.exp_b_up = din("exp_b_up", [L, NE, 2 * D])
    k.exp_w_down = din("exp_w_down", [L, NE, D, D]); k.exp_b_down = din("exp_b_down", [L, NE, D])
    k.inject_nsa = inject_nsa
    if dbg and inject_nsa:
        k.ynsa_in = din("ynsa_in", [L, S, 512], BF16)
    k.cst = {n: din("c_" + n, v.shape) for n, v in host_consts().items()}
    k.out = nc.dram_tensor("out", [S, D], F32, kind="ExternalOutput").ap()
    k.ft = dscr("ft", [12, 128, S], BF16, out=True)
    k.tmv = dscr("tmv", [S, 256], BF16, out=True)
    k.gts = dscr("gts", [S, 24], F32, out=True)
    k.y = dscr("y", [S, D], BF16, out=True)
    k.x1 = dscr("x1", [S, D], F32, out=True)
    k.x2 = dscr("x2", [S, D], F32, out=True)
    k.xbuf = dscr("xbuf", [TRASH + 128, D], BF16)
    k.ybuf = dscr("ybuf", [TRASH + 128, D], BF16)
    k.moe_dbg = dscr("moe_dbg", [S, D], F32, out=True) if dbg else None
    k.dbgout = dscr("dbgout", [24, 128, 512], F32, out=True)
    k.ndump = 0
    k.dumpnames = []

    with ExitStack() as es:
        sems = {}
        for sk in SEM_KEYS:
            nm = sk if isinstance(sk, str) else f"{sk[0]}{sk[1]}"
            sems[sk] = es.enter_context(nc.semaphore("s_" + nm))
        P = Prog(nc, sems)
        k.P = P
        k.es = es
        setup_consts(k)
        for l in range(nlayers if stop_after != "setup" else 0):
            if 1 in phases:
                phase1(k, l)
            if stop_after == (l, 1):
                break
            if 2 in phases:
                phase2(k, l)
            if stop_after == (l, 2):
                break
            if 3 in phases:
                phase3(k, l)
            if stop_after == (l, 3):
                break
            if 4 in phases:
                phase4(k, l)
            if 5 in phases:
                phase5(k, l, last=(l == nlayers - 1))
        P.finish()
        print("[build] ops", P.ntotal, "waits", P.nwait, "cnt", P.cnt, "dcnt", P.dcnt)
    return nc


def setup_consts(k):
    nc, P, es = k.nc, k.P, k.es
    sb = lambda name, shape, dt: es.enter_context(nc.sbuf_tensor(name, shape, dt))
    k.ident_f = sb("ident_f", [128, 128], F32)
    k.ident_b = sb("ident_b", [128, 128], BF16)
    k.mcum = sb("mcum", [128, 128], F32)
    k.mmid = sb("mmid", [128, 128], F32)
    k.mrem = sb("mrem", [128, 128], F32)
    k.tri = sb("tri", [128, 128], F32)
    k.trit = sb("trit", [128, 128], F32)
    k.mcum_b = sb("mcum_b", [128, 128], BF16)
    k.mmid_b = sb("mmid_b", [128, 128], BF16)
    k.mrem_b = sb("mrem_b", [128, 128], BF16)
    k.ones_b = sb("ones_b", [128, 128], BF16)
    k.ropecol = sb("ropecol", [128, 4], F32)
    k.ones_f = sb("ones_f", [128, 128], F32)
    k.idx4 = sb("idx4", [128, NT, 4], I32)
    k.w4 = sb("w4", [128, NT, 4], F32)
    k.eoff = sb("eoff", [128, NE], F32)
    k.trashp = sb("trashp", [128, 1], F32)
    k.ustrict_b = sb("ustrict_b", [128, 128], BF16)
    for n, t in (("ident", k.ident_f), ("mcum", k.mcum), ("mmid", k.mmid), ("mrem", k.mrem), ("tri", k.tri),
                 ("trit", k.trit), ("ropecol", k.ropecol), ("eoff", k.eoff), ("trashp", k.trashp)):
        P.dma("sp", t[:], k.cst[n], writes=[n])
    P.op("dve", lambda: nc.vector.tensor_copy(out=k.ident_b[:], in_=k.ident_f[:]), reads=["ident"], writes=["ident_b"])
    P.op("dve", lambda: nc.vector.memset(k.ones_f[:], 1.0), writes=["ones_f"])
    P.op("dve", lambda: nc.vector.memset(k.ones_b[:], 1.0), writes=["ones_b"])
    P.op("dve", lambda: nc.vector.tensor_sub(out=k.ones_f[:], in0=k.tri[:], in1=k.ident_f[:]), reads=["tri", "ident", "ones_f"], writes=["ustr_tmp"])
    P.op("dve", lambda: nc.vector.tensor_copy(out=k.ustrict_b[:], in_=k.ones_f[:]), reads=["ustr_tmp"], writes=["ustrict_b"])
    P.op("dve", lambda: nc.vector.memset(k.ones_f[:], 1.0), reads=["ustrict_b"], writes=["ones_f"])
    P.op("dve", lambda: nc.vector.tensor_copy(out=k.mcum_b[:], in_=k.mcum[:]), reads=["mcum"], writes=["mcum_b"])
    P.op("dve", lambda: nc.vector.tensor_copy(out=k.mmid_b[:], in_=k.mmid[:]), reads=["mmid"], writes=["mmid_b"])
    P.op("dve", lambda: nc.vector.tensor_copy(out=k.mrem_b[:], in_=k.mrem[:]), reads=["mrem"], writes=["mrem_b"])
    P.barrier()


def setup_rope(k, es, l):
    nc, P = k.nc, k.P
    sb = lambda name, shape, dt: es.enter_context(nc.sbuf_tensor(f"{name}l{l}", shape, dt))
    k.ropeC = sb("ropeC", [128, S], F32)
    k.ropeS = sb("ropeS", [128, S], F32)
    with nc.sbuf_tensor(f"posil{l}", [128, S], I32) as posi, nc.sbuf_tensor(f"ropeTl{l}", [128, S], F32) as ropeT, nc.sbuf_tensor(f"ropeT2l{l}", [128, S], F32) as ropeT2:
        k.ropeT, k.ropeT2 = ropeT, ropeT2
        P.dma("sp", posi[:], k.pos.partition_broadcast(128), writes=["posi"])
        C, Sg, col = k.ropeC, k.ropeS, k.ropecol
        P.op("dve", lambda: nc.vector.tensor_copy(out=C[:], in_=posi[:]), reads=["posi"], writes=["C"])
        P.op("dve", lambda: nc.vector.tensor_scalar(out=C[:], in0=C[:], scalar1=col[:, 0:1], scalar2=None, op0=ALU.mult),
             reads=["C", "ropecol"], writes=["C"])
        def red(T, add):
            P.op("dve", lambda: nc.vector.tensor_scalar(out=T[:], in0=C[:], scalar1=1.0 / (2 * PI), scalar2=add, op0=ALU.mult, op1=ALU.add),
                 reads=["C"], writes=["T" + str(add)])
            P.op("dve", lambda: nc.vector.tensor_copy(out=posi[:], in_=T[:]), reads=["T" + str(add)], writes=["posi"])
            P.op("dve", lambda: nc.vector.tensor_copy(out=k.ropeT[:], in_=posi[:]), reads=["posi"], writes=["ropeT"])
            P.op("dve", lambda: nc.vector.tensor_sub(out=T[:], in0=T[:], in1=k.ropeT[:]), reads=["ropeT", "T" + str(add)], writes=["T" + str(add)])
            P.op("dve", lambda: nc.vector.tensor_scalar(out=k.ropeT[:], in0=T[:], scalar1=0.5, scalar2=None, op0=ALU.is_gt), reads=["T" + str(add)], writes=["ropeT"])
            P.op("dve", lambda: nc.vector.tensor_sub(out=T[:], in0=T[:], in1=k.ropeT[:]), reads=["ropeT", "T" + str(add)], writes=["T" + str(add)])
        red(Sg, 0.0)
        P.op("act", lambda: nc.scalar.activation(out=Sg[:], in_=Sg[:], func=AF.Sin, scale=col[:, 2:3]), reads=["T0.0", "ropecol"], writes=["S"])
        red(k.ropeT2, 0.25)
        P.op("act", lambda: nc.scalar.activation(out=C[:], in_=k.ropeT2[:], func=AF.Sin, scale=2 * PI), reads=["T0.25", "C"], writes=["C"])
        P.barrier()


def dump(k, name, ap, key, width, parts=128):
    if not k.dbg:
        return
    i = k.ndump
    k.ndump += 1
    k.dumpnames.append(name)
    k.P.dma("pool", k.dbgout[i, 0:parts, 0:width], ap, reads=[key], writes=[("dbgout", i)])


def bcast_row(k, P, q, dst, src_row, key):
    P.dma(q, dst, src_row.partition_broadcast(128), writes=[key])


class _Stop(Exception):
    pass


def phase1(k, l):
    with ExitStack() as es:
        setup_rope(k, es, l)
        try:
            _phase1(k, l, es)
        except _Stop:
            pass
        k.P.barrier()


def ck(k, n):
    if k.stage == n:
        raise _Stop()


def _phase1(k, l, es):
    nc, P = k.nc, k.P
    if True:
        sb = lambda name, shape, dt: es.enter_context(nc.sbuf_tensor(f"p1l{l}_{name}", shape, dt))
        ps = lambda name, shape, dt: es.enter_context(nc.psum_tensor(f"p1l{l}_{name}", shape, dt))
        wb = sb("wb", [128, 8, WCOLS], BF16)
        xt = [sb(f"xt{i}", [128, D], F32) for i in range(2)]
        xb = [sb(f"xb{i}", [128, D], BF16) for i in range(2)]
        xTb = [sb(f"xTb{i}", [128, 8, 512], BF16) for i in range(2)]
        hg_nw = sb("hg_nw", [128, 256], F32)
        gm_lnw = sb("gm_lnw", [128, 256], F32)
        gm_lnb = sb("gm_lnb", [128, 256], F32)
        gm_nw = sb("gm_nw", [128, 256], F32)
        lbt = sb("lbt", [128, 2, 256], F32)
        lb = sb("lb", [128, 256], F32)
        oml = sb("oml", [128, 256], F32)
        wsn = sb("wsn", [128, 4, 128], F32)
        wsb = sb("wsb", [128, 4, 128], BF16)
        wsT = sb("wsT", [128, 4, 128], BF16)
        bsT = sb("bsT", [128, 4], F32)
        pT = ps("pT", [128, 8, 128], BF16)
        pm = [ps(f"pm{i}", [128, 512], F32) for i in range(4)]
        pa = ps("pa", [128, 512], F32)
        pb = ps("pb", [128, 512], F32)
        pc = ps("pc", [128, 512], F32)
        pcb = pc[:].bitcast(BF16)

        x_src = k.x if l == 0 else k.x2
        wv = k.w_in[l].rearrange("(c p) n -> p c n", p=128)
        half = WCOLS // 2
        for c in range(8):
            for hh in range(2):
                P.dma("pool", wb[:, c, hh * half:(hh + 1) * half], wv[:, c, hh * half:(hh + 1) * half], writes=[("wb", c), ("wbser", (2 * c + hh) % 2)])
        bcast_row(k, P, "sp", hg_nw[:], k.hg_nw[l:l + 1, :], "hg_nw")
        bcast_row(k, P, "sp", gm_lnw[:], k.gm_lnw[l:l + 1, :], "gm_lnw")
        bcast_row(k, P, "sp", gm_lnb[:], k.gm_lnb[l:l + 1, :], "gm_lnb")
        bcast_row(k, P, "sp", gm_nw[:], k.gm_nw[l:l + 1, :], "gm_nw")
        if l > 0:
            P.dma("sp", lbt[:].rearrange("p a n -> p (a n)"),
                  k.lbraw.rearrange("a n -> (a n)").unsqueeze(0).partition_broadcast(128), writes=["lbt"])
            P.op("dve", lambda: nc.vector.tensor_sub(out=lb[:], in0=lbt[:, 1, :], in1=lbt[:, 0, :]), reads=["lbt"], writes=["lb"])
            P.op("act", lambda: nc.scalar.activation(out=lb[:], in_=lb[:], func=AF.Sigmoid), reads=["lb"], writes=["lb"])
            P.op("dve", lambda: nc.vector.tensor_scalar(out=oml[:], in0=lb[:], scalar1=-1.0, scalar2=1.0, op0=ALU.mult, op1=ALU.add),
                 reads=["lb"], writes=["oml"])
        P.dma("sp", wsn[:], k.gm_ws[l].rearrange("g t s -> t g s"), writes=["wsn"])
        P.dma("sp", bsT[:], k.gm_bs[l].rearrange("g t -> t g"), writes=["bsT"], allow_slow_non_contiguous=True)
        trit = k.trit
        for g in range(4):
            P.op("dve", lambda g=g: nc.vector.tensor_tensor(out=wsb[:, g, :], in0=wsn[:, g, :], in1=trit[:], op=ALU.mult),
                 reads=["wsn"], writes=["wsb"])
        for g in range(4):
            P.op("pe", lambda g=g: nc.tensor.transpose(out=pT[:, g, :], in_=wsb[:, g, :], identity=k.ident_b[:]),
                 reads=["wsb", "ident_b"], writes=["pT"])
        P.op("dve", lambda: nc.vector.tensor_copy(out=wsT[:], in_=pT[:, 0:4, :]), reads=["pT"], writes=["wsT"])

        st_f = sb("st_f", [128, 2, 64], F32)
        st_b = sb("st_b", [128, 2, 64], BF16)
        P.op("dve", lambda: nc.vector.memset(st_f[:], 0.0), writes=["st_f"])
        P.op("dve", lambda: nc.vector.memset(st_b[:], 0.0), writes=["st_b"])

        W = {}
        for n in ("qf", "sg", "sgate", "logf", "kk", "ebm", "enbm", "eb", "erem", "t1", "t2", "osq", "nwg", "gsq", "ginn", "gsv"):
            W[n] = sb(n, [128, 256], F32)
        gx = sb("gx", [128, 512], F32)
        gg = sb("gg", [128, 512], F32)
        gt = sb("gt", [128, 512], F32)
        vb = sb("vb", [128, 256], BF16)
        qe = sb("qe", [128, 256], BF16)
        ke = sb("ke", [128, 256], BF16)
        qb = sb("qb", [128, 256], BF16)
        kd = sb("kd", [128, 256], BF16)
        vln = sb("vln", [128, 256], BF16)
        lfh = sb("lfh", [128, 256], BF16)
        lfl = sb("lfl", [128, 256], BF16)
        trT = sb("trT", [128, 6, 128], BF16)
        attb = sb("attb", [128, 4, 128], BF16)
        ebl = sb("ebl", [128, 2, 2], F32)
        ss4 = sb("ss4", [128, 8], F32)
        rs4 = sb("rs4", [128, 8], F32)
        bnst = sb("bnst", [128, 6], F32)
        bnag = sb("bnag", [128, 2], F32)
        ytile = [sb(f"ytile{i}", [128, 512], BF16) for i in range(2)]
        vstage = [sb(f"vstage{i}", [128, 256], BF16) for i in range(2)]
        gstage = [sb(f"gstage{i}", [128, 24], F32) for i in range(2)]
        fst_raw = [sb(f"fst_raw{i}", [128, 512], BF16) for i in range(2)]
        fst_rot = [sb(f"fst_rot{i}", [128, 512], BF16) for i in range(2)]
        r1 = [sb(f"r1_{i}", [128, 512], F32) for i in range(2)]
        r2 = [sb(f"r2_{i}", [128, 512], F32) for i in range(2)]

        V = nc.vector
        A = nc.scalar
        nfm = 0
        ck(k, 0)
        for tb in range(S // 512):
            xTc = xTb[tb % 2]
            kx = ("xTb", tb % 2)
            for tt in range(4):
                t = tb * 4 + tt
                i2 = t % 2
                P.dma("sp", xt[i2][:], x_src[t * 128:(t + 1) * 128, :], writes=[("xt", i2)])
                P.op("act", lambda i2=i2: A.copy(out=xb[i2][:], in_=xt[i2][:]), reads=[("xt", i2)], writes=[("xb", i2)])
                for c in range(8):
                    P.op("pe", lambda c=c, i2=i2: nc.tensor.transpose(out=pT[:, c, :], in_=xb[i2][:, c * 128:(c + 1) * 128], identity=k.ident_b[:]),
                         reads=[("xb", i2), "ident_b"], writes=["pT"])
                P.op("dve", lambda tt=tt, xTc=xTc: V.tensor_copy(out=xTc[:, :, tt * 128:(tt + 1) * 128], in_=pT[:]), reads=["pT"], writes=[kx])
            ck(k, 1)
            blk = slice(tb * 512, (tb + 1) * 512)

            def fm(ch, dst, xTc=xTc, kx=kx):
                for c in range(8):
                    P.op("pe", lambda c=c, ch=ch, dst=dst, xTc=xTc: nc.tensor.matmul(dst[:], lhsT=wb[:, c, NCOLA + ch * 128:NCOLA + (ch + 1) * 128], rhs=xTc[:, c, :],
                                                                                     start=(c == 0), stop=(c == 7)), reads=[("wb", c), kx], writes=[("pm", id(dst))])
            plan = [(0, 4, 0, 4), (1, 5, 1, 5), (2, 6, 2, 6), (3, 7, 3, 7), (8, 9, None, 8), (10, 11, None, 9)]
            for (cr, cs, fraw, frot) in plan:
                ck(k, 1.5 + 0.01 * nfm)
                j = nfm % 2
                nfm += 1
                pA, pB = pm[2 * j], pm[2 * j + 1]
                fm(cr, pA)
                ck(k, 1.21)
                fm(cs, pB)
                ck(k, 1.22)
                if k.dbgv == 13:
                    P.op("dve", lambda j=j, pA=pA: V.tensor_tensor(out=r1[j][:], in0=pA[:], in1=k.ropeC[:, blk], op=ALU.mult),
                         reads=[("pm", id(pA))], writes=[("r1", j)])
                if fraw is not None and k.dbgv == 14:
                    P.op("dve", lambda j=j, pA=pA: V.tensor_copy(out=fst_raw[j][:], in_=pA[:]), reads=[("pm", id(pA))], writes=[("fst_raw", j)])
                elif fraw is not None:
                    P.op("act", lambda j=j, pA=pA: A.copy(out=fst_raw[j][:], in_=pA[:]), reads=[("pm", id(pA))] + ([("r1", j)] if k.dbgv == 13 else []), writes=[("fst_raw", j)])
                    ck(k, 1.23)
                    P.dma("sp", k.ft[fraw, :, blk], fst_raw[j][:], reads=[("fst_raw", j)], writes=[("ft", fraw, tb)])
                ck(k, 1.24)
                P.op("act", lambda j=j, pA=pA: A.copy(out=r1[j][:], in_=pA[:]), reads=[("pm", id(pA))], writes=[("r1", j)])
                P.op("act", lambda j=j, pB=pB: A.copy(out=r2[j][:], in_=pB[:]), reads=[("pm", id(pB))], writes=[("r2", j)])
                P.op("dve", lambda j=j, blk=blk: V.tensor_tensor(out=r1[j][:], in0=r1[j][:], in1=k.ropeC[:, blk], op=ALU.mult), reads=[("r1", j)], writes=[("r1", j)])
                P.op("dve", lambda j=j, blk=blk: V.tensor_tensor(out=r2[j][:], in0=r2[j][:], in1=k.ropeS[:, blk], op=ALU.mult), reads=[("r2", j)], writes=[("r2", j)])
                P.op("dve", lambda j=j: V.tensor_tensor(out=fst_rot[j][:], in0=r1[j][:], in1=r2[j][:], op=ALU.add),
                     reads=[("r1", j), ("r2", j)], writes=[("fst_rot", j)])
                ck(k, 1.243)
                P.dma("sp", k.ft[frot, :, blk], fst_rot[j][:], reads=[("fst_rot", j)], writes=[("ft", frot, tb)])
            for (cr, fi) in ((12, 10), (13, 11)):
                j = nfm % 2
                nfm += 1
                pA = pm[2 * j]
                fm(cr, pA)
                P.op("act", lambda j=j, pA=pA: A.copy(out=fst_raw[j][:], in_=pA[:]), reads=[("pm", id(pA))], writes=[("fst_raw", j)])
                P.dma("sp", k.ft[fi, :, blk], fst_raw[j][:], reads=[("fst_raw", j)], writes=[("ft", fi, tb)])

            ck(k, 2)
            for tt in range(4):
                t = tb * 4 + tt
                i2 = t % 2
                rows = slice(t * 128, (t + 1) * 128)
                xs = lambda c, xTc=xTc, tt=tt: xTc[:, c, tt * 128:(tt + 1) * 128]
                colgrp = [(0, 512), (512, 512), (1024, 512), (1536, 280)]
                for gi, (c0, wdt) in enumerate(colgrp):
                    for c in range(8):
                        P.op("pe", lambda c=c, gi=gi, c0=c0, wdt=wdt, xs=xs: nc.tensor.matmul(pm[gi][:, 0:wdt], lhsT=xs(c), rhs=wb[:, c, c0:c0 + wdt],
                                                                                       start=(c == 0), stop=(c == 7)),
                             reads=[("wb", c), kx], writes=[("pm", id(pm[gi]))])
                kp = [("pm", id(pm[gi])) for gi in range(4)]
                P.op("act", lambda: A.activation(out=W["qf"][:], in_=pm[0][:, 0:256], func=AF.Silu), reads=[kp[0]], writes=["qf"])
                P.op("act", lambda: A.activation(out=W["sg"][:], in_=pm[0][:, 256:512], func=AF.Sigmoid), reads=[kp[0]], writes=["sg"])
                P.op("act", lambda: A.activation(out=W["sgate"][:], in_=pm[1][:, 256:512], func=AF.Sigmoid), reads=[kp[1]], writes=["sgate"])
                P.op("act", lambda i2=i2: A.activation(out=gstage[i2][:], in_=pm[3][:, 256:280], func=AF.Sigmoid), reads=[kp[3]], writes=[("gstage", i2)])
                P.op("act", lambda: A.copy(out=vb[:], in_=pm[1][:, 0:256]), reads=[kp[1]], writes=["vb"])
                P.op("act", lambda i2=i2: A.copy(out=vstage[i2][:], in_=pm[3][:, 0:256]), reads=[kp[3]], writes=[("vstage", i2)])
                P.op("act", lambda: A.copy(out=gx[:], in_=pm[2][:]), reads=[kp[2]], writes=["gx"])
                P.dma("sp", k.tmv[rows, :], vstage[i2][:], reads=[("vstage", i2)], writes=[("tmv", t)])
                P.dma("sp", k.gts[rows, :], gstage[i2][:], reads=[("gstage", i2)], writes=[("gts", t)])

                ck(k, 3)
                if l == 0:
                    fsrc = W["sg"]
                    kf = "sg"
                else:
                    P.op("dve", lambda: V.tensor_tensor(out=W["t1"][:], in0=W["sg"][:], in1=oml[:], op=ALU.mult), reads=["sg", "oml"], writes=["t1"])
                    P.op("dve", lambda: V.tensor_tensor(out=W["t1"][:], in0=W["t1"][:], in1=lb[:], op=ALU.add), reads=["t1", "lb"], writes=["t1"])
                    fsrc = W["t1"]
                    kf = "t1"
                P.op("act", lambda fsrc=fsrc: A.activation(out=W["logf"][:], in_=fsrc[:], func=AF.Ln), reads=[kf], writes=["logf"])
                P.op("dve", lambda fsrc=fsrc: V.tensor_scalar(out=W["kk"][:], in0=fsrc[:], scalar1=-1.0, scalar2=1.0, op0=ALU.mult, op1=ALU.add),
                     reads=[kf], writes=["kk"])
                ck(k, 3.5)
                lf = W["logf"]
                P.op("dve", lambda: V.tensor_copy(out=lfh[:], in_=lf[:]), reads=["logf"], writes=["lfh"])
                P.op("dve", lambda: V.tensor_tensor(out=W["t2"][:], in0=lf[:], in1=lfh[:], op=ALU.subtract), reads=["logf", "lfh"], writes=["t2"])
                P.op("dve", lambda: V.tensor_copy(out=lfl[:], in_=W["t2"][:]), reads=["t2"], writes=["lfl"])
                for (dst, mat, kn) in ((pa[:, 0:256], k.mmid_b, "pa"), (pa[:, 256:512], k.mcum_b, "pa"), (pb[:, 0:256], k.mrem_b, "pb")):
                    P.op("pe", lambda dst=dst, mat=mat: nc.tensor.matmul(dst, lhsT=mat[:], rhs=lfh[:], start=True, stop=False), reads=["lfh"], writes=[kn])
                    P.op("pe", lambda dst=dst, mat=mat: nc.tensor.matmul(dst, lhsT=mat[:], rhs=lfl[:], start=False, stop=True), reads=["lfl"], writes=[kn])
                for c2 in range(2):
                    for hh in range(2):
                        for pi, part in enumerate((lfh, lfl)):
                            P.op("pe", lambda c2=c2, hh=hh, part=part, pi=pi: nc.tensor.matmul((pb[:, 256 + hh:257 + hh] if c2 == 0 else pm[2][:, hh:hh + 1]),
                                                                                             lhsT=part[c2 * 64:(c2 + 1) * 64, hh * 128:(hh + 1) * 128],
                                                                                             rhs=k.ones_b[c2 * 64:(c2 + 1) * 64, 0:1], start=(pi == 0), stop=(pi == 1)),
                                 reads=["lfh", "lfl", "ones_b"], writes=(["pb"] if c2 == 0 else [kp[2]]))
                P.op("act", lambda: A.activation(out=W["ebm"][:], in_=pa[:, 0:256], func=AF.Exp), reads=["pa"], writes=["ebm"])
                P.op("act", lambda: A.activation(out=W["enbm"][:], in_=pa[:, 0:256], func=AF.Exp, scale=-1.0), reads=["pa"], writes=["enbm"])
                P.op("act", lambda: A.activation(out=W["eb"][:], in_=pa[:, 256:512], func=AF.Exp), reads=["pa"], writes=["eb"])
                P.op("act", lambda: A.activation(out=W["erem"][:], in_=pb[:, 0:256], func=AF.Exp), reads=["pb"], writes=["erem"])
                P.op("act", lambda: A.activation(out=ebl[:, 0, :], in_=pb[:, 256:258], func=AF.Exp), reads=["pb"], writes=["ebl"])
                P.op("act", lambda: A.activation(out=ebl[:, 1, :], in_=pm[2][:, 0:2], func=AF.Exp), reads=[kp[2]], writes=["ebl"])
                P.op("dve", lambda: V.tensor_tensor(out=qe[:], in0=W["qf"][:], in1=W["ebm"][:], op=ALU.mult), reads=["qf", "ebm"], writes=["qe"])
                P.op("dve", lambda: V.tensor_tensor(out=ke[:], in0=W["kk"][:], in1=W["enbm"][:], op=ALU.mult), reads=["kk", "enbm"], writes=["ke"])
                P.op("dve", lambda: V.tensor_tensor(out=qb[:], in0=W["qf"][:], in1=W["eb"][:], op=ALU.mult), reads=["qf", "eb"], writes=["qb"])
                P.op("dve", lambda: V.tensor_tensor(out=kd[:], in0=W["kk"][:], in1=W["erem"][:], op=ALU.mult), reads=["kk", "erem"], writes=["kd"])
                for i, (src, kn) in enumerate(((qe, "qe"), (ke, "ke"), (qb, "qb"))):
                    for hh in range(2):
                        P.op("pe", lambda i=i, hh=hh, src=src: nc.tensor.transpose(out=pcb[:, (i * 2 + hh) * 128:(i * 2 + hh + 1) * 128],
                                                                                   in_=src[:, hh * 128:(hh + 1) * 128], identity=k.ident_b[:]),
                             reads=[kn, "ident_b"], writes=["pc"])
                P.op("dve", lambda: V.tensor_copy(out=trT[:].rearrange("p a b -> p (a b)"), in_=pcb[:, 0:768]), reads=["pc"], writes=["trT"])

                ck(k, 4)

                def hT(i, h):
                    return trT[(h % 2) * 64:(h % 2) * 64 + 64, i * 2 + h // 2, :]
                for h in range(4):
                    ck(k, 4.01 + 0.01 * h)
                    dst = (pa if h % 2 == 0 else pm[0])[:, (h // 2) * 128:(h // 2 + 1) * 128]
                    P.op("pe", lambda h=h, dst=dst: nc.tensor.matmul(dst, lhsT=hT(1, h), rhs=hT(0, h), start=True, stop=True),
                         reads=["trT"], writes=["pa" if h % 2 == 0 else kp[0]])
                ck(k, 4.1)
                P.op("act", lambda: A.copy(out=gt[:, 0:256], in_=pa[:, 0:256]), reads=["pa"], writes=["gt"])
                P.op("act", lambda: A.copy(out=gt[:, 256:512], in_=pm[0][:, 0:256]), reads=[kp[0]], writes=["gt"])
                ck(k, 4.2)
                for h in range(4):
                    gb = (h % 2) * 2 + h // 2
                    P.op("dve", lambda h=h, gb=gb: V.tensor_tensor(out=attb[:, h, :], in0=gt[:, gb * 128:(gb + 1) * 128], in1=k.mcum[:], op=ALU.mult),
                         reads=["gt", "mcum"], writes=["attb"])
                ck(k, 4.5)
                def obank(h):
                    return ((pc, "pc"), (pm[1], kp[1]), (pm[3], kp[3]), (pm[0], kp[0]))[h]
                for h in range(4):
                    ob, okey = obank(h)
                    P.op("pe", lambda h=h, ob=ob: nc.tensor.matmul(ob[:, 0:64], lhsT=attb[:, h, :], rhs=vb[:, h * 64:(h + 1) * 64],
                                                                   start=True, stop=False), reads=["attb", "vb", "trT"], writes=[okey])
                for c2 in range(2):
                    rs = slice(c2 * 64, (c2 + 1) * 64)
                    for h in range(4):
                        hp = slice((h % 2) * 64, (h % 2) * 64 + 64)
                        ob, okey = obank(h)
                        P.op("pe", lambda h=h, rs=rs, hp=hp, ob=ob: nc.tensor.matmul(ob[rs, 0:64], lhsT=hT(2, h)[:, rs], rhs=st_b[hp, h // 2, :],
                                                                                     start=False, stop=True), reads=["trT", "st_b"], writes=[okey])
                    for hh in range(2):
                        P.op("pe", lambda hh=hh, rs=rs: nc.tensor.matmul(pa[:, hh * 128:(hh + 1) * 128], lhsT=kd[rs, hh * 128:(hh + 1) * 128],
                                                                         rhs=vb[rs, hh * 128:(hh + 1) * 128], start=True, stop=True),
                             reads=["kd", "vb", "attb"], writes=["pa"])
                    P.op("act", lambda: A.copy(out=W["t2"][:], in_=pa[:, 0:256]), reads=["pa"], writes=["t2"])
                    for h in range(4):
                        hp = slice((h % 2) * 64, (h % 2) * 64 + 64)
                        P.op("dve", lambda h=h, c2=c2, hp=hp: V.scalar_tensor_tensor(out=st_f[hp, h // 2, :], in0=st_f[hp, h // 2, :],
                                                                                    scalar=ebl[hp, c2, h // 2:h // 2 + 1],
                                                                                    in1=W["t2"][hp, h * 64:(h + 1) * 64], op0=ALU.mult, op1=ALU.add),
                             reads=["st_f", "ebl", "t2"], writes=["st_f"])
                    P.op("dve", lambda: V.tensor_copy(out=st_b[:], in_=st_f[:]), reads=["st_f"], writes=["st_b"])
                ck(k, 5)
                yt = ytile[i2]
                ky = ("ytile", i2)
                tk = (["t1"] if l > 0 else [])
                for h in range(4):
                    ob, okey = obank(h)
                    P.op("act", lambda h=h, ob=ob: A.copy(out=W["t1"][:, h * 64:(h + 1) * 64], in_=ob[:, 0:64]), reads=[okey] + tk, writes=["osb"] + tk)
                P.op("act", lambda: A.activation(out=W["osq"][:], in_=W["t1"][:], func=AF.Square), reads=["osb"], writes=["osq"])
                P.op("dve", lambda: V.tensor_reduce(out=ss4[:, 0:4], in_=W["osq"][:].rearrange("p (h d) -> p h d", h=4), axis=AX.X, op=ALU.add),
                     reads=["osq"], writes=["ss4"])
                P.op("dve", lambda: V.tensor_scalar(out=rs4[:, 0:4], in0=ss4[:, 0:4], scalar1=1.0 / 64, scalar2=1e-6, op0=ALU.mult, op1=ALU.add),
                     reads=["ss4"], writes=["rs4"])
                P.op("act", lambda: A.activation(out=rs4[:, 0:4], in_=rs4[:, 0:4], func=AF.Ln), reads=["rs4"], writes=["rs4"])
                P.op("act", lambda: A.activation(out=rs4[:, 0:4], in_=rs4[:, 0:4], func=AF.Exp, scale=-0.5), reads=["rs4"], writes=["rs4"])
                P.op("dve", lambda: V.tensor_tensor(out=W["nwg"][:], in0=W["sgate"][:], in1=hg_nw[:], op=ALU.mult), reads=["sgate", "hg_nw"], writes=["nwg"])
                for h in range(4):
                    P.op("dve", lambda h=h, yt=yt: V.scalar_tensor_tensor(out=yt[:, h * 64:(h + 1) * 64], in0=W["t1"][:, h * 64:(h + 1) * 64],
                                                                         scalar=rs4[:, h:h + 1], in1=W["nwg"][:, h * 64:(h + 1) * 64],
                                                                         op0=ALU.mult, op1=ALU.mult), reads=["osb", "rs4", "nwg"], writes=[ky])

                ck(k, 6)
                P.op("dve", lambda: V.tensor_tensor(out=gt[:], in0=gx[:], in1=gx[:], op=ALU.mult), reads=["gx"], writes=["gt"])
                P.op("dve", lambda: V.tensor_scalar(out=gt[:], in0=gt[:], scalar1=0.044715, scalar2=1.0, op0=ALU.mult, op1=ALU.add), reads=["gt"], writes=["gt"])
                P.op("dve", lambda: V.tensor_tensor(out=gt[:], in0=gt[:], in1=gx[:], op=ALU.mult), reads=["gt", "gx"], writes=["gt"])
                P.op("act", lambda: A.activation(out=gt[:], in_=gt[:], func=AF.Sigmoid, scale=1.5957691216057308), reads=["gt"], writes=["gt"])
                P.op("dve", lambda: V.tensor_tensor(out=gg[:], in0=gt[:], in1=gx[:], op=ALU.mult), reads=["gt", "gx"], writes=["gg"])
                P.op("dve", lambda: V.bn_stats(out=bnst[:], in_=gg[:, 256:512]), reads=["gg"], writes=["bnst"])
                P.op("dve", lambda: V.bn_aggr(out=bnag[:], in_=bnst[:]), reads=["bnst"], writes=["bnag"])
                P.op("dve", lambda: V.tensor_scalar(out=rs4[:, 4:5], in0=bnag[:, 1:2], scalar1=1e-5, scalar2=None, op0=ALU.add),
                     reads=["bnag"], writes=["rs4b"])
                P.op("act", lambda: A.activation(out=rs4[:, 4:5], in_=rs4[:, 4:5], func=AF.Ln), reads=["rs4b"], writes=["rs4b"])
                P.op("act", lambda: A.activation(out=rs4[:, 4:5], in_=rs4[:, 4:5], func=AF.Exp, scale=-0.5), reads=["rs4b"], writes=["rs4b"])
                P.op("dve", lambda: V.tensor_scalar(out=W["ginn"][:], in0=gg[:, 256:512], scalar1=bnag[:, 0:1], scalar2=rs4[:, 4:5],
                                                    op0=ALU.subtract, op1=ALU.mult), reads=["gg", "bnag", "rs4b"], writes=["ginn"])
                P.op("dve", lambda: V.tensor_tensor(out=W["ginn"][:], in0=W["ginn"][:], in1=gm_lnw[:], op=ALU.mult), reads=["ginn", "gm_lnw"], writes=["ginn"])
                P.op("dve", lambda: V.tensor_tensor(out=vln[:], in0=W["ginn"][:], in1=gm_lnb[:], op=ALU.add), reads=["ginn", "gm_lnb"], writes=["vln"])
                for g in range(4):
                    P.op("pe", lambda g=g: nc.tensor.matmul(pb[:, 256 + g * 64:256 + (g + 1) * 64], lhsT=wsT[:, g, :], rhs=vln[:, g * 64:(g + 1) * 64],
                                                            start=True, stop=True), reads=["wsT", "vln"], writes=["pb"])
                P.op("act", lambda: A.copy(out=W["ginn"][:], in_=pb[:, 256:512]), reads=["pb", "ginn"], writes=["ginn"])
                for g in range(4):
                    P.op("dve", lambda g=g: V.scalar_tensor_tensor(out=W["gsv"][:, g * 64:(g + 1) * 64], in0=W["ginn"][:, g * 64:(g + 1) * 64],
                                                                  scalar=bsT[:, g:g + 1], in1=gg[:, g * 64:(g + 1) * 64], op0=ALU.add, op1=ALU.mult),
                         reads=["ginn", "bsT", "gg"], writes=["gsv"])
                P.op("act", lambda: A.activation(out=W["gsq"][:], in_=W["gsv"][:], func=AF.Square), reads=["gsv"], writes=["gsq"])
                P.op("dve", lambda: V.tensor_reduce(out=ss4[:, 4:8], in_=W["gsq"][:].rearrange("p (h d) -> p h d", h=4), axis=AX.X, op=ALU.add),
                     reads=["gsq"], writes=["ss4b"])
                P.op("dve", lambda: V.tensor_scalar(out=ss4[:, 4:8], in0=ss4[:, 4:8], scalar1=1.0 / 64, scalar2=1e-6, op0=ALU.mult, op1=ALU.add),
                     reads=["ss4b"], writes=["ss4b"])
                P.op("act", lambda: A.activation(out=ss4[:, 4:8], in_=ss4[:, 4:8], func=AF.Ln), reads=["ss4b"], writes=["ss4b"])
                P.op("act", lambda: A.activation(out=ss4[:, 4:8], in_=ss4[:, 4:8], func=AF.Exp, scale=-0.5), reads=["ss4b"], writes=["ss4b"])
                for g in range(4):
                    P.op("dve", lambda g=g, yt=yt: V.scalar_tensor_tensor(out=yt[:, 256 + g * 64:256 + (g + 1) * 64], in0=W["gsv"][:, g * 64:(g + 1) * 64],
                                                                         scalar=ss4[:, 4 + g:5 + g], in1=gm_nw[:, g * 64:(g + 1) * 64],
                                                                         op0=ALU.mult, op1=ALU.mult), reads=["gsv", "ss4b", "gm_nw"], writes=[ky])
                P.dma("sp", k.y[rows, 0:512], yt[:], reads=[ky], writes=[("y", t)])
                if t == 0:
                    dump(k, "gg", gg[:], "gg", 512)
                    dump(k, "bnag", bnag[:], "bnag", 2)
                    dump(k, "rs4", rs4[:], "rs4b", 8)
                    dump(k, "vln", vln[:], "vln", 256)
                    dump(k, "gsv", W["gsv"][:], "gsv", 256)
                    dump(k, "ss4", ss4[:], "ss4b", 8)
                    dump(k, "wsT", wsT[:].rearrange("p a b -> p (a b)"), "wsT", 512)
                    dump(k, "logf", W["logf"][:], "logf", 256)
                    dump(k, "kk", W["kk"][:], "kk", 256)
                    dump(k, "qf", W["qf"][:], "qf", 256)
                    dump(k, "ebm", W["ebm"][:], "ebm", 256)
                    dump(k, "eb", W["eb"][:], "eb", 256)
                    dump(k, "erem", W["erem"][:], "erem", 256)
                    dump(k, "ebl", ebl[:].rearrange("p a b -> p (a b)"), "ebl", 4)
                    dump(k, "osb", W["t1"][:], "osb", 256)
                    dump(k, "attb", attb[:].rearrange("p a b -> p (a b)"), "attb", 512)
                    dump(k, "st_f", st_f[:].rearrange("p a b -> p (a b)"), "st_f", 128)
                    dump(k, "rs4h", rs4[:], "rs4", 8)
                    dump(k, "nwg", W["nwg"][:], "nwg", 256)
                ck(k, 7)


NSA_BIG = 30000.0


def phase2(k, l):
    nc, P = k.nc, k.P
    V, A, G = nc.vector, nc.scalar, nc.gpsimd
    with ExitStack() as es:
        sb = lambda name, shape, dt: es.enter_context(nc.sbuf_tensor(f"p2l{l}_{name}", shape, dt))
        ps = lambda name, shape, dt: es.enter_context(nc.psum_tensor(f"p2l{l}_{name}", shape, dt))
        edup = sb("edup", [128, 32, 128], BF16)
        win01 = sb("win01", [128, 8, 512], BF16)
        cmp01 = sb("cmp01", [128, 5, 512], BF16)
        impkeep = sb("impkeep", [128, NT, 64], F32)
        impadd = sb("impadd", [128, NT, 64], F32)
        ovext = sb("ovext", [128, 2, 65], BF16)
        wpad = sb("wpad", [128, NT], F32)
        gts = sb("gts", [128, NT, 24], F32)
        nw = sb("nw", [128, 512], F32)
        w1 = [sb(f"w1_{i}", [64, 32, 128], BF16) for i in range(2)]
        w2kd = sb("w2kd", [128, 128], BF16)
        w2v = sb("w2v", [128, 64], BF16)
        peT = sb("peT", [64, 2, 32], BF16)
        craw = sb("craw", [64, S], BF16)
        cb = sb("cb", [128, 1], F32)
        hx = sb("hx", [128, 256], F32)
        ht = sb("ht", [128, 256], F32)
        hb = sb("hb", [128, 256], BF16)
        kcd = sb("kcd", [128, 256], BF16)
        vc = sb("vc", [128, 2, 65], BF16)
        qr = [sb(f"qr{i}", [128, S], BF16) for i in range(2)]
        qo = [sb(f"qo{i}", [128, S], BF16) for i in range(2)]
        ksd = sb("ksd", [128, S], BF16)
        kwd = sb("kwd", [128, S], BF16)
        vsg = sb("vsg", [128, NT, 65], BF16)
        vwg = sb("vwg", [128, NT, 65], BF16)
        ec = [[[sb(f"ec{ci}{par}{ct}", [128, 512], BF16) for ct in range(2)] for par in range(2)] for ci in range(2)]
        pt = [[sb(f"pt{par}{i}", [128, 512], BF16) for i in range(2)] for par in range(2)]
        osb = [sb(f"osb{par}", [65, 512], F32) for par in range(2)]
        obs = [sb(f"ob{i}", [128, 3, 4, 4, 65], F32) for i in range(2)]
        impsb = sb("impsb", [128, 4, 4, 64], F32)
        impw = sb("impw", [128, 4, 64], F32)
        imp = sb("imp", [128, 64], F32)
        imp3 = sb("imp3", [128, 64], F32)
        m8a = sb("m8a", [128, 8], F32)
        m8b = sb("m8b", [128, 8], F32)
        rdn = sb("rdn", [128, 4], F32)
        mdup = sb("mdup", [128, 4, 128], BF16)
        MT = sb("MT", [128, 512], BF16)
        den3 = sb("den3", [128, 3, 4], F32)
        coef = sb("coef", [128, 3, 4], F32)
        acc = sb("acc", [128, 4, 64], F32)
        tm2 = sb("tm2", [128, 4, 64], F32)
        ss = sb("ss", [128, 4], F32)
        yst = [sb(f"yst{i}", [128, 256], BF16) for i in range(2)]
        Sb = [[ps(f"S{par}{i}", [128, 512], F32) for i in range(2)] for par in range(2)]
        Ob = [ps(f"O{par}", [128, 512], F32) for par in range(2)]
        X = ps("X", [128, 512], F32)
        Y = ps("Y", [128, 512], F32)
        Xb = X[:].bitcast(BF16)

        flat = lambda t: t[:].rearrange("p a b -> p (a b)")

        def cast_load(dst_flat, src_flat, n, key):
            for o in range(0, n, 2048):
                w = min(2048, n - o)
                P.dma("pool", dst_flat[:, o:o + w], src_flat[:, o:o + w], writes=[key])
        cast_load(flat(edup), k.cst["edup"].rearrange("p a b -> p (a b)"), 32 * 128, "edup")
        cast_load(flat(win01), k.cst["win01"].rearrange("p a b -> p (a b)"), 8 * 512, "win01")
        cast_load(flat(cmp01), k.cst["cmp01"].rearrange("p a b -> p (a b)"), 5 * 512, "cmp01")
        cast_load(flat(ovext), k.cst["ovext"].rearrange("p a b -> p (a b)"), 130, "ovext")
        P.op("dve", lambda: V.tensor_scalar(out=flat(win01), in0=flat(win01), scalar1=-1.0, scalar2=NSA_BIG, op0=ALU.add, op1=ALU.mult), reads=["win01"], writes=["win01"])
        P.op("dve", lambda: V.tensor_scalar(out=flat(cmp01), in0=flat(cmp01), scalar1=-1.0, scalar2=NSA_BIG, op0=ALU.add, op1=ALU.mult), reads=["cmp01"], writes=["cmp01"])
        P.dma("sp", impkeep[:], k.cst["impkeep"], writes=["impkeep"])
        P.dma("sp", impadd[:], k.cst["impadd"], writes=["impadd"])
        P.dma("sp", wpad[:], k.cst["wpad"], writes=["wpad"])
        P.dma("sp", gts[:], k.gts.rearrange("(t p) c -> p t c", p=128), writes=["gts"])
        bcast_row(k, P, "sp", nw[:], k.nsa_nw[l:l + 1, :], "nw")
        for kv in range(2):
            wv = k.nsa_w1[l, kv].rearrange("(j d) m -> d j m", d=64)
            for hh in range(2):
                P.dma("pool", w1[kv][:, hh * 16:(hh + 1) * 16, :], wv[:, hh * 16:(hh + 1) * 16, :], writes=[("w1", kv)])
        P.dma("pool", w2kd[:, 0:64], k.nsa_w2[l, 0], writes=["w2kd"])
        P.dma("pool", w2kd[:, 64:128], k.nsa_w2[l, 0], writes=["w2kd"])
        P.dma("pool", w2v[:], k.nsa_w2[l, 1], writes=["w2v"])
        P.dma("pool", peT[:], k.nsa_pe[l].rearrange("k j d -> d k j"), writes=["peT"], allow_slow_non_contiguous=True)

        def evac_O(par, br, hl, ob, okey):
            P.op("act", lambda par=par: A.copy(out=osb[par][:], in_=Ob[par][0:65, :]), reads=[("O", par)], writes=[("osb", par)])
            for qt in range(4):
                P.op("pe", lambda par=par, qt=qt: nc.tensor.transpose(out=X[:, qt * 65:(qt + 1) * 65], in_=osb[par][:, qt * 128:(qt + 1) * 128], identity=k.ident_f[0:65, 0:65]),
                     reads=[("osb", par), "ident"], writes=["X"])
            P.op("act", lambda br=br, hl=hl, ob=ob: A.copy(out=ob[:, br, :, hl, :], in_=X[:, 0:260].rearrange("p (a b) -> p a b", a=4)), reads=["X"], writes=[(okey, br)])

        for g in range(2):
            for kv in range(2):
                P.dma("sp", craw[:], k.ft[10 + kv, g * 64:(g + 1) * 64, :], writes=["craw"])
                for j in range(32):
                    P.op("pe", lambda j=j, kv=kv: nc.tensor.matmul(X[:, 300:301], lhsT=w1[kv][:, j, :], rhs=peT[:, kv, j:j + 1], start=(j == 0), stop=(j == 31)),
                         reads=[("w1", kv), "peT"], writes=["X"])
                for j in range(32):
                    P.op("pe", lambda j=j, kv=kv: nc.tensor.matmul(X[:, 0:255], lhsT=w1[kv][:, j, :], rhs=craw[:, j:j + 16 * 254 + 1:16], start=(j == 0), stop=(j == 31)),
                         reads=[("w1", kv), "craw"], writes=["X"])
                P.op("act", lambda: A.copy(out=cb[:], in_=X[:, 300:301]), reads=["X"], writes=["cb"])
                P.op("dve", lambda: V.memset(hx[:], 0.0), writes=["hx"])
                P.op("act", lambda: A.activation(out=hx[:, 0:255], in_=X[:, 0:255], func=AF.Identity, bias=cb[:, 0:1], scale=1.0), reads=["X", "cb", "hx"], writes=["hx"])
                P.op("dve", lambda: V.tensor_tensor(out=ht[:], in0=hx[:], in1=hx[:], op=ALU.mult), reads=["hx"], writes=["ht"])
                P.op("dve", lambda: V.tensor_scalar(out=ht[:], in0=ht[:], scalar1=0.044715, scalar2=1.0, op0=ALU.mult, op1=ALU.add), reads=["ht"], writes=["ht"])
                P.op("dve", lambda: V.tensor_tensor(out=ht[:], in0=ht[:], in1=hx[:], op=ALU.mult), reads=["ht", "hx"], writes=["ht"])
                P.op("act", lambda: A.activation(out=ht[:], in_=ht[:], func=AF.Sigmoid, scale=1.5957691216057308), reads=["ht"], writes=["ht"])
                P.op("dve", lambda: V.tensor_tensor(out=hb[:], in0=ht[:], in1=hx[:], op=ALU.mult), reads=["ht", "hx"], writes=["hb"])
                if kv == 0:
                    P.op("pe", lambda: nc.tensor.matmul(X[:, 0:256], lhsT=w2kd[:], rhs=hb[:], start=True, stop=True), reads=["w2kd", "hb"], writes=["X"])
                    P.op("act", lambda: A.copy(out=kcd[:], in_=X[:, 0:256]), reads=["X"], writes=["kcd"])
                else:
                    for ct in range(2):
                        P.op("pe", lambda ct=ct: nc.tensor.matmul(X[:, ct * 64:(ct + 1) * 64], lhsT=hb[:, ct * 128:(ct + 1) * 128], rhs=w2v[:], start=True, stop=True),
                             reads=["w2v", "hb"], writes=["X"])
                    P.op("dve", lambda: V.memset(vc[:], 1.0), writes=["vc"])
                    P.op("act", lambda: A.copy(out=vc[:, :, 0:64], in_=X[:, 0:128].rearrange("p (a b) -> p a b", a=2)), reads=["X", "vc"], writes=["vc"])
            for ci in range(2):
                P.dma("sp", qr[ci][:], k.ft[2 * g + ci], writes=[("qr", ci)])
                P.dma("act", qo[ci][:], k.ft[4 + 2 * g + ci], writes=[("qo", ci)])
            for hh in range(2):
                P.dma("sp", ksd[hh * 64:(hh + 1) * 64, :], k.ft[8, g * 64:(g + 1) * 64, :], writes=["ksd"])
                P.dma("act", kwd[hh * 64:(hh + 1) * 64, :], k.ft[9, g * 64:(g + 1) * 64, :], writes=["kwd"])
            P.op("dve", lambda: V.memset(vsg[:], 1.0), writes=["vsg"])
            P.op("dve", lambda: V.memset(vwg[:], 1.0), writes=["vwg"])
            P.dma("sp", vsg[:, :, 0:64], k.tmv[:, g * 64:(g + 1) * 64].rearrange("(t p) c -> p t c", p=128), reads=["vsg"], writes=["vsg"])
            P.dma("act", vwg[:, :, 0:64], k.tmv[:, 128 + g * 64:128 + (g + 1) * 64].rearrange("(t p) c -> p t c", p=128), reads=["vwg"], writes=["vwg"])

            for Q in range(S // 512):
                qs = slice(Q * 512, (Q + 1) * 512)
                ob = obs[Q % 2]
                okey = "ob%d" % (Q % 2)
                cts = []
                for ct in range(2):
                    u = 512 * Q - 2048 * ct
                    if u < -480:
                        continue
                    cts.append((ct, (None if u > 2048 else (0, 512, 1024, 1536, 2048).index(u))))
                for ci in range(2):
                    for n_, (ct, mi) in enumerate(cts):
                        for par in range(2):
                            hp = slice(par * 64, (par + 1) * 64)
                            P.op("pe", lambda par=par, hp=hp, ct=ct, ci=ci, qs=qs, mi=mi: nc.tensor.matmul(Sb[par][0][:], lhsT=kcd[hp, ct * 128:(ct + 1) * 128], rhs=qr[ci][hp, qs], start=True, stop=(mi is None)),
                                 reads=["kcd", ("qr", ci)], writes=[("S", par, 0)])
                        if mi is not None:
                            for par in range(2):
                                P.op("pe", lambda par=par, mi=mi: nc.tensor.matmul(Sb[par][0][:], lhsT=k.ident_b[:], rhs=cmp01[:, mi, :], start=False, stop=True),
                                     reads=["cmp01", "ident_b"], writes=[("S", par, 0)])
                        for par in range(2):
                            e_ = ec[ci][par][ct]
                            ke = ("ec", ci, par, ct)
                            P.op("act", lambda par=par, e_=e_: A.activation(out=e_[:], in_=Sb[par][0][:], func=AF.Exp, scale=0.125), reads=[("S", par, 0)], writes=[ke])
                            P.op("pe", lambda par=par, ct=ct, e_=e_, n_=n_, nk=len(cts): nc.tensor.matmul(Ob[par][0:65, :], lhsT=vc[:, ct, :], rhs=e_[:], start=(n_ == 0), stop=(n_ == nk - 1)),
                                 reads=["vc", ke], writes=[("O", par)])
                    for par in range(2):
                        evac_O(par, 0, 2 * ci + par, ob, okey)
                for half in range(2):
                    for q2 in range(2):
                        qt = half * 2 + q2
                        for hl in range(4):
                            ci, par = hl // 2, hl % 2
                            for n_, (ct, mi) in enumerate(cts):
                                e_ = ec[ci][par][ct]
                                P.op("pe", lambda e_=e_, ct=ct, qt=qt, q2=q2, hl=hl, n_=n_, nk=len(cts): nc.tensor.matmul(Y[:, q2 * 256 + hl * 64:q2 * 256 + (hl + 1) * 64],
                                                                                                                        lhsT=e_[:, qt * 128:(qt + 1) * 128], rhs=ovext[:, ct, 0:64],
                                                                                                                        start=(n_ == 0), stop=(n_ == nk - 1)),
                                     reads=[("ec", ci, par, ct), "ovext"], writes=["Y"])
                    P.op("act", lambda half=half: A.copy(out=impsb[:, half * 2:(half + 1) * 2, :, :].rearrange("p a b c -> p (a b c)"), in_=Y[:]), reads=["Y"], writes=["impsb"])
                for qt in range(4):
                    t = Q * 4 + qt
                    P.op("dve", lambda qt=qt, ob=ob: V.tensor_scalar(out=rdn[:], in0=ob[:, 0, qt, :, 64], scalar1=1e-30, scalar2=None, op0=ALU.max), reads=[(okey, 0)], writes=["rdn"])
                    P.op("dve", lambda: V.reciprocal(out=rdn[:], in_=rdn[:]), reads=["rdn"], writes=["rdn"])
                    P.op("dve", lambda qt=qt: V.tensor_tensor(out=impw[:], in0=impsb[:, qt, :, :], in1=rdn[:].unsqueeze(2).to_broadcast([128, 4, 64]), op=ALU.mult),
                         reads=["impsb", "rdn"], writes=["impw"])
                    P.op("dve", lambda: V.tensor_reduce(out=imp[:], in_=impw[:].rearrange("p h j -> p j h"), axis=AX.X, op=ALU.add), reads=["impw"], writes=["imp"])
                    P.op("dve", lambda t=t: V.tensor_tensor(out=imp[:], in0=imp[:], in1=impkeep[:, t, :], op=ALU.mult), reads=["imp", "impkeep"], writes=["imp"])
                    P.op("dve", lambda t=t: V.tensor_tensor(out=imp[:], in0=imp[:], in1=impadd[:, t, :], op=ALU.add), reads=["imp", "impadd"], writes=["imp"])
                    P.op("dve", lambda: V.max(out=m8a[:], in_=imp[:]), reads=["imp"], writes=["m8a"])
                    P.op("dve", lambda: V.match_replace(out=imp3[:], in_to_replace=m8a[:], in_values=imp[:], imm_value=-3.0e9), reads=["imp", "m8a"], writes=["imp3"])
                    P.op("dve", lambda: V.max(out=m8b[:], in_=imp3[:]), reads=["imp3"], writes=["m8b"])
                    P.op("dve", lambda: V.tensor_scalar(out=imp3[:], in0=imp[:], scalar1=m8b[:, 7:8], scalar2=None, op0=ALU.is_ge), reads=["imp", "m8b", "imp3"], writes=["imp3"])
                    for dd in range(2):
                        P.op("dve", lambda qt=qt, dd=dd: V.tensor_scalar(out=mdup[:, qt, dd * 64:(dd + 1) * 64], in0=imp3[:], scalar1=-1.0, scalar2=NSA_BIG, op0=ALU.add, op1=ALU.mult),
                             reads=["imp3"], writes=["mdup"])

                def attend(br, kd_, kkey, vt, vkey):
                    kt0 = 0 if br == 1 else max(0, 4 * Q - 4)
                    kts = list(range(kt0, 4 * Q + 4))
                    for ci in range(2):
                        def scores(n_, ci=ci):
                            kt = kts[n_]
                            bi = n_ % 2
                            dl = kt - 4 * Q
                            masked = (br == 2 or dl >= 0)
                            for par in range(2):
                                hp = slice(par * 64, (par + 1) * 64)
                                P.op("pe", lambda par=par, hp=hp, kt=kt, ci=ci, bi=bi, qs=qs, kd_=kd_, last=(br == 2 and not masked): nc.tensor.matmul(Sb[par][bi][:], lhsT=kd_[hp, kt * 128:(kt + 1) * 128], rhs=qo[ci][hp, qs],
                                                                                                                              start=True, stop=last),
                                     reads=[kkey, ("qo", ci)], writes=[("S", par, bi)])
                            if br == 1:
                                for par in range(2):
                                    hp = slice(par * 64, (par + 1) * 64)
                                    P.op("pe", lambda par=par, hp=hp, kt=kt, bi=bi, masked=masked: nc.tensor.matmul(Sb[par][bi][:], lhsT=edup[hp, kt, :], rhs=MT[hp, :], start=False, stop=(not masked)),
                                         reads=["edup", "MT"], writes=[("S", par, bi)])
                            if masked:
                                for par in range(2):
                                    P.op("pe", lambda par=par, bi=bi, dl=dl: nc.tensor.matmul(Sb[par][bi][:], lhsT=k.ident_b[:], rhs=win01[:, dl + 4, :], start=False, stop=True),
                                         reads=["win01", "ident_b"], writes=[("S", par, bi)])
                        scores(0)
                        for n_, kt in enumerate(kts):
                            bi = n_ % 2
                            if n_ + 1 < len(kts):
                                scores(n_ + 1)
                            for par in range(2):
                                p_ = pt[par][bi]
                                kp_ = ("pt", par, bi)
                                P.op("act", lambda par=par, bi=bi, p_=p_: A.activation(out=p_[:], in_=Sb[par][bi][:], func=AF.Exp, scale=0.125), reads=[("S", par, bi)], writes=[kp_])
                                P.op("pe", lambda par=par, kt=kt, p_=p_, n_=n_, vt=vt, nk=len(kts): nc.tensor.matmul(Ob[par][0:65, :], lhsT=vt[:, kt, :], rhs=p_[:], start=(n_ == 0), stop=(n_ == nk - 1)),
                                     reads=[vkey, kp_], writes=[("O", par)])
                        for par in range(2):
                            evac_O(par, br, 2 * ci + par, ob, okey)
                attend(2, kwd, "kwd", vwg, "vwg")
                for qt in range(4):
                    P.op("pe", lambda qt=qt: nc.tensor.transpose(out=Xb[:, qt * 128:(qt + 1) * 128], in_=mdup[:, qt, :], identity=k.ident_b[:]), reads=["mdup", "ident_b"], writes=["X"])
                P.op("dve", lambda: V.tensor_copy(out=MT[:], in_=Xb[:, 0:512]), reads=["X"], writes=["MT"])
                attend(1, ksd, "ksd", vsg, "vsg")
                for qt in range(4):
                    t = Q * 4 + qt
                    i2 = qt % 2
                    P.op("dve", lambda qt=qt, ob=ob: V.tensor_scalar(out=den3[:], in0=ob[:, :, qt, :, 64], scalar1=1e-30, scalar2=None, op0=ALU.max), reads=[(okey, 0), (okey, 1), (okey, 2)], writes=["den3"])
                    if Q == 0:
                        P.op("dve", lambda t=t: V.tensor_scalar(out=den3[:, 2, :], in0=den3[:, 2, :], scalar1=wpad[:, t:t + 1], scalar2=None, op0=ALU.add),
                             reads=["den3", "wpad"], writes=["den3"])
                    P.op("dve", lambda: V.reciprocal(out=den3[:], in_=den3[:]), reads=["den3"], writes=["den3"])
                    P.op("dve", lambda t=t, g=g: V.tensor_tensor(out=coef[:], in0=den3[:], in1=gts[:, t, g * 12:(g + 1) * 12].rearrange("p (h b) -> p b h", b=3), op=ALU.mult),
                         reads=["den3", "gts"], writes=["coef"])
                    for br in range(3):
                        dst = acc if br == 0 else tm2
                        P.op("dve", lambda br=br, qt=qt, dst=dst, ob=ob: V.tensor_tensor(out=dst[:], in0=ob[:, br, qt, :, 0:64], in1=coef[:, br, :].unsqueeze(2).to_broadcast([128, 4, 64]), op=ALU.mult),
                             reads=[(okey, br), "coef"], writes=["acc" if br == 0 else "tm2"])
                        if br > 0:
                            P.op("dve", lambda: V.tensor_tensor(out=acc[:], in0=acc[:], in1=tm2[:], op=ALU.add), reads=["acc", "tm2"], writes=["acc"])
                    P.op("act", lambda: A.activation(out=tm2[:], in_=acc[:], func=AF.Square), reads=["acc", "tm2"], writes=["tm2"])
                    P.op("dve", lambda: V.tensor_reduce(out=ss[:], in_=tm2[:], axis=AX.X, op=ALU.add), reads=["tm2"], writes=["ss"])
                    P.op("dve", lambda: V.tensor_scalar(out=ss[:], in0=ss[:], scalar1=1.0 / 64, scalar2=1e-6, op0=ALU.mult, op1=ALU.add), reads=["ss"], writes=["ss"])
                    P.op("act", lambda: A.activation(out=ss[:], in_=ss[:], func=AF.Sqrt), reads=["ss"], writes=["ss"])
                    P.op("dve", lambda: V.reciprocal(out=ss[:], in_=ss[:]), reads=["ss"], writes=["ss"])
                    P.op("dve", lambda: V.tensor_tensor(out=acc[:], in0=acc[:], in1=ss[:].unsqueeze(2).to_broadcast([128, 4, 64]), op=ALU.mult), reads=["acc", "ss"], writes=["acc"])
                    P.op("dve", lambda g=g, i2=i2: V.tensor_tensor(out=yst[i2][:], in0=acc[:].rearrange("p h d -> p (h d)"), in1=nw[:, g * 256:(g + 1) * 256], op=ALU.mult),
                         reads=["acc", "nw"], writes=[("yst", i2)])
                    P.dma("sp", k.y[t * 128:(t + 1) * 128, 512 + g * 256:512 + (g + 1) * 256], yst[i2][:], reads=[("yst", i2)], writes=[("ynsa", t, g)])
        P.barrier()


TRASH = NE * CAP


def layer_norm_tile(k, P, src, dst, lnw, lnb, tmp, key_src, key_dst, sfx):
    nc = k.nc
    V, A = nc.vector, nc.scalar
    st, ag, rs = k.ln_st, k.ln_ag, k.ln_rs
    for hlf in range(2):
        P.op("dve", lambda hlf=hlf: V.bn_stats(out=st[:, hlf * 6:(hlf + 1) * 6], in_=src[:, hlf * 512:(hlf + 1) * 512]), reads=[key_src], writes=["ln_st"])
    P.op("dve", lambda: V.bn_aggr(out=ag[:], in_=st[:]), reads=["ln_st"], writes=["ln_ag"])
    P.op("dve", lambda: V.tensor_scalar(out=rs[:], in0=ag[:, 1:2], scalar1=1e-5, scalar2=None, op0=ALU.add), reads=["ln_ag"], writes=["ln_rs"])
    P.op("act", lambda: A.activation(out=rs[:], in_=rs[:], func=AF.Sqrt), reads=["ln_rs"], writes=["ln_rs"])
    P.op("dve", lambda: V.reciprocal(out=rs[:], in_=rs[:]), reads=["ln_rs"], writes=["ln_rs"])
    P.op("dve", lambda: V.tensor_scalar(out=tmp[:], in0=src[:], scalar1=ag[:, 0:1], scalar2=rs[:, 0:1], op0=ALU.subtract, op1=ALU.mult),
         reads=[key_src, "ln_ag", "ln_rs"], writes=["ln_tmp" + sfx])
    P.op("dve", lambda: V.tensor_tensor(out=tmp[:], in0=tmp[:], in1=lnw[:], op=ALU.mult), reads=["ln_tmp" + sfx, "lnw" + sfx], writes=["ln_tmp" + sfx])
    P.op("dve", lambda: V.tensor_tensor(out=dst[:], in0=tmp[:], in1=lnb[:], op=ALU.add), reads=["ln_tmp" + sfx, "lnb" + sfx], writes=[key_dst])


def phase3(k, l):
    nc, P = k.nc, k.P
    V, A = nc.vector, nc.scalar
    with ExitStack() as es:
        sb = lambda name, shape, dt: es.enter_context(nc.sbuf_tensor(f"p3l{l}_{name}", shape, dt))
        ps = lambda name, shape, dt: es.enter_context(nc.psum_tensor(f"p3l{l}_{name}", shape, dt))
        wo = sb("wo", [128, 8, D], BF16)
        rw = sb("rw", [128, 8, NE], F32)
        rb = sb("rb", [128, NE], F32)
        lnw = sb("lnw", [128, D], F32)
        lnb = sb("lnb", [128, D], F32)
        k.ln_st = sb("ln_st", [128, 12], F32)
        k.ln_ag = sb("ln_ag", [128, 2], F32)
        k.ln_rs = sb("ln_rs", [128, 1], F32)
        ytl = [sb(f"ytl{i}", [128, D], BF16) for i in range(2)]
        yT = sb("yT", [128, 8, 128], BF16)
        xt = [sb(f"xt{i}", [128, D], F32) for i in range(2)]
        mix = sb("mix", [128, D], F32)
        rr = sb("rr", [128, D], F32)
        tmp = sb("tmp", [128, D], F32)
        x1 = [sb(f"x1_{i}", [128, D], F32) for i in range(2)]
        x1b = [sb(f"x1b{i}", [128, D], BF16) for i in range(2)]
        x1T = sb("x1T", [128, 8, 128], F32)
        lg = sb("lg", [128, NE], F32)
        m8 = sb("m8", [128, 8], F32)
        nm = sb("nm", [128, 1], F32)
        msk = sb("msk", [128, NE], F32)
        mskb = sb("mskb", [128, NE], BF16)
        ex = sb("ex", [128, NE], F32)
        ssum = sb("ssum", [128, 1], F32)
        wts = sb("wts", [128, NE], F32)
        posf = sb("posf", [128, NE], F32)
        carry = sb("carry", [128, NE], F32)
        val = sb("val", [128, NE], F32)
        nsel = sb("nsel", [128, NE], F32)
        t8 = sb("t8", [128, 8], F32)
        oh = sb("oh", [128, NE], F32)
        idxf = sb("idxf", [128, 4], F32)
        pT = ps("pT", [128, 8, 128], BF16)
        pm = [ps(f"pm{i}", [128, 512], F32) for i in range(2)]
        pX = [ps(f"pX{i}", [128, 512], F32) for i in range(2)]
        pr = ps("pr", [128, 512], F32)

        x_src = k.x if l == 0 else k.x2
        wov = k.w_out[l].rearrange("(c p) n -> p c n", p=128)
        for c in range(8):
            P.dma("pool", wo[:, c, :], wov[:, c, :], writes=[("wo", c)])
        P.dma("sp", rw[:], k.router_w[l].rearrange("(c p) n -> p c n", p=128), writes=["rw"])
        bcast_row(k, P, "sp", rb[:], k.router_b[l:l + 1, :], "rb")
        bcast_row(k, P, "sp", lnw[:], k.ln1_w[l:l + 1, :], "lnw1")
        bcast_row(k, P, "sp", lnb[:], k.ln1_b[l:l + 1, :], "lnb1")
        P.op("dve", lambda: V.memset(carry[:], 0.0), writes=["carry"])
        if k.dbg and k.inject_nsa:
            P.dma("sp", k.y[:, 512:1024], k.ynsa_in[l], writes=["yinj"])
            P.barrier()

        for t in range(NT):
            i2 = t % 2
            rows = slice(t * 128, (t + 1) * 128)
            P.dma("sp", ytl[i2][:], k.y[rows, :], writes=[("ytl", i2)])
            P.dma("act", xt[i2][:], x_src[rows, :], writes=[("xt", i2)])
            for c in range(8):
                P.op("pe", lambda c=c, i2=i2: nc.tensor.transpose(out=pT[:, c, :], in_=ytl[i2][:, c * 128:(c + 1) * 128], identity=k.ident_b[:]),
                     reads=[("ytl", i2), "ident_b"], writes=["pT"])
            P.op("dve", lambda: V.tensor_copy(out=yT[:], in_=pT[:]), reads=["pT"], writes=["yT"])
            for hf in range(2):
                for c in range(8):
                    P.op("pe", lambda c=c, hf=hf: nc.tensor.matmul(pm[hf][:], lhsT=yT[:, c, :], rhs=wo[:, c, hf * 512:(hf + 1) * 512], start=(c == 0), stop=(c == 7)),
                         reads=["yT", ("wo", c)], writes=[("pm", hf)])
                P.op("act", lambda hf=hf: A.copy(out=mix[:, hf * 512:(hf + 1) * 512], in_=pm[hf][:]), reads=[("pm", hf)], writes=["mix"])
            P.op("dve", lambda i2=i2: V.scalar_tensor_tensor(out=rr[:], in0=xt[i2][:], scalar=ALPHA, in1=mix[:], op0=ALU.mult, op1=ALU.add),
                 reads=[("xt", i2), "mix"], writes=["rr"])
            layer_norm_tile(k, P, rr, x1[i2], lnw, lnb, tmp, "rr", ("x1", i2), "1")
            P.dma("sp", k.x1[rows, :], x1[i2][:], reads=[("x1", i2)], writes=[("x1d", t)])
            P.op("act", lambda i2=i2: A.copy(out=x1b[i2][:], in_=x1[i2][:]), reads=[("x1", i2)], writes=[("x1b", i2)])
            for c in range(8):
                P.op("pe", lambda c=c, i2=i2: nc.tensor.transpose(out=pX[c // 4][:, (c % 4) * 128:(c % 4 + 1) * 128], in_=x1[i2][:, c * 128:(c + 1) * 128], identity=k.ident_f[:]),
                     reads=[("x1", i2), "ident"], writes=[("pX", c // 4)])
            for q in range(2):
                P.op("act", lambda q=q: A.copy(out=x1T[:, q * 4:(q + 1) * 4, :].rearrange("p a b -> p (a b)"), in_=pX[q][:]), reads=[("pX", q)], writes=["x1T"])
            for c in range(8):
                P.op("pe", lambda c=c: nc.tensor.matmul(pr[:, 0:NE], lhsT=x1T[:, c, :], rhs=rw[:, c, :], start=(c == 0), stop=(c == 7)),
                     reads=["x1T", "rw"], writes=["pr"])
            P.op("act", lambda: A.copy(out=lg[:], in_=pr[:, 0:NE]), reads=["pr"], writes=["lg"])
            P.op("dve", lambda: V.tensor_tensor(out=lg[:], in0=lg[:], in1=rb[:], op=ALU.add), reads=["lg", "rb"], writes=["lg"])
            P.op("dve", lambda: V.max(out=m8[:], in_=lg[:]), reads=["lg"], writes=["m8"])
            P.op("dve", lambda: V.tensor_scalar(out=msk[:], in0=lg[:], scalar1=m8[:, 3:4], scalar2=None, op0=ALU.is_ge), reads=["lg", "m8"], writes=["msk"])
            P.op("dve", lambda: V.tensor_copy(out=mskb[:], in_=msk[:]), reads=["msk"], writes=["mskb"])
            P.op("dve", lambda: V.tensor_scalar(out=nm[:], in0=m8[:, 0:1], scalar1=-1.0, scalar2=None, op0=ALU.mult), reads=["m8"], writes=["nm"])
            P.op("act", lambda: A.activation(out=ex[:], in_=lg[:], func=AF.Exp, bias=nm[:, 0:1], scale=1.0), reads=["lg", "nm"], writes=["ex"])
            P.op("dve", lambda: V.tensor_tensor(out=ex[:], in0=ex[:], in1=msk[:], op=ALU.mult), reads=["ex", "msk"], writes=["ex"])
            P.op("dve", lambda: V.tensor_reduce(out=ssum[:], in_=ex[:], axis=AX.X, op=ALU.add), reads=["ex"], writes=["ssum"])
            P.op("dve", lambda: V.reciprocal(out=ssum[:], in_=ssum[:]), reads=["ssum"], writes=["ssum"])
            P.op("dve", lambda: V.tensor_scalar(out=wts[:], in0=ex[:], scalar1=ssum[:, 0:1], scalar2=None, op0=ALU.mult), reads=["ex", "ssum"], writes=["wts"])
            P.op("pe", lambda: nc.tensor.matmul(pr[:, 64:64 + NE], lhsT=k.ustrict_b[:], rhs=mskb[:], start=True, stop=True), reads=["mskb", "ustrict_b"], writes=["pr"])
            P.op("pe", lambda: nc.tensor.matmul(pr[:, 128:128 + NE], lhsT=k.ones_b[:], rhs=mskb[:], start=True, stop=True), reads=["mskb", "ones_b"], writes=["pr"])
            P.op("act", lambda: A.copy(out=posf[:], in_=pr[:, 64:64 + NE]), reads=["pr"], writes=["posf"])
            P.op("act", lambda: A.copy(out=oh[:], in_=pr[:, 128:128 + NE]), reads=["pr"], writes=["oh"])
            P.op("dve", lambda: V.tensor_tensor(out=posf[:], in0=posf[:], in1=carry[:], op=ALU.add), reads=["posf", "carry"], writes=["posf"])
            P.op("dve", lambda: V.tensor_tensor(out=carry[:], in0=carry[:], in1=oh[:], op=ALU.add), reads=["carry", "oh", "posf"], writes=["carry"])
            P.op("dve", lambda: V.tensor_scalar(out=val[:], in0=posf[:], scalar1=float(CAP) - 0.5, scalar2=None, op0=ALU.is_lt), reads=["posf"], writes=["val"])
            P.op("dve", lambda: V.tensor_tensor(out=val[:], in0=val[:], in1=msk[:], op=ALU.mult), reads=["val", "msk"], writes=["val"])
            P.op("dve", lambda: V.tensor_tensor(out=nsel[:], in0=posf[:], in1=k.eoff[:], op=ALU.add), reads=["posf", "eoff"], writes=["nsel"])
            P.op("dve", lambda: V.tensor_scalar(out=nsel[:], in0=nsel[:], scalar1=k.trashp[:, 0:1], scalar2=None, op0=ALU.subtract), reads=["nsel", "trashp"], writes=["nsel"])
            P.op("dve", lambda: V.tensor_tensor(out=nsel[:], in0=nsel[:], in1=val[:], op=ALU.mult), reads=["nsel", "val"], writes=["nsel"])
            P.op("dve", lambda: V.tensor_scalar(out=nsel[:], in0=nsel[:], scalar1=k.trashp[:, 0:1], scalar2=-1.0, op0=ALU.add, op1=ALU.mult), reads=["nsel", "trashp"], writes=["nsel"])
            P.op("dve", lambda: V.max(out=t8[:], in_=nsel[:]), reads=["nsel"], writes=["t8"])
            P.op("dve", lambda: V.tensor_scalar(out=idxf[:], in0=t8[:, 0:4], scalar1=-1.0, scalar2=None, op0=ALU.mult), reads=["t8"], writes=["idxf"])
            P.op("dve", lambda t=t: V.tensor_copy(out=k.idx4[:, t, :], in_=idxf[:]), reads=["idxf"], writes=[("idx4", t)])
            for kk_ in range(4):
                P.op("dve", lambda kk_=kk_: V.tensor_scalar(out=oh[:], in0=nsel[:], scalar1=t8[:, kk_:kk_ + 1], scalar2=None, op0=ALU.is_equal), reads=["nsel", "t8", "carry"], writes=["oh"])
                P.op("dve", lambda: V.tensor_tensor(out=oh[:], in0=oh[:], in1=wts[:], op=ALU.mult), reads=["oh", "wts"], writes=["oh"])
                P.op("dve", lambda kk_=kk_, t=t: V.tensor_reduce(out=k.w4[:, t, kk_:kk_ + 1], in_=oh[:], axis=AX.X, op=ALU.add), reads=["oh"], writes=[("w4", t)])
            for kk_ in range(4):
                P.op("pool", lambda kk_=kk_, t=t, i2=i2: nc.gpsimd.indirect_dma_start(out=k.xbuf, out_offset=bass.IndirectOffsetOnAxis(ap=k.idx4[:, t, kk_:kk_ + 1], axis=0),
                                                                                       in_=x1b[i2][:], in_offset=None),
                     reads=[("x1b", i2), ("idx4", t)], writes=[("xbuf", t, kk_)], dma=True)
            if k.dbg and t == 0:
                dump(k, "lg", lg[:], "lg", NE)
                dump(k, "wts", wts[:], "wts", NE)
                dump(k, "nsel", nsel[:], "nsel", NE)
                dump(k, "w4", k.w4[:, 0, :], ("w4", 0), 4)
                dump(k, "idxf", idxf[:], "idxf", 4)
        P.barrier()


def phase4(k, l):
    nc, P = k.nc, k.P
    V, A = nc.vector, nc.scalar
    NSL = CAP // 128
    with ExitStack() as es:
        sb = lambda name, shape, dt: es.enter_context(nc.sbuf_tensor(f"p4l{l}_{name}", shape, dt))
        ps = lambda name, shape, dt: es.enter_context(nc.psum_tensor(f"p4l{l}_{name}", shape, dt))
        wu = [sb(f"wu{i}", [128, 8, 2 * D], BF16) for i in range(2)]
        wd = [sb(f"wd{i}", [128, 8, D], BF16) for i in range(2)]
        bupT = sb("bupT", [128, 16, NE], F32)
        bdn = [sb(f"bdn{i}", [128, D], F32) for i in range(2)]
        xe = [sb(f"xe{i}", [128, D], BF16) for i in range(4)]
        XeTs = [sb(f"XeT{i}", [128, 8, CAP], BF16) for i in range(2)]
        actT = sb("actT", [128, 8, CAP], BF16)
        gsb = [sb(f"gsb{i}", [128, CAP], F32) for i in range(2)]
        lsb = [sb(f"lsb{i}", [128, CAP], F32) for i in range(2)]
        sig = [sb(f"sig{i}", [128, CAP], F32) for i in range(2)]
        ysb = [sb(f"ysb{i}", [128, D], F32) for i in range(2)]
        yst = [sb(f"yst{i}", [128, D], BF16) for i in range(2)]
        pT = ps("pT", [128, 8, 128], BF16)
        pA = ps("pA", [128, 512], F32)
        pB = ps("pB", [128, 512], F32)
        pC = ps("pC", [128, 512], F32)
        pD = ps("pD", [128, 512], F32)

        P.op("dve", lambda: V.memset(yst[0][:], 0.0), writes=[("yst", 0)])
        P.dma("sp", k.ybuf[TRASH:TRASH + 128, :], yst[0][:], reads=[("yst", 0)], writes=["ybuf_trash"])
        P.dma("sp", gsb[0][0:NE, :], k.exp_b_up[l][:, 0:D], writes=[("gsb", 0)])
        P.dma("sp", lsb[0][0:NE, :], k.exp_b_up[l][:, D:2 * D], writes=[("lsb", 0)])
        for j in range(16):
            src = gsb[0] if j < 8 else lsb[0]
            P.op("pe", lambda j=j, src=src: nc.tensor.transpose(out=pA[:, j * NE:(j + 1) * NE], in_=src[0:NE, (j % 8) * 128:(j % 8 + 1) * 128], identity=k.ident_f[0:NE, 0:NE]),
                 reads=[("gsb", 0), ("lsb", 0), "ident"], writes=["pA"])
        P.op("act", lambda: A.copy(out=bupT[:].rearrange("p a b -> p (a b)"), in_=pA[:, 0:16 * NE]), reads=["pA"], writes=["bupT"])

        def load_w(e):
            import os
            if os.environ.get("NOLOADW") and e > 1:
                return
            b = e % 2
            wuv = k.exp_w_up[l, e].rearrange("(c p) n -> p c n", p=128)
            wdv = k.exp_w_down[l, e].rearrange("(c p) n -> p c n", p=128)
            for c in range(8):
                P.dma("pool", wu[b][:, c, :], wuv[:, c, :], writes=[("wu", b, c)])
            for c in range(8):
                P.dma("pool", wd[b][:, c, :], wdv[:, c, :], writes=[("wd", b, c)])
            P.dma("act", bdn[b][:], k.exp_b_down[l, e:e + 1, :].partition_broadcast(128), writes=[("bdn", b)])

        def gather(e):
            XeT = XeTs[e % 2]
            for i in range(NSL):
                i4 = i % 4
                P.dma("sp", xe[i4][:], k.xbuf[e * CAP + i * 128:e * CAP + (i + 1) * 128, :], writes=[("xe", i4)])
                for c in range(8):
                    P.op("pe", lambda c=c, i4=i4: nc.tensor.transpose(out=pT[:, c, :], in_=xe[i4][:, c * 128:(c + 1) * 128], identity=k.ident_b[:]),
                         reads=[("xe", i4), "ident_b"], writes=["pT"])
                P.op("dve", lambda i=i, XeT=XeT: V.tensor_copy(out=XeT[:, :, i * 128:(i + 1) * 128], in_=pT[:]), reads=["pT"], writes=[("XeT", e % 2)])

        load_w(0)
        for e in range(NE):
            b = e % 2
            if e + 1 < NE:
                load_w(e + 1)
            XeT = XeTs[b]
            kxe = ("XeT", b)
            if e == 0:
                gather(0)
            for j in range(8):
                groups = ((pA, "pA", j, 0), (pB, "pB", j, 512), (pC, "pC", 8 + j, 0), (pD, "pD", 8 + j, 512))
                for (dst, key, fc, n0) in groups:
                    for c in range(8):
                        P.op("pe", lambda c=c, dst=dst, fc=fc, n0=n0, b=b, XeT=XeT: nc.tensor.matmul(dst[:], lhsT=wu[b][:, c, fc * 128:(fc + 1) * 128], rhs=XeT[:, c, n0:n0 + 512],
                                                                                           start=(c == 0), stop=(c == 7)),
                             reads=[("wu", b, c), kxe], writes=[key])
                jb = j % 2
                G, Lq, Sg = gsb[jb], lsb[jb], sig[jb]
                kg, kl, ks = ("gsb", jb), ("lsb", jb), ("sig", jb)
                for (dst, key, fc, n0) in groups:
                    T_, kt_ = (G, kg) if fc < 8 else (Lq, kl)
                    P.op("act", lambda dst=dst, fc=fc, n0=n0, e=e, T_=T_: A.activation(out=T_[:, n0:n0 + 512], in_=dst[:], func=AF.Identity, bias=bupT[:, fc, e:e + 1], scale=1.0),
                         reads=[key, "bupT"], writes=[kt_])
                P.op("dve", lambda G=G: V.tensor_scalar(out=G[:], in0=G[:], scalar1=7.0, scalar2=None, op0=ALU.min), reads=[kg], writes=[kg])
                P.op("act", lambda G=G, Sg=Sg: A.activation(out=Sg[:], in_=G[:], func=AF.Sigmoid, scale=1.702), reads=[kg], writes=[ks])
                P.op("dve", lambda Lq=Lq: V.tensor_scalar(out=Lq[:], in0=Lq[:], scalar1=-7.0, scalar2=7.0, op0=ALU.max, op1=ALU.min), reads=[kl], writes=[kl])
                P.op("dve", lambda G=G, Sg=Sg: V.tensor_tensor(out=Sg[:], in0=G[:], in1=Sg[:], op=ALU.mult), reads=[kg, ks], writes=[ks])
                P.op("dve", lambda j=j, Lq=Lq, Sg=Sg: V.scalar_tensor_tensor(out=actT[:, j, :], in0=Lq[:], scalar=1.0, in1=Sg[:], op0=ALU.add, op1=ALU.mult),
                     reads=[kl, ks], writes=[("actT", j)])
            if e + 1 < NE:
                gather(e + 1)
            for i in range(NSL):
                i2 = i % 2
                for hf in range(2):
                    dstb, key = ((pA, "pA"), (pC, "pC"), (pB, "pB"), (pD, "pD"))[2 * i2 + hf]
                    for c in range(8):
                        P.op("pe", lambda c=c, dstb=dstb, hf=hf, i=i, b=b: nc.tensor.matmul(dstb[:], lhsT=actT[:, c, i * 128:(i + 1) * 128], rhs=wd[b][:, c, hf * 512:(hf + 1) * 512],
                                                                                           start=(c == 0), stop=(c == 7)),
                             reads=[("actT", c), ("wd", b, c)], writes=[key])
                    P.op("act", lambda dstb=dstb, hf=hf, i2=i2: A.copy(out=ysb[i2][:, hf * 512:(hf + 1) * 512], in_=dstb[:]), reads=[key], writes=[("ysb", i2)])
                P.op("dve", lambda i2=i2, b=b: V.tensor_tensor(out=yst[i2][:], in0=ysb[i2][:], in1=bdn[b][:], op=ALU.add),
                     reads=[("ysb", i2), ("bdn", b)], writes=[("yst", i2)])
                P.dma("sp", k.ybuf[e * CAP + i * 128:e * CAP + (i + 1) * 128, :], yst[i2][:], reads=[("yst", i2)], writes=[("ybuf", e, i)])
        P.barrier()


def phase5(k, l, last):
    nc, P = k.nc, k.P
    V, A = nc.vector, nc.scalar
    with ExitStack() as es:
        sb = lambda name, shape, dt: es.enter_context(nc.sbuf_tensor(f"p5l{l}_{name}", shape, dt))
        lnw = sb("lnw", [128, D], F32)
        lnb = sb("lnb", [128, D], F32)
        k.ln_st = sb("ln_st", [128, 12], F32)
        k.ln_ag = sb("ln_ag", [128, 2], F32)
        k.ln_rs = sb("ln_rs", [128, 1], F32)
        gk = [[sb(f"gk{i}_{q}", [128, D], BF16) for q in range(4)] for i in range(2)]
        x1t = [sb(f"x1t{i}", [128, D], F32) for i in range(2)]
        acc = sb("acc", [128, D], F32)
        tmp = sb("tmp", [128, D], F32)
        x2 = [sb(f"x2_{i}", [128, D], F32) for i in range(2)]
        bcast_row(k, P, "sp", lnw[:], k.ln2_w[l:l + 1, :], "lnw2")
        bcast_row(k, P, "sp", lnb[:], k.ln2_b[l:l + 1, :], "lnb2")
        dst_d = k.out if last else k.x2
        for t in range(NT):
            i2 = t % 2
            rows = slice(t * 128, (t + 1) * 128)
            P.dma("sp", x1t[i2][:], k.x1[rows, :], writes=[("x1t", i2)])
            for q in range(4):
                P.op("pool", lambda q=q, t=t, i2=i2: nc.gpsimd.indirect_dma_start(out=gk[i2][q][:], out_offset=None, in_=k.ybuf,
                                                                                 in_offset=bass.IndirectOffsetOnAxis(ap=k.idx4[:, t, q:q + 1], axis=0)),
                     writes=[("gk", i2, q)], dma=True)
            P.op("dve", lambda t=t, i2=i2: V.tensor_scalar(out=acc[:], in0=gk[i2][0][:], scalar1=k.w4[:, t, 0:1], scalar2=None, op0=ALU.mult),
                 reads=[("gk", i2, 0)], writes=["acc"])
            for q in range(1, 4):
                P.op("dve", lambda q=q, t=t, i2=i2: V.scalar_tensor_tensor(out=acc[:], in0=gk[i2][q][:], scalar=k.w4[:, t, q:q + 1], in1=acc[:], op0=ALU.mult, op1=ALU.add),
                     reads=[("gk", i2, q), "acc"], writes=["acc"])
            if k.dbg and k.moe_dbg is not None:
                P.dma("sp", k.moe_dbg[rows, :], acc[:], reads=["acc"], writes=[("moe_dbg", t)])
            P.op("dve", lambda i2=i2: V.scalar_tensor_tensor(out=acc[:], in0=x1t[i2][:], scalar=ALPHA, in1=acc[:], op0=ALU.mult, op1=ALU.add),
                 reads=[("x1t", i2), "acc"], writes=["acc"])
            layer_norm_tile(k, P, acc, x2[i2], lnw, lnb, tmp, "acc", ("x2", i2), "2")
            P.dma("sp", dst_d[rows, :], x2[i2][:], reads=[("x2", i2)], writes=[("x2d", t)])
        P.barrier()


def make_inputs(inputs, b, perm, w_in_r=None):
    f32 = lambda n: np.ascontiguousarray(np.asarray(inputs[n], dtype=np.float32))
    if w_in_r is None:
        w_in_r = np.ascontiguousarray(np.asarray(inputs["w_in"], dtype=np.float32)[:, :, perm])
    m = {"x": np.ascontiguousarray(np.asarray(inputs["x"], dtype=np.float32)[b]),
         "pos": np.ascontiguousarray(np.asarray(inputs["positions"], dtype=np.int32)[b].reshape(1, -1)), "w_in": w_in_r,
         "hg_lb": f32("hg_lower_bounds"), "hg_nw": f32("hg_norm_w"), "gm_lnw": f32("gm_ln_w"), "gm_lnb": f32("gm_ln_b"),
         "gm_ws": f32("gm_spatial_w"), "gm_bs": f32("gm_spatial_b"), "gm_nw": f32("gm_norm_w"),
         "nsa_pe": f32("nsa_cmp_pe"), "nsa_w1": f32("nsa_cmp_w1"), "nsa_w2": f32("nsa_cmp_w2"), "nsa_nw": f32("nsa_norm_w"),
         "w_out": f32("w_out"), "ln1_w": f32("ln1_w"), "ln1_b": f32("ln1_b"), "ln2_w": f32("ln2_w"), "ln2_b": f32("ln2_b"),
         "router_w": f32("router_w"), "router_b": f32("router_b"), "exp_w_up": f32("exp_w_up"), "exp_b_up": f32("exp_b_up"),
         "exp_w_down": f32("exp_w_down"), "exp_b_down": f32("exp_b_down")}
    for n, v in host_consts().items():
        m["c_" + n] = v
    return m


def kernel(**inputs):
    perm = w_in_perm()
    w_in_r = np.ascontiguousarray(np.asarray(inputs["w_in"], dtype=np.float32)[:, :, perm])
    nc = build(nlayers=L, dbg=False)
    in_maps = [make_inputs(inputs, b, perm, w_in_r) for b in range(8)]
    res = run_bass_kernel_spmd(nc, in_maps, core_ids=list(range(8)))
    return np.stack([np.asarray(r["out"], dtype=np.float32) for r in res.results], axis=0)
```

```python
import math
from contextlib import ExitStack

import numpy as np
import concourse.bass as bass
import concourse.mybir as mybir
from concourse.bass_utils import run_bass_kernel_spmd

F32 = mybir.dt.float32
BF16 = mybir.dt.bfloat16
I32 = mybir.dt.int32
U32 = mybir.dt.uint32
AF = mybir.ActivationFunctionType
ALU = mybir.AluOpType
AX = mybir.AxisListType

S = 4096
D = 1024
NT = S // 128
L = 2
NE = 32
CAP = 1024
NCOLA = 1816
NCH_B = 14
WCOLS = NCOLA + NCH_B * 128
ALPHA = (2 * L) ** 0.25
PI = math.pi
TRASH = NE * CAP

COMPUTE = ("pe", "dve", "act", "pool")
DMAQ = ("sp", "act", "pool")
NDMASEM = 6
NCSEM = 8
SEM_KEYS = [(e, i) for e in COMPUTE for i in range(NCSEM)] + [(q + "_q", i) for q in DMAQ for i in range(NDMASEM)]


class Op:
    __slots__ = ("eng", "fn", "reads", "writes", "dma", "deps", "raw", "need_inc", "sem", "val", "idx")


class Prog:
    def __init__(self, nc, sems):
        self.nc = nc
        self.sems = sems
        self.eng = {"pe": nc.tensor, "dve": nc.vector, "act": nc.scalar, "pool": nc.gpsimd, "sp": nc.sync}
        self.ops = []
        self.last_writer = {}
        self.readers = {}
        self.cnt = {e: 0 for e in COMPUTE}
        self.dcnt = {q: 0 for q in DMAQ}
        self.waited = {}
        self.dma_last = {}
        self.nwait = 0
        self.ntotal = 0
        import os
        self.verbose = bool(os.environ.get("VERB"))

    def op(self, eng, fn, reads=(), writes=(), dma=False):
        o = Op()
        o.eng, o.fn, o.dma = eng, fn, dma
        o.reads, o.writes = tuple(reads), tuple(writes)
        o.need_inc = False
        o.idx = len(self.ops)
        deps = set()
        raw = set()
        lw, rd = self.last_writer, self.readers
        for k in o.reads:
            w = lw.get(k)
            if w is not None:
                deps.add(w)
                raw.add(w)
        for k in o.writes:
            w = lw.get(k)
            if w is not None:
                deps.add(w)
            r = rd.get(k)
            if r:
                deps.update(r)
        for k in o.reads:
            rd.setdefault(k, []).append(o.idx)
        for k in o.writes:
            lw[k] = o.idx
            rd[k] = []
        deps.discard(o.idx)
        o.deps = deps
        o.raw = raw
        self.ops.append(o)
        return o

    def dma(self, q, out, in_, reads=(), writes=(), **kw):
        e = self.eng[q]
        return self.op(q, lambda: e.dma_start(out=out, in_=in_, **kw), reads, writes, dma=True)

    def barrier(self):
        ops = self.ops
        tails = set()
        last_by_eng = {}
        for o in ops:
            if o.dma:
                tails.add(o.idx)
            else:
                last_by_eng[o.eng] = o.idx
        tails.update(last_by_eng.values())
        for en in ("pe", "dve", "act", "pool", "sp"):
            o = Op()
            o.eng, o.dma, o.reads, o.writes, o.need_inc = en, False, (), (), False
            e = self.eng[en]
            o.fn = (lambda e=e: e.nop())
            o.idx = len(ops)
            o.deps = set(tails)
            o.raw = set()
            ops.append(o)
        self.emit()

    def emit(self):
        ops, sems = self.ops, self.sems
        def needs_sync(o, d):
            p = ops[d]
            return p.dma or p.eng != o.eng or (o.eng != "pe" and d in o.raw)
        for o in ops:
            for d in o.deps:
                if needs_sync(o, d):
                    ops[d].need_inc = True
        cnt, dcnt, waited = self.cnt, self.dcnt, self.waited
        for o in ops:
            e = self.eng[o.eng]
            pre = []
            if o.dma:
                i = dcnt[o.eng]
                dcnt[o.eng] += 1
                sk = (o.eng + "_q", i % NDMASEM)
                o.sem = sk
                o.val = 16 * (i // NDMASEM + 1)
                self.dma_last[sk] = o.val
                if o.val > 16:
                    pre.append((sk, o.val - 16))
            for d in o.deps:
                p = ops[d]
                if p.dma or needs_sync(o, d):
                    pre.append((p.sem, p.val))
            best = {}
            for sk, v in pre:
                if v > best.get(sk, 0):
                    best[sk] = v
            for sk, v in best.items():
                if waited.get((o.eng, sk), 0) >= v:
                    continue
                waited[(o.eng, sk)] = v
                e.wait_ge(sems[sk], v)
                self.nwait += 1
                if self.verbose:
                    print("   wait", o.eng, sk, v)
            ins = o.fn()
            if self.verbose:
                print("op", o.idx, o.eng, "dma" if o.dma else "", o.writes, "inc" if (o.need_inc or o.dma) else "", getattr(o, "sem", None) if o.dma else "", (o.val if o.dma else ""))
            if o.dma:
                ins.then_inc(sems[o.sem], 16)
            elif o.need_inc:
                i = cnt[o.eng]
                cnt[o.eng] += 1
                o.sem = (o.eng, i % NCSEM)
                o.val = i // NCSEM + 1
                ins.then_inc(sems[o.sem], 1)
        self.ntotal += len(ops)
        self.ops = []
        self.last_writer = {}
        self.readers = {}

    def finish(self):
        self.emit()
        e = self.eng["sp"]
        for sk, v in self.dma_last.items():
            e.wait_ge(self.sems[sk], v)


class Rec:
    def __init__(self, P, keymap=None):
        self.P = P
        self.eng = P.eng
        self.items = []
        self.km = keymap or (lambda x: x)

    def op(self, eng, fn, reads=(), writes=(), dma=False):
        self.items.append((eng, fn, [self.km(r) for r in reads], [self.km(w) for w in writes], dma))

    def dma(self, q, out, in_, reads=(), writes=(), **kw):
        e = self.eng[q]
        self.op(q, lambda: e.dma_start(out=out, in_=in_, **kw), reads, writes, dma=True)


def replay_interleaved(P, recs):
    its = [r.items for r in recs if r is not None and r.items]
    pos = [0] * len(its)
    tot = sum(len(x) for x in its)
    lens = [len(x) for x in its]
    done = 0
    while done < tot:
        for i, x in enumerate(its):
            if pos[i] < lens[i] and pos[i] * tot <= done * lens[i] + lens[i]:
                eng, fn, rd, wr, dma = x[pos[i]]
                P.op(eng, fn, rd, wr, dma=dma)
                pos[i] += 1
                done += 1


def host_consts():
    c = {}
    c["ident"] = np.eye(128, dtype=np.float32)
    s = np.arange(128)[:, None]
    t = np.arange(128)[None, :]
    same = (s // 64) == (t // 64)
    mid = (t // 64) * 64 + 31
    c["mcum"] = (same & (s <= t)).astype(np.float32)
    c["mmid"] = (c["mcum"] - (same & (s <= mid)).astype(np.float32))
    c["mrem"] = (same & (s > t)).astype(np.float32)
    c["tri"] = (s <= t).astype(np.float32)
    c["trit"] = (s >= t).astype(np.float32)
    inv = (10000.0 ** (-np.arange(0, 64, 2, dtype=np.float32) / 64)).astype(np.float32)
    p = np.arange(128)
    sgn = np.where((p % 64) < 32, -1.0, 1.0).astype(np.float32)
    col = np.zeros((128, 4), np.float32)
    col[:, 0] = inv[p % 32]
    col[:, 1] = sgn
    col[:, 2] = 2 * PI * sgn
    col[:, 3] = -PI
    c["ropecol"] = col
    jj = np.arange(64)[:, None, None]
    kt_ = np.arange(32)[None, :, None]
    kk_ = np.arange(128)[None, None, :]
    E = (jj == 2 * kt_ + kk_ // 64).astype(np.float32)
    c["edup"] = np.concatenate([E, E], 0)
    p_ = np.arange(128)[:, None]
    q_ = np.arange(512)[None, :]
    c["win01"] = np.stack([((128 * dl + p_ - q_ <= 0) & (128 * dl + p_ - q_ > -512)).astype(np.float32) for dl in range(-4, 4)], 1)
    c["cmp01"] = np.stack([(16 * p_ + 31 - q_ <= u).astype(np.float32) for u in (0, 512, 1024, 1536, 2048)], 1)
    tq = np.arange(S).reshape(NT, 128)
    cur = (tq // 64)[:, :, None]
    jb = np.arange(64)[None, None, :]
    fut = jb > cur
    frc = (jb == 0) | (jb == cur) | (jb == cur - 1)
    keep = (~fut & ~frc).astype(np.float32)
    add = np.where(frc, 1e9, np.where(fut, -1e9, 0.0)).astype(np.float32)
    c["wpad"] = np.ascontiguousarray(np.maximum(0, 511 - tq).T.astype(np.float32))
    c["impkeep"] = np.ascontiguousarray(keep.transpose(1, 0, 2))
    c["impadd"] = np.ascontiguousarray(add.transpose(1, 0, 2))
    cc = np.arange(256)
    ov = np.zeros((256, 65), np.float32)
    for c_ in range(255):
        for uu in (c_, c_ + 1):
            ov[c_, uu // 4] += 1.0
    ov[:, 64] = 1.0
    c["ovext"] = np.ascontiguousarray(ov.reshape(2, 128, 65).transpose(1, 0, 2))
    c["eoff"] = np.tile((np.arange(NE, dtype=np.float32) * CAP)[None, :], (128, 1)).astype(np.float32)
    c["trashp"] = (TRASH + np.arange(128, dtype=np.float32)).reshape(128, 1).astype(np.float32)
    return c


def w_in_perm():
    hg_q, hg_f, hg_i, hg_g, gm_u, gm_v, q, k_c, v_c, k_s, v_s, k_w, v_w, gts = (
        0, 256, 512, 768, 1024, 1280, 1536, 2048, 2176, 2304, 2432, 2560, 2688, 2816)
    A = list(range(0, 1536)) + list(range(v_s, v_s + 128)) + list(range(v_w, v_w + 128)) + list(range(gts, gts + 24))

    def sw(c0, n):
        out = []
        for h in range(n // 64):
            b = c0 + h * 64
            out += list(range(b + 32, b + 64)) + list(range(b, b + 32))
        return out
    B = (list(range(q, q + 512)) + sw(q, 512) + list(range(k_s, k_s + 128)) + sw(k_s, 128)
         + list(range(k_w, k_w + 128)) + sw(k_w, 128) + list(range(k_c, k_c + 128)) + list(range(v_c, v_c + 128)))
    perm = np.array(A + B, dtype=np.int64)
    assert perm.shape[0] == WCOLS
    return perm


class K:
    pass


def build(nlayers=L, dbg=False, stop_after=None, stage=None, inject_nsa=False, phases=(1, 2, 3, 4, 5)):
    nc = bass.Bass("TRN2", target_bir_lowering=False)
    k = K()
    global _LASTK
    _LASTK = k
    k.stage = stage
    import os
    k.dbgv = int(os.environ.get('DBGV', '0'))
    k.nc = nc
    k.dbg = dbg

    def din(name, shape, dt=F32):
        return nc.dram_tensor(name, list(shape), dt, kind="ExternalInput").ap()

    def dscr(name, shape, dt, out=False):
        return nc.dram_tensor(name, list(shape), dt, kind=("ExternalOutput" if (out and dbg) else "Internal")).ap()

    k.x = din("x", [S, D])
    k.pos = din("pos", [1, S], I32)
    k.w_in = din("w_in", [L, D, WCOLS])
    k.lbraw = din("hg_lb", [L, 256])
    k.hg_nw = din("hg_nw", [L, 256])
    k.gm_lnw = din("gm_lnw", [L, 256])
    k.gm_lnb = din("gm_lnb", [L, 256])
    k.gm_ws = din("gm_ws", [L, 4, 128, 128])
    k.gm_bs = din("gm_bs", [L, 4, 128])
    k.gm_nw = din("gm_nw", [L, 256])
    k.nsa_pe = din("nsa_pe", [L, 2, 32, 64]); k.nsa_w1 = din("nsa_w1", [L, 2, 2048, 128])
    k.nsa_w2 = din("nsa_w2", [L, 2, 128, 64]); k.nsa_nw = din("nsa_nw", [L, 512])
    k.w_out = din("w_out", [L, D, D])
    k.ln1_w = din("ln1_w", [L, D]); k.ln1_b = din("ln1_b", [L, D])
    k.ln2_w = din("ln2_w", [L, D]); k.ln2_b = din("ln2_b", [L, D])
    k.router_w = din("router_w", [L, D, NE]); k.router_b = din("router_b", [L, NE])
    k.exp_w_up = din("exp_w_up", [L, NE, D, 2 * D]); k.exp_b_up = din("exp_b_up", [L, NE, 2 * D])
    k.exp_w_down = din("exp_w_down", [L, NE, D, D]); k.exp_b_down = din("exp_b_down", [L, NE, D])
    k.inject_nsa = inject_nsa
    if dbg and inject_nsa:
        k.ynsa_in = din("ynsa_in", [L, S, 512], BF16)
    k.cst = {n: din("c_" + n, v.shape) for n, v in host_consts().items()}
    k.out = nc.dram_tensor("out", [S, D], F32, kind="ExternalOutput").ap()
    k.ft = dscr("ft", [12, 128, S], BF16, out=True)
    k.tmv = dscr("tmv", [S, 256], BF16, out=True)
    k.gts = dscr("gts", [S, 24], F32, out=True)
    k.y = dscr("y", [S, D], BF16, out=True)
    k.x1 = dscr("x1", [S, D], F32, out=True)
    k.x2 = dscr("x2", [S, D], F32, out=True)
    k.xbuf = dscr("xbuf", [TRASH + 128, D], BF16)
    k.ybuf = dscr("ybuf", [TRASH + 128, D], BF16)
    k.moe_dbg = dscr("moe_dbg", [S, D], F32, out=True) if dbg else None
    k.dbgout = dscr("dbgout", [24, 128, 512], F32, out=True)
    k.ndump = 0
    k.dumpnames = []

    with ExitStack() as es:
        sems = {}
        for sk in SEM_KEYS:
            nm = sk if isinstance(sk, str) else f"{sk[0]}{sk[1]}"
            sems[sk] = es.enter_context(nc.semaphore("s_" + nm))
        P = Prog(nc, sems)
        k.P = P
        k.es = es
        setup_consts(k)
        for l in range(nlayers if stop_after != "setup" else 0):
            if 1 in phases:
                phase1(k, l)
            if stop_after == (l, 1):
                break
            if 2 in phases:
                phase2(k, l)
            if stop_after == (l, 2):
                break
            if 3 in phases:
                phase3(k, l)
            if stop_after == (l, 3):
                break
            if 4 in phases:
                phase4(k, l)
            if 5 in phases:
                phase5(k, l, last=(l == nlayers - 1))
        P.finish()
        print("[build] ops", P.ntotal, "waits", P.nwait, "cnt", P.cnt, "dcnt", P.dcnt)
    return nc


def setup_consts(k):
    nc, P, es = k.nc, k.P, k.es
    sb = lambda name, shape, dt: es.enter_context(nc.sbuf_tensor(name, shape, dt))
    k.ident_f = sb("ident_f", [128, 128], F32)
    k.ident_b = sb("ident_b", [128, 128], BF16)
    k.mcum = sb("mcum", [128, 128], F32)
    k.mmid = sb("mmid", [128, 128], F32)
    k.mrem = sb("mrem", [128, 128], F32)
    k.tri = sb("tri", [128, 128], F32)
    k.trit = sb("trit", [128, 128], F32)
    k.mcum_b = sb("mcum_b", [128, 128], BF16)
    k.mmid_b = sb("mmid_b", [128, 128], BF16)
    k.mrem_b = sb("mrem_b", [128, 128], BF16)
    k.ones_b = sb("ones_b", [128, 128], BF16)
    k.ropecol = sb("ropecol", [128, 4], F32)
    k.ones_f = sb("ones_f", [128, 128], F32)
    k.idx4 = sb("idx4", [128, NT, 4], I32)
    k.w4 = sb("w4", [128, NT, 4], F32)
    k.eoff = sb("eoff", [128, NE], F32)
    k.trashp = sb("trashp", [128, 1], F32)
    k.ustrict_b = sb("ustrict_b", [128, 128], BF16)
    for n, t in (("ident", k.ident_f), ("mcum", k.mcum), ("mmid", k.mmid), ("mrem", k.mrem), ("tri", k.tri),
                 ("trit", k.trit), ("ropecol", k.ropecol), ("eoff", k.eoff), ("trashp", k.trashp)):
        P.dma("sp", t[:], k.cst[n], writes=[n])
    P.op("dve", lambda: nc.vector.tensor_copy(out=k.ident_b[:], in_=k.ident_f[:]), reads=["ident"], writes=["ident_b"])
    P.op("dve", lambda: nc.vector.memset(k.ones_f[:], 1.0), writes=["ones_f"])
    P.op("dve", lambda: nc.vector.memset(k.ones_b[:], 1.0), writes=["ones_b"])
    P.op("dve", lambda: nc.vector.tensor_sub(out=k.ones_f[:], in0=k.tri[:], in1=k.ident_f[:]), reads=["tri", "ident", "ones_f"], writes=["ustr_tmp"])
    P.op("dve", lambda: nc.vector.tensor_copy(out=k.ustrict_b[:], in_=k.ones_f[:]), reads=["ustr_tmp"], writes=["ustrict_b"])
    P.op("dve", lambda: nc.vector.memset(k.ones_f[:], 1.0), reads=["ustrict_b"], writes=["ones_f"])
    P.op("dve", lambda: nc.vector.tensor_copy(out=k.mcum_b[:], in_=k.mcum[:]), reads=["mcum"], writes=["mcum_b"])
    P.op("dve", lambda: nc.vector.tensor_copy(out=k.mmid_b[:], in_=k.mmid[:]), reads=["mmid"], writes=["mmid_b"])
    P.op("dve", lambda: nc.vector.tensor_copy(out=k.mrem_b[:], in_=k.mrem[:]), reads=["mrem"], writes=["mrem_b"])
    P.barrier()


def setup_rope(k, es, l):
    nc, P = k.nc, k.P
    sb = lambda name, shape, dt: es.enter_context(nc.sbuf_tensor(f"{name}l{l}", shape, dt))
    k.ropeC = sb("ropeC", [128, S], F32)
    k.ropeS = sb("ropeS", [128, S], F32)
    with nc.sbuf_tensor(f"posil{l}", [128, S], I32) as posi, nc.sbuf_tensor(f"ropeTl{l}", [128, S], F32) as ropeT, nc.sbuf_tensor(f"ropeT2l{l}", [128, S], F32) as ropeT2:
        k.ropeT, k.ropeT2 = ropeT, ropeT2
        P.dma("sp", posi[:], k.pos.partition_broadcast(128), writes=["posi"])
        C, Sg, col = k.ropeC, k.ropeS, k.ropecol
        P.op("dve", lambda: nc.vector.tensor_copy(out=C[:], in_=posi[:]), reads=["posi"], writes=["C"])
        P.op("dve", lambda: nc.vector.tensor_scalar(out=C[:], in0=C[:], scalar1=col[:, 0:1], scalar2=None, op0=ALU.mult),
             reads=["C", "ropecol"], writes=["C"])
        def red(T, add):
            P.op("dve", lambda: nc.vector.tensor_scalar(out=T[:], in0=C[:], scalar1=1.0 / (2 * PI), scalar2=add, op0=ALU.mult, op1=ALU.add),
                 reads=["C"], writes=["T" + str(add)])
            P.op("dve", lambda: nc.vector.tensor_copy(out=posi[:], in_=T[:]), reads=["T" + str(add)], writes=["posi"])
            P.op("dve", lambda: nc.vector.tensor_copy(out=k.ropeT[:], in_=posi[:]), reads=["posi"], writes=["ropeT"])
            P.op("dve", lambda: nc.vector.tensor_sub(out=T[:], in0=T[:], in1=k.ropeT[:]), reads=["ropeT", "T" + str(add)], writes=["T" + str(add)])
            P.op("dve", lambda: nc.vector.tensor_scalar(out=k.ropeT[:], in0=T[:], scalar1=0.5, scalar2=None, op0=ALU.is_gt), reads=["T" + str(add)], writes=["ropeT"])
            P.op("dve", lambda: nc.vector.tensor_sub(out=T[:], in0=T[:], in1=k.ropeT[:]), reads=["ropeT", "T" + str(add)], writes=["T" + str(add)])
        red(Sg, 0.0)
        P.op("act", lambda: nc.scalar.activation(out=Sg[:], in_=Sg[:], func=AF.Sin, scale=col[:, 2:3]), reads=["T0.0", "ropecol"], writes=["S"])
        red(k.ropeT2, 0.25)
        P.op("act", lambda: nc.scalar.activation(out=C[:], in_=k.ropeT2[:], func=AF.Sin, scale=2 * PI), reads=["T0.25", "C"], writes=["C"])
        P.barrier()


def dump(k, name, ap, key, width, parts=128):
    if not k.dbg:
        return
    i = k.ndump
    k.ndump += 1
    k.dumpnames.append(name)
    k.P.dma("pool", k.dbgout[i, 0:parts, 0:width], ap, reads=[key], writes=[("dbgout", i)])


def bcast_row(k, P, q, dst, src_row, key):
    P.dma(q, dst, src_row.partition_broadcast(128), writes=[key])


class _Stop(Exception):
    pass


def phase1(k, l):
    with ExitStack() as es:
        setup_rope(k, es, l)
        try:
            _phase1(k, l, es)
        except _Stop:
            pass
        k.P.barrier()


def ck(k, n):
    if k.stage == n:
        raise _Stop()


def _phase1(k, l, es):
    nc, P = k.nc, k.P
    if True:
        sb = lambda name, shape, dt: es.enter_context(nc.sbuf_tensor(f"p1l{l}_{name}", shape, dt))
        ps = lambda name, shape, dt: es.enter_context(nc.psum_tensor(f"p1l{l}_{name}", shape, dt))
        wb = sb("wb", [128, 8, WCOLS], BF16)
        xt = [sb(f"xt{i}", [128, D], F32) for i in range(2)]
        xb = [sb(f"xb{i}", [128, D], BF16) for i in range(2)]
        xTb = [sb(f"xTb{i}", [128, 8, 512], BF16) for i in range(2)]
        hg_nw = sb("hg_nw", [128, 256], F32)
        gm_lnw = sb("gm_lnw", [128, 256], F32)
        gm_lnb = sb("gm_lnb", [128, 256], F32)
        gm_nw = sb("gm_nw", [128, 256], F32)
        lbt = sb("lbt", [128, 2, 256], F32)
        lb = sb("lb", [128, 256], F32)
        oml = sb("oml", [128, 256], F32)
        wsn = sb("wsn", [128, 4, 128], F32)
        wsb = sb("wsb", [128, 4, 128], BF16)
        wsT = sb("wsT", [128, 4, 128], BF16)
        bsT = sb("bsT", [128, 4], F32)
        pT = ps("pT", [128, 8, 128], BF16)
        pm = [ps(f"pm{i}", [128, 512], F32) for i in range(4)]
        pa = ps("pa", [128, 512], F32)
        pb = ps("pb", [128, 512], F32)
        pc = ps("pc", [128, 512], F32)
        pcb = pc[:].bitcast(BF16)

        x_src = k.x if l == 0 else k.x2
        wv = k.w_in[l].rearrange("(c p) n -> p c n", p=128)
        half = WCOLS // 2
        for c in range(8):
            for hh in range(2):
                P.dma("pool", wb[:, c, hh * half:(hh + 1) * half], wv[:, c, hh * half:(hh + 1) * half], writes=[("wb", c), ("wbser", (2 * c + hh) % 2)])
        bcast_row(k, P, "sp", hg_nw[:], k.hg_nw[l:l + 1, :], "hg_nw")
        bcast_row(k, P, "sp", gm_lnw[:], k.gm_lnw[l:l + 1, :], "gm_lnw")
        bcast_row(k, P, "sp", gm_lnb[:], k.gm_lnb[l:l + 1, :], "gm_lnb")
        bcast_row(k, P, "sp", gm_nw[:], k.gm_nw[l:l + 1, :], "gm_nw")
        if l > 0:
            P.dma("sp", lbt[:].rearrange("p a n -> p (a n)"),
                  k.lbraw.rearrange("a n -> (a n)").unsqueeze(0).partition_broadcast(128), writes=["lbt"])
            P.op("dve", lambda: nc.vector.tensor_sub(out=lb[:], in0=lbt[:, 1, :], in1=lbt[:, 0, :]), reads=["lbt"], writes=["lb"])
            P.op("act", lambda: nc.scalar.activation(out=lb[:], in_=lb[:], func=AF.Sigmoid), reads=["lb"], writes=["lb"])
            P.op("dve", lambda: nc.vector.tensor_scalar(out=oml[:], in0=lb[:], scalar1=-1.0, scalar2=1.0, op0=ALU.mult, op1=ALU.add),
                 reads=["lb"], writes=["oml"])
        P.dma("sp", wsn[:], k.gm_ws[l].rearrange("g t s -> t g s"), writes=["wsn"])
        P.dma("sp", bsT[:], k.gm_bs[l].rearrange("g t -> t g"), writes=["bsT"], allow_slow_non_contiguous=True)
        trit = k.trit
        for g in range(4):
            P.op("dve", lambda g=g: nc.vector.tensor_tensor(out=wsb[:, g, :], in0=wsn[:, g, :], in1=trit[:], op=ALU.mult),
                 reads=["wsn"], writes=["wsb"])
        for g in range(4):
            P.op("pe", lambda g=g: nc.tensor.transpose(out=pT[:, g, :], in_=wsb[:, g, :], identity=k.ident_b[:]),
                 reads=["wsb", "ident_b"], writes=["pT"])
        P.op("dve", lambda: nc.vector.tensor_copy(out=wsT[:], in_=pT[:, 0:4, :]), reads=["pT"], writes=["wsT"])

        st_f = sb("st_f", [128, 2, 64], F32)
        st_b = sb("st_b", [128, 2, 64], BF16)
        P.op("dve", lambda: nc.vector.memset(st_f[:], 0.0), writes=["st_f"])
        P.op("dve", lambda: nc.vector.memset(st_b[:], 0.0), writes=["st_b"])

        W = {}
        for n in ("qf", "sg", "sgate", "logf", "kk", "ebm", "enbm", "eb", "erem", "t1", "t2", "osq", "nwg", "gsq", "ginn", "gsv"):
            W[n] = sb(n, [128, 256], F32)
        gx = sb("gx", [128, 512], F32)
        gg = sb("gg", [128, 512], F32)
        gt = sb("gt", [128, 512], F32)
        vb = sb("vb", [128, 256], BF16)
        qe = sb("qe", [128, 256], BF16)
        ke = sb("ke", [128, 256], BF16)
        qb = sb("qb", [128, 256], BF16)
        kd = sb("kd", [128, 256], BF16)
        vln = sb("vln", [128, 256], BF16)
        lfh = sb("lfh", [128, 256], BF16)
        lfl = sb("lfl", [128, 256], BF16)
        trT = sb("trT", [128, 6, 128], BF16)
        attb = sb("attb", [128, 4, 128], BF16)
        ebl = sb("ebl", [128, 2, 2], F32)
        ss4 = sb("ss4", [128, 8], F32)
        rs4 = sb("rs4", [128, 8], F32)
        bnst = sb("bnst", [128, 6], F32)
        bnag = sb("bnag", [128, 2], F32)
        ytile = [sb(f"ytile{i}", [128, 512], BF16) for i in range(2)]
        vstage = [sb(f"vstage{i}", [128, 256], BF16) for i in range(2)]
        gstage = [sb(f"gstage{i}", [128, 24], F32) for i in range(2)]
        fst_raw = [sb(f"fst_raw{i}", [128, 512], BF16) for i in range(2)]
        fst_rot = [sb(f"fst_rot{i}", [128, 512], BF16) for i in range(2)]
        r1 = [sb(f"r1_{i}", [128, 512], F32) for i in range(2)]
        r2 = [sb(f"r2_{i}", [128, 512], F32) for i in range(2)]

        V = nc.vector
        A = nc.scalar
        nfm = 0
        ck(k, 0)
        for tb in range(S // 512):
            xTc = xTb[tb % 2]
            kx = ("xTb", tb % 2)
            for tt in range(4):
                t = tb * 4 + tt
                i2 = t % 2
                P.dma("sp", xt[i2][:], x_src[t * 128:(t + 1) * 128, :], writes=[("xt", i2)])
                P.op("act", lambda i2=i2: A.copy(out=xb[i2][:], in_=xt[i2][:]), reads=[("xt", i2)], writes=[("xb", i2)])
                for c in range(8):
                    P.op("pe", lambda c=c, i2=i2: nc.tensor.transpose(out=pT[:, c, :], in_=xb[i2][:, c * 128:(c + 1) * 128], identity=k.ident_b[:]),
                         reads=[("xb", i2), "ident_b"], writes=["pT"])
                P.op("dve", lambda tt=tt, xTc=xTc: V.tensor_copy(out=xTc[:, :, tt * 128:(tt + 1) * 128], in_=pT[:]), reads=["pT"], writes=[kx])
            ck(k, 1)
            blk = slice(tb * 512, (tb + 1) * 512)

            def fm(ch, dst, xTc=xTc, kx=kx):
                for c in range(8):
                    P.op("pe", lambda c=c, ch=ch, dst=dst, xTc=xTc: nc.tensor.matmul(dst[:], lhsT=wb[:, c, NCOLA + ch * 128:NCOLA + (ch + 1) * 128], rhs=xTc[:, c, :],
                                                                                     start=(c == 0), stop=(c == 7)), reads=[("wb", c), kx], writes=[("pm", id(dst))])
            plan = [(0, 4, 0, 4), (1, 5, 1, 5), (2, 6, 2, 6), (3, 7, 3, 7), (8, 9, None, 8), (10, 11, None, 9)]
            for (cr, cs, fraw, frot) in plan:
                ck(k, 1.5 + 0.01 * nfm)
                j = nfm % 2
                nfm += 1
                pA, pB = pm[2 * j], pm[2 * j + 1]
                fm(cr, pA)
                ck(k, 1.21)
                fm(cs, pB)
                ck(k, 1.22)
                if k.dbgv == 13:
                    P.op("dve", lambda j=j, pA=pA: V.tensor_tensor(out=r1[j][:], in0=pA[:], in1=k.ropeC[:, blk], op=ALU.mult),
                         reads=[("pm", id(pA))], writes=[("r1", j)])
                if fraw is not None and k.dbgv == 14:
                    P.op("dve", lambda j=j, pA=pA: V.tensor_copy(out=fst_raw[j][:], in_=pA[:]), reads=[("pm", id(pA))], writes=[("fst_raw", j)])
                elif fraw is not None:
                    P.op("act", lambda j=j, pA=pA: A.copy(out=fst_raw[j][:], in_=pA[:]), reads=[("pm", id(pA))] + ([("r1", j)] if k.dbgv == 13 else []), writes=[("fst_raw", j)])
                    ck(k, 1.23)
                    P.dma("pool", k.ft[fraw, :, blk], fst_raw[j][:], reads=[("fst_raw", j)], writes=[("ft", fraw, tb)])
                ck(k, 1.24)
                P.op("act", lambda j=j, pA=pA: A.copy(out=r1[j][:], in_=pA[:]), reads=[("pm", id(pA))], writes=[("r1", j)])
                P.op("act", lambda j=j, pB=pB: A.copy(out=r2[j][:], in_=pB[:]), reads=[("pm", id(pB))], writes=[("r2", j)])
                P.op("dve", lambda j=j, blk=blk: V.tensor_tensor(out=r1[j][:], in0=r1[j][:], in1=k.ropeC[:, blk], op=ALU.mult), reads=[("r1", j)], writes=[("r1", j)])
                P.op("dve", lambda j=j, blk=blk: V.tensor_tensor(out=r2[j][:], in0=r2[j][:], in1=k.ropeS[:, blk], op=ALU.mult), reads=[("r2", j)], writes=[("r2", j)])
                P.op("dve", lambda j=j: V.tensor_tensor(out=fst_rot[j][:], in0=r1[j][:], in1=r2[j][:], op=ALU.add),
                     reads=[("r1", j), ("r2", j)], writes=[("fst_rot", j)])
                ck(k, 1.243)
                P.dma("pool", k.ft[frot, :, blk], fst_rot[j][:], reads=[("fst_rot", j)], writes=[("ft", frot, tb)])
            for (cr, fi) in ((12, 10), (13, 11)):
                j = nfm % 2
                nfm += 1
                pA = pm[2 * j]
                fm(cr, pA)
                P.op("act", lambda j=j, pA=pA: A.copy(out=fst_raw[j][:], in_=pA[:]), reads=[("pm", id(pA))], writes=[("fst_raw", j)])
                P.dma("pool", k.ft[fi, :, blk], fst_raw[j][:], reads=[("fst_raw", j)], writes=[("ft", fi, tb)])

            ck(k, 2)
            for tt in range(4):
                t = tb * 4 + tt
                i2 = t % 2
                rows = slice(t * 128, (t + 1) * 128)
                xs = lambda c, xTc=xTc, tt=tt: xTc[:, c, tt * 128:(tt + 1) * 128]
                colgrp = [(0, 512), (512, 512), (1024, 512), (1536, 280)]
                for gi, (c0, wdt) in enumerate(colgrp):
                    for c in range(8):
                        P.op("pe", lambda c=c, gi=gi, c0=c0, wdt=wdt, xs=xs: nc.tensor.matmul(pm[gi][:, 0:wdt], lhsT=xs(c), rhs=wb[:, c, c0:c0 + wdt],
                                                                                       start=(c == 0), stop=(c == 7)),
                             reads=[("wb", c), kx], writes=[("pm", id(pm[gi]))])
                kp = [("pm", id(pm[gi])) for gi in range(4)]
                P.op("act", lambda: A.activation(out=W["qf"][:], in_=pm[0][:, 0:256], func=AF.Silu), reads=[kp[0]], writes=["qf"])
                P.op("act", lambda: A.activation(out=W["sg"][:], in_=pm[0][:, 256:512], func=AF.Sigmoid), reads=[kp[0]], writes=["sg"])
                P.op("act", lambda: A.activation(out=W["sgate"][:], in_=pm[1][:, 256:512], func=AF.Sigmoid), reads=[kp[1]], writes=["sgate"])
                P.op("act", lambda i2=i2: A.activation(out=gstage[i2][:], in_=pm[3][:, 256:280], func=AF.Sigmoid), reads=[kp[3]], writes=[("gstage", i2)])
                P.op("act", lambda: A.copy(out=vb[:], in_=pm[1][:, 0:256]), reads=[kp[1]], writes=["vb"])
                P.op("act", lambda i2=i2: A.copy(out=vstage[i2][:], in_=pm[3][:, 0:256]), reads=[kp[3]], writes=[("vstage", i2)])
                P.op("act", lambda: A.copy(out=gx[:], in_=pm[2][:]), reads=[kp[2]], writes=["gx"])
                P.dma("pool", k.tmv[rows, :], vstage[i2][:], reads=[("vstage", i2)], writes=[("tmv", t)])
                P.dma("pool", k.gts[rows, :], gstage[i2][:], reads=[("gstage", i2)], writes=[("gts", t)])

                yt = ytile[i2]
                ky = ("ytile", i2)
                def G1():
                    P.op("dve", lambda: V.tensor_tensor(out=gt[:], in0=gx[:], in1=gx[:], op=ALU.mult), reads=["gx"], writes=["gt"])
                    P.op("dve", lambda: V.tensor_scalar(out=gt[:], in0=gt[:], scalar1=0.044715, scalar2=1.0, op0=ALU.mult, op1=ALU.add), reads=["gt"], writes=["gt"])
                    P.op("dve", lambda: V.tensor_tensor(out=gt[:], in0=gt[:], in1=gx[:], op=ALU.mult), reads=["gt", "gx"], writes=["gt"])
                    P.op("act", lambda: A.activation(out=gt[:], in_=gt[:], func=AF.Sigmoid, scale=1.5957691216057308), reads=["gt"], writes=["gt"])
                    P.op("dve", lambda: V.tensor_tensor(out=gg[:], in0=gt[:], in1=gx[:], op=ALU.mult), reads=["gt", "gx"], writes=["gg"])
                def G2():
                    P.op("dve", lambda: V.bn_stats(out=bnst[:], in_=gg[:, 256:512]), reads=["gg"], writes=["bnst"])
                    P.op("dve", lambda: V.bn_aggr(out=bnag[:], in_=bnst[:]), reads=["bnst"], writes=["bnag"])
                    P.op("dve", lambda: V.tensor_scalar(out=rs4[:, 4:5], in0=bnag[:, 1:2], scalar1=1e-5, scalar2=None, op0=ALU.add),
                         reads=["bnag"], writes=["rs4b"])
                    P.op("act", lambda: A.activation(out=rs4[:, 4:5], in_=rs4[:, 4:5], func=AF.Ln), reads=["rs4b"], writes=["rs4b"])
                    P.op("act", lambda: A.activation(out=rs4[:, 4:5], in_=rs4[:, 4:5], func=AF.Exp, scale=-0.5), reads=["rs4b"], writes=["rs4b"])
                    P.op("dve", lambda: V.tensor_scalar(out=W["ginn"][:], in0=gg[:, 256:512], scalar1=bnag[:, 0:1], scalar2=rs4[:, 4:5],
                                                        op0=ALU.subtract, op1=ALU.mult), reads=["gg", "bnag", "rs4b"], writes=["ginn"])
                    P.op("dve", lambda: V.tensor_tensor(out=W["ginn"][:], in0=W["ginn"][:], in1=gm_lnw[:], op=ALU.mult), reads=["ginn", "gm_lnw"], writes=["ginn"])
                    P.op("dve", lambda: V.tensor_tensor(out=vln[:], in0=W["ginn"][:], in1=gm_lnb[:], op=ALU.add), reads=["ginn", "gm_lnb"], writes=["vln"])
                def G3():
                    for g in range(4):
                        P.op("pe", lambda g=g: nc.tensor.matmul(pb[:, 256 + g * 64:256 + (g + 1) * 64], lhsT=wsT[:, g, :], rhs=vln[:, g * 64:(g + 1) * 64],
                                                                start=True, stop=True), reads=["wsT", "vln"], writes=["pb"])
                    P.op("act", lambda: A.copy(out=W["ginn"][:], in_=pb[:, 256:512]), reads=["pb", "ginn"], writes=["ginn"])
                    for g in range(4):
                        P.op("dve", lambda g=g: V.scalar_tensor_tensor(out=W["gsv"][:, g * 64:(g + 1) * 64], in0=W["ginn"][:, g * 64:(g + 1) * 64],
                                                                      scalar=bsT[:, g:g + 1], in1=gg[:, g * 64:(g + 1) * 64], op0=ALU.add, op1=ALU.mult),
                             reads=["ginn", "bsT", "gg"], writes=["gsv"])
                def G4():
                    P.op("act", lambda: A.activation(out=W["gsq"][:], in_=W["gsv"][:], func=AF.Square), reads=["gsv"], writes=["gsq"])
                    P.op("dve", lambda: V.tensor_reduce(out=ss4[:, 4:8], in_=W["gsq"][:].rearrange("p (h d) -> p h d", h=4), axis=AX.X, op=ALU.add),
                         reads=["gsq"], writes=["ss4b"])
                    P.op("dve", lambda: V.tensor_scalar(out=ss4[:, 4:8], in0=ss4[:, 4:8], scalar1=1.0 / 64, scalar2=1e-6, op0=ALU.mult, op1=ALU.add),
                         reads=["ss4b"], writes=["ss4b"])
                    P.op("act", lambda: A.activation(out=ss4[:, 4:8], in_=ss4[:, 4:8], func=AF.Ln), reads=["ss4b"], writes=["ss4b"])
                    P.op("act", lambda: A.activation(out=ss4[:, 4:8], in_=ss4[:, 4:8], func=AF.Exp, scale=-0.5), reads=["ss4b"], writes=["ss4b"])
                    for g in range(4):
                        P.op("dve", lambda g=g, yt=yt: V.scalar_tensor_tensor(out=yt[:, 256 + g * 64:256 + (g + 1) * 64], in0=W["gsv"][:, g * 64:(g + 1) * 64],
                                                                             scalar=ss4[:, 4 + g:5 + g], in1=gm_nw[:, g * 64:(g + 1) * 64],
                                                                             op0=ALU.mult, op1=ALU.mult), reads=["gsv", "ss4b", "gm_nw"], writes=[ky])
                ck(k, 3)
                if l == 0:
                    fsrc = W["sg"]
                    kf = "sg"
                else:
                    P.op("dve", lambda: V.tensor_tensor(out=W["t1"][:], in0=W["sg"][:], in1=oml[:], op=ALU.mult), reads=["sg", "oml"], writes=["t1"])
                    P.op("dve", lambda: V.tensor_tensor(out=W["t1"][:], in0=W["t1"][:], in1=lb[:], op=ALU.add), reads=["t1", "lb"], writes=["t1"])
                    fsrc = W["t1"]
                    kf = "t1"
                P.op("act", lambda fsrc=fsrc: A.activation(out=W["logf"][:], in_=fsrc[:], func=AF.Ln), reads=[kf], writes=["logf"])
                P.op("dve", lambda fsrc=fsrc: V.tensor_scalar(out=W["kk"][:], in0=fsrc[:], scalar1=-1.0, scalar2=1.0, op0=ALU.mult, op1=ALU.add),
                     reads=[kf], writes=["kk"])
                G1()
                ck(k, 3.5)
                lf = W["logf"]
                P.op("dve", lambda: V.tensor_copy(out=lfh[:], in_=lf[:]), reads=["logf"], writes=["lfh"])
                P.op("dve", lambda: V.tensor_tensor(out=W["t2"][:], in0=lf[:], in1=lfh[:], op=ALU.subtract), reads=["logf", "lfh"], writes=["t2"])
                P.op("dve", lambda: V.tensor_copy(out=lfl[:], in_=W["t2"][:]), reads=["t2"], writes=["lfl"])
                for (dst, mat, kn) in ((pa[:, 0:256], k.mmid_b, "pa"), (pa[:, 256:512], k.mcum_b, "pa"), (pb[:, 0:256], k.mrem_b, "pb")):
                    P.op("pe", lambda dst=dst, mat=mat: nc.tensor.matmul(dst, lhsT=mat[:], rhs=lfh[:], start=True, stop=False), reads=["lfh"], writes=[kn])
                    P.op("pe", lambda dst=dst, mat=mat: nc.tensor.matmul(dst, lhsT=mat[:], rhs=lfl[:], start=False, stop=True), reads=["lfl"], writes=[kn])
                for c2 in range(2):
                    for hh in range(2):
                        for pi, part in enumerate((lfh, lfl)):
                            P.op("pe", lambda c2=c2, hh=hh, part=part, pi=pi: nc.tensor.matmul((pb[:, 256 + hh:257 + hh] if c2 == 0 else pm[2][:, hh:hh + 1]),
                                                                                             lhsT=part[c2 * 64:(c2 + 1) * 64, hh * 128:(hh + 1) * 128],
                                                                                             rhs=k.ones_b[c2 * 64:(c2 + 1) * 64, 0:1], start=(pi == 0), stop=(pi == 1)),
                                 reads=["lfh", "lfl", "ones_b"], writes=(["pb"] if c2 == 0 else [kp[2]]))
                G2()
                P.op("act", lambda: A.activation(out=W["ebm"][:], in_=pa[:, 0:256], func=AF.Exp), reads=["pa"], writes=["ebm"])
                P.op("act", lambda: A.activation(out=W["enbm"][:], in_=pa[:, 0:256], func=AF.Exp, scale=-1.0), reads=["pa"], writes=["enbm"])
                P.op("act", lambda: A.activation(out=W["eb"][:], in_=pa[:, 256:512], func=AF.Exp), reads=["pa"], writes=["eb"])
                P.op("act", lambda: A.activation(out=W["erem"][:], in_=pb[:, 0:256], func=AF.Exp), reads=["pb"], writes=["erem"])
                P.op("act", lambda: A.activation(out=ebl[:, 0, :], in_=pb[:, 256:258], func=AF.Exp), reads=["pb"], writes=["ebl"])
                P.op("act", lambda: A.activation(out=ebl[:, 1, :], in_=pm[2][:, 0:2], func=AF.Exp), reads=[kp[2]], writes=["ebl"])
                P.op("dve", lambda: V.tensor_tensor(out=qe[:], in0=W["qf"][:], in1=W["ebm"][:], op=ALU.mult), reads=["qf", "ebm"], writes=["qe"])
                P.op("dve", lambda: V.tensor_tensor(out=ke[:], in0=W["kk"][:], in1=W["enbm"][:], op=ALU.mult), reads=["kk", "enbm"], writes=["ke"])
                P.op("dve", lambda: V.tensor_tensor(out=qb[:], in0=W["qf"][:], in1=W["eb"][:], op=ALU.mult), reads=["qf", "eb"], writes=["qb"])
                P.op("dve", lambda: V.tensor_tensor(out=kd[:], in0=W["kk"][:], in1=W["erem"][:], op=ALU.mult), reads=["kk", "erem"], writes=["kd"])
                G3()
                for i, (src, kn) in enumerate(((qe, "qe"), (ke, "ke"), (qb, "qb"))):
                    for hh in range(2):
                        P.op("pe", lambda i=i, hh=hh, src=src: nc.tensor.transpose(out=pcb[:, (i * 2 + hh) * 128:(i * 2 + hh + 1) * 128],
                                                                                   in_=src[:, hh * 128:(hh + 1) * 128], identity=k.ident_b[:]),
                             reads=[kn, "ident_b"], writes=["pc"])
                P.op("dve", lambda: V.tensor_copy(out=trT[:].rearrange("p a b -> p (a b)"), in_=pcb[:, 0:768]), reads=["pc"], writes=["trT"])

                ck(k, 4)

                def hT(i, h):
                    return trT[(h % 2) * 64:(h % 2) * 64 + 64, i * 2 + h // 2, :]
                for h in range(4):
                    ck(k, 4.01 + 0.01 * h)
                    dst = (pa if h % 2 == 0 else pm[0])[:, (h // 2) * 128:(h // 2 + 1) * 128]
                    P.op("pe", lambda h=h, dst=dst: nc.tensor.matmul(dst, lhsT=hT(1, h), rhs=hT(0, h), start=True, stop=True),
                         reads=["trT"], writes=["pa" if h % 2 == 0 else kp[0]])
                ck(k, 4.1)
                P.op("act", lambda: A.copy(out=gt[:, 0:256], in_=pa[:, 0:256]), reads=["pa"], writes=["gt"])
                P.op("act", lambda: A.copy(out=gt[:, 256:512], in_=pm[0][:, 0:256]), reads=[kp[0]], writes=["gt"])
                ck(k, 4.2)
                for h in range(4):
                    gb = (h % 2) * 2 + h // 2
                    P.op("dve", lambda h=h, gb=gb: V.tensor_tensor(out=attb[:, h, :], in0=gt[:, gb * 128:(gb + 1) * 128], in1=k.mcum[:], op=ALU.mult),
                         reads=["gt", "mcum"], writes=["attb"])
                G4()
                ck(k, 4.5)
                def obank(h):
                    return ((pc, "pc"), (pm[1], kp[1]), (pm[3], kp[3]), (pm[0], kp[0]))[h]
                for h in range(4):
                    ob, okey = obank(h)
                    P.op("pe", lambda h=h, ob=ob: nc.tensor.matmul(ob[:, 0:64], lhsT=attb[:, h, :], rhs=vb[:, h * 64:(h + 1) * 64],
                                                                   start=True, stop=False), reads=["attb", "vb", "trT"], writes=[okey])
                for c2 in range(2):
                    rs = slice(c2 * 64, (c2 + 1) * 64)
                    for h in range(4):
                        hp = slice((h % 2) * 64, (h % 2) * 64 + 64)
                        ob, okey = obank(h)
                        P.op("pe", lambda h=h, rs=rs, hp=hp, ob=ob: nc.tensor.matmul(ob[rs, 0:64], lhsT=hT(2, h)[:, rs], rhs=st_b[hp, h // 2, :],
                                                                                     start=False, stop=True), reads=["trT", "st_b"], writes=[okey])
                    for hh in range(2):
                        P.op("pe", lambda hh=hh, rs=rs: nc.tensor.matmul(pa[:, hh * 128:(hh + 1) * 128], lhsT=kd[rs, hh * 128:(hh + 1) * 128],
                                                                         rhs=vb[rs, hh * 128:(hh + 1) * 128], start=True, stop=True),
                             reads=["kd", "vb", "attb"], writes=["pa"])
                    P.op("act", lambda: A.copy(out=W["t2"][:], in_=pa[:, 0:256]), reads=["pa"], writes=["t2"])
                    for h in range(4):
                        hp = slice((h % 2) * 64, (h % 2) * 64 + 64)
                        P.op("dve", lambda h=h, c2=c2, hp=hp: V.scalar_tensor_tensor(out=st_f[hp, h // 2, :], in0=st_f[hp, h // 2, :],
                                                                                    scalar=ebl[hp, c2, h // 2:h // 2 + 1],
                                                                                    in1=W["t2"][hp, h * 64:(h + 1) * 64], op0=ALU.mult, op1=ALU.add),
                             reads=["st_f", "ebl", "t2"], writes=["st_f"])
                    P.op("dve", lambda: V.tensor_copy(out=st_b[:], in_=st_f[:]), reads=["st_f"], writes=["st_b"])
                ck(k, 5)
                tk = (["t1"] if l > 0 else [])
                for h in range(4):
                    ob, okey = obank(h)
                    P.op("act", lambda h=h, ob=ob: A.copy(out=W["t1"][:, h * 64:(h + 1) * 64], in_=ob[:, 0:64]), reads=[okey] + tk, writes=["osb"] + tk)
                P.op("act", lambda: A.activation(out=W["osq"][:], in_=W["t1"][:], func=AF.Square), reads=["osb"], writes=["osq"])
                P.op("dve", lambda: V.tensor_reduce(out=ss4[:, 0:4], in_=W["osq"][:].rearrange("p (h d) -> p h d", h=4), axis=AX.X, op=ALU.add),
                     reads=["osq"], writes=["ss4"])
                P.op("dve", lambda: V.tensor_scalar(out=rs4[:, 0:4], in0=ss4[:, 0:4], scalar1=1.0 / 64, scalar2=1e-6, op0=ALU.mult, op1=ALU.add),
                     reads=["ss4"], writes=["rs4"])
                P.op("act", lambda: A.activation(out=rs4[:, 0:4], in_=rs4[:, 0:4], func=AF.Ln), reads=["rs4"], writes=["rs4"])
                P.op("act", lambda: A.activation(out=rs4[:, 0:4], in_=rs4[:, 0:4], func=AF.Exp, scale=-0.5), reads=["rs4"], writes=["rs4"])
                P.op("dve", lambda: V.tensor_tensor(out=W["nwg"][:], in0=W["sgate"][:], in1=hg_nw[:], op=ALU.mult), reads=["sgate", "hg_nw"], writes=["nwg"])
                for h in range(4):
                    P.op("dve", lambda h=h, yt=yt: V.scalar_tensor_tensor(out=yt[:, h * 64:(h + 1) * 64], in0=W["t1"][:, h * 64:(h + 1) * 64],
                                                                         scalar=rs4[:, h:h + 1], in1=W["nwg"][:, h * 64:(h + 1) * 64],
                                                                         op0=ALU.mult, op1=ALU.mult), reads=["osb", "rs4", "nwg"], writes=[ky])

                P.dma("pool", k.y[rows, 0:512], yt[:], reads=[ky], writes=[("y", t)])
                if t == 0:
                    dump(k, "gg", gg[:], "gg", 512)
                    dump(k, "bnag", bnag[:], "bnag", 2)
                    dump(k, "rs4", rs4[:], "rs4b", 8)
                    dump(k, "vln", vln[:], "vln", 256)
                    dump(k, "gsv", W["gsv"][:], "gsv", 256)
                    dump(k, "ss4", ss4[:], "ss4b", 8)
                    dump(k, "wsT", wsT[:].rearrange("p a b -> p (a b)"), "wsT", 512)
                    dump(k, "logf", W["logf"][:], "logf", 256)
                    dump(k, "kk", W["kk"][:], "kk", 256)
                    dump(k, "qf", W["qf"][:], "qf", 256)
                    dump(k, "ebm", W["ebm"][:], "ebm", 256)
                    dump(k, "eb", W["eb"][:], "eb", 256)
                    dump(k, "erem", W["erem"][:], "erem", 256)
                    dump(k, "ebl", ebl[:].rearrange("p a b -> p (a b)"), "ebl", 4)
                    dump(k, "osb", W["t1"][:], "osb", 256)
                    dump(k, "attb", attb[:].rearrange("p a b -> p (a b)"), "attb", 512)
                    dump(k, "st_f", st_f[:].rearrange("p a b -> p (a b)"), "st_f", 128)
                    dump(k, "rs4h", rs4[:], "rs4", 8)
                    dump(k, "nwg", W["nwg"][:], "nwg", 256)
                ck(k, 7)


NSA_BIG = 30000.0


def phase2(k, l):
    nc, P = k.nc, k.P
    V, A, G = nc.vector, nc.scalar, nc.gpsimd
    with ExitStack() as es:
        sb = lambda name, shape, dt: es.enter_context(nc.sbuf_tensor(f"p2l{l}_{name}", shape, dt))
        ps = lambda name, shape, dt: es.enter_context(nc.psum_tensor(f"p2l{l}_{name}", shape, dt))
        edup = sb("edup", [128, 32, 128], BF16)
        win01 = sb("win01", [128, 8, 512], BF16)
        cmp01 = sb("cmp01", [128, 5, 512], BF16)
        impkeep = sb("impkeep", [128, NT, 64], F32)
        impadd = sb("impadd", [128, NT, 64], F32)
        ovext = sb("ovext", [128, 2, 65], BF16)
        wpad = sb("wpad", [128, NT], F32)
        gts = sb("gts", [128, NT, 24], F32)
        nw = sb("nw", [128, 512], F32)
        w1 = [sb(f"w1_{i}", [64, 32, 128], BF16) for i in range(2)]
        w2kd = sb("w2kd", [128, 128], BF16)
        w2v = sb("w2v", [128, 64], BF16)
        peT = sb("peT", [64, 2, 32], BF16)
        craw = sb("craw", [64, S], BF16)
        cb = sb("cb", [128, 1], F32)
        hx = sb("hx", [128, 256], F32)
        ht = sb("ht", [128, 256], F32)
        hb = sb("hb", [128, 256], BF16)
        kcd = sb("kcd", [128, 256], BF16)
        vc = sb("vc", [128, 2, 65], BF16)
        qr = [sb(f"qr{i}", [128, S], BF16) for i in range(2)]
        qo = [sb(f"qo{i}", [128, S], BF16) for i in range(2)]
        ksd = sb("ksd", [128, S], BF16)
        kwd = sb("kwd", [128, S], BF16)
        vsg = sb("vsg", [128, NT, 65], BF16)
        vwg = sb("vwg", [128, NT, 65], BF16)
        ec = [[[sb(f"ec{ci}{par}{ct}", [128, 512], BF16) for ct in range(2)] for par in range(2)] for ci in range(2)]
        pt = [[sb(f"pt{par}{i}", [128, 512], BF16) for i in range(2)] for par in range(2)]
        osb = [sb(f"osb{par}", [65, 512], F32) for par in range(2)]
        obs = [sb(f"ob{i}", [128, 3, 4, 4, 65], F32) for i in range(2)]
        impsb = sb("impsb", [128, 4, 4, 64], F32)
        impw = sb("impw", [128, 4, 64], F32)
        imp = sb("imp", [128, 64], F32)
        imp3 = sb("imp3", [128, 64], F32)
        m8a = sb("m8a", [128, 8], F32)
        m8b = sb("m8b", [128, 8], F32)
        rdn = sb("rdn", [128, 4], F32)
        mdup = sb("mdup", [128, 4, 128], BF16)
        MT = sb("MT", [128, 512], BF16)
        den3 = sb("den3", [128, 3, 4], F32)
        coef = sb("coef", [128, 3, 4], F32)
        acc = sb("acc", [128, 4, 64], F32)
        tm2 = sb("tm2", [128, 4, 64], F32)
        ss = sb("ss", [128, 4], F32)
        yst = [sb(f"yst{i}", [128, 256], BF16) for i in range(2)]
        Sb = [[ps(f"S{par}{i}", [128, 512], F32) for i in range(2)] for par in range(2)]
        Ob = [ps(f"O{par}", [128, 512], F32) for par in range(2)]
        X = ps("X", [128, 512], F32)
        Y = ps("Y", [128, 512], F32)
        Xb = X[:].bitcast(BF16)

        flat = lambda t: t[:].rearrange("p a b -> p (a b)")

        def cast_load(dst_flat, src_flat, n, key):
            for o in range(0, n, 2048):
                w = min(2048, n - o)
                P.dma("pool", dst_flat[:, o:o + w], src_flat[:, o:o + w], writes=[key])
        cast_load(flat(edup), k.cst["edup"].rearrange("p a b -> p (a b)"), 32 * 128, "edup")
        cast_load(flat(win01), k.cst["win01"].rearrange("p a b -> p (a b)"), 8 * 512, "win01")
        cast_load(flat(cmp01), k.cst["cmp01"].rearrange("p a b -> p (a b)"), 5 * 512, "cmp01")
        cast_load(flat(ovext), k.cst["ovext"].rearrange("p a b -> p (a b)"), 130, "ovext")
        P.op("dve", lambda: V.tensor_scalar(out=flat(win01), in0=flat(win01), scalar1=-1.0, scalar2=NSA_BIG, op0=ALU.add, op1=ALU.mult), reads=["win01"], writes=["win01"])
        P.op("dve", lambda: V.tensor_scalar(out=flat(cmp01), in0=flat(cmp01), scalar1=-1.0, scalar2=NSA_BIG, op0=ALU.add, op1=ALU.mult), reads=["cmp01"], writes=["cmp01"])
        P.dma("sp", impkeep[:], k.cst["impkeep"], writes=["impkeep"])
        P.dma("sp", impadd[:], k.cst["impadd"], writes=["impadd"])
        P.dma("sp", wpad[:], k.cst["wpad"], writes=["wpad"])
        P.dma("sp", gts[:], k.gts.rearrange("(t p) c -> p t c", p=128), writes=["gts"])
        bcast_row(k, P, "sp", nw[:], k.nsa_nw[l:l + 1, :], "nw")
        for kv in range(2):
            wv = k.nsa_w1[l, kv].rearrange("(j d) m -> d j m", d=64)
            for hh in range(2):
                P.dma("pool", w1[kv][:, hh * 16:(hh + 1) * 16, :], wv[:, hh * 16:(hh + 1) * 16, :], writes=[("w1", kv)])
        P.dma("pool", w2kd[:, 0:64], k.nsa_w2[l, 0], writes=["w2kd"])
        P.dma("pool", w2kd[:, 64:128], k.nsa_w2[l, 0], writes=["w2kd"])
        P.dma("pool", w2v[:], k.nsa_w2[l, 1], writes=["w2v"])
        P.dma("pool", peT[:], k.nsa_pe[l].rearrange("k j d -> d k j"), writes=["peT"], allow_slow_non_contiguous=True)

        def evac_O(par, br, hl, ob, okey):
            P.op("act", lambda par=par: A.copy(out=osb[par][:], in_=Ob[par][0:65, :]), reads=[("O", par)], writes=[("osb", par)])
            for qt in range(4):
                P.op("pe", lambda par=par, qt=qt: nc.tensor.transpose(out=X[:, qt * 65:(qt + 1) * 65], in_=osb[par][:, qt * 128:(qt + 1) * 128], identity=k.ident_f[0:65, 0:65]),
                     reads=[("osb", par), "ident"], writes=["X"])
            P.op("act", lambda br=br, hl=hl, ob=ob: A.copy(out=ob[:, br, :, hl, :], in_=X[:, 0:260].rearrange("p (a b) -> p a b", a=4)), reads=["X"], writes=[(okey, br)])

        for g in range(2):
            for kv in range(2):
                P.dma("sp", craw[:], k.ft[10 + kv, g * 64:(g + 1) * 64, :], writes=["craw"])
                for j in range(32):
                    P.op("pe", lambda j=j, kv=kv: nc.tensor.matmul(X[:, 300:301], lhsT=w1[kv][:, j, :], rhs=peT[:, kv, j:j + 1], start=(j == 0), stop=(j == 31)),
                         reads=[("w1", kv), "peT"], writes=["X"])
                for j in range(32):
                    P.op("pe", lambda j=j, kv=kv: nc.tensor.matmul(X[:, 0:255], lhsT=w1[kv][:, j, :], rhs=craw[:, j:j + 16 * 254 + 1:16], start=(j == 0), stop=(j == 31)),
                         reads=[("w1", kv), "craw"], writes=["X"])
                P.op("act", lambda: A.copy(out=cb[:], in_=X[:, 300:301]), reads=["X"], writes=["cb"])
                P.op("dve", lambda: V.memset(hx[:], 0.0), writes=["hx"])
                P.op("act", lambda: A.activation(out=hx[:, 0:255], in_=X[:, 0:255], func=AF.Identity, bias=cb[:, 0:1], scale=1.0), reads=["X", "cb", "hx"], writes=["hx"])
                P.op("dve", lambda: V.tensor_tensor(out=ht[:], in0=hx[:], in1=hx[:], op=ALU.mult), reads=["hx"], writes=["ht"])
                P.op("dve", lambda: V.tensor_scalar(out=ht[:], in0=ht[:], scalar1=0.044715, scalar2=1.0, op0=ALU.mult, op1=ALU.add), reads=["ht"], writes=["ht"])
                P.op("dve", lambda: V.tensor_tensor(out=ht[:], in0=ht[:], in1=hx[:], op=ALU.mult), reads=["ht", "hx"], writes=["ht"])
                P.op("act", lambda: A.activation(out=ht[:], in_=ht[:], func=AF.Sigmoid, scale=1.5957691216057308), reads=["ht"], writes=["ht"])
                P.op("dve", lambda: V.tensor_tensor(out=hb[:], in0=ht[:], in1=hx[:], op=ALU.mult), reads=["ht", "hx"], writes=["hb"])
                if kv == 0:
                    P.op("pe", lambda: nc.tensor.matmul(X[:, 0:256], lhsT=w2kd[:], rhs=hb[:], start=True, stop=True), reads=["w2kd", "hb"], writes=["X"])
                    P.op("act", lambda: A.copy(out=kcd[:], in_=X[:, 0:256]), reads=["X"], writes=["kcd"])
                else:
                    for ct in range(2):
                        P.op("pe", lambda ct=ct: nc.tensor.matmul(X[:, ct * 64:(ct + 1) * 64], lhsT=hb[:, ct * 128:(ct + 1) * 128], rhs=w2v[:], start=True, stop=True),
                             reads=["w2v", "hb"], writes=["X"])
                    P.op("dve", lambda: V.memset(vc[:], 1.0), writes=["vc"])
                    P.op("act", lambda: A.copy(out=vc[:, :, 0:64], in_=X[:, 0:128].rearrange("p (a b) -> p a b", a=2)), reads=["X", "vc"], writes=["vc"])
            for ci in range(2):
                P.dma("sp", qr[ci][:], k.ft[2 * g + ci], writes=[("qr", ci)])
                P.dma("act", qo[ci][:], k.ft[4 + 2 * g + ci], writes=[("qo", ci)])
            for hh in range(2):
                P.dma("sp", ksd[hh * 64:(hh + 1) * 64, :], k.ft[8, g * 64:(g + 1) * 64, :], writes=["ksd"])
                P.dma("act", kwd[hh * 64:(hh + 1) * 64, :], k.ft[9, g * 64:(g + 1) * 64, :], writes=["kwd"])
            P.op("dve", lambda: V.memset(vsg[:], 1.0), writes=["vsg"])
            P.op("dve", lambda: V.memset(vwg[:], 1.0), writes=["vwg"])
            P.dma("sp", vsg[:, :, 0:64], k.tmv[:, g * 64:(g + 1) * 64].rearrange("(t p) c -> p t c", p=128), reads=["vsg"], writes=["vsg"])
            P.dma("act", vwg[:, :, 0:64], k.tmv[:, 128 + g * 64:128 + (g + 1) * 64].rearrange("(t p) c -> p t c", p=128), reads=["vwg"], writes=["vwg"])

            for Q in range(S // 512):
                qs = slice(Q * 512, (Q + 1) * 512)
                ob = obs[Q % 2]
                okey = "ob%d" % (Q % 2)
                cts = []
                for ct in range(2):
                    u = 512 * Q - 2048 * ct
                    if u < -480:
                        continue
                    cts.append((ct, (None if u > 2048 else (0, 512, 1024, 1536, 2048).index(u))))
                for ci in range(2):
                    for n_, (ct, mi) in enumerate(cts):
                        for par in range(2):
                            hp = slice(par * 64, (par + 1) * 64)
                            P.op("pe", lambda par=par, hp=hp, ct=ct, ci=ci, qs=qs, mi=mi: nc.tensor.matmul(Sb[par][0][:], lhsT=kcd[hp, ct * 128:(ct + 1) * 128], rhs=qr[ci][hp, qs], start=True, stop=(mi is None)),
                                 reads=["kcd", ("qr", ci)], writes=[("S", par, 0)])
                        if mi is not None:
                            for par in range(2):
                                P.op("pe", lambda par=par, mi=mi: nc.tensor.matmul(Sb[par][0][:], lhsT=k.ident_b[:], rhs=cmp01[:, mi, :], start=False, stop=True),
                                     reads=["cmp01", "ident_b"], writes=[("S", par, 0)])
                        for par in range(2):
                            e_ = ec[ci][par][ct]
                            ke = ("ec", ci, par, ct)
                            P.op("act", lambda par=par, e_=e_: A.activation(out=e_[:], in_=Sb[par][0][:], func=AF.Exp, scale=0.125), reads=[("S", par, 0)], writes=[ke])
                            P.op("pe", lambda par=par, ct=ct, e_=e_, n_=n_, nk=len(cts): nc.tensor.matmul(Ob[par][0:65, :], lhsT=vc[:, ct, :], rhs=e_[:], start=(n_ == 0), stop=(n_ == nk - 1)),
                                 reads=["vc", ke], writes=[("O", par)])
                    for par in range(2):
                        evac_O(par, 0, 2 * ci + par, ob, okey)
                for half in range(2):
                    for q2 in range(2):
                        qt = half * 2 + q2
                        for hl in range(4):
                            ci, par = hl // 2, hl % 2
                            for n_, (ct, mi) in enumerate(cts):
                                e_ = ec[ci][par][ct]
                                P.op("pe", lambda e_=e_, ct=ct, qt=qt, q2=q2, hl=hl, n_=n_, nk=len(cts): nc.tensor.matmul(Y[:, q2 * 256 + hl * 64:q2 * 256 + (hl + 1) * 64],
                                                                                                                        lhsT=e_[:, qt * 128:(qt + 1) * 128], rhs=ovext[:, ct, 0:64],
                                                                                                                        start=(n_ == 0), stop=(n_ == nk - 1)),
                                     reads=[("ec", ci, par, ct), "ovext"], writes=["Y"])
                    P.op("act", lambda half=half: A.copy(out=impsb[:, half * 2:(half + 1) * 2, :, :].rearrange("p a b c -> p (a b c)"), in_=Y[:]), reads=["Y"], writes=["impsb"])
                for qt in range(4):
                    t = Q * 4 + qt
                    P.op("dve", lambda qt=qt, ob=ob: V.tensor_scalar(out=rdn[:], in0=ob[:, 0, qt, :, 64], scalar1=1e-30, scalar2=None, op0=ALU.max), reads=[(okey, 0)], writes=["rdn"])
                    P.op("dve", lambda: V.reciprocal(out=rdn[:], in_=rdn[:]), reads=["rdn"], writes=["rdn"])
                    P.op("dve", lambda qt=qt: V.tensor_tensor(out=impw[:], in0=impsb[:, qt, :, :], in1=rdn[:].unsqueeze(2).to_broadcast([128, 4, 64]), op=ALU.mult),
                         reads=["impsb", "rdn"], writes=["impw"])
                    P.op("dve", lambda: V.tensor_reduce(out=imp[:], in_=impw[:].rearrange("p h j -> p j h"), axis=AX.X, op=ALU.add), reads=["impw"], writes=["imp"])
                    P.op("dve", lambda t=t: V.tensor_tensor(out=imp[:], in0=imp[:], in1=impkeep[:, t, :], op=ALU.mult), reads=["imp", "impkeep"], writes=["imp"])
                    P.op("dve", lambda t=t: V.tensor_tensor(out=imp[:], in0=imp[:], in1=impadd[:, t, :], op=ALU.add), reads=["imp", "impadd"], writes=["imp"])
                    P.op("dve", lambda: V.max(out=m8a[:], in_=imp[:]), reads=["imp"], writes=["m8a"])
                    P.op("dve", lambda: V.match_replace(out=imp3[:], in_to_replace=m8a[:], in_values=imp[:], imm_value=-3.0e9), reads=["imp", "m8a"], writes=["imp3"])
                    P.op("dve", lambda: V.max(out=m8b[:], in_=imp3[:]), reads=["imp3"], writes=["m8b"])
                    P.op("dve", lambda: V.tensor_scalar(out=imp3[:], in0=imp[:], scalar1=m8b[:, 7:8], scalar2=None, op0=ALU.is_ge), reads=["imp", "m8b", "imp3"], writes=["imp3"])
                    for dd in range(2):
                        P.op("dve", lambda qt=qt, dd=dd: V.tensor_scalar(out=mdup[:, qt, dd * 64:(dd + 1) * 64], in0=imp3[:], scalar1=-1.0, scalar2=NSA_BIG, op0=ALU.add, op1=ALU.mult),
                             reads=["imp3"], writes=["mdup"])

                def attend(br, kd_, kkey, vt, vkey):
                    kt0 = 0 if br == 1 else max(0, 4 * Q - 4)
                    kts = list(range(kt0, 4 * Q + 4))
                    for ci in range(2):
                        def scores(n_, ci=ci):
                            kt = kts[n_]
                            bi = n_ % 2
                            dl = kt - 4 * Q
                            masked = (br == 2 or dl >= 0)
                            for par in range(2):
                                hp = slice(par * 64, (par + 1) * 64)
                                P.op("pe", lambda par=par, hp=hp, kt=kt, ci=ci, bi=bi, qs=qs, kd_=kd_, last=(br == 2 and not masked): nc.tensor.matmul(Sb[par][bi][:], lhsT=kd_[hp, kt * 128:(kt + 1) * 128], rhs=qo[ci][hp, qs],
                                                                                                                              start=True, stop=last),
                                     reads=[kkey, ("qo", ci)], writes=[("S", par, bi)])
                            if br == 1:
                                for par in range(2):
                                    hp = slice(par * 64, (par + 1) * 64)
                                    P.op("pe", lambda par=par, hp=hp, kt=kt, bi=bi, masked=masked: nc.tensor.matmul(Sb[par][bi][:], lhsT=edup[hp, kt, :], rhs=MT[hp, :], start=False, stop=(not masked)),
                                         reads=["edup", "MT"], writes=[("S", par, bi)])
                            if masked:
                                for par in range(2):
                                    P.op("pe", lambda par=par, bi=bi, dl=dl: nc.tensor.matmul(Sb[par][bi][:], lhsT=k.ident_b[:], rhs=win01[:, dl + 4, :], start=False, stop=True),
                                         reads=["win01", "ident_b"], writes=[("S", par, bi)])
                        scores(0)
                        for n_, kt in enumerate(kts):
                            bi = n_ % 2
                            if n_ + 1 < len(kts):
                                scores(n_ + 1)
                            for par in range(2):
                                p_ = pt[par][bi]
                                kp_ = ("pt", par, bi)
                                P.op("act", lambda par=par, bi=bi, p_=p_: A.activation(out=p_[:], in_=Sb[par][bi][:], func=AF.Exp, scale=0.125), reads=[("S", par, bi)], writes=[kp_])
                                P.op("pe", lambda par=par, kt=kt, p_=p_, n_=n_, vt=vt, nk=len(kts): nc.tensor.matmul(Ob[par][0:65, :], lhsT=vt[:, kt, :], rhs=p_[:], start=(n_ == 0), stop=(n_ == nk - 1)),
                                     reads=[vkey, kp_], writes=[("O", par)])
                        for par in range(2):
                            evac_O(par, br, 2 * ci + par, ob, okey)
                attend(2, kwd, "kwd", vwg, "vwg")
                for qt in range(4):
                    P.op("pe", lambda qt=qt: nc.tensor.transpose(out=Xb[:, qt * 128:(qt + 1) * 128], in_=mdup[:, qt, :], identity=k.ident_b[:]), reads=["mdup", "ident_b"], writes=["X"])
                P.op("dve", lambda: V.tensor_copy(out=MT[:], in_=Xb[:, 0:512]), reads=["X"], writes=["MT"])
                attend(1, ksd, "ksd", vsg, "vsg")
                for qt in range(4):
                    t = Q * 4 + qt
                    i2 = qt % 2
                    P.op("dve", lambda qt=qt, ob=ob: V.tensor_scalar(out=den3[:], in0=ob[:, :, qt, :, 64], scalar1=1e-30, scalar2=None, op0=ALU.max), reads=[(okey, 0), (okey, 1), (okey, 2)], writes=["den3"])
                    if Q == 0:
                        P.op("dve", lambda t=t: V.tensor_scalar(out=den3[:, 2, :], in0=den3[:, 2, :], scalar1=wpad[:, t:t + 1], scalar2=None, op0=ALU.add),
                             reads=["den3", "wpad"], writes=["den3"])
                    P.op("dve", lambda: V.reciprocal(out=den3[:], in_=den3[:]), reads=["den3"], writes=["den3"])
                    P.op("dve", lambda t=t, g=g: V.tensor_tensor(out=coef[:], in0=den3[:], in1=gts[:, t, g * 12:(g + 1) * 12].rearrange("p (h b) -> p b h", b=3), op=ALU.mult),
                         reads=["den3", "gts"], writes=["coef"])
                    for br in range(3):
                        dst = acc if br == 0 else tm2
                        P.op("dve", lambda br=br, qt=qt, dst=dst, ob=ob: V.tensor_tensor(out=dst[:], in0=ob[:, br, qt, :, 0:64], in1=coef[:, br, :].unsqueeze(2).to_broadcast([128, 4, 64]), op=ALU.mult),
                             reads=[(okey, br), "coef"], writes=["acc" if br == 0 else "tm2"])
                        if br > 0:
                            P.op("dve", lambda: V.tensor_tensor(out=acc[:], in0=acc[:], in1=tm2[:], op=ALU.add), reads=["acc", "tm2"], writes=["acc"])
                    P.op("act", lambda: A.activation(out=tm2[:], in_=acc[:], func=AF.Square), reads=["acc", "tm2"], writes=["tm2"])
                    P.op("dve", lambda: V.tensor_reduce(out=ss[:], in_=tm2[:], axis=AX.X, op=ALU.add), reads=["tm2"], writes=["ss"])
                    P.op("dve", lambda: V.tensor_scalar(out=ss[:], in0=ss[:], scalar1=1.0 / 64, scalar2=1e-6, op0=ALU.mult, op1=ALU.add), reads=["ss"], writes=["ss"])
                    P.op("act", lambda: A.activation(out=ss[:], in_=ss[:], func=AF.Sqrt), reads=["ss"], writes=["ss"])
                    P.op("dve", lambda: V.reciprocal(out=ss[:], in_=ss[:]), reads=["ss"], writes=["ss"])
                    P.op("dve", lambda: V.tensor_tensor(out=acc[:], in0=acc[:], in1=ss[:].unsqueeze(2).to_broadcast([128, 4, 64]), op=ALU.mult), reads=["acc", "ss"], writes=["acc"])
                    P.op("dve", lambda g=g, i2=i2: V.tensor_tensor(out=yst[i2][:], in0=acc[:].rearrange("p h d -> p (h d)"), in1=nw[:, g * 256:(g + 1) * 256], op=ALU.mult),
                         reads=["acc", "nw"], writes=[("yst", i2)])
                    P.dma("pool", k.y[t * 128:(t + 1) * 128, 512 + g * 256:512 + (g + 1) * 256], yst[i2][:], reads=[("yst", i2)], writes=[("ynsa", t, g)])
        P.barrier()


TRASH = NE * CAP


def layer_norm_tile(k, P, src, dst, lnw, lnb, tmp, key_src, key_dst, sfx):
    nc = k.nc
    V, A = nc.vector, nc.scalar
    st, ag, rs = k.ln_st, k.ln_ag, k.ln_rs
    for hlf in range(2):
        P.op("dve", lambda hlf=hlf: V.bn_stats(out=st[:, hlf * 6:(hlf + 1) * 6], in_=src[:, hlf * 512:(hlf + 1) * 512]), reads=[key_src], writes=["ln_st"])
    P.op("dve", lambda: V.bn_aggr(out=ag[:], in_=st[:]), reads=["ln_st"], writes=["ln_ag"])
    P.op("dve", lambda: V.tensor_scalar(out=rs[:], in0=ag[:, 1:2], scalar1=1e-5, scalar2=None, op0=ALU.add), reads=["ln_ag"], writes=["ln_rs"])
    P.op("act", lambda: A.activation(out=rs[:], in_=rs[:], func=AF.Sqrt), reads=["ln_rs"], writes=["ln_rs"])
    P.op("dve", lambda: V.reciprocal(out=rs[:], in_=rs[:]), reads=["ln_rs"], writes=["ln_rs"])
    P.op("dve", lambda: V.tensor_scalar(out=tmp[:], in0=src[:], scalar1=ag[:, 0:1], scalar2=rs[:, 0:1], op0=ALU.subtract, op1=ALU.mult),
         reads=[key_src, "ln_ag", "ln_rs"], writes=["ln_tmp" + sfx])
    P.op("dve", lambda: V.tensor_tensor(out=tmp[:], in0=tmp[:], in1=lnw[:], op=ALU.mult), reads=["ln_tmp" + sfx, "lnw" + sfx], writes=["ln_tmp" + sfx])
    P.op("dve", lambda: V.tensor_tensor(out=dst[:], in0=tmp[:], in1=lnb[:], op=ALU.add), reads=["ln_tmp" + sfx, "lnb" + sfx], writes=[key_dst])


def phase3(k, l):
    nc, P = k.nc, k.P
    V, A = nc.vector, nc.scalar
    with ExitStack() as es:
        sb = lambda name, shape, dt: es.enter_context(nc.sbuf_tensor(f"p3l{l}_{name}", shape, dt))
        ps = lambda name, shape, dt: es.enter_context(nc.psum_tensor(f"p3l{l}_{name}", shape, dt))
        wo = sb("wo", [128, 8, D], BF16)
        rw = sb("rw", [128, 8, NE], F32)
        rb = sb("rb", [128, NE], F32)
        lnw = sb("lnw", [128, D], F32)
        lnb = sb("lnb", [128, D], F32)
        ytl = [sb(f"ytl{i}", [128, D], BF16) for i in range(2)]
        xt = [sb(f"xt{i}", [128, D], F32) for i in range(2)]
        x1 = [sb(f"x1_{i}", [128, D], F32) for i in range(2)]
        NXB = 4
        x1b = [sb(f"x1b{i}", [128, D], BF16) for i in range(NXB)]
        carry = sb("carry", [128, NE], F32)
        BUF = []
        for pi in range(2):
            bd = {}
            for nm_, shp, dt_ in (("yT", [128, 8, 128], BF16), ("mix", [128, D], F32), ("rr", [128, D], F32), ("tmp", [128, D], F32), ("x1T", [128, 8, 128], F32),
                                  ("lg", [128, NE], F32), ("m8", [128, 8], F32), ("nm", [128, 1], F32), ("msk", [128, NE], F32), ("mskb", [128, NE], BF16),
                                  ("ex", [128, NE], F32), ("ssum", [128, 1], F32), ("wts", [128, NE], F32), ("posf", [128, NE], F32), ("val", [128, NE], F32),
                                  ("nsel", [128, NE], F32), ("t8", [128, 8], F32), ("oh", [128, NE], F32), ("idxf", [128, 4], F32),
                                  ("ln_st", [128, 12], F32), ("ln_ag", [128, 2], F32), ("ln_rs", [128, 1], F32)):
                bd[nm_] = sb(f"{nm_}_{pi}", shp, dt_)
            BUF.append(bd)
        SHARED = {"pT", "pr", "ident_b", "ident", "rw", "rb", "lnw1", "lnb1", "carry", "eoff", "trashp", "ustrict_b", "ones_b"}

        def keymap(i2):
            def km(x):
                if x in SHARED or (isinstance(x, tuple) and x[0] in ("pm", "pX", "wo")):
                    return x
                return ("par", i2, x)
            return km
        pT = ps("pT", [128, 8, 128], BF16)
        pm = [ps(f"pm{i}", [128, 512], F32) for i in range(2)]
        pX = [ps(f"pX{i}", [128, 512], F32) for i in range(2)]
        pr = ps("pr", [128, 512], F32)

        x_src = k.x if l == 0 else k.x2
        wov = k.w_out[l].rearrange("(c p) n -> p c n", p=128)
        for c in range(8):
            P.dma("pool", wo[:, c, :], wov[:, c, :], writes=[("wo", c)])
        P.dma("sp", rw[:], k.router_w[l].rearrange("(c p) n -> p c n", p=128), writes=["rw"])
        bcast_row(k, P, "sp", rb[:], k.router_b[l:l + 1, :], "rb")
        bcast_row(k, P, "sp", lnw[:], k.ln1_w[l:l + 1, :], "lnw1")
        bcast_row(k, P, "sp", lnb[:], k.ln1_b[l:l + 1, :], "lnb1")
        P.op("dve", lambda: V.memset(carry[:], 0.0), writes=["carry"])
        if k.dbg and k.inject_nsa:
            P.dma("sp", k.y[:, 512:1024], k.ynsa_in[l], writes=["yinj"])
            P.barrier()

        def stageA(t, P):
            i2 = t % 2
            rows = slice(t * 128, (t + 1) * 128)
            B_ = BUF[i2]
            yT, mix, rr, tmp, x1T, lg, m8, nm, msk, mskb, ex, ssum, wts, posf, val, nsel, t8, oh, idxf = (B_[n_] for n_ in (
                "yT", "mix", "rr", "tmp", "x1T", "lg", "m8", "nm", "msk", "mskb", "ex", "ssum", "wts", "posf", "val", "nsel", "t8", "oh", "idxf"))
            k.ln_st, k.ln_ag, k.ln_rs = B_["ln_st"], B_["ln_ag"], B_["ln_rs"]
            P.dma("sp", ytl[i2][:], k.y[rows, :], writes=[("ytl", i2)])
            P.dma("act", xt[i2][:], x_src[rows, :], writes=[("xt", i2)])
            for c in range(8):
                P.op("pe", lambda c=c, i2=i2: nc.tensor.transpose(out=pT[:, c, :], in_=ytl[i2][:, c * 128:(c + 1) * 128], identity=k.ident_b[:]),
                     reads=[("ytl", i2), "ident_b"], writes=["pT"])
            P.op("dve", lambda: V.tensor_copy(out=yT[:], in_=pT[:]), reads=["pT"], writes=["yT"])
            for hf in range(2):
                for c in range(8):
                    P.op("pe", lambda c=c, hf=hf: nc.tensor.matmul(pm[hf][:], lhsT=yT[:, c, :], rhs=wo[:, c, hf * 512:(hf + 1) * 512], start=(c == 0), stop=(c == 7)),
                         reads=["yT", ("wo", c)], writes=[("pm", hf)])
                P.op("act", lambda hf=hf: A.copy(out=mix[:, hf * 512:(hf + 1) * 512], in_=pm[hf][:]), reads=[("pm", hf)], writes=["mix"])
            P.op("dve", lambda i2=i2: V.scalar_tensor_tensor(out=rr[:], in0=xt[i2][:], scalar=ALPHA, in1=mix[:], op0=ALU.mult, op1=ALU.add),
                 reads=[("xt", i2), "mix"], writes=["rr"])
            layer_norm_tile(k, P, rr, x1[i2], lnw, lnb, tmp, "rr", ("x1", i2), "1")
            P.dma("pool", k.x1[rows, :], x1[i2][:], reads=[("x1", i2)], writes=[("x1d", t)])
            ib = t % NXB
            P.op("act", lambda i2=i2, ib=ib: A.copy(out=x1b[ib][:], in_=x1[i2][:]), reads=[("x1", i2)], writes=[("x1b", ib)])

        def stageB(t, P):
            i2 = t % 2
            rows = slice(t * 128, (t + 1) * 128)
            B_ = BUF[i2]
            yT, mix, rr, tmp, x1T, lg, m8, nm, msk, mskb, ex, ssum, wts, posf, val, nsel, t8, oh, idxf = (B_[n_] for n_ in (
                "yT", "mix", "rr", "tmp", "x1T", "lg", "m8", "nm", "msk", "mskb", "ex", "ssum", "wts", "posf", "val", "nsel", "t8", "oh", "idxf"))
            k.ln_st, k.ln_ag, k.ln_rs = B_["ln_st"], B_["ln_ag"], B_["ln_rs"]
            for c in range(8):
                P.op("pe", lambda c=c, i2=i2: nc.tensor.transpose(out=pX[c // 4][:, (c % 4) * 128:(c % 4 + 1) * 128], in_=x1[i2][:, c * 128:(c + 1) * 128], identity=k.ident_f[:]),
                     reads=[("x1", i2), "ident"], writes=[("pX", c // 4)])
            for q in range(2):
                P.op("act", lambda q=q: A.copy(out=x1T[:, q * 4:(q + 1) * 4, :].rearrange("p a b -> p (a b)"), in_=pX[q][:]), reads=[("pX", q)], writes=["x1T"])
            for c in range(8):
                P.op("pe", lambda c=c: nc.tensor.matmul(pr[:, 0:NE], lhsT=x1T[:, c, :], rhs=rw[:, c, :], start=(c == 0), stop=(c == 7)),
                     reads=["x1T", "rw"], writes=["pr"])
            P.op("act", lambda: A.copy(out=lg[:], in_=pr[:, 0:NE]), reads=["pr"], writes=["lg"])
            P.op("dve", lambda: V.tensor_tensor(out=lg[:], in0=lg[:], in1=rb[:], op=ALU.add), reads=["lg", "rb"], writes=["lg"])
            P.op("dve", lambda: V.max(out=m8[:], in_=lg[:]), reads=["lg"], writes=["m8"])
            P.op("dve", lambda: V.tensor_scalar(out=msk[:], in0=lg[:], scalar1=m8[:, 3:4], scalar2=None, op0=ALU.is_ge), reads=["lg", "m8"], writes=["msk"])
            P.op("dve", lambda: V.tensor_copy(out=mskb[:], in_=msk[:]), reads=["msk"], writes=["mskb"])
            P.op("dve", lambda: V.tensor_scalar(out=nm[:], in0=m8[:, 0:1], scalar1=-1.0, scalar2=None, op0=ALU.mult), reads=["m8"], writes=["nm"])
            P.op("act", lambda: A.activation(out=ex[:], in_=lg[:], func=AF.Exp, bias=nm[:, 0:1], scale=1.0), reads=["lg", "nm"], writes=["ex"])
            P.op("dve", lambda: V.tensor_tensor(out=ex[:], in0=ex[:], in1=msk[:], op=ALU.mult), reads=["ex", "msk"], writes=["ex"])
            P.op("dve", lambda: V.tensor_reduce(out=ssum[:], in_=ex[:], axis=AX.X, op=ALU.add), reads=["ex"], writes=["ssum"])
            P.op("dve", lambda: V.reciprocal(out=ssum[:], in_=ssum[:]), reads=["ssum"], writes=["ssum"])
            P.op("dve", lambda: V.tensor_scalar(out=wts[:], in0=ex[:], scalar1=ssum[:, 0:1], scalar2=None, op0=ALU.mult), reads=["ex", "ssum"], writes=["wts"])
            P.op("pe", lambda: nc.tensor.matmul(pr[:, 64:64 + NE], lhsT=k.ustrict_b[:], rhs=mskb[:], start=True, stop=True), reads=["mskb", "ustrict_b"], writes=["pr"])
            P.op("pe", lambda: nc.tensor.matmul(pr[:, 128:128 + NE], lhsT=k.ones_b[:], rhs=mskb[:], start=True, stop=True), reads=["mskb", "ones_b"], writes=["pr"])
            P.op("act", lambda: A.copy(out=posf[:], in_=pr[:, 64:64 + NE]), reads=["pr"], writes=["posf"])
            P.op("act", lambda: A.copy(out=oh[:], in_=pr[:, 128:128 + NE]), reads=["pr"], writes=["oh"])
            P.op("dve", lambda: V.tensor_tensor(out=posf[:], in0=posf[:], in1=carry[:], op=ALU.add), reads=["posf", "carry"], writes=["posf"])
            P.op("dve", lambda: V.tensor_tensor(out=carry[:], in0=carry[:], in1=oh[:], op=ALU.add), reads=["carry", "oh", "posf"], writes=["carry"])
            P.op("dve", lambda: V.tensor_scalar(out=val[:], in0=posf[:], scalar1=float(CAP) - 0.5, scalar2=None, op0=ALU.is_lt), reads=["posf"], writes=["val"])
            P.op("dve", lambda: V.tensor_tensor(out=val[:], in0=val[:], in1=msk[:], op=ALU.mult), reads=["val", "msk"], writes=["val"])
            P.op("dve", lambda: V.tensor_tensor(out=nsel[:], in0=posf[:], in1=k.eoff[:], op=ALU.add), reads=["posf", "eoff"], writes=["nsel"])
            P.op("dve", lambda: V.tensor_scalar(out=nsel[:], in0=nsel[:], scalar1=k.trashp[:, 0:1], scalar2=None, op0=ALU.subtract), reads=["nsel", "trashp"], writes=["nsel"])
            P.op("dve", lambda: V.tensor_tensor(out=nsel[:], in0=nsel[:], in1=val[:], op=ALU.mult), reads=["nsel", "val"], writes=["nsel"])
            P.op("dve", lambda: V.tensor_scalar(out=nsel[:], in0=nsel[:], scalar1=k.trashp[:, 0:1], scalar2=-1.0, op0=ALU.add, op1=ALU.mult), reads=["nsel", "trashp"], writes=["nsel"])
            P.op("dve", lambda: V.max(out=t8[:], in_=nsel[:]), reads=["nsel"], writes=["t8"])
            P.op("dve", lambda: V.tensor_scalar(out=idxf[:], in0=t8[:, 0:4], scalar1=-1.0, scalar2=None, op0=ALU.mult), reads=["t8"], writes=["idxf"])
            P.op("dve", lambda t=t: V.tensor_copy(out=k.idx4[:, t, :], in_=idxf[:]), reads=["idxf"], writes=[("idx4", t)])
            for kk_ in range(4):
                P.op("dve", lambda kk_=kk_: V.tensor_scalar(out=oh[:], in0=nsel[:], scalar1=t8[:, kk_:kk_ + 1], scalar2=None, op0=ALU.is_equal), reads=["nsel", "t8", "carry"], writes=["oh"])
                P.op("dve", lambda: V.tensor_tensor(out=oh[:], in0=oh[:], in1=wts[:], op=ALU.mult), reads=["oh", "wts"], writes=["oh"])
                P.op("dve", lambda kk_=kk_, t=t: V.tensor_reduce(out=k.w4[:, t, kk_:kk_ + 1], in_=oh[:], axis=AX.X, op=ALU.add), reads=["oh"], writes=[("w4", t)])
            for kk_ in range(4):
                ib = t % NXB
                P.op("pool", lambda kk_=kk_, t=t, ib=ib: nc.gpsimd.indirect_dma_start(out=k.xbuf, out_offset=bass.IndirectOffsetOnAxis(ap=k.idx4[:, t, kk_:kk_ + 1], axis=0),
                                                                                       in_=x1b[ib][:], in_offset=None),
                     reads=[("x1b", ib), ("idx4", t)], writes=[("xbuf", t, kk_)], dma=True)
            if k.dbg and t == 0:
                dump(k, "lg", lg[:], "lg", NE)
                dump(k, "wts", wts[:], "wts", NE)
                dump(k, "nsel", nsel[:], "nsel", NE)
                dump(k, "w4", k.w4[:, 0, :], ("w4", 0), 4)
                dump(k, "idxf", idxf[:], "idxf", 4)

        r0 = Rec(P, keymap(0))
        stageA(0, r0)
        replay_interleaved(P, [r0])
        for t in range(NT):
            ra = None
            if t + 1 < NT:
                ra = Rec(P, keymap((t + 1) % 2))
                stageA(t + 1, ra)
            rb_ = Rec(P, keymap(t % 2))
            stageB(t, rb_)
            replay_interleaved(P, [ra, rb_])
        P.barrier()


def phase4(k, l):
    nc, P = k.nc, k.P
    V, A = nc.vector, nc.scalar
    NSL = CAP // 128
    with ExitStack() as es:
        sb = lambda name, shape, dt: es.enter_context(nc.sbuf_tensor(f"p4l{l}_{name}", shape, dt))
        ps = lambda name, shape, dt: es.enter_context(nc.psum_tensor(f"p4l{l}_{name}", shape, dt))
        wu = [sb(f"wu{i}", [128, 8, 2 * D], BF16) for i in range(2)]
        wd = [sb(f"wd{i}", [128, 8, D], BF16) for i in range(2)]
        bupT = sb("bupT", [128, 16, NE], F32)
        bdn = [sb(f"bdn{i}", [128, D], F32) for i in range(2)]
        xe = [sb(f"xe{i}", [128, D], BF16) for i in range(4)]
        XeTs = [sb(f"XeT{i}", [128, 8, CAP], BF16) for i in range(2)]
        actT = sb("actT", [128, 8, CAP], BF16)
        gsb = [sb(f"gsb{i}", [128, CAP], F32) for i in range(2)]
        lsb = [sb(f"lsb{i}", [128, CAP], F32) for i in range(2)]
        sig = [sb(f"sig{i}", [128, CAP], F32) for i in range(2)]
        ysb = [sb(f"ysb{i}", [128, D], F32) for i in range(2)]
        yst = [sb(f"yst{i}", [128, D], BF16) for i in range(2)]
        pT = ps("pT", [128, 8, 128], BF16)
        pA = ps("pA", [128, 512], F32)
        pB = ps("pB", [128, 512], F32)
        pC = ps("pC", [128, 512], F32)
        pD = ps("pD", [128, 512], F32)

        P.op("dve", lambda: V.memset(yst[0][:], 0.0), writes=[("yst", 0)])
        P.dma("sp", k.ybuf[TRASH:TRASH + 128, :], yst[0][:], reads=[("yst", 0)], writes=["ybuf_trash"])
        P.dma("sp", gsb[0][0:NE, :], k.exp_b_up[l][:, 0:D], writes=[("gsb", 0)])
        P.dma("sp", lsb[0][0:NE, :], k.exp_b_up[l][:, D:2 * D], writes=[("lsb", 0)])
        for j in range(16):
            src = gsb[0] if j < 8 else lsb[0]
            P.op("pe", lambda j=j, src=src: nc.tensor.transpose(out=pA[:, j * NE:(j + 1) * NE], in_=src[0:NE, (j % 8) * 128:(j % 8 + 1) * 128], identity=k.ident_f[0:NE, 0:NE]),
                 reads=[("gsb", 0), ("lsb", 0), "ident"], writes=["pA"])
        P.op("act", lambda: A.copy(out=bupT[:].rearrange("p a b -> p (a b)"), in_=pA[:, 0:16 * NE]), reads=["pA"], writes=["bupT"])

        def load_w(e):
            import os
            if os.environ.get("NOLOADW") and e > 1:
                return
            b = e % 2
            wuv = k.exp_w_up[l, e].rearrange("(c p) n -> p c n", p=128)
            wdv = k.exp_w_down[l, e].rearrange("(c p) n -> p c n", p=128)
            for c in range(8):
                P.dma("pool", wu[b][:, c, :], wuv[:, c, :], writes=[("wu", b, c)])
            for c in range(8):
                P.dma("pool", wd[b][:, c, :], wdv[:, c, :], writes=[("wd", b, c)])
            P.dma("act", bdn[b][:], k.exp_b_down[l, e:e + 1, :].partition_broadcast(128), writes=[("bdn", b)])

        def gather(e):
            XeT = XeTs[e % 2]
            for i in range(NSL):
                i4 = i % 4
                P.dma("sp", xe[i4][:], k.xbuf[e * CAP + i * 128:e * CAP + (i + 1) * 128, :], writes=[("xe", i4)])
                for c in range(8):
                    P.op("pe", lambda c=c, i4=i4: nc.tensor.transpose(out=pT[:, c, :], in_=xe[i4][:, c * 128:(c + 1) * 128], identity=k.ident_b[:]),
                         reads=[("xe", i4), "ident_b"], writes=["pT"])
                P.op("dve", lambda i=i, XeT=XeT: V.tensor_copy(out=XeT[:, :, i * 128:(i + 1) * 128], in_=pT[:]), reads=["pT"], writes=[("XeT", e % 2)])

        load_w(0)
        for e in range(NE):
            b = e % 2
            if e + 1 < NE:
                load_w(e + 1)
            XeT = XeTs[b]
            kxe = ("XeT", b)
            if e == 0:
                gather(0)
            for j in range(8):
                groups = ((pA, "pA", j, 0), (pB, "pB", j, 512), (pC, "pC", 8 + j, 0), (pD, "pD", 8 + j, 512))
                for (dst, key, fc, n0) in groups:
                    for c in range(8):
                        P.op("pe", lambda c=c, dst=dst, fc=fc, n0=n0, b=b, XeT=XeT: nc.tensor.matmul(dst[:], lhsT=wu[b][:, c, fc * 128:(fc + 1) * 128], rhs=XeT[:, c, n0:n0 + 512],
                                                                                           start=(c == 0), stop=(c == 7)),
                             reads=[("wu", b, c), kxe], writes=[key])
                jb = j % 2
                G, Lq, Sg = gsb[jb], lsb[jb], sig[jb]
                kg, kl, ks = ("gsb", jb), ("lsb", jb), ("sig", jb)
                for (dst, key, fc, n0) in groups:
                    T_, kt_ = (G, kg) if fc < 8 else (Lq, kl)
                    P.op("act", lambda dst=dst, fc=fc, n0=n0, e=e, T_=T_: A.activation(out=T_[:, n0:n0 + 512], in_=dst[:], func=AF.Identity, bias=bupT[:, fc, e:e + 1], scale=1.0),
                         reads=[key, "bupT"], writes=[kt_])
                P.op("dve", lambda G=G: V.tensor_scalar(out=G[:], in0=G[:], scalar1=7.0, scalar2=None, op0=ALU.min), reads=[kg], writes=[kg])
                P.op("act", lambda G=G, Sg=Sg: A.activation(out=Sg[:], in_=G[:], func=AF.Sigmoid, scale=1.702), reads=[kg], writes=[ks])
                P.op("dve", lambda Lq=Lq: V.tensor_scalar(out=Lq[:], in0=Lq[:], scalar1=-7.0, scalar2=7.0, op0=ALU.max, op1=ALU.min), reads=[kl], writes=[kl])
                P.op("dve", lambda G=G, Sg=Sg: V.tensor_tensor(out=Sg[:], in0=G[:], in1=Sg[:], op=ALU.mult), reads=[kg, ks], writes=[ks])
                P.op("dve", lambda j=j, Lq=Lq, Sg=Sg: V.scalar_tensor_tensor(out=actT[:, j, :], in0=Lq[:], scalar=1.0, in1=Sg[:], op0=ALU.add, op1=ALU.mult),
                     reads=[kl, ks], writes=[("actT", j)])
            if e + 1 < NE:
                gather(e + 1)
            for i in range(NSL):
                i2 = i % 2
                for hf in range(2):
                    dstb, key = ((pA, "pA"), (pC, "pC"), (pB, "pB"), (pD, "pD"))[2 * i2 + hf]
                    for c in range(8):
                        P.op("pe", lambda c=c, dstb=dstb, hf=hf, i=i, b=b: nc.tensor.matmul(dstb[:], lhsT=actT[:, c, i * 128:(i + 1) * 128], rhs=wd[b][:, c, hf * 512:(hf + 1) * 512],
                                                                                           start=(c == 0), stop=(c == 7)),
                             reads=[("actT", c), ("wd", b, c)], writes=[key])
                    P.op("act", lambda dstb=dstb, hf=hf, i2=i2: A.copy(out=ysb[i2][:, hf * 512:(hf + 1) * 512], in_=dstb[:]), reads=[key], writes=[("ysb", i2)])
                P.op("dve", lambda i2=i2, b=b: V.tensor_tensor(out=yst[i2][:], in0=ysb[i2][:], in1=bdn[b][:], op=ALU.add),
                     reads=[("ysb", i2), ("bdn", b)], writes=[("yst", i2)])
                P.dma("pool", k.ybuf[e * CAP + i * 128:e * CAP + (i + 1) * 128, :], yst[i2][:], reads=[("yst", i2)], writes=[("ybuf", e, i)])
        P.barrier()


def phase5(k, l, last):
    nc, P = k.nc, k.P
    V, A = nc.vector, nc.scalar
    with ExitStack() as es:
        sb = lambda name, shape, dt: es.enter_context(nc.sbuf_tensor(f"p5l{l}_{name}", shape, dt))
        lnw = sb("lnw", [128, D], F32)
        lnb = sb("lnb", [128, D], F32)
        k.ln_st = sb("ln_st", [128, 12], F32)
        k.ln_ag = sb("ln_ag", [128, 2], F32)
        k.ln_rs = sb("ln_rs", [128, 1], F32)
        gk = [[sb(f"gk{i}_{q}", [128, D], BF16) for q in range(4)] for i in range(2)]
        x1t = [sb(f"x1t{i}", [128, D], F32) for i in range(2)]
        acc = sb("acc", [128, D], F32)
        tmp = sb("tmp", [128, D], F32)
        x2 = [sb(f"x2_{i}", [128, D], F32) for i in range(2)]
        bcast_row(k, P, "sp", lnw[:], k.ln2_w[l:l + 1, :], "lnw2")
        bcast_row(k, P, "sp", lnb[:], k.ln2_b[l:l + 1, :], "lnb2")
        dst_d = k.out if last else k.x2
        for t in range(NT):
            i2 = t % 2
            rows = slice(t * 128, (t + 1) * 128)
            P.dma("sp", x1t[i2][:], k.x1[rows, :], writes=[("x1t", i2)])
            for q in range(4):
                P.op("pool", lambda q=q, t=t, i2=i2: nc.gpsimd.indirect_dma_start(out=gk[i2][q][:], out_offset=None, in_=k.ybuf,
                                                                                 in_offset=bass.IndirectOffsetOnAxis(ap=k.idx4[:, t, q:q + 1], axis=0)),
                     writes=[("gk", i2, q)], dma=True)
            P.op("dve", lambda t=t, i2=i2: V.tensor_scalar(out=acc[:], in0=gk[i2][0][:], scalar1=k.w4[:, t, 0:1], scalar2=None, op0=ALU.mult),
                 reads=[("gk", i2, 0)], writes=["acc"])
            for q in range(1, 4):
                P.op("dve", lambda q=q, t=t, i2=i2: V.scalar_tensor_tensor(out=acc[:], in0=gk[i2][q][:], scalar=k.w4[:, t, q:q + 1], in1=acc[:], op0=ALU.mult, op1=ALU.add),
                     reads=[("gk", i2, q), "acc"], writes=["acc"])
            if k.dbg and k.moe_dbg is not None:
                P.dma("sp", k.moe_dbg[rows, :], acc[:], reads=["acc"], writes=[("moe_dbg", t)])
            P.op("dve", lambda i2=i2: V.scalar_tensor_tensor(out=acc[:], in0=x1t[i2][:], scalar=ALPHA, in1=acc[:], op0=ALU.mult, op1=ALU.add),
                 reads=[("x1t", i2), "acc"], writes=["acc"])
            layer_norm_tile(k, P, acc, x2[i2], lnw, lnb, tmp, "acc", ("x2", i2), "2")
            P.dma("pool", dst_d[rows, :], x2[i2][:], reads=[("x2", i2)], writes=[("x2d", t)])
        P.barrier()


def make_inputs(inputs, b, perm, w_in_r=None):
    f32 = lambda n: np.ascontiguousarray(np.asarray(inputs[n], dtype=np.float32))
    if w_in_r is None:
        w_in_r = np.ascontiguousarray(np.asarray(inputs["w_in"], dtype=np.float32)[:, :, perm])
    m = {"x": np.ascontiguousarray(np.asarray(inputs["x"], dtype=np.float32)[b]),
         "pos": np.ascontiguousarray(np.asarray(inputs["positions"], dtype=np.int32)[b].reshape(1, -1)), "w_in": w_in_r,
         "hg_lb": f32("hg_lower_bounds"), "hg_nw": f32("hg_norm_w"), "gm_lnw": f32("gm_ln_w"), "gm_lnb": f32("gm_ln_b"),
         "gm_ws": f32("gm_spatial_w"), "gm_bs": f32("gm_spatial_b"), "gm_nw": f32("gm_norm_w"),
         "nsa_pe": f32("nsa_cmp_pe"), "nsa_w1": f32("nsa_cmp_w1"), "nsa_w2": f32("nsa_cmp_w2"), "nsa_nw": f32("nsa_norm_w"),
         "w_out": f32("w_out"), "ln1_w": f32("ln1_w"), "ln1_b": f32("ln1_b"), "ln2_w": f32("ln2_w"), "ln2_b": f32("ln2_b"),
         "router_w": f32("router_w"), "router_b": f32("router_b"), "exp_w_up": f32("exp_w_up"), "exp_b_up": f32("exp_b_up"),
         "exp_w_down": f32("exp_w_down"), "exp_b_down": f32("exp_b_down")}
    for n, v in host_consts().items():
        m["c_" + n] = v
    return m


def kernel(**inputs):
    perm = w_in_perm()
    w_in_r = np.ascontiguousarray(np.asarray(inputs["w_in"], dtype=np.float32)[:, :, perm])
    nc = build(nlayers=L, dbg=False)
    in_maps = [make_inputs(inputs, b, perm, w_in_r) for b in range(8)]
    res = run_bass_kernel_spmd(nc, in_maps, core_ids=list(range(8)))
    return np.stack([np.asarray(r["out"], dtype=np.float32) for r in res.results], axis=0)
```
